# Optimizing a Trainium2 kernel written in Bass

```python
import math
import jax, jax.numpy as jnp
from jax import lax
import numpy as np

D_MODEL = 2048
BATCH = 1
SEQ = 8192
DEPTH = 2

GRID_W = 64
CTX_LEN = 256
N_BRANCH = 4
BRANCH_W = D_MODEL // 4
CHUNK = 128
A_GROUPS = 4
CONV_W = 31
F_GROUPS = 4
QK_NOPE = 64
QK_ROPE = 32
V_HEAD = 64
MLA_HEADS = BRANCH_W // V_HEAD
Q_LORA = D_MODEL // 4
KV_LORA = D_MODEL // 8
ROPE_THETA = 10000.0
Q_BLOCK = 128
MLA_SCALE = (QK_NOPE + QK_ROPE) ** -0.5
N_GROUPS = 4
EXP_PER_GROUP = 8
N_EXPERTS = N_GROUPS * EXP_PER_GROUP
TOP_K = 2
D_EXPERT = D_MODEL // 2
MOE_BLOCK = 128
COL_A = 0
COL_B = COL_A + 2 * BRANCH_W
COL_C = COL_B + 2 * BRANCH_W
COL_D = COL_C + BRANCH_W
COL_KV = COL_D + Q_LORA
COL_KR = COL_KV + KV_LORA
COL_G = COL_KR + QK_ROPE
D_IN = COL_G + N_BRANCH * D_MODEL
ALPHA = (2 * DEPTH) ** 0.25
BETA = (8 * DEPTH) ** -0.25
LN_EPS = 1e-6

kernel_name = "hybrid_gated_mixers_mla_hmoe_dit"


def _ln0(x):
    xf = x.astype(jnp.float32)
    mu = xf.mean(-1, keepdims=True)
    var = jnp.square(xf - mu).mean(-1, keepdims=True)
    return ((xf - mu) * lax.rsqrt(var + LN_EPS)).astype(x.dtype)


def _ln(x, g, b):
    xf = x.astype(jnp.float32)
    mu = xf.mean(-1, keepdims=True)
    var = jnp.square(xf - mu).mean(-1, keepdims=True)
    y = (xf - mu) * lax.rsqrt(var + LN_EPS)
    return (y * g.astype(jnp.float32) + b.astype(jnp.float32)).astype(x.dtype)


def _rms(x, g):
    xf = x.astype(jnp.float32)
    y = xf * lax.rsqrt(jnp.mean(xf * xf, -1, keepdims=True) + LN_EPS)
    return (y * g.astype(jnp.float32)).astype(x.dtype)


def _modulate(x, shift, scale):
    return _ln0(x) * (1 + scale) + shift


def _axial_rope_tables(s_len, dtype):
    rows = s_len // GRID_W
    row = jnp.repeat(jnp.arange(rows, dtype=jnp.float32), GRID_W)
    col = jnp.tile(jnp.arange(GRID_W, dtype=jnp.float32), rows)
    half = QK_ROPE // 2
    inv = ROPE_THETA ** (-jnp.arange(0, half, 2, dtype=jnp.float32) / half)
    ar = row[:, None] * inv
    ac = col[:, None] * inv
    ang = jnp.concatenate([ar, ar, ac, ac], -1)
    return jnp.cos(ang).astype(dtype), jnp.sin(ang).astype(dtype)


def _rope(x, cos, sin):
    x1, x2, x3, x4 = jnp.split(x, 4, -1)
    rot = jnp.concatenate([-x2, x1, -x4, x3], -1)
    return x * cos + rot * sin


def _chunk_spatial_gate(uv, p):
    b_, n_tok, _ = uv.shape
    uv = jax.nn.gelu(uv)
    u, v = jnp.split(uv, 2, -1)
    v = _ln(v, p["a_ln_g"], p["a_ln_b"])
    v = v.reshape(b_, n_tok // CHUNK, CHUNK, A_GROUPS, BRANCH_W // A_GROUPS)
    mixed = jnp.einsum("gpq,bnqgc->bnpgc", p["a_w_s"], v) + p["a_b_s"].T[None, None, :, :, None]
    return u * mixed.reshape(b_, n_tok, BRANCH_W)


def _conformer_conv(ab, p):
    a, g = jnp.split(ab, 2, -1)
    y = a * jax.nn.sigmoid(g)
    y = lax.conv_general_dilated(y, p["b_w_dw"][:, None, :], (1,), [(CONV_W // 2, CONV_W // 2)],
                                 dimension_numbers=("NWC", "WIO", "NWC"), feature_group_count=BRANCH_W)
    y = y + p["b_b_dw"]
    return jax.nn.silu(_ln(y, p["b_ln_g"], p["b_ln_b"]))


def _fourier_mix(z):
    b_, n_tok, ch = z.shape
    zf = z.astype(jnp.float32).reshape(b_, n_tok, F_GROUPS, ch // F_GROUPS)
    y = jnp.fft.fft2(zf, axes=(1, 3), norm="ortho").real
    return y.reshape(b_, n_tok, ch).astype(z.dtype)


def _local_mixers(z, p):
    return (_chunk_spatial_gate(z[..., COL_A:COL_B], p),
            _conformer_conv(z[..., COL_B:COL_C], p),
            _fourier_mix(z[..., COL_C:COL_D]))


def _mla_queries(cq, p):
    b_, n_tok, _ = cq.shape
    q = (_rms(cq, p["d_q_g"]) @ p["d_w_uq"]).reshape(b_, n_tok, MLA_HEADS, QK_NOPE + QK_ROPE)
    return q[..., :QK_NOPE], q[..., QK_NOPE:]


def _mla_kv(ckv, p):
    b_, n_tok, _ = ckv.shape
    ckv = _rms(ckv, p["d_kv_g"])
    kn = (ckv @ p["d_w_uk"]).reshape(b_, n_tok, MLA_HEADS, QK_NOPE)
    v = (ckv @ p["d_w_uv"]).reshape(b_, n_tok, MLA_HEADS, V_HEAD)
    return kn, v


def _attend(qn, qr, kn, kr, v):
    s = jnp.einsum("bqhd,bkhd->bhqk", qn, kn) + jnp.einsum("bqhr,bkr->bhqk", qr, kr)
    pr = jax.nn.softmax(s.astype(jnp.float32) * MLA_SCALE, axis=-1).astype(v.dtype)
    o = jnp.einsum("bhqk,bkhd->bqhd", pr, v)
    return o.reshape(o.shape[0], o.shape[1], MLA_HEADS * V_HEAD)


def _attend_blocked(qn, qr, kn, kr, v):
    b_, n_tok = qn.shape[:2]
    nb = n_tok // Q_BLOCK

    def blk(t):
        return t.reshape(b_, nb, Q_BLOCK, *t.shape[2:]).swapaxes(0, 1)

    o = lax.map(lambda a: _attend(a[0], a[1], kn, kr, v), (blk(qn), blk(qr)))
    return o.swapaxes(0, 1).reshape(b_, n_tok, MLA_HEADS * V_HEAD)


def _merge(z, branches, p):
    b_, n_tok, _ = z.shape
    gates = jax.nn.sigmoid(z[..., COL_G:].reshape(b_, n_tok, N_BRANCH, D_MODEL))
    y = 0
    for i, br in enumerate(branches):
        y = y + gates[:, :, i, :] * (br @ p["w_branch"][i])
    return y @ p["w_o"]


def _hier_moe(h, w_grp, b_grp, w_exp, b_exp, w1, w3, w2):
    n_t, d = h.shape
    g_logit = (h @ w_grp).astype(jnp.float32) + b_grp.astype(jnp.float32)
    g_idx = jnp.argmax(g_logit, -1).astype(jnp.int32)
    g_p = jnp.take_along_axis(jax.nn.softmax(g_logit, -1), g_idx[:, None], -1)
    e_logit = ((h @ w_exp).astype(jnp.float32) + b_exp.astype(jnp.float32)).reshape(n_t, N_GROUPS, EXP_PER_GROUP)
    e_logit = jnp.take_along_axis(e_logit, g_idx[:, None, None], 1)[:, 0]
    top_v, top_i = lax.top_k(e_logit, TOP_K)
    gate = (jax.nn.softmax(top_v, -1) * g_p).astype(h.dtype).reshape(-1)
    expert = (g_idx[:, None] * EXP_PER_GROUP + top_i.astype(jnp.int32)).reshape(-1)
    tok = jnp.repeat(jnp.arange(n_t, dtype=jnp.int32), TOP_K)
    n = n_t * TOP_K
    order = jnp.argsort(expert)
    se, stok, sgate = expert[order], tok[order], gate[order]
    counts = jax.ops.segment_sum(jnp.ones((n,), jnp.int32), expert, num_segments=N_EXPERTS)
    starts = jnp.cumsum(counts) - counts
    padded = (counts + MOE_BLOCK - 1) // MOE_BLOCK * MOE_BLOCK
    pends = jnp.cumsum(padded)
    pstarts = pends - padded
    dest = pstarts[se] + jnp.arange(n, dtype=jnp.int32) - starts[se]
    n_rows = -(-(n + N_EXPERTS * (MOE_BLOCK - 1)) // MOE_BLOCK) * MOE_BLOCK
    n_blk = n_rows // MOE_BLOCK
    row_tok = jnp.zeros((n_rows,), jnp.int32).at[dest].set(stok)
    row_gate = jnp.zeros((n_rows,), h.dtype).at[dest].set(sgate)
    blk_start = jnp.arange(n_blk, dtype=jnp.int32) * MOE_BLOCK
    blk_exp = jnp.minimum(jnp.sum(pends[None, :] <= blk_start[:, None], axis=1), N_EXPERTS - 1).astype(jnp.int32)

    def run(args):
        t, e = args
        xb = h[t]
        return (jax.nn.silu(xb @ w1[e]) * (xb @ w3[e])) @ w2[e]

    y = lax.map(run, (row_tok.reshape(n_blk, MOE_BLOCK), blk_exp)).reshape(n_rows, d)
    return jnp.zeros_like(h).at[row_tok].add(y * row_gate[:, None])


def setup_inputs(seed: int = 0) -> dict:
    key = jax.random.key(seed)
    ks = iter(jax.random.split(key, 48))
    L, D = DEPTH, D_MODEL

    def nrm(shape, s):
        return jax.random.normal(next(ks), shape, jnp.float32) * s

    return {
        "x": nrm((BATCH, SEQ, D), 1.0),
        "c": nrm((BATCH, D), 1.0),
        "ctx": nrm((BATCH, CTX_LEN, D), 1.0),
        "c_ctx": nrm((D,), 1.0),
        "w_ada": nrm((L, D, 6 * D), 0.5 * D ** -0.5),
        "b_ada": nrm((L, 6 * D), 0.01),
        "w_in": nrm((L, D, D_IN), D ** -0.5),
        "b_in": nrm((L, D_IN), 0.01),
        "a_ln_g": 1.0 + nrm((L, BRANCH_W), 0.05),
        "a_ln_b": nrm((L, BRANCH_W), 0.01),
        "a_w_s": nrm((L, A_GROUPS, CHUNK, CHUNK), CHUNK ** -0.5),
        "a_b_s": 1.0 + nrm((L, A_GROUPS, CHUNK), 0.05),
        "b_w_dw": nrm((L, CONV_W, BRANCH_W), CONV_W ** -0.5),
        "b_b_dw": nrm((L, BRANCH_W), 0.01),
        "b_ln_g": 1.0 + nrm((L, BRANCH_W), 0.05),
        "b_ln_b": nrm((L, BRANCH_W), 0.01),
        "d_q_g": 1.0 + nrm((L, Q_LORA), 0.05),
        "d_w_uq": nrm((L, Q_LORA, MLA_HEADS * (QK_NOPE + QK_ROPE)), Q_LORA ** -0.5),
        "d_kv_g": 1.0 + nrm((L, KV_LORA), 0.05),
        "d_w_uk": nrm((L, KV_LORA, MLA_HEADS * QK_NOPE), KV_LORA ** -0.5),
        "d_w_uv": nrm((L, KV_LORA, MLA_HEADS * V_HEAD), KV_LORA ** -0.5),
        "w_branch": nrm((L, N_BRANCH, BRANCH_W, D), BETA * BRANCH_W ** -0.5),
        "w_o": nrm((L, D, D), BETA * D ** -0.5),
        "ln1_g": 1.0 + nrm((L, D), 0.05),
        "ln1_b": nrm((L, D), 0.01),
        "r_w_grp": nrm((L, D, N_GROUPS), D ** -0.5),
        "r_b_grp": nrm((L, N_GROUPS), 0.01),
        "r_w_exp": nrm((L, D, N_EXPERTS), D ** -0.5),
        "r_b_exp": nrm((L, N_EXPERTS), 0.01),
        "e_w1": nrm((L, N_EXPERTS, D, D_EXPERT), D ** -0.5),
        "e_w3": nrm((L, N_EXPERTS, D, D_EXPERT), D ** -0.5),
        "e_w2": nrm((L, N_EXPERTS, D_EXPERT, D), BETA * D_EXPERT ** -0.5),
        "ln2_g": 1.0 + nrm((L, D), 0.05),
        "ln2_b": nrm((L, D), 0.01),
    }


def reference(x, c, ctx, c_ctx, w_ada, b_ada, w_in, b_in, a_ln_g, a_ln_b, a_w_s, a_b_s,
              b_w_dw, b_b_dw, b_ln_g, b_ln_b, d_q_g, d_w_uq, d_kv_g, d_w_uk, d_w_uv,
              w_branch, w_o, ln1_g, ln1_b, r_w_grp, r_b_grp, r_w_exp, r_b_exp,
              e_w1, e_w3, e_w2, ln2_g, ln2_b):
    b_, s_len, d = x.shape
    n_ctx = ctx.shape[1]
    cos, sin = _axial_rope_tables(s_len, x.dtype)
    s_lat = jax.nn.silu(c)
    s_ctx = jax.nn.silu(c_ctx)[None]
    xl, xc = x, ctx
    for l in range(DEPTH):
        last = l == DEPTH - 1
        p = {"a_ln_g": a_ln_g[l], "a_ln_b": a_ln_b[l], "a_w_s": a_w_s[l], "a_b_s": a_b_s[l],
             "b_w_dw": b_w_dw[l], "b_b_dw": b_b_dw[l], "b_ln_g": b_ln_g[l], "b_ln_b": b_ln_b[l],
             "d_q_g": d_q_g[l], "d_w_uq": d_w_uq[l], "d_kv_g": d_kv_g[l], "d_w_uk": d_w_uk[l],
             "d_w_uv": d_w_uv[l], "w_branch": w_branch[l], "w_o": w_o[l]}
        ml = jnp.split((s_lat @ w_ada[l] + b_ada[l])[:, None, :], 6, -1)
        mc = jnp.split((s_ctx @ w_ada[l] + b_ada[l])[:, None, :], 6, -1)

        hl = _modulate(xl, ml[0], ml[1])
        hc = _modulate(xc, mc[0], mc[1])
        zl = hl @ w_in[l] + b_in[l]
        if last:
            zc = hc @ w_in[l][:, COL_KV:COL_G] + b_in[l][COL_KV:COL_G]
            zc_kv = zc
        else:
            zc = hc @ w_in[l] + b_in[l]
            zc_kv = zc[..., COL_KV:COL_G]
        kn_c, v_c = _mla_kv(zc_kv[..., :KV_LORA], p)
        kr_c = zc_kv[..., KV_LORA:]
        qn_l, qr_l = _mla_queries(zl[..., COL_D:COL_KV], p)
        qr_l = _rope(qr_l, cos[:, None, :], sin[:, None, :])
        kn_l, v_l = _mla_kv(zl[..., COL_KV:COL_KR], p)
        kr_l = _rope(zl[..., COL_KR:COL_G], cos, sin)
        yd_l = _attend_blocked(qn_l, qr_l,
                               jnp.concatenate([kn_c, kn_l], 1),
                               jnp.concatenate([kr_c, kr_l], 1),
                               jnp.concatenate([v_c, v_l], 1))
        mix_l = _merge(zl, _local_mixers(zl, p) + (yd_l,), p)
        if not last:
            qn_c, qr_c = _mla_queries(zc[..., COL_D:COL_KV], p)
            yd_c = _attend(qn_c, qr_c, kn_c, kr_c, v_c)
            mix_c = _merge(zc, _local_mixers(zc, p) + (yd_c,), p)
            xc = _ln(ALPHA * xc + mc[2] * mix_c, ln1_g[l], ln1_b[l])
        xl = _ln(ALPHA * xl + ml[2] * mix_l, ln1_g[l], ln1_b[l])

        hl = _modulate(xl, ml[3], ml[4]).reshape(-1, d)
        if last:
            fl = _hier_moe(hl, r_w_grp[l], r_b_grp[l], r_w_exp[l], r_b_exp[l],
                           e_w1[l], e_w3[l], e_w2[l]).reshape(b_, s_len, d)
        else:
            hc = _modulate(xc, mc[3], mc[4]).reshape(-1, d)
            f = _hier_moe(jnp.concatenate([hc, hl], 0), r_w_grp[l], r_b_grp[l], r_w_exp[l], r_b_exp[l],
                          e_w1[l], e_w3[l], e_w2[l])
            fc = f[:b_ * n_ctx].reshape(b_, n_ctx, d)
            fl = f[b_ * n_ctx:].reshape(b_, s_len, d)
            xc = _ln(ALPHA * xc + mc[5] * fc, ln2_g[l], ln2_b[l])
        xl = _ln(ALPHA * xl + ml[5] * fl, ln2_g[l], ln2_b[l])
    return xl
```

```python
import contextlib
import math
import numpy as np
import ml_dtypes
import concourse.bass as bass
import concourse.mybir as mybir
from concourse.bass_utils import run_bass_kernel_spmd

F32 = mybir.dt.float32
BF16 = mybir.dt.bfloat16
AF = mybir.ActivationFunctionType
ALU = mybir.AluOpType
AX = mybir.AxisListType
NCORES = 8

D_MODEL = 2048
SEQ = 8192
DEPTH = 2
GRID_W = 64
CTX_LEN = 256
BRANCH_W = 512
CHUNK = 128
CONV_W = 31
QK_NOPE = 64
QK_ROPE = 32
V_HEAD = 64
MLA_HEADS = 8
Q_LORA = 512
KV_LORA = 256
ROPE_THETA = 10000.0
MLA_SCALE = (QK_NOPE + QK_ROPE) ** -0.5
N_GROUPS = 4
EXP_PER_GROUP = 8
N_EXPERTS = 32
D_EXPERT = 1024
COL_A = 0
COL_B = 1024
COL_C = 2048
COL_D = 2560
COL_KV = 3072
COL_KR = 3328
COL_G = 3360
D_IN = COL_G + 4 * D_MODEL
ALPHA = (2 * DEPTH) ** 0.25
LN_EPS = 1e-6
TL = SEQ // NCORES


class Buf:
    __slots__ = ("t", "lw", "rd", "name")

    def __init__(self, t, name=""):
        self.t = t
        self.lw = None
        self.rd = {}
        self.name = name

    def __getitem__(self, k):
        return self.t[k]


class Sched:
    RING = 12

    def __init__(self, nc, es):
        self.nc = nc
        self.es = es
        self.engs = {"pe": nc.tensor, "act": nc.scalar, "dve": nc.vector, "pool": nc.gpsimd, "sp": nc.sync}
        self.sem = {e: es.enter_context(nc.semaphore("s_" + e)) for e in ("pe", "act", "dve", "pool")}
        self.cnt = {e: 0 for e in self.sem}
        self.dsem = {q: [es.enter_context(nc.semaphore("d_%s%d" % (q, i))) for i in range(self.RING)]
                     for q in ("sp", "pool")}
        self.dcnt = {"sp": 0, "pool": 0}
        self.waited = {e: {} for e in self.engs}
        self.n = 0

    def sb(self, name, shape, dt):
        return Buf(self.es.enter_context(self.nc.sbuf_tensor("sb%d_" % getattr(self, "uid", 0) + name, list(shape), dt)), name)

    def ps(self, name, shape, dt=F32):
        return Buf(self.es.enter_context(self.nc.psum_tensor("ps_" + name, list(shape), dt)), name)

    def _semval(self, tok):
        if tok[0] == "c":
            return ("c", tok[1]), self.sem[tok[1]], tok[2]
        q, idx = tok[1], tok[2]
        return ("d", q, idx % self.RING), self.dsem[q][idx % self.RING], 16 * (idx // self.RING + 1)

    def _wait(self, eng, toks):
        need = {}
        for tok in toks:
            if tok is None:
                continue
            if tok[0] == "c" and tok[1] == eng and eng == "pe":
                continue
            key, sem, val = self._semval(tok)
            if need.get(key, (None, 0))[1] < val:
                need[key] = (sem, val)
        w = self.waited[eng]
        for key, (sem, val) in need.items():
            if w.get(key, 0) >= val:
                continue
            self.engs[eng].wait_ge(sem, val)
            w[key] = val

    def _deps(self, reads, writes):
        toks = []
        for r in reads:
            toks.append(r.lw)
        for wv in writes:
            toks.append(wv.lw)
            toks.extend(wv.rd.values())
        return toks

    def _commit(self, tok, reads, writes, rkey):
        for r in reads:
            r.rd[rkey] = tok
        for wv in writes:
            wv.lw = tok
            wv.rd = {}

    def op(self, eng, fn, reads=(), writes=()):
        self._wait(eng, self._deps(reads, writes))
        ins = fn()
        self.cnt[eng] += 1
        ins.then_inc(self.sem[eng], 1)
        tok = ("c", eng, self.cnt[eng])
        self._commit(tok, reads, writes, eng)
        self.n += 1
        return tok

    def dma(self, q, out, in_, reads=(), writes=(), **kw):
        idx = self.dcnt[q]
        toks = self._deps(reads, writes)
        if idx >= self.RING:
            toks.append(("d", q, idx - self.RING))
        self._wait(q, toks)
        ins = self.engs[q].dma_start(out=out, in_=in_, **kw)
        ins.then_inc(self.dsem[q][idx % self.RING], 16)
        self.dcnt[q] += 1
        tok = ("d", q, idx)
        self._commit(tok, reads, writes, "dma_" + q + str(idx % self.RING))
        self.n += 1
        return tok

    def barrier(self):
        toks = []
        for q in ("sp", "pool"):
            for i in range(max(0, self.dcnt[q] - self.RING), self.dcnt[q]):
                toks.append(("d", q, i))
        for e in self.cnt:
            if self.cnt[e]:
                toks.append(("c", e, self.cnt[e]))
        for e in self.engs:
            self._wait(e, [t for t in toks if not (t[0] == "c" and t[1] == e)])

    @contextlib.contextmanager
    def scope(self):
        old = self.es
        with contextlib.ExitStack() as es2:
            self.es = es2
            self.uid = getattr(self, "uid", 0) + 1
            yield
            self.barrier()
        self.es = old

    def finish(self):
        toks = []
        for q in ("sp", "pool"):
            for i in range(max(0, self.dcnt[q] - self.RING), self.dcnt[q]):
                toks.append(("d", q, i))
        for e in self.cnt:
            if self.cnt[e]:
                toks.append(("c", e, self.cnt[e]))
        self._wait("sp", toks)


def new_prog():
    nc = bass.Bass("TRN2", target_bir_lowering=False)
    es = contextlib.ExitStack()
    return nc, es


def dram_in(nc, name, shape, dt=F32):
    return nc.dram_tensor(name, list(shape), dt, kind="ExternalInput").ap()


def dram_out(nc, name, shape, dt=F32):
    return nc.dram_tensor(name, list(shape), dt, kind="ExternalOutput").ap()


_PROG_CACHE = {}


def dram_tmp(nc, name, shape, dt=F32):
    return nc.dram_tensor(name, list(shape), dt).ap()


def rstd_of(S, m):
    nc = S.nc
    S.op("act", lambda: nc.scalar.activation(out=m[:, 3:4], in_=m[:, 3:4], func=AF.Sqrt), reads=[m], writes=[m])
    S.op("dve", lambda: nc.vector.reciprocal(out=m[:, 2:3], in_=m[:, 3:4]), reads=[m], writes=[m])


def ln_stats(S, x, K, tag, ring):
    nc = S.nc
    nch = (K + 511) // 512
    st, mv = ring["st"], ring["mv"]
    for c in range(nch):
        w = min(512, K - c * 512)
        S.op("dve", lambda c=c, w=w: nc.vector.bn_stats(out=st[:, c, :], in_=x[:, c * 512:c * 512 + w]),
             reads=[x], writes=[st])
    S.op("dve", lambda: nc.vector.bn_aggr(out=mv[:, 0:2], in_=st[:, 0:nch, :]), reads=[st], writes=[mv])
    S.op("dve", lambda: nc.vector.tensor_scalar(out=mv[:, 3:4], in0=mv[:, 1:2], scalar1=LN_EPS, scalar2=None,
                                                op0=ALU.add), reads=[mv], writes=[mv])
    rstd_of(S, mv)
    return mv


class Ctx:
    def __init__(self, nc, es, ident_d):
        self.nc = nc
        self.S = S = Sched(nc, es)
        self.pT = [S.ps("pT%d" % i, [128, 1024], BF16) for i in range(2)]
        self.acc = [S.ps("acc%d" % i, [128, 512], F32) for i in range(4)]
        self.pB = [S.ps("pB%d" % i, [128, 512], F32) for i in range(2)]
        self.identb = S.sb("identb", [128, 128], BF16)
        S.dma("pool", self.identb[:], ident_d, writes=[self.identb])
        self.identf = S.sb("identf", [128, 128], F32)
        S.dma("sp", self.identf[:], ident_d, writes=[self.identf])
        self.nT = 0
        self.nA = 0


def emit_linear(C, xs, w_ap, y_d, R, K, chunks, prologue="none", modrows=None, modsel=None, bias_d=None,
                mul_d=None, g_d=None, colscale=None, RB=16):
    S, nc = C.S, C.nc
    NT = R // 128
    KC = K // 128
    nsum = len(xs)
    with S.scope():
        hT = [S.sb("hT%d" % t, [128, KC, 128], BF16) for t in range(min(RB, NT))]
        xin = [S.sb("xin%d" % i, [128, K], F32) for i in range(2)]
        xaux = [S.sb("xaux%d" % i, [128, K], F32) for i in range(2)] if nsum > 1 else None
        hb = [S.sb("hb%d" % i, [128, K], BF16) for i in range(2)]
        st = [S.sb("st%d" % i, [128, 8, 6], F32) for i in range(2)]
        mv = [S.sb("mv%d" % i, [128, 4], F32) for i in range(2)]
        if prologue == "ln0mod":
            nsets = len(modrows)
            modA = S.sb("modA", [128, nsets, K], F32)
            modB = S.sb("modB", [128, nsets, K], F32)
            for s_, (ra, rb_) in enumerate(modrows):
                S.dma("sp", modA[:, s_, :], ra.partition_broadcast(128), writes=[modA])
                S.dma("sp", modB[:, s_, :], rb_.partition_broadcast(128), writes=[modB])
            S.op("dve", lambda: nc.vector.tensor_scalar(out=modA[:], in0=modA[:], scalar1=1.0, scalar2=None, op0=ALU.add),
                 reads=[modA], writes=[modA])
        if prologue == "rms":
            gb = S.sb("gb", [128, K], F32)
            S.dma("sp", gb[:], g_d.partition_broadcast(128), writes=[gb])
            junk = S.sb("junk", [128, K], F32)
        if prologue == "attn":
            oin = [S.sb("oin%d" % i, [128, MLA_HEADS, V_HEAD + 1], F32) for i in range(2)]
            rden = [S.sb("rden%d" % i, [128, MLA_HEADS, 1], F32) for i in range(2)]
        wb = [S.sb("wb%d" % i, [128, KC, 512], BF16) for i in range(2)]
        bb = [S.sb("bb%d" % i, [128, 512], F32) for i in range(2)] if bias_d is not None else None
        ot = [S.sb("ot%d" % i, [128, 512], F32) for i in range(3)]
        mt = [S.sb("mt%d" % i, [128, 512], F32) for i in range(3)] if mul_d is not None else None
        if colscale is not None:
            gcol_d, gfn = colscale
            ncol = gcol_d.shape[1]
            gt = [S.sb("gt%d" % t, [128, ncol], F32) for t in range(min(RB, NT))]
        wi = 0
        it = 0
        for t0 in range(0, NT, RB):
            tiles = list(range(t0, min(NT, t0 + RB)))
            for t in tiles:
                x = xin[t % 2]
                h = hb[t % 2]
                rows = slice(t * 128, (t + 1) * 128)
                if colscale is not None:
                    S.dma("sp", gt[t - t0][:], gcol_d[rows], writes=[gt[t - t0]])
                if prologue == "attn":
                    o = oin[t % 2]
                    rd_ = rden[t % 2]
                    S.dma("sp", o[:], xs[0][rows], writes=[o])
                    S.op("dve", lambda o=o, rd_=rd_: nc.vector.reciprocal(out=rd_[:], in_=o[:, :, V_HEAD:V_HEAD + 1]),
                         reads=[o], writes=[rd_])
                    for hh in range(MLA_HEADS):
                        S.op("dve", lambda o=o, rd_=rd_, h=h, hh=hh: nc.vector.tensor_scalar(
                            out=h[:, hh * V_HEAD:(hh + 1) * V_HEAD], in0=o[:, hh, 0:V_HEAD], scalar1=rd_[:, hh, :],
                            scalar2=None, op0=ALU.mult), reads=[o, rd_], writes=[h])
                else:
                    S.dma("sp", x[:], xs[0][rows], writes=[x])
                    for i in range(1, nsum):
                        xa = xaux[i % 2]
                        S.dma("sp", xa[:], xs[i][rows], writes=[xa])
                        S.op("dve", lambda x=x, xa=xa: nc.vector.tensor_tensor(out=x[:], in0=x[:], in1=xa[:], op=ALU.add),
                             reads=[x, xa], writes=[x])
                    if prologue == "none":
                        S.op("act", lambda x=x, h=h: nc.scalar.copy(out=h[:], in_=x[:]), reads=[x], writes=[h])
                    elif prologue == "silu":
                        S.op("act", lambda x=x, h=h: nc.scalar.activation(out=h[:], in_=x[:], func=AF.Silu),
                             reads=[x], writes=[h])
                    elif prologue == "ln0mod":
                        m = ln_stats(S, x, K, "p", {"st": st[t % 2], "mv": mv[t % 2]})
                        ms = modsel(t)
                        S.op("dve", lambda x=x, m=m: nc.vector.tensor_scalar(
                            out=x[:], in0=x[:], scalar1=m[:, 0:1], scalar2=m[:, 2:3], op0=ALU.subtract, op1=ALU.mult),
                            reads=[x, m], writes=[x])
                        S.op("dve", lambda x=x, ms=ms: nc.vector.tensor_tensor(out=x[:], in0=x[:], in1=modA[:, ms, :],
                                                                               op=ALU.mult), reads=[x, modA], writes=[x])
                        S.op("dve", lambda x=x, h=h, ms=ms: nc.vector.tensor_tensor(
                            out=h[:], in0=x[:], in1=modB[:, ms, :], op=ALU.add), reads=[x, modB], writes=[h])
                    elif prologue == "rms":
                        m = mv[t % 2]
                        S.op("act", lambda x=x, m=m: nc.scalar.activation(out=junk[:], in_=x[:], func=AF.Square,
                                                                          accum_out=m[:, 0:1]),
                             reads=[x], writes=[junk, m])
                        S.op("dve", lambda m=m: nc.vector.tensor_scalar(out=m[:, 3:4], in0=m[:, 0:1], scalar1=1.0 / K,
                                                                        scalar2=LN_EPS, op0=ALU.mult, op1=ALU.add),
                             reads=[m], writes=[m])
                        rstd_of(S, m)
                        S.op("dve", lambda x=x, h=h, m=m: nc.vector.scalar_tensor_tensor(
                            out=h[:], in0=x[:], scalar=m[:, 2:3], in1=gb[:], op0=ALU.mult, op1=ALU.mult),
                            reads=[x, m, gb], writes=[h])
                for k0 in range(0, KC, 8):
                    kn = min(8, KC - k0)
                    p = C.pT[C.nT % 2]
                    C.nT += 1
                    for j in range(kn):
                        S.op("pe", lambda p=p, h=h, j=j, k0=k0: nc.tensor.transpose(
                            out=p[:, j * 128:(j + 1) * 128], in_=h[:, (k0 + j) * 128:(k0 + j + 1) * 128],
                            identity=C.identb[:]), reads=[h, C.identb], writes=[p])
                    S.op("act", lambda p=p, t=t, k0=k0, kn=kn: nc.scalar.copy(
                        out=hT[t - t0][:, k0:k0 + kn, :], in_=p[:, 0:kn * 128].rearrange("p (a b) -> p a b", b=128)),
                        reads=[p], writes=[hT[t - t0]])
            for ci, (c0, nw, act) in enumerate(chunks):
                w = wb[wi % 2]
                wi += 1
                wv = w_ap(ci).rearrange("(kc p) n -> p kc n", p=128)
                for k0 in range(0, KC, 16):
                    k1 = min(KC, k0 + 16)
                    S.dma("pool", w[:, k0:k1, 0:nw], wv[:, k0:k1, :], writes=[w])
                if bias_d is not None:
                    b = bb[wi % 2]
                    S.dma("sp", b[:, 0:nw], bias_d[c0:c0 + nw].partition_broadcast(128), writes=[b])
                for t in tiles:
                    a = C.acc[C.nA % 4]
                    C.nA += 1
                    o = ot[it % 3]
                    for kc in range(KC):
                        S.op("pe", lambda a=a, w=w, t=t, kc=kc, nw=nw: nc.tensor.matmul(
                            out=a[:, 0:nw], lhsT=hT[t - t0][:, kc, :], rhs=w[:, kc, 0:nw], start=(kc == 0),
                            stop=(kc == KC - 1)), reads=[hT[t - t0], w], writes=[a])
                    if bias_d is not None:
                        S.op("dve", lambda a=a, o=o, b=b, nw=nw: nc.vector.tensor_tensor(
                            out=o[:, 0:nw], in0=a[:, 0:nw], in1=b[:, 0:nw], op=ALU.add), reads=[a, b], writes=[o])
                        if act is not None:
                            S.op("act", lambda o=o, nw=nw, act=act: nc.scalar.activation(
                                out=o[:, 0:nw], in_=o[:, 0:nw], func=act), reads=[o], writes=[o])
                    else:
                        S.op("act", lambda a=a, o=o, nw=nw, act=act: nc.scalar.activation(
                            out=o[:, 0:nw], in_=a[:, 0:nw], func=(act if act is not None else AF.Copy)),
                            reads=[a], writes=[o])
                    if colscale is not None:
                        e_ = gfn(ci)
                        S.op("dve", lambda o=o, t=t, e_=e_, nw=nw: nc.vector.tensor_scalar(
                            out=o[:, 0:nw], in0=o[:, 0:nw], scalar1=gt[t - t0][:, e_:e_ + 1], scalar2=None, op0=ALU.mult),
                            reads=[o, gt[t - t0]], writes=[o])
                    if mul_d is not None:
                        m_ = mt[it % 3]
                        S.dma("sp", m_[:, 0:nw], mul_d[t * 128:(t + 1) * 128, c0:c0 + nw], writes=[m_])
                        S.op("dve", lambda o=o, m_=m_, nw=nw: nc.vector.tensor_tensor(
                            out=o[:, 0:nw], in0=o[:, 0:nw], in1=m_[:, 0:nw], op=ALU.mult), reads=[o, m_], writes=[o])
                    S.dma("sp", y_d[t * 128:(t + 1) * 128, c0:c0 + nw], o[:, 0:nw], reads=[o])
                    it += 1


def emit_resln(C, x_d, fs, m_rows, msel, g_row, b_row, y_d, R):
    S, nc = C.S, C.nc
    D = D_MODEL
    with S.scope():
        mm = S.sb("mm", [128, len(m_rows), D], F32)
        for i, r in enumerate(m_rows):
            S.dma("sp", mm[:, i, :], r.partition_broadcast(128), writes=[mm])
        gb = S.sb("g", [128, D], F32)
        bb = S.sb("b", [128, D], F32)
        S.dma("sp", gb[:], g_row.partition_broadcast(128), writes=[gb])
        S.dma("sp", bb[:], b_row.partition_broadcast(128), writes=[bb])
        xt = [S.sb("x%d" % i, [128, D], F32) for i in range(2)]
        ft = [S.sb("f%d" % i, [128, D], F32) for i in range(2)]
        fa = [S.sb("fa%d" % i, [128, D], F32) for i in range(2)]
        st = [S.sb("st%d" % i, [128, 8, 6], F32) for i in range(2)]
        mv = [S.sb("mv%d" % i, [128, 4], F32) for i in range(2)]
        for t in range(R // 128):
            rows = slice(t * 128, (t + 1) * 128)
            x, f = xt[t % 2], ft[t % 2]
            S.dma("sp", x[:], x_d[rows], writes=[x])
            S.dma("sp", f[:], fs[0][rows], writes=[f])
            for i in range(1, len(fs)):
                a = fa[i % 2]
                S.dma("sp", a[:], fs[i][rows], writes=[a])
                S.op("dve", lambda f=f, a=a: nc.vector.tensor_tensor(out=f[:], in0=f[:], in1=a[:], op=ALU.add),
                     reads=[f, a], writes=[f])
            ms = msel(t)
            S.op("dve", lambda f=f, ms=ms: nc.vector.tensor_tensor(out=f[:], in0=f[:], in1=mm[:, ms, :], op=ALU.mult),
                 reads=[f, mm], writes=[f])
            S.op("dve", lambda x=x, f=f: nc.vector.scalar_tensor_tensor(out=x[:], in0=x[:], scalar=ALPHA, in1=f[:],
                                                                        op0=ALU.mult, op1=ALU.add),
                 reads=[x, f], writes=[x])
            m = ln_stats(S, x, D, "r", {"st": st[t % 2], "mv": mv[t % 2]})
            S.op("dve", lambda x=x, m=m: nc.vector.tensor_scalar(out=x[:], in0=x[:], scalar1=m[:, 0:1], scalar2=m[:, 2:3],
                                                                 op0=ALU.subtract, op1=ALU.mult), reads=[x, m], writes=[x])
            S.op("dve", lambda x=x: nc.vector.tensor_tensor(out=x[:], in0=x[:], in1=gb[:], op=ALU.mult),
                 reads=[x, gb], writes=[x])
            S.op("dve", lambda x=x: nc.vector.tensor_tensor(out=x[:], in0=x[:], in1=bb[:], op=ALU.add),
                 reads=[x, bb], writes=[x])
            S.dma("sp", y_d[rows], x[:], reads=[x])


def gelu_tanh(S, x, t, W):
    nc = S.nc
    S.op("dve", lambda: nc.vector.tensor_tensor(out=t[:, 0:W], in0=x[:, 0:W], in1=x[:, 0:W], op=ALU.mult), reads=[x], writes=[t])
    S.op("dve", lambda: nc.vector.tensor_scalar(out=t[:, 0:W], in0=t[:, 0:W], scalar1=0.044715, scalar2=1.0, op0=ALU.mult,
                                                op1=ALU.add), reads=[t], writes=[t])
    S.op("dve", lambda: nc.vector.tensor_tensor(out=t[:, 0:W], in0=t[:, 0:W], in1=x[:, 0:W], op=ALU.mult), reads=[t, x], writes=[t])
    S.op("act", lambda: nc.scalar.activation(out=t[:, 0:W], in_=t[:, 0:W], func=AF.Sigmoid, scale=1.5957691216057308),
         reads=[t], writes=[t])
    S.op("dve", lambda: nc.vector.tensor_tensor(out=x[:, 0:W], in0=x[:, 0:W], in1=t[:, 0:W], op=ALU.mult), reads=[t, x], writes=[x])


def ln_rows(S, x, W, st, mv, gb, bb, out=None):
    nc = S.nc
    out = out if out is not None else x
    m = ln_stats(S, x, W, "l", {"st": st, "mv": mv})
    S.op("dve", lambda: nc.vector.tensor_scalar(out=x[:, 0:W], in0=x[:, 0:W], scalar1=m[:, 0:1], scalar2=m[:, 2:3],
                                                op0=ALU.subtract, op1=ALU.mult), reads=[x, m], writes=[x])
    S.op("dve", lambda: nc.vector.tensor_tensor(out=x[:, 0:W], in0=x[:, 0:W], in1=gb[:, 0:W], op=ALU.mult), reads=[x, gb], writes=[x])
    S.op("dve", lambda: nc.vector.tensor_tensor(out=out[:, 0:W], in0=x[:, 0:W], in1=bb[:, 0:W], op=ALU.add),
         reads=[x, bb], writes=[out])


def emit_chunkgate(C, z_d, lng, lnb, ws_d, bs_d, y_d, R):
    S, nc = C.S, C.nc
    with S.scope():
        gb = S.sb("g", [128, 512], F32)
        bb = S.sb("b", [128, 512], F32)
        S.dma("sp", gb[:], lng.partition_broadcast(128), writes=[gb])
        S.dma("sp", bb[:], lnb.partition_broadcast(128), writes=[bb])
        wraw = S.sb("wraw", [128, 4, 128], BF16)
        wT = S.sb("wT", [128, 4, 128], BF16)
        S.dma("pool", wraw[:], ws_d.rearrange("g p q -> p g q"), writes=[wraw])
        p = C.pT[C.nT % 2]
        C.nT += 1
        for g in range(4):
            S.op("pe", lambda g=g: nc.tensor.transpose(out=p[:, g * 128:(g + 1) * 128], in_=wraw[:, g, :], identity=C.identb[:]),
                 reads=[wraw, C.identb], writes=[p])
        S.op("act", lambda: nc.scalar.copy(out=wT[:], in_=p[:, 0:512].rearrange("p (a b) -> p a b", b=128)), reads=[p], writes=[wT])
        bs = S.sb("bs", [128, 4], F32)
        S.dma("sp", bs[:], bs_d.rearrange("g p -> p g"), writes=[bs], allow_slow_non_contiguous=True)
        uv = [S.sb("uv%d" % i, [128, 1024], F32) for i in range(2)]
        tt = [S.sb("tt%d" % i, [128, 1024], F32) for i in range(2)]
        vb = [S.sb("vb%d" % i, [128, 512], BF16) for i in range(2)]
        ot = [S.sb("ot%d" % i, [128, 512], F32) for i in range(2)]
        st = [S.sb("st%d" % i, [128, 8, 6], F32) for i in range(2)]
        mv = [S.sb("mv%d" % i, [128, 4], F32) for i in range(2)]
        for t in range(R // 128):
            rows = slice(t * 128, (t + 1) * 128)
            x, tmp, v, o = uv[t % 2], tt[t % 2], vb[t % 2], ot[t % 2]
            S.dma("sp", x[:], z_d[rows, 0:1024], writes=[x])
            gelu_tanh(S, x, tmp, 1024)
            m = ln_stats(S, Buf(x.t[:, 512:1024]) if False else x, 512, "c", {"st": st[t % 2], "mv": mv[t % 2]}) if False else None
            nchs = 1
            S.op("dve", lambda x=x, t=t: nc.vector.bn_stats(out=st[t % 2][:, 0, :], in_=x[:, 512:1024]), reads=[x], writes=[st[t % 2]])
            mvt = mv[t % 2]
            S.op("dve", lambda t=t, mvt=mvt: nc.vector.bn_aggr(out=mvt[:, 0:2], in_=st[t % 2][:, 0:1, :]), reads=[st[t % 2]], writes=[mvt])
            S.op("dve", lambda mvt=mvt: nc.vector.tensor_scalar(out=mvt[:, 3:4], in0=mvt[:, 1:2], scalar1=LN_EPS, scalar2=None,
                                                                op0=ALU.add), reads=[mvt], writes=[mvt])
            rstd_of(S, mvt)
            S.op("dve", lambda x=x, mvt=mvt: nc.vector.tensor_scalar(out=x[:, 512:1024], in0=x[:, 512:1024], scalar1=mvt[:, 0:1],
                                                                     scalar2=mvt[:, 2:3], op0=ALU.subtract, op1=ALU.mult),
                 reads=[x, mvt], writes=[x])
            S.op("dve", lambda x=x: nc.vector.tensor_tensor(out=x[:, 512:1024], in0=x[:, 512:1024], in1=gb[:], op=ALU.mult),
                 reads=[x, gb], writes=[x])
            S.op("dve", lambda x=x, v=v: nc.vector.tensor_tensor(out=v[:], in0=x[:, 512:1024], in1=bb[:], op=ALU.add),
                 reads=[x, bb], writes=[v])
            a = C.acc[C.nA % 4]
            C.nA += 1
            for g in range(4):
                S.op("pe", lambda a=a, v=v, g=g: nc.tensor.matmul(out=a[:, g * 128:(g + 1) * 128], lhsT=wT[:, g, :],
                                                                  rhs=v[:, g * 128:(g + 1) * 128], start=True, stop=True),
                     reads=[wT, v], writes=[a])
            for g in range(4):
                S.op("dve", lambda a=a, o=o, x=x, g=g: nc.vector.scalar_tensor_tensor(
                    out=o[:, g * 128:(g + 1) * 128], in0=a[:, g * 128:(g + 1) * 128], scalar=bs[:, g:g + 1],
                    in1=x[:, g * 128:(g + 1) * 128], op0=ALU.add, op1=ALU.mult), reads=[a, bs, x], writes=[o])
            S.dma("sp", y_d[rows], o[:], reads=[o])


def emit_conv(C, z_d, wdw_d, bdw_d, lng, lnb, y_d, segs):
    S, nc = C.S, C.nc
    with S.scope():
        gb = S.sb("g", [128, 512], F32)
        bb = S.sb("b", [128, 512], F32)
        S.dma("sp", gb[:], lng.partition_broadcast(128), writes=[gb])
        S.dma("sp", bb[:], lnb.partition_broadcast(128), writes=[bb])
        wd = S.sb("wd", [128, 4, CONV_W], F32)
        bd = S.sb("bd", [128, 4], F32)
        for c in range(4):
            S.dma("sp", wd[:, c, :], wdw_d[:, c * 128:(c + 1) * 128].rearrange("k p -> p k"), writes=[wd],
                  allow_slow_non_contiguous=True)
            S.dma("sp", bd[:, c:c + 1], bdw_d[c * 128:(c + 1) * 128].rearrange("(p o) -> p o", o=1), writes=[bd],
                  allow_slow_non_contiguous=True)
        yT = S.sb("yT", [128, 4, 1280], F32)
        cacc = S.sb("cacc", [128, 4, 1024], F32)
        ab = [S.sb("ab%d" % i, [128, 1024], F32) for i in range(2)]
        yy = [S.sb("yy%d" % i, [128, 512], F32) for i in range(2)]
        ot = [S.sb("ot%d" % i, [128, 512], F32) for i in range(2)]
        st = [S.sb("st%d" % i, [128, 8, 6], F32) for i in range(2)]
        mv = [S.sb("mv%d" % i, [128, 4], F32) for i in range(2)]
        it = 0
        for (r0, nr) in segs:
            for b0 in range(0, nr, 1024):
                bl = min(1024, nr - b0)
                nti = bl // 128
                S.op("pool", lambda: nc.gpsimd.memset(yT[:], 0.0), writes=[yT])
                for j in range(-1, nti + 1):
                    tr = b0 + j * 128
                    if tr < 0 or tr >= nr:
                        continue
                    x, y = ab[it % 2], yy[it % 2]
                    it += 1
                    S.dma("sp", x[:], z_d[r0 + tr:r0 + tr + 128, 1024:2048], writes=[x])
                    S.op("dve", lambda x=x, y=y: nc.vector.tensor_tensor(out=y[:], in0=x[:, 0:512], in1=x[:, 512:1024], op=ALU.mult),
                         reads=[x], writes=[y])
                    a = C.acc[C.nA % 4]
                    C.nA += 1
                    for c in range(4):
                        S.op("pe", lambda a=a, y=y, c=c: nc.tensor.transpose(out=a[:, c * 128:(c + 1) * 128],
                                                                             in_=y[:, c * 128:(c + 1) * 128], identity=C.identf[:]),
                             reads=[y, C.identf], writes=[a])
                    S.op("act", lambda a=a, j=j: nc.scalar.copy(out=yT[:, :, (j + 1) * 128:(j + 2) * 128],
                                                                in_=a[:, 0:512].rearrange("p (c t) -> p c t", t=128)),
                         reads=[a], writes=[yT])
                for c in range(4):
                    for k in range(CONV_W):
                        src = yT[:, c, 113 + k:113 + k + bl]
                        if k == 0:
                            S.op("dve", lambda c=c, src=src: nc.vector.tensor_scalar(
                                out=cacc[:, c, 0:bl], in0=src, scalar1=wd[:, c, 0:1], scalar2=bd[:, c:c + 1], op0=ALU.mult,
                                op1=ALU.add), reads=[yT, wd, bd], writes=[cacc])
                        else:
                            S.op("dve", lambda c=c, k=k, src=src: nc.vector.scalar_tensor_tensor(
                                out=cacc[:, c, 0:bl], in0=src, scalar=wd[:, c, k:k + 1], in1=cacc[:, c, 0:bl], op0=ALU.mult,
                                op1=ALU.add), reads=[yT, wd, cacc], writes=[cacc])
                for j in range(nti):
                    a = C.acc[C.nA % 4]
                    C.nA += 1
                    o = ot[j % 2]
                    for c in range(4):
                        S.op("pe", lambda a=a, c=c, j=j: nc.tensor.transpose(out=a[:, c * 128:(c + 1) * 128],
                                                                             in_=cacc[:, c, j * 128:(j + 1) * 128], identity=C.identf[:]),
                             reads=[cacc, C.identf], writes=[a])
                    S.op("act", lambda a=a, o=o: nc.scalar.copy(out=o[:], in_=a[:]), reads=[a], writes=[o])
                    ln_rows(S, o, 512, st[j % 2], mv[j % 2], gb, bb)
                    S.op("act", lambda o=o: nc.scalar.activation(out=o[:], in_=o[:], func=AF.Silu), reads=[o], writes=[o])
                    S.dma("sp", y_d[r0 + b0 + j * 128:r0 + b0 + (j + 1) * 128], o[:], reads=[o])


def emit_rope(C, src_d, c0, H, stride, dst_d, d0, cos_d, sin_d, nrows):
    S, nc = C.S, C.nc
    W = (H - 1) * stride + 32
    with S.scope():
        xt = [S.sb("x%d" % i, [128, W], F32) for i in range(2)]
        cs = [S.sb("cs%d" % i, [128, 32], F32) for i in range(2)]
        sn = [S.sb("sn%d" % i, [128, 32], F32) for i in range(2)]
        t1 = [S.sb("t1%d" % i, [128, 32], F32) for i in range(2)]
        for t in range(nrows // 128):
            rows = slice(t * 128, (t + 1) * 128)
            x, c_, s_, u = xt[t % 2], cs[t % 2], sn[t % 2], t1[t % 2]
            S.dma("sp", x[:], src_d[rows, c0:c0 + W], writes=[x])
            S.dma("sp", c_[:], cos_d[rows], writes=[c_])
            S.dma("sp", s_[:], sin_d[rows], writes=[s_])
            for h in range(H):
                o = h * stride
                for (a0, b0_) in ((0, 8), (8, 0), (16, 24), (24, 16)):
                    S.op("dve", lambda x=x, s_=s_, u=u, o=o, a0=a0, b0_=b0_: nc.vector.tensor_tensor(
                        out=u[:, a0:a0 + 8], in0=x[:, o + b0_:o + b0_ + 8], in1=s_[:, a0:a0 + 8], op=ALU.mult),
                        reads=[x, s_], writes=[u])
                S.op("dve", lambda x=x, c_=c_, o=o: nc.vector.tensor_tensor(out=x[:, o:o + 32], in0=x[:, o:o + 32], in1=c_[:], op=ALU.mult),
                     reads=[x, c_], writes=[x])
                S.op("dve", lambda x=x, u=u, o=o: nc.vector.tensor_tensor(out=x[:, o:o + 32], in0=x[:, o:o + 32], in1=u[:], op=ALU.add),
                     reads=[x, u], writes=[x])
            S.dma("sp", dst_d[rows, d0:d0 + W], x[:], reads=[x])


def emit_attention(C, q_d, kv_d, kr_d, o_d, q_rows, k_rows):
    S, nc = C.S, C.nc
    ktiles = [r0 + i * 128 for (r0, n) in k_rows for i in range(n // 128)]
    NKT = len(ktiles)
    q0, nq = q_rows
    NQT = nq // 128
    DK = QK_NOPE + QK_ROPE
    with S.scope():
        KT = S.sb("KT", [DK, NKT * 128], BF16)
        QT = S.sb("QT", [DK, NQT * 128], BF16)
        Vt = S.sb("Vt", [128, NKT, V_HEAD + 1], BF16)
        kin = [S.sb("kin%d" % i, [128, DK], F32) for i in range(2)]
        kb = [S.sb("kb%d" % i, [128, DK], BF16) for i in range(2)]
        pt = [S.sb("pt%d" % i, [128, 512], BF16) for i in range(3)]
        of = [S.sb("of%d" % i, [V_HEAD + 1, 512], F32) for i in range(2)]
        oo = [S.sb("oo%d" % i, [128, 4, V_HEAD + 1], F32) for i in range(2)]
        ones = S.sb("ones", [128, NKT, 1], F32)
        S.op("pool", lambda: nc.gpsimd.memset(ones[:], 1.0), writes=[ones])
        S.op("act", lambda: nc.scalar.copy(out=Vt[:, :, V_HEAD:V_HEAD + 1], in_=ones[:]), reads=[ones], writes=[Vt])
        n_ = 0
        for h in range(MLA_HEADS):
            for i, r in enumerate(ktiles):
                x, xb = kin[n_ % 2], kb[n_ % 2]
                n_ += 1
                S.dma("sp", x[:, 0:QK_NOPE], kv_d[r:r + 128, h * QK_NOPE:(h + 1) * QK_NOPE], writes=[x])
                S.dma("sp", x[:, QK_NOPE:DK], kr_d[r:r + 128, :], writes=[x])
                S.dma("pool", Vt[:, i, 0:V_HEAD], kv_d[r:r + 128, 512 + h * V_HEAD:512 + (h + 1) * V_HEAD], writes=[Vt])
                S.op("act", lambda x=x, xb=xb: nc.scalar.copy(out=xb[:], in_=x[:]), reads=[x], writes=[xb])
                p = C.pT[C.nT % 2]
                C.nT += 1
                S.op("pe", lambda p=p, xb=xb: nc.tensor.transpose(out=p[0:DK, 0:128], in_=xb[:], identity=C.identb[:]),
                     reads=[xb, C.identb], writes=[p])
                S.op("act", lambda p=p, i=i: nc.scalar.copy(out=KT[:, i * 128:(i + 1) * 128], in_=p[0:DK, 0:128]),
                     reads=[p], writes=[KT])
            for i in range(NQT):
                r = q0 + i * 128
                x, xb = kin[n_ % 2], kb[n_ % 2]
                n_ += 1
                S.dma("sp", x[:], q_d[r:r + 128, h * DK:(h + 1) * DK], writes=[x])
                S.op("act", lambda x=x, xb=xb: nc.scalar.copy(out=xb[:], in_=x[:]), reads=[x], writes=[xb])
                p = C.pT[C.nT % 2]
                C.nT += 1
                S.op("pe", lambda p=p, xb=xb: nc.tensor.transpose(out=p[0:DK, 0:128], in_=xb[:], identity=C.identb[:]),
                     reads=[xb, C.identb], writes=[p])
                S.op("act", lambda p=p, i=i: nc.scalar.copy(out=QT[:, i * 128:(i + 1) * 128], in_=p[0:DK, 0:128]),
                     reads=[p], writes=[QT])
            for qb in range(0, NQT * 128, 512):
                qw = min(512, NQT * 128 - qb)
                ao = C.pB[(qb // 512) % 2]
                for i in range(NKT):
                    a = C.acc[C.nA % 4]
                    C.nA += 1
                    pp = pt[i % 3]
                    S.op("pe", lambda a=a, i=i, qb=qb, qw=qw: nc.tensor.matmul(out=a[:, 0:qw], lhsT=KT[:, i * 128:(i + 1) * 128],
                                                                              rhs=QT[:, qb:qb + qw], start=True, stop=True),
                         reads=[KT, QT], writes=[a])
                    S.op("act", lambda a=a, pp=pp, qw=qw: nc.scalar.activation(out=pp[:, 0:qw], in_=a[:, 0:qw], func=AF.Exp,
                                                                              scale=MLA_SCALE), reads=[a], writes=[pp])
                    S.op("pe", lambda ao=ao, pp=pp, i=i, qw=qw: nc.tensor.matmul(out=ao[0:V_HEAD + 1, 0:qw], lhsT=Vt[:, i, :],
                                                                                rhs=pp[:, 0:qw], start=(i == 0), stop=(i == NKT - 1)),
                         reads=[Vt, pp], writes=[ao])
                o1 = of[(qb // 512) % 2]
                o2 = oo[(qb // 512) % 2]
                S.op("act", lambda ao=ao, o1=o1, qw=qw: nc.scalar.copy(out=o1[:, 0:qw], in_=ao[0:V_HEAD + 1, 0:qw]), reads=[ao], writes=[o1])
                a = C.acc[C.nA % 4]
                C.nA += 1
                nsub = qw // 128
                for j in range(nsub):
                    S.op("pe", lambda a=a, o1=o1, j=j: nc.tensor.transpose(out=a[:, j * 65:(j + 1) * 65], in_=o1[:, j * 128:(j + 1) * 128],
                                                                           identity=C.identf[0:V_HEAD + 1, 0:V_HEAD + 1]),
                         reads=[o1, C.identf], writes=[a])
                S.op("dve", lambda a=a, o2=o2, nsub=nsub: nc.vector.tensor_copy(
                    out=o2[:, 0:nsub, :], in_=a[:, 0:nsub * 65].rearrange("p (j d) -> p j d", d=65)), reads=[a], writes=[o2])
                for j in range(nsub):
                    r = q0 + qb + j * 128
                    S.dma("sp", o_d[r:r + 128, h, :], o2[:, j, :], reads=[o2])


def emit_route(C, lg_d, G_d, R):
    S, nc = C.S, C.nc
    with S.scope():
        L = [S.sb("L%d" % i, [128, 36], F32) for i in range(2)]
        W = [S.sb("W%d" % i, [128, 64], F32) for i in range(2)]
        G = [S.sb("G%d" % i, [128, 32], F32) for i in range(2)]
        for t in range(R // 128):
            rows = slice(t * 128, (t + 1) * 128)
            l, w, g = L[t % 2], W[t % 2], G[t % 2]
            S.dma("sp", l[:], lg_d[rows], writes=[l])
            S.op("dve", lambda l=l, w=w: nc.vector.tensor_reduce(out=w[:, 0:1], in_=l[:, 0:4], axis=AX.X, op=ALU.max), reads=[l], writes=[w])
            S.op("dve", lambda l=l, w=w: nc.vector.tensor_scalar(out=w[:, 4:8], in0=l[:, 0:4], scalar1=w[:, 0:1], scalar2=None, op0=ALU.is_ge),
                 reads=[l, w], writes=[w])
            S.op("dve", lambda w=w: nc.vector.tensor_scalar(out=w[:, 1:2], in0=w[:, 0:1], scalar1=-1.0, scalar2=None, op0=ALU.mult),
                 reads=[w], writes=[w])
            S.op("act", lambda l=l, w=w: nc.scalar.activation(out=w[:, 44:48], in_=l[:, 0:4], func=AF.Exp, bias=w[:, 1:2], scale=1.0),
                 reads=[l, w], writes=[w])
            S.op("dve", lambda w=w: nc.vector.tensor_reduce(out=w[:, 2:3], in_=w[:, 44:48], axis=AX.X, op=ALU.add), reads=[w], writes=[w])
            S.op("dve", lambda w=w: nc.vector.reciprocal(out=w[:, 3:4], in_=w[:, 2:3]), reads=[w], writes=[w])
            S.op("dve", lambda l=l, w=w: nc.vector.tensor_scalar(out=w[:, 8:16], in0=l[:, 4:12], scalar1=w[:, 4:5], scalar2=None, op0=ALU.mult),
                 reads=[l, w], writes=[w])
            for gi in range(1, 4):
                S.op("dve", lambda l=l, w=w, gi=gi: nc.vector.scalar_tensor_tensor(
                    out=w[:, 8:16], in0=l[:, 4 + gi * 8:12 + gi * 8], scalar=w[:, 4 + gi:5 + gi], in1=w[:, 8:16], op0=ALU.mult, op1=ALU.add),
                    reads=[l, w], writes=[w])
            S.op("dve", lambda w=w: nc.vector.max(out=w[:, 16:24], in_=w[:, 8:16]), reads=[w], writes=[w])
            S.op("dve", lambda w=w: nc.vector.tensor_scalar(out=w[:, 24:32], in0=w[:, 8:16], scalar1=w[:, 17:18], scalar2=None, op0=ALU.is_ge),
                 reads=[w], writes=[w])
            S.op("dve", lambda w=w: nc.vector.tensor_scalar(out=w[:, 40:41], in0=w[:, 16:17], scalar1=-1.0, scalar2=None, op0=ALU.mult),
                 reads=[w], writes=[w])
            S.op("act", lambda w=w: nc.scalar.activation(out=w[:, 32:40], in_=w[:, 8:16], func=AF.Exp, bias=w[:, 40:41], scale=1.0),
                 reads=[w], writes=[w])
            S.op("act", lambda w=w: nc.scalar.activation(out=w[:, 41:42], in_=w[:, 17:18], func=AF.Exp, bias=w[:, 40:41], scale=1.0),
                 reads=[w], writes=[w])
            S.op("dve", lambda w=w: nc.vector.tensor_scalar(out=w[:, 41:42], in0=w[:, 41:42], scalar1=1.0, scalar2=None, op0=ALU.add),
                 reads=[w], writes=[w])
            S.op("dve", lambda w=w: nc.vector.reciprocal(out=w[:, 42:43], in_=w[:, 41:42]), reads=[w], writes=[w])
            S.op("dve", lambda w=w: nc.vector.tensor_tensor(out=w[:, 42:43], in0=w[:, 42:43], in1=w[:, 3:4], op=ALU.mult), reads=[w], writes=[w])
            S.op("dve", lambda w=w: nc.vector.tensor_tensor(out=w[:, 24:32], in0=w[:, 24:32], in1=w[:, 32:40], op=ALU.mult), reads=[w], writes=[w])
            S.op("dve", lambda w=w: nc.vector.tensor_scalar(out=w[:, 24:32], in0=w[:, 24:32], scalar1=w[:, 42:43], scalar2=None, op0=ALU.mult),
                 reads=[w], writes=[w])
            for gi in range(4):
                S.op("dve", lambda w=w, g=g, gi=gi: nc.vector.tensor_scalar(out=g[:, gi * 8:(gi + 1) * 8], in0=w[:, 24:32],
                                                                            scalar1=w[:, 4 + gi:5 + gi], scalar2=None, op0=ALU.mult),
                     reads=[w], writes=[g])
            S.dma("sp", G_d[rows], g[:], reads=[g])


WNAMES = ["w_ada", "b_ada", "w_in", "b_in", "a_ln_g", "a_ln_b", "a_w_s", "a_b_s", "b_w_dw", "b_b_dw", "b_ln_g", "b_ln_b",
          "d_q_g", "d_w_uq", "d_kv_g", "w_branch", "w_o", "ln1_g", "ln1_b", "e_w1", "e_w3", "e_w2", "ln2_g", "ln2_b"]


def build_program(shapes, RL, NCTX):
    R = RL + NCTX
    D = D_MODEL
    nc, es = new_prog()
    I = {}
    for k, shp in shapes.items():
        I[k] = dram_in(nc, k, shp)
    y_out = dram_out(nc, "y", [RL, D])
    T = lambda n, s: dram_tmp(nc, n, s)
    mod_d = [T("mod%d" % l, [128, 6 * D]) for l in range(DEPTH)]
    z_d = T("z", [R, COL_G])
    zg_d = [T("zg%d" % i, [R, D]) for i in range(4)]
    brA, brB = T("brA", [R, 512]), T("brB", [R, 512])
    PQ = T("PQ", [R, 1024])
    brC = [T("brC%d" % p, [R, 512]) for p in range(4)]
    q_d, kv_d, kr_d = T("q", [R, 768]), T("kv", [R, 1024]), T("kr", [R, 32])
    o_d = T("o", [R, MLA_HEADS, V_HEAD + 1])
    GP = [T("GP%d" % i, [R, D]) for i in range(4)]
    mix_d, x1_d, xb_d = T("mix", [R, D]), T("x1", [R, D]), T("xb", [R, D])
    lg_d, G_d = T("lg", [R, 36]), T("G", [R, 32])
    S1_d = [T("S1_%d" % p, [R, 4096]) for p in range(8)]
    H_d = [T("H_%d" % p, [R, 4096]) for p in range(8)]
    f_d = [T("f%d" % p, [R, D]) for p in range(8)]
    NTL = RL // 128
    msel = lambda t: 0 if t < NTL else 1
    with es:
        C = Ctx(nc, es, I["ident"])
        S = C.S
        c512 = [(i * 512, 512, None) for i in range(4)]
        for l in range(DEPTH):
            emit_linear(C, [I["cc"]], lambda ci, l=l: I["w_ada"][l][:, ci * 512:(ci + 1) * 512], mod_d[l], 128, D,
                        [(i * 512, 512, None) for i in range(24)], prologue="silu", bias_d=I["b_ada"][l])
        x_cur = I["x_all"]
        for l in range(DEPTH):
            last = l == DEPTH - 1
            Rm = RL if last else R
            mr = lambda who, i, l=l: mod_d[l][who:who + 1, i * D:(i + 1) * D]
            chunks_in = []
            for c0 in range(0, COL_KV, 512):
                chunks_in.append((c0, 512, AF.Sigmoid if c0 == 1536 else None))
            chunks_in.append((COL_KV, COL_G - COL_KV, None))
            modr1 = [(mr(0, 1), mr(0, 0)), (mr(1, 1), mr(1, 0))]
            emit_linear(C, [x_cur], lambda ci, l=l, ch=chunks_in: I["w_in"][l][:, ch[ci][0]:ch[ci][0] + ch[ci][1]], z_d, R, D,
                        chunks_in, prologue="ln0mod", modrows=modr1, modsel=msel, bias_d=I["b_in"][l])
            for i in range(4):
                g0 = COL_G + i * D
                emit_linear(C, [x_cur], lambda ci, l=l, g0=g0: I["w_in"][l][:, g0 + ci * 512:g0 + (ci + 1) * 512], zg_d[i], Rm, D,
                            [(j * 512, 512, AF.Sigmoid) for j in range(4)], prologue="ln0mod", modrows=modr1, modsel=msel,
                            bias_d=I["b_in"][l][g0:g0 + D])
            emit_chunkgate(C, z_d, I["a_ln_g"][l], I["a_ln_b"][l], I["a_w_s"][l], I["a_b_s"][l], brA, Rm)
            emit_conv(C, z_d, I["b_w_dw"][l], I["b_b_dw"][l], I["b_ln_g"][l], I["b_ln_b"][l], brB,
                      [(0, RL)] + ([] if last else [(RL, NCTX)]))
            emit_linear(C, [z_d[:, COL_C:COL_D]], lambda ci: I["cs128"][:, ci * 512:(ci + 1) * 512], PQ, R, 512,
                        [(0, 512, None), (512, 512, None)])
            K4 = RL // 2
            for p in range(4):
                wsl = PQ[(p % 2) * K4:(p % 2 + 1) * K4, (p // 2) * 512:(p // 2 + 1) * 512]
                emit_linear(C, [I["dftlat"][:, p * K4:(p + 1) * K4]], lambda ci, wsl=wsl: wsl, brC[p], RL, K4, [(0, 512, None)], RB=8)
                if not last:
                    Kc = NCTX // 2
                    wsl2 = PQ[RL + (p % 2) * Kc:RL + (p % 2 + 1) * Kc, (p // 2) * 512:(p // 2 + 1) * 512]
                    emit_linear(C, [I["dftctx"][:, p * Kc:(p + 1) * Kc]], lambda ci, wsl2=wsl2: wsl2, brC[p][RL:R], NCTX, Kc,
                                [(0, 512, None)])
            emit_linear(C, [z_d[:, COL_D:COL_KV]], lambda ci, l=l: I["d_w_uq"][l][:, ci * 512:ci * 512 + (512 if ci == 0 else 256)],
                        q_d, R, 512, [(0, 512, None), (512, 256, None)], prologue="rms", g_d=I["d_q_g"][l])
            emit_linear(C, [z_d[:, COL_KV:COL_KR]], lambda ci, l=l: I["w_ukv"][l][:, ci * 512:(ci + 1) * 512],
                        kv_d, R, 256, [(0, 512, None), (512, 512, None)], prologue="rms", g_d=I["d_kv_g"][l])
            emit_rope(C, q_d, QK_NOPE, MLA_HEADS, QK_NOPE + QK_ROPE, q_d, QK_NOPE, I["rcos"], I["rsin"], RL)
            emit_rope(C, z_d, COL_KR, 1, 32, kr_d, 0, I["rcos"], I["rsin"], RL)
            with S.scope():
                tmpk = S.sb("tmpk", [128, NCTX // 128, 32], F32)
                S.dma("sp", tmpk[:], z_d[RL:R, COL_KR:COL_G].rearrange("(a p) n -> p a n", p=128), writes=[tmpk])
                S.dma("sp", kr_d[RL:R, :].rearrange("(a p) n -> p a n", p=128), tmpk[:], reads=[tmpk])
            emit_attention(C, q_d, kv_d, kr_d, o_d, (0, RL), [(0, R)])
            if not last:
                emit_attention(C, q_d, kv_d, kr_d, o_d, (RL, NCTX), [(RL, NCTX)])
            for i in range(4):
                xs = [[brA], [brB], brC, [o_d]][i]
                emit_linear(C, xs, lambda ci, l=l, i=i: I["w_branch"][l][i][:, ci * 512:(ci + 1) * 512], GP[i], Rm, 512, c512,
                            prologue=("attn" if i == 3 else "none"), mul_d=zg_d[i])
            emit_linear(C, GP, lambda ci, l=l: I["w_o"][l][:, ci * 512:(ci + 1) * 512], mix_d, Rm, D, c512)
            emit_resln(C, x_cur, [mix_d], [mr(0, 2), mr(1, 2)], msel, I["ln1_g"][l], I["ln1_b"][l], x1_d, Rm)
            modr2 = [(mr(0, 4), mr(0, 3)), (mr(1, 4), mr(1, 3))]
            emit_linear(C, [x1_d], lambda ci, l=l: I["r_w"][l], lg_d, Rm, D, [(0, 36, None)], prologue="ln0mod",
                        modrows=modr2, modsel=msel, bias_d=I["r_b"][l])
            emit_route(C, lg_d, G_d, Rm)
            w2f = I["e_w2"][l].rearrange("e j n -> (e j) n")
            for p in range(8):
                che = [(i * 512, 512, AF.Silu) for i in range(8)]
                emit_linear(C, [x1_d], lambda ci, l=l, p=p: I["e_w1"][l][4 * p + ci // 2][:, (ci % 2) * 512:(ci % 2 + 1) * 512],
                            S1_d[p], Rm, D, che, prologue="ln0mod", modrows=modr2, modsel=msel,
                            colscale=(G_d, lambda ci, p=p: 4 * p + ci // 2))
                che = [(i * 512, 512, None) for i in range(8)]
                emit_linear(C, [x1_d], lambda ci, l=l, p=p: I["e_w3"][l][4 * p + ci // 2][:, (ci % 2) * 512:(ci % 2 + 1) * 512],
                            H_d[p], Rm, D, che, prologue="ln0mod", modrows=modr2, modsel=msel, mul_d=S1_d[p])
                emit_linear(C, [H_d[p]], lambda ci, p=p, w2f=w2f: w2f[p * 4096:(p + 1) * 4096, ci * 512:(ci + 1) * 512],
                            f_d[p], Rm, 4096, c512, RB=8)
            dst = y_out if last else xb_d
            emit_resln(C, x1_d, f_d, [mr(0, 5), mr(1, 5)], msel, I["ln2_g"][l], I["ln2_b"][l], dst, Rm)
            x_cur = xb_d
        S.finish()
        print("program instructions:", S.n, S.cnt, S.dcnt)
    return nc


def host_consts(RL, NCTX):
    rows = RL // GRID_W
    row = np.repeat(np.arange(rows, dtype=np.float32), GRID_W)
    col = np.tile(np.arange(GRID_W, dtype=np.float32), rows)
    half = QK_ROPE // 2
    inv = (ROPE_THETA ** (-np.arange(0, half, 2, dtype=np.float32) / half)).astype(np.float32)
    ar = row[:, None] * inv
    ac = col[:, None] * inv
    ang = np.concatenate([ar, ar, ac, ac], -1)
    rcos = np.cos(ang).astype(np.float32)
    rsin = np.concatenate([-np.sin(ar), np.sin(ar), -np.sin(ac), np.sin(ac)], -1).astype(np.float32)
    cw = BRANCH_W // 4
    k = np.arange(cw)
    a = 2 * np.pi * np.outer(k, k) / cw
    cs = np.zeros((512, 1024), np.float32)
    for g in range(4):
        cs[g * cw:(g + 1) * cw, g * cw:(g + 1) * cw] = np.cos(a)
        cs[g * cw:(g + 1) * cw, 512 + g * cw:512 + (g + 1) * cw] = np.sin(a)

    def dft(n):
        kk = np.arange(n, dtype=np.int64)
        ph = (np.outer(kk, kk) % n).astype(np.float64) * (2 * np.pi / n)
        sc = 1.0 / math.sqrt(n * cw)
        return np.concatenate([np.cos(ph) * sc, -np.sin(ph) * sc], 1).astype(np.float32)

    return {"rcos": rcos, "rsin": rsin, "cs128": cs, "dftlat": dft(RL), "dftctx": dft(NCTX),
            "ident": np.eye(128, dtype=np.float32)}


def kernel(**inp):
    x = np.asarray(inp["x"], np.float32)
    RL = x.shape[1]
    NCTX = inp["ctx"].shape[1]
    ins = {}
    ins["x_all"] = np.concatenate([x[0], np.asarray(inp["ctx"], np.float32)[0]], 0)
    cc = np.zeros((128, D_MODEL), np.float32)
    cc[0] = np.asarray(inp["c"])[0]
    cc[1] = np.asarray(inp["c_ctx"])
    ins["cc"] = cc
    for k in WNAMES:
        ins[k] = np.ascontiguousarray(np.asarray(inp[k], np.float32))
    ins["w_ukv"] = np.concatenate([np.asarray(inp["d_w_uk"]), np.asarray(inp["d_w_uv"])], -1).astype(np.float32)
    ins["r_w"] = np.concatenate([np.asarray(inp["r_w_grp"]), np.asarray(inp["r_w_exp"])], -1).astype(np.float32)
    ins["r_b"] = np.concatenate([np.asarray(inp["r_b_grp"]), np.asarray(inp["r_b_exp"])], -1).astype(np.float32)
    ins.update(host_consts(RL, NCTX))
    shapes = {k: list(v.shape) for k, v in ins.items()}
    key = ("main", RL, NCTX)
    if key not in _PROG_CACHE:
        _PROG_CACHE[key] = build_program(shapes, RL, NCTX)
    res = run_bass_kernel_spmd(_PROG_CACHE[key], [ins], core_ids=[0])
    return np.asarray(res.results[0]["y"], np.float32)[None]
```

```python
import contextlib
import math
import numpy as np
import ml_dtypes
import concourse.bass as bass
import concourse.mybir as mybir
from concourse.bass_utils import run_bass_kernel_spmd

F32 = mybir.dt.float32
BF16 = mybir.dt.bfloat16
AF = mybir.ActivationFunctionType
ALU = mybir.AluOpType
AX = mybir.AxisListType
NCORES = 8

D_MODEL = 2048
SEQ = 8192
DEPTH = 2
GRID_W = 64
CTX_LEN = 256
BRANCH_W = 512
CHUNK = 128
CONV_W = 31
QK_NOPE = 64
QK_ROPE = 32
V_HEAD = 64
MLA_HEADS = 8
Q_LORA = 512
KV_LORA = 256
ROPE_THETA = 10000.0
MLA_SCALE = (QK_NOPE + QK_ROPE) ** -0.5
N_GROUPS = 4
EXP_PER_GROUP = 8
N_EXPERTS = 32
D_EXPERT = 1024
COL_A = 0
COL_B = 1024
COL_C = 2048
COL_D = 2560
COL_KV = 3072
COL_KR = 3328
COL_G = 3360
D_IN = COL_G + 4 * D_MODEL
ALPHA = (2 * DEPTH) ** 0.25
LN_EPS = 1e-6
TL = SEQ // NCORES


class Buf:
    __slots__ = ("t", "lw", "rd", "name")

    def __init__(self, t, name=""):
        self.t = t
        self.lw = None
        self.rd = {}
        self.name = name

    def __getitem__(self, k):
        return self.t[k]


class Sched:
    RING = 12

    def __init__(self, nc, es):
        self.nc = nc
        self.es = es
        self.engs = {"pe": nc.tensor, "act": nc.scalar, "dve": nc.vector, "pool": nc.gpsimd, "sp": nc.sync}
        self.sem = {e: es.enter_context(nc.semaphore("s_" + e)) for e in ("pe", "act", "dve", "pool")}
        self.cnt = {e: 0 for e in self.sem}
        self.dsem = {q: [es.enter_context(nc.semaphore("d_%s%d" % (q, i))) for i in range(self.RING)]
                     for q in ("sp", "pool")}
        self.dcnt = {"sp": 0, "pool": 0}
        self.waited = {e: {} for e in self.engs}
        self.n = 0

    def sb(self, name, shape, dt):
        return Buf(self.es.enter_context(self.nc.sbuf_tensor("sb%d_" % getattr(self, "uid", 0) + name, list(shape), dt)), name)

    def ps(self, name, shape, dt=F32):
        return Buf(self.es.enter_context(self.nc.psum_tensor("ps_" + name, list(shape), dt)), name)

    def _semval(self, tok):
        if tok[0] == "c":
            return ("c", tok[1]), self.sem[tok[1]], tok[2]
        q, idx = tok[1], tok[2]
        return ("d", q, idx % self.RING), self.dsem[q][idx % self.RING], 16 * (idx // self.RING + 1)

    def _wait(self, eng, toks):
        need = {}
        for tok in toks:
            if tok is None:
                continue
            if tok[0] == "c" and tok[1] == eng and eng == "pe":
                continue
            key, sem, val = self._semval(tok)
            if need.get(key, (None, 0))[1] < val:
                need[key] = (sem, val)
        w = self.waited[eng]
        for key, (sem, val) in need.items():
            if w.get(key, 0) >= val:
                continue
            self.engs[eng].wait_ge(sem, val)
            w[key] = val

    def _deps(self, reads, writes):
        toks = []
        for r in reads:
            toks.append(r.lw)
        for wv in writes:
            toks.append(wv.lw)
            toks.extend(wv.rd.values())
        return toks

    def _commit(self, tok, reads, writes, rkey):
        for r in reads:
            r.rd[rkey] = tok
        for wv in writes:
            wv.lw = tok
            wv.rd = {}

    def op(self, eng, fn, reads=(), writes=()):
        self._wait(eng, self._deps(reads, writes))
        ins = fn()
        self.cnt[eng] += 1
        ins.then_inc(self.sem[eng], 1)
        tok = ("c", eng, self.cnt[eng])
        self._commit(tok, reads, writes, eng)
        self.n += 1
        return tok

    def dma(self, q, out, in_, reads=(), writes=(), **kw):
        idx = self.dcnt[q]
        toks = self._deps(reads, writes)
        if idx >= self.RING:
            toks.append(("d", q, idx - self.RING))
        self._wait(q, toks)
        ins = self.engs[q].dma_start(out=out, in_=in_, **kw)
        ins.then_inc(self.dsem[q][idx % self.RING], 16)
        self.dcnt[q] += 1
        tok = ("d", q, idx)
        self._commit(tok, reads, writes, "dma_" + q + str(idx % self.RING))
        self.n += 1
        return tok

    def idma(self, out, in_, idx_ap, scatter, reads=(), writes=()):
        q = "pool"
        idx = self.dcnt[q]
        toks = self._deps(reads, writes)
        if idx >= self.RING:
            toks.append(("d", q, idx - self.RING))
        self._wait(q, toks)
        off = bass.IndirectOffsetOnAxis(ap=idx_ap, axis=0)
        if scatter:
            ins = self.nc.gpsimd.indirect_dma_start(out=out, out_offset=off, in_=in_, in_offset=None)
        else:
            ins = self.nc.gpsimd.indirect_dma_start(out=out, out_offset=None, in_=in_, in_offset=off)
        ins.then_inc(self.dsem[q][idx % self.RING], 16)
        self.dcnt[q] += 1
        tok = ("d", q, idx)
        self._commit(tok, reads, writes, "dma_" + q + str(idx % self.RING))
        self.n += 1
        return tok

    def barrier(self):
        toks = []
        for q in ("sp", "pool"):
            for i in range(max(0, self.dcnt[q] - self.RING), self.dcnt[q]):
                toks.append(("d", q, i))
        for e in self.cnt:
            if self.cnt[e]:
                toks.append(("c", e, self.cnt[e]))
        for e in self.engs:
            self._wait(e, [t for t in toks if not (t[0] == "c" and t[1] == e)])

    @contextlib.contextmanager
    def scope(self):
        old = self.es
        with contextlib.ExitStack() as es2:
            self.es = es2
            self.uid = getattr(self, "uid", 0) + 1
            yield
            self.barrier()
        self.es = old

    def finish(self):
        toks = []
        for q in ("sp", "pool"):
            for i in range(max(0, self.dcnt[q] - self.RING), self.dcnt[q]):
                toks.append(("d", q, i))
        for e in self.cnt:
            if self.cnt[e]:
                toks.append(("c", e, self.cnt[e]))
        self._wait("sp", toks)


def new_prog():
    nc = bass.Bass("TRN2", target_bir_lowering=False)
    es = contextlib.ExitStack()
    return nc, es


def dram_in(nc, name, shape, dt=F32):
    return nc.dram_tensor(name, list(shape), dt, kind="ExternalInput").ap()


def dram_out(nc, name, shape, dt=F32):
    return nc.dram_tensor(name, list(shape), dt, kind="ExternalOutput").ap()


_PROG_CACHE = {}


def dram_tmp(nc, name, shape, dt=F32):
    return nc.dram_tensor(name, list(shape), dt).ap()


def rstd_of(S, m):
    nc = S.nc
    S.op("act", lambda: nc.scalar.activation(out=m[:, 3:4], in_=m[:, 3:4], func=AF.Sqrt), reads=[m], writes=[m])
    S.op("dve", lambda: nc.vector.reciprocal(out=m[:, 2:3], in_=m[:, 3:4]), reads=[m], writes=[m])


def ln_stats(S, x, K, tag, ring):
    nc = S.nc
    nch = (K + 511) // 512
    st, mv = ring["st"], ring["mv"]
    for c in range(nch):
        w = min(512, K - c * 512)
        S.op("dve", lambda c=c, w=w: nc.vector.bn_stats(out=st[:, c, :], in_=x[:, c * 512:c * 512 + w]),
             reads=[x], writes=[st])
    S.op("dve", lambda: nc.vector.bn_aggr(out=mv[:, 0:2], in_=st[:, 0:nch, :]), reads=[st], writes=[mv])
    S.op("dve", lambda: nc.vector.tensor_scalar(out=mv[:, 3:4], in0=mv[:, 1:2], scalar1=LN_EPS, scalar2=None,
                                                op0=ALU.add), reads=[mv], writes=[mv])
    rstd_of(S, mv)
    return mv


class Ctx:
    def __init__(self, nc, es, ident_d):
        self.nc = nc
        self.S = S = Sched(nc, es)
        self.pT = [S.ps("pT%d" % i, [128, 1024], BF16) for i in range(2)]
        self.acc = [S.ps("acc%d" % i, [128, 512], F32) for i in range(4)]
        self.pB = [S.ps("pB%d" % i, [128, 512], F32) for i in range(2)]
        self.identb = S.sb("identb", [128, 128], BF16)
        S.dma("pool", self.identb[:], ident_d, writes=[self.identb])
        self.identf = S.sb("identf", [128, 128], F32)
        S.dma("sp", self.identf[:], ident_d, writes=[self.identf])
        self.nT = 0
        self.nA = 0


def emit_linear(C, xs, w_ap, y_d, R, K, chunks, prologue="none", modrows=None, modsel=None, bias_d=None,
                mul_d=None, g_d=None, colscale=None, RB=16):
    S, nc = C.S, C.nc
    NT = R // 128
    KC = K // 128
    nsum = len(xs)
    with S.scope():
        hT = [S.sb("hT%d" % t, [128, KC, 128], BF16) for t in range(min(RB, NT))]
        xin = [S.sb("xin%d" % i, [128, K], F32) for i in range(2)]
        xaux = [S.sb("xaux%d" % i, [128, K], F32) for i in range(2)] if nsum > 1 else None
        hb = [S.sb("hb%d" % i, [128, K], BF16) for i in range(2)]
        st = [S.sb("st%d" % i, [128, 8, 6], F32) for i in range(2)]
        mv = [S.sb("mv%d" % i, [128, 4], F32) for i in range(2)]
        if prologue == "ln0mod":
            nsets = len(modrows)
            modA = S.sb("modA", [128, nsets, K], F32)
            modB = S.sb("modB", [128, nsets, K], F32)
            for s_, (ra, rb_) in enumerate(modrows):
                S.dma("sp", modA[:, s_, :], ra.partition_broadcast(128), writes=[modA])
                S.dma("sp", modB[:, s_, :], rb_.partition_broadcast(128), writes=[modB])
            S.op("dve", lambda: nc.vector.tensor_scalar(out=modA[:], in0=modA[:], scalar1=1.0, scalar2=None, op0=ALU.add),
                 reads=[modA], writes=[modA])
        if prologue == "rms":
            gb = S.sb("gb", [128, K], F32)
            S.dma("sp", gb[:], g_d.partition_broadcast(128), writes=[gb])
            junk = S.sb("junk", [128, K], F32)
        if prologue == "attn":
            oin = [S.sb("oin%d" % i, [128, MLA_HEADS, V_HEAD + 1], F32) for i in range(2)]
            rden = [S.sb("rden%d" % i, [128, MLA_HEADS, 1], F32) for i in range(2)]
        wb = [S.sb("wb%d" % i, [128, KC, 512], BF16) for i in range(2)]
        bb = [S.sb("bb%d" % i, [128, 512], F32) for i in range(2)] if bias_d is not None else None
        ot = [S.sb("ot%d" % i, [128, 512], F32) for i in range(3)]
        mt = [S.sb("mt%d" % i, [128, 512], F32) for i in range(3)] if mul_d is not None else None
        if colscale is not None:
            gcol_d, gfn = colscale
            ncol = gcol_d.shape[1]
            gt = [S.sb("gt%d" % t, [128, ncol], F32) for t in range(min(RB, NT))]
        wi = 0
        it = 0
        for t0 in range(0, NT, RB):
            tiles = list(range(t0, min(NT, t0 + RB)))
            for t in tiles:
                x = xin[t % 2]
                h = hb[t % 2]
                rows = slice(t * 128, (t + 1) * 128)
                if colscale is not None:
                    S.dma("sp", gt[t - t0][:], gcol_d[rows], writes=[gt[t - t0]])
                if prologue == "attn":
                    o = oin[t % 2]
                    rd_ = rden[t % 2]
                    S.dma("sp", o[:], xs[0][rows], writes=[o])
                    S.op("dve", lambda o=o, rd_=rd_: nc.vector.reciprocal(out=rd_[:], in_=o[:, :, V_HEAD:V_HEAD + 1]),
                         reads=[o], writes=[rd_])
                    for hh in range(MLA_HEADS):
                        S.op("dve", lambda o=o, rd_=rd_, h=h, hh=hh: nc.vector.tensor_scalar(
                            out=h[:, hh * V_HEAD:(hh + 1) * V_HEAD], in0=o[:, hh, 0:V_HEAD], scalar1=rd_[:, hh, :],
                            scalar2=None, op0=ALU.mult), reads=[o, rd_], writes=[h])
                else:
                    S.dma("sp", x[:], xs[0][rows], writes=[x])
                    for i in range(1, nsum):
                        xa = xaux[i % 2]
                        S.dma("sp", xa[:], xs[i][rows], writes=[xa])
                        S.op("dve", lambda x=x, xa=xa: nc.vector.tensor_tensor(out=x[:], in0=x[:], in1=xa[:], op=ALU.add),
                             reads=[x, xa], writes=[x])
                    if prologue == "none":
                        S.op("act", lambda x=x, h=h: nc.scalar.copy(out=h[:], in_=x[:]), reads=[x], writes=[h])
                    elif prologue == "silu":
                        S.op("act", lambda x=x, h=h: nc.scalar.activation(out=h[:], in_=x[:], func=AF.Silu),
                             reads=[x], writes=[h])
                    elif prologue == "ln0mod":
                        m = ln_stats(S, x, K, "p", {"st": st[t % 2], "mv": mv[t % 2]})
                        ms = modsel(t)
                        S.op("dve", lambda x=x, m=m: nc.vector.tensor_scalar(
                            out=x[:], in0=x[:], scalar1=m[:, 0:1], scalar2=m[:, 2:3], op0=ALU.subtract, op1=ALU.mult),
                            reads=[x, m], writes=[x])
                        S.op("dve", lambda x=x, ms=ms: nc.vector.tensor_tensor(out=x[:], in0=x[:], in1=modA[:, ms, :],
                                                                               op=ALU.mult), reads=[x, modA], writes=[x])
                        S.op("dve", lambda x=x, h=h, ms=ms: nc.vector.tensor_tensor(
                            out=h[:], in0=x[:], in1=modB[:, ms, :], op=ALU.add), reads=[x, modB], writes=[h])
                    elif prologue == "rms":
                        m = mv[t % 2]
                        S.op("act", lambda x=x, m=m: nc.scalar.activation(out=junk[:], in_=x[:], func=AF.Square,
                                                                          accum_out=m[:, 0:1]),
                             reads=[x], writes=[junk, m])
                        S.op("dve", lambda m=m: nc.vector.tensor_scalar(out=m[:, 3:4], in0=m[:, 0:1], scalar1=1.0 / K,
                                                                        scalar2=LN_EPS, op0=ALU.mult, op1=ALU.add),
                             reads=[m], writes=[m])
                        rstd_of(S, m)
                        S.op("dve", lambda x=x, h=h, m=m: nc.vector.scalar_tensor_tensor(
                            out=h[:], in0=x[:], scalar=m[:, 2:3], in1=gb[:], op0=ALU.mult, op1=ALU.mult),
                            reads=[x, m, gb], writes=[h])
                for k0 in range(0, KC, 8):
                    kn = min(8, KC - k0)
                    p = C.pT[C.nT % 2]
                    C.nT += 1
                    for j in range(kn):
                        S.op("pe", lambda p=p, h=h, j=j, k0=k0: nc.tensor.transpose(
                            out=p[:, j * 128:(j + 1) * 128], in_=h[:, (k0 + j) * 128:(k0 + j + 1) * 128],
                            identity=C.identb[:]), reads=[h, C.identb], writes=[p])
                    S.op("act", lambda p=p, t=t, k0=k0, kn=kn: nc.scalar.copy(
                        out=hT[t - t0][:, k0:k0 + kn, :], in_=p[:, 0:kn * 128].rearrange("p (a b) -> p a b", b=128)),
                        reads=[p], writes=[hT[t - t0]])
            for ci, (c0, nw, act) in enumerate(chunks):
                w = wb[wi % 2]
                wi += 1
                wv = w_ap(ci).rearrange("(kc p) n -> p kc n", p=128)
                for k0 in range(0, KC, 16):
                    k1 = min(KC, k0 + 16)
                    S.dma("pool", w[:, k0:k1, 0:nw], wv[:, k0:k1, :], writes=[w])
                if bias_d is not None:
                    b = bb[wi % 2]
                    S.dma("sp", b[:, 0:nw], bias_d[c0:c0 + nw].partition_broadcast(128), writes=[b])
                for t in tiles:
                    a = C.acc[C.nA % 4]
                    C.nA += 1
                    o = ot[it % 3]
                    for kc in range(KC):
                        S.op("pe", lambda a=a, w=w, t=t, kc=kc, nw=nw: nc.tensor.matmul(
                            out=a[:, 0:nw], lhsT=hT[t - t0][:, kc, :], rhs=w[:, kc, 0:nw], start=(kc == 0),
                            stop=(kc == KC - 1)), reads=[hT[t - t0], w], writes=[a])
                    if bias_d is not None:
                        S.op("dve", lambda a=a, o=o, b=b, nw=nw: nc.vector.tensor_tensor(
                            out=o[:, 0:nw], in0=a[:, 0:nw], in1=b[:, 0:nw], op=ALU.add), reads=[a, b], writes=[o])
                        if act is not None:
                            S.op("act", lambda o=o, nw=nw, act=act: nc.scalar.activation(
                                out=o[:, 0:nw], in_=o[:, 0:nw], func=act), reads=[o], writes=[o])
                    else:
                        S.op("act", lambda a=a, o=o, nw=nw, act=act: nc.scalar.activation(
                            out=o[:, 0:nw], in_=a[:, 0:nw], func=(act if act is not None else AF.Copy)),
                            reads=[a], writes=[o])
                    if colscale is not None:
                        e_ = gfn(ci)
                        S.op("dve", lambda o=o, t=t, e_=e_, nw=nw: nc.vector.tensor_scalar(
                            out=o[:, 0:nw], in0=o[:, 0:nw], scalar1=gt[t - t0][:, e_:e_ + 1], scalar2=None, op0=ALU.mult),
                            reads=[o, gt[t - t0]], writes=[o])
                    if mul_d is not None:
                        m_ = mt[it % 3]
                        S.dma("sp", m_[:, 0:nw], mul_d[t * 128:(t + 1) * 128, c0:c0 + nw], writes=[m_])
                        S.op("dve", lambda o=o, m_=m_, nw=nw: nc.vector.tensor_tensor(
                            out=o[:, 0:nw], in0=o[:, 0:nw], in1=m_[:, 0:nw], op=ALU.mult), reads=[o, m_], writes=[o])
                    S.dma("sp", y_d[t * 128:(t + 1) * 128, c0:c0 + nw], o[:, 0:nw], reads=[o])
                    it += 1


def emit_resln(C, x_d, fs, m_rows, msel, g_row, b_row, y_d, R):
    S, nc = C.S, C.nc
    D = D_MODEL
    with S.scope():
        mm = S.sb("mm", [128, len(m_rows), D], F32)
        for i, r in enumerate(m_rows):
            S.dma("sp", mm[:, i, :], r.partition_broadcast(128), writes=[mm])
        gb = S.sb("g", [128, D], F32)
        bb = S.sb("b", [128, D], F32)
        S.dma("sp", gb[:], g_row.partition_broadcast(128), writes=[gb])
        S.dma("sp", bb[:], b_row.partition_broadcast(128), writes=[bb])
        xt = [S.sb("x%d" % i, [128, D], F32) for i in range(2)]
        ft = [S.sb("f%d" % i, [128, D], F32) for i in range(2)]
        fa = [S.sb("fa%d" % i, [128, D], F32) for i in range(2)]
        st = [S.sb("st%d" % i, [128, 8, 6], F32) for i in range(2)]
        mv = [S.sb("mv%d" % i, [128, 4], F32) for i in range(2)]
        for t in range(R // 128):
            rows = slice(t * 128, (t + 1) * 128)
            x, f = xt[t % 2], ft[t % 2]
            S.dma("sp", x[:], x_d[rows], writes=[x])
            S.dma("sp", f[:], fs[0][rows], writes=[f])
            for i in range(1, len(fs)):
                a = fa[i % 2]
                S.dma("sp", a[:], fs[i][rows], writes=[a])
                S.op("dve", lambda f=f, a=a: nc.vector.tensor_tensor(out=f[:], in0=f[:], in1=a[:], op=ALU.add),
                     reads=[f, a], writes=[f])
            ms = msel(t)
            S.op("dve", lambda f=f, ms=ms: nc.vector.tensor_tensor(out=f[:], in0=f[:], in1=mm[:, ms, :], op=ALU.mult),
                 reads=[f, mm], writes=[f])
            S.op("dve", lambda x=x, f=f: nc.vector.scalar_tensor_tensor(out=x[:], in0=x[:], scalar=ALPHA, in1=f[:],
                                                                        op0=ALU.mult, op1=ALU.add),
                 reads=[x, f], writes=[x])
            m = ln_stats(S, x, D, "r", {"st": st[t % 2], "mv": mv[t % 2]})
            S.op("dve", lambda x=x, m=m: nc.vector.tensor_scalar(out=x[:], in0=x[:], scalar1=m[:, 0:1], scalar2=m[:, 2:3],
                                                                 op0=ALU.subtract, op1=ALU.mult), reads=[x, m], writes=[x])
            S.op("dve", lambda x=x: nc.vector.tensor_tensor(out=x[:], in0=x[:], in1=gb[:], op=ALU.mult),
                 reads=[x, gb], writes=[x])
            S.op("dve", lambda x=x: nc.vector.tensor_tensor(out=x[:], in0=x[:], in1=bb[:], op=ALU.add),
                 reads=[x, bb], writes=[x])
            S.dma("sp", y_d[rows], x[:], reads=[x])


def gelu_tanh(S, x, t, W):
    nc = S.nc
    S.op("dve", lambda: nc.vector.tensor_tensor(out=t[:, 0:W], in0=x[:, 0:W], in1=x[:, 0:W], op=ALU.mult), reads=[x], writes=[t])
    S.op("dve", lambda: nc.vector.tensor_scalar(out=t[:, 0:W], in0=t[:, 0:W], scalar1=0.044715, scalar2=1.0, op0=ALU.mult,
                                                op1=ALU.add), reads=[t], writes=[t])
    S.op("dve", lambda: nc.vector.tensor_tensor(out=t[:, 0:W], in0=t[:, 0:W], in1=x[:, 0:W], op=ALU.mult), reads=[t, x], writes=[t])
    S.op("act", lambda: nc.scalar.activation(out=t[:, 0:W], in_=t[:, 0:W], func=AF.Sigmoid, scale=1.5957691216057308),
         reads=[t], writes=[t])
    S.op("dve", lambda: nc.vector.tensor_tensor(out=x[:, 0:W], in0=x[:, 0:W], in1=t[:, 0:W], op=ALU.mult), reads=[t, x], writes=[x])


def ln_rows(S, x, W, st, mv, gb, bb, out=None):
    nc = S.nc
    out = out if out is not None else x
    m = ln_stats(S, x, W, "l", {"st": st, "mv": mv})
    S.op("dve", lambda: nc.vector.tensor_scalar(out=x[:, 0:W], in0=x[:, 0:W], scalar1=m[:, 0:1], scalar2=m[:, 2:3],
                                                op0=ALU.subtract, op1=ALU.mult), reads=[x, m], writes=[x])
    S.op("dve", lambda: nc.vector.tensor_tensor(out=x[:, 0:W], in0=x[:, 0:W], in1=gb[:, 0:W], op=ALU.mult), reads=[x, gb], writes=[x])
    S.op("dve", lambda: nc.vector.tensor_tensor(out=out[:, 0:W], in0=x[:, 0:W], in1=bb[:, 0:W], op=ALU.add),
         reads=[x, bb], writes=[out])


def emit_chunkgate(C, z_d, lng, lnb, ws_d, bs_d, y_d, R):
    S, nc = C.S, C.nc
    with S.scope():
        gb = S.sb("g", [128, 512], F32)
        bb = S.sb("b", [128, 512], F32)
        S.dma("sp", gb[:], lng.partition_broadcast(128), writes=[gb])
        S.dma("sp", bb[:], lnb.partition_broadcast(128), writes=[bb])
        wraw = S.sb("wraw", [128, 4, 128], BF16)
        wT = S.sb("wT", [128, 4, 128], BF16)
        S.dma("pool", wraw[:], ws_d.rearrange("g p q -> p g q"), writes=[wraw])
        p = C.pT[C.nT % 2]
        C.nT += 1
        for g in range(4):
            S.op("pe", lambda g=g: nc.tensor.transpose(out=p[:, g * 128:(g + 1) * 128], in_=wraw[:, g, :], identity=C.identb[:]),
                 reads=[wraw, C.identb], writes=[p])
        S.op("act", lambda: nc.scalar.copy(out=wT[:], in_=p[:, 0:512].rearrange("p (a b) -> p a b", b=128)), reads=[p], writes=[wT])
        bs = S.sb("bs", [128, 4], F32)
        S.dma("sp", bs[:], bs_d.rearrange("g p -> p g"), writes=[bs], allow_slow_non_contiguous=True)
        uv = [S.sb("uv%d" % i, [128, 1024], F32) for i in range(2)]
        tt = [S.sb("tt%d" % i, [128, 1024], F32) for i in range(2)]
        vb = [S.sb("vb%d" % i, [128, 512], BF16) for i in range(2)]
        ot = [S.sb("ot%d" % i, [128, 512], F32) for i in range(2)]
        st = [S.sb("st%d" % i, [128, 8, 6], F32) for i in range(2)]
        mv = [S.sb("mv%d" % i, [128, 4], F32) for i in range(2)]
        for t in range(R // 128):
            rows = slice(t * 128, (t + 1) * 128)
            x, tmp, v, o = uv[t % 2], tt[t % 2], vb[t % 2], ot[t % 2]
            S.dma("sp", x[:], z_d[rows, 0:1024], writes=[x])
            gelu_tanh(S, x, tmp, 1024)
            m = ln_stats(S, Buf(x.t[:, 512:1024]) if False else x, 512, "c", {"st": st[t % 2], "mv": mv[t % 2]}) if False else None
            nchs = 1
            S.op("dve", lambda x=x, t=t: nc.vector.bn_stats(out=st[t % 2][:, 0, :], in_=x[:, 512:1024]), reads=[x], writes=[st[t % 2]])
            mvt = mv[t % 2]
            S.op("dve", lambda t=t, mvt=mvt: nc.vector.bn_aggr(out=mvt[:, 0:2], in_=st[t % 2][:, 0:1, :]), reads=[st[t % 2]], writes=[mvt])
            S.op("dve", lambda mvt=mvt: nc.vector.tensor_scalar(out=mvt[:, 3:4], in0=mvt[:, 1:2], scalar1=LN_EPS, scalar2=None,
                                                                op0=ALU.add), reads=[mvt], writes=[mvt])
            rstd_of(S, mvt)
            S.op("dve", lambda x=x, mvt=mvt: nc.vector.tensor_scalar(out=x[:, 512:1024], in0=x[:, 512:1024], scalar1=mvt[:, 0:1],
                                                                     scalar2=mvt[:, 2:3], op0=ALU.subtract, op1=ALU.mult),
                 reads=[x, mvt], writes=[x])
            S.op("dve", lambda x=x: nc.vector.tensor_tensor(out=x[:, 512:1024], in0=x[:, 512:1024], in1=gb[:], op=ALU.mult),
                 reads=[x, gb], writes=[x])
            S.op("dve", lambda x=x, v=v: nc.vector.tensor_tensor(out=v[:], in0=x[:, 512:1024], in1=bb[:], op=ALU.add),
                 reads=[x, bb], writes=[v])
            a = C.acc[C.nA % 4]
            C.nA += 1
            for g in range(4):
                S.op("pe", lambda a=a, v=v, g=g: nc.tensor.matmul(out=a[:, g * 128:(g + 1) * 128], lhsT=wT[:, g, :],
                                                                  rhs=v[:, g * 128:(g + 1) * 128], start=True, stop=True),
                     reads=[wT, v], writes=[a])
            for g in range(4):
                S.op("dve", lambda a=a, o=o, x=x, g=g: nc.vector.scalar_tensor_tensor(
                    out=o[:, g * 128:(g + 1) * 128], in0=a[:, g * 128:(g + 1) * 128], scalar=bs[:, g:g + 1],
                    in1=x[:, g * 128:(g + 1) * 128], op0=ALU.add, op1=ALU.mult), reads=[a, bs, x], writes=[o])
            S.dma("sp", y_d[rows], o[:], reads=[o])


def emit_conv(C, z_d, wdw_d, bdw_d, lng, lnb, y_d, segs):
    S, nc = C.S, C.nc
    with S.scope():
        gb = S.sb("g", [128, 512], F32)
        bb = S.sb("b", [128, 512], F32)
        S.dma("sp", gb[:], lng.partition_broadcast(128), writes=[gb])
        S.dma("sp", bb[:], lnb.partition_broadcast(128), writes=[bb])
        wd = S.sb("wd", [128, 4, CONV_W], F32)
        bd = S.sb("bd", [128, 4], F32)
        for c in range(4):
            S.dma("sp", wd[:, c, :], wdw_d[:, c * 128:(c + 1) * 128].rearrange("k p -> p k"), writes=[wd],
                  allow_slow_non_contiguous=True)
            S.dma("sp", bd[:, c:c + 1], bdw_d[c * 128:(c + 1) * 128].rearrange("(p o) -> p o", o=1), writes=[bd],
                  allow_slow_non_contiguous=True)
        yT = S.sb("yT", [128, 4, 1280], F32)
        cacc = S.sb("cacc", [128, 4, 1024], F32)
        ab = [S.sb("ab%d" % i, [128, 1024], F32) for i in range(2)]
        yy = [S.sb("yy%d" % i, [128, 512], F32) for i in range(2)]
        ot = [S.sb("ot%d" % i, [128, 512], F32) for i in range(2)]
        st = [S.sb("st%d" % i, [128, 8, 6], F32) for i in range(2)]
        mv = [S.sb("mv%d" % i, [128, 4], F32) for i in range(2)]
        it = 0
        for (r0, nr) in segs:
            for b0 in range(0, nr, 1024):
                bl = min(1024, nr - b0)
                nti = bl // 128
                S.op("pool", lambda: nc.gpsimd.memset(yT[:], 0.0), writes=[yT])
                for j in range(-1, nti + 1):
                    tr = b0 + j * 128
                    if tr < 0 or tr >= nr:
                        continue
                    x, y = ab[it % 2], yy[it % 2]
                    it += 1
                    S.dma("sp", x[:], z_d[r0 + tr:r0 + tr + 128, 1024:2048], writes=[x])
                    S.op("dve", lambda x=x, y=y: nc.vector.tensor_tensor(out=y[:], in0=x[:, 0:512], in1=x[:, 512:1024], op=ALU.mult),
                         reads=[x], writes=[y])
                    a = C.acc[C.nA % 4]
                    C.nA += 1
                    for c in range(4):
                        S.op("pe", lambda a=a, y=y, c=c: nc.tensor.transpose(out=a[:, c * 128:(c + 1) * 128],
                                                                             in_=y[:, c * 128:(c + 1) * 128], identity=C.identf[:]),
                             reads=[y, C.identf], writes=[a])
                    S.op("act", lambda a=a, j=j: nc.scalar.copy(out=yT[:, :, (j + 1) * 128:(j + 2) * 128],
                                                                in_=a[:, 0:512].rearrange("p (c t) -> p c t", t=128)),
                         reads=[a], writes=[yT])
                for c in range(4):
                    for k in range(CONV_W):
                        src = yT[:, c, 113 + k:113 + k + bl]
                        if k == 0:
                            S.op("dve", lambda c=c, src=src: nc.vector.tensor_scalar(
                                out=cacc[:, c, 0:bl], in0=src, scalar1=wd[:, c, 0:1], scalar2=bd[:, c:c + 1], op0=ALU.mult,
                                op1=ALU.add), reads=[yT, wd, bd], writes=[cacc])
                        else:
                            S.op("dve", lambda c=c, k=k, src=src: nc.vector.scalar_tensor_tensor(
                                out=cacc[:, c, 0:bl], in0=src, scalar=wd[:, c, k:k + 1], in1=cacc[:, c, 0:bl], op0=ALU.mult,
                                op1=ALU.add), reads=[yT, wd, cacc], writes=[cacc])
                for j in range(nti):
                    a = C.acc[C.nA % 4]
                    C.nA += 1
                    o = ot[j % 2]
                    for c in range(4):
                        S.op("pe", lambda a=a, c=c, j=j: nc.tensor.transpose(out=a[:, c * 128:(c + 1) * 128],
                                                                             in_=cacc[:, c, j * 128:(j + 1) * 128], identity=C.identf[:]),
                             reads=[cacc, C.identf], writes=[a])
                    S.op("act", lambda a=a, o=o: nc.scalar.copy(out=o[:], in_=a[:]), reads=[a], writes=[o])
                    ln_rows(S, o, 512, st[j % 2], mv[j % 2], gb, bb)
                    S.op("act", lambda o=o: nc.scalar.activation(out=o[:], in_=o[:], func=AF.Silu), reads=[o], writes=[o])
                    S.dma("sp", y_d[r0 + b0 + j * 128:r0 + b0 + (j + 1) * 128], o[:], reads=[o])


def emit_rope(C, src_d, c0, H, stride, dst_d, d0, cos_d, sin_d, nrows):
    S, nc = C.S, C.nc
    W = (H - 1) * stride + 32
    with S.scope():
        xt = [S.sb("x%d" % i, [128, W], F32) for i in range(2)]
        cs = [S.sb("cs%d" % i, [128, 32], F32) for i in range(2)]
        sn = [S.sb("sn%d" % i, [128, 32], F32) for i in range(2)]
        t1 = [S.sb("t1%d" % i, [128, 32], F32) for i in range(2)]
        for t in range(nrows // 128):
            rows = slice(t * 128, (t + 1) * 128)
            x, c_, s_, u = xt[t % 2], cs[t % 2], sn[t % 2], t1[t % 2]
            S.dma("sp", x[:], src_d[rows, c0:c0 + W], writes=[x])
            S.dma("sp", c_[:], cos_d[rows], writes=[c_])
            S.dma("sp", s_[:], sin_d[rows], writes=[s_])
            for h in range(H):
                o = h * stride
                for (a0, b0_) in ((0, 8), (8, 0), (16, 24), (24, 16)):
                    S.op("dve", lambda x=x, s_=s_, u=u, o=o, a0=a0, b0_=b0_: nc.vector.tensor_tensor(
                        out=u[:, a0:a0 + 8], in0=x[:, o + b0_:o + b0_ + 8], in1=s_[:, a0:a0 + 8], op=ALU.mult),
                        reads=[x, s_], writes=[u])
                S.op("dve", lambda x=x, c_=c_, o=o: nc.vector.tensor_tensor(out=x[:, o:o + 32], in0=x[:, o:o + 32], in1=c_[:], op=ALU.mult),
                     reads=[x, c_], writes=[x])
                S.op("dve", lambda x=x, u=u, o=o: nc.vector.tensor_tensor(out=x[:, o:o + 32], in0=x[:, o:o + 32], in1=u[:], op=ALU.add),
                     reads=[x, u], writes=[x])
            S.dma("sp", dst_d[rows, d0:d0 + W], x[:], reads=[x])


def emit_attention(C, q_d, kv_d, kr_d, o_d, q_rows, k_rows):
    S, nc = C.S, C.nc
    ktiles = [r0 + i * 128 for (r0, n) in k_rows for i in range(n // 128)]
    NKT = len(ktiles)
    q0, nq = q_rows
    NQT = nq // 128
    DK = QK_NOPE + QK_ROPE
    with S.scope():
        KT = S.sb("KT", [DK, NKT * 128], BF16)
        QT = S.sb("QT", [DK, NQT * 128], BF16)
        Vt = S.sb("Vt", [128, NKT, V_HEAD + 1], BF16)
        kin = [S.sb("kin%d" % i, [128, DK], F32) for i in range(2)]
        kb = [S.sb("kb%d" % i, [128, DK], BF16) for i in range(2)]
        pt = [S.sb("pt%d" % i, [128, 512], BF16) for i in range(3)]
        of = [S.sb("of%d" % i, [V_HEAD + 1, 512], F32) for i in range(2)]
        oo = [S.sb("oo%d" % i, [128, 4, V_HEAD + 1], F32) for i in range(2)]
        ones = S.sb("ones", [128, NKT, 1], F32)
        S.op("pool", lambda: nc.gpsimd.memset(ones[:], 1.0), writes=[ones])
        S.op("act", lambda: nc.scalar.copy(out=Vt[:, :, V_HEAD:V_HEAD + 1], in_=ones[:]), reads=[ones], writes=[Vt])
        n_ = 0
        for h in range(MLA_HEADS):
            for i, r in enumerate(ktiles):
                x, xb = kin[n_ % 2], kb[n_ % 2]
                n_ += 1
                S.dma("sp", x[:, 0:QK_NOPE], kv_d[r:r + 128, h * QK_NOPE:(h + 1) * QK_NOPE], writes=[x])
                S.dma("sp", x[:, QK_NOPE:DK], kr_d[r:r + 128, :], writes=[x])
                S.dma("pool", Vt[:, i, 0:V_HEAD], kv_d[r:r + 128, 512 + h * V_HEAD:512 + (h + 1) * V_HEAD], writes=[Vt])
                S.op("act", lambda x=x, xb=xb: nc.scalar.copy(out=xb[:], in_=x[:]), reads=[x], writes=[xb])
                p = C.pT[C.nT % 2]
                C.nT += 1
                S.op("pe", lambda p=p, xb=xb: nc.tensor.transpose(out=p[0:DK, 0:128], in_=xb[:], identity=C.identb[:]),
                     reads=[xb, C.identb], writes=[p])
                S.op("act", lambda p=p, i=i: nc.scalar.copy(out=KT[:, i * 128:(i + 1) * 128], in_=p[0:DK, 0:128]),
                     reads=[p], writes=[KT])
            for i in range(NQT):
                r = q0 + i * 128
                x, xb = kin[n_ % 2], kb[n_ % 2]
                n_ += 1
                S.dma("sp", x[:], q_d[r:r + 128, h * DK:(h + 1) * DK], writes=[x])
                S.op("act", lambda x=x, xb=xb: nc.scalar.copy(out=xb[:], in_=x[:]), reads=[x], writes=[xb])
                p = C.pT[C.nT % 2]
                C.nT += 1
                S.op("pe", lambda p=p, xb=xb: nc.tensor.transpose(out=p[0:DK, 0:128], in_=xb[:], identity=C.identb[:]),
                     reads=[xb, C.identb], writes=[p])
                S.op("act", lambda p=p, i=i: nc.scalar.copy(out=QT[:, i * 128:(i + 1) * 128], in_=p[0:DK, 0:128]),
                     reads=[p], writes=[QT])
            for qb in range(0, NQT * 128, 512):
                qw = min(512, NQT * 128 - qb)
                ao = C.pB[(qb // 512) % 2]
                for i in range(NKT):
                    a = C.acc[C.nA % 4]
                    C.nA += 1
                    pp = pt[i % 3]
                    S.op("pe", lambda a=a, i=i, qb=qb, qw=qw: nc.tensor.matmul(out=a[:, 0:qw], lhsT=KT[:, i * 128:(i + 1) * 128],
                                                                              rhs=QT[:, qb:qb + qw], start=True, stop=True),
                         reads=[KT, QT], writes=[a])
                    S.op("act", lambda a=a, pp=pp, qw=qw: nc.scalar.activation(out=pp[:, 0:qw], in_=a[:, 0:qw], func=AF.Exp,
                                                                              scale=MLA_SCALE), reads=[a], writes=[pp])
                    S.op("pe", lambda ao=ao, pp=pp, i=i, qw=qw: nc.tensor.matmul(out=ao[0:V_HEAD + 1, 0:qw], lhsT=Vt[:, i, :],
                                                                                rhs=pp[:, 0:qw], start=(i == 0), stop=(i == NKT - 1)),
                         reads=[Vt, pp], writes=[ao])
                o1 = of[(qb // 512) % 2]
                o2 = oo[(qb // 512) % 2]
                S.op("act", lambda ao=ao, o1=o1, qw=qw: nc.scalar.copy(out=o1[:, 0:qw], in_=ao[0:V_HEAD + 1, 0:qw]), reads=[ao], writes=[o1])
                a = C.acc[C.nA % 4]
                C.nA += 1
                nsub = qw // 128
                for j in range(nsub):
                    S.op("pe", lambda a=a, o1=o1, j=j: nc.tensor.transpose(out=a[:, j * 65:(j + 1) * 65], in_=o1[:, j * 128:(j + 1) * 128],
                                                                           identity=C.identf[0:V_HEAD + 1, 0:V_HEAD + 1]),
                         reads=[o1, C.identf], writes=[a])
                S.op("dve", lambda a=a, o2=o2, nsub=nsub: nc.vector.tensor_copy(
                    out=o2[:, 0:nsub, :], in_=a[:, 0:nsub * 65].rearrange("p (j d) -> p j d", d=65)), reads=[a], writes=[o2])
                for j in range(nsub):
                    r = q0 + qb + j * 128
                    S.dma("sp", o_d[r:r + 128, h, :], o2[:, j, :], reads=[o2])


def emit_route(C, lg_d, IDX_d, GAB_d, lstrict_d, R, CAP):
    S, nc = C.S, C.nc
    I32 = mybir.dt.int32
    with S.scope():
        L = [S.sb("L%d" % i, [128, 36], F32) for i in range(2)]
        W = [S.sb("W%d" % i, [128, 256], F32) for i in range(2)]
        Mi = [S.sb("Mi%d" % i, [128, 32], BF16) for i in range(2)]
        II = [S.sb("II%d" % i, [128, 2], I32) for i in range(2)]
        Macc = S.sb("Macc", [128, 32], BF16)
        lst = S.sb("lst", [128, 128], BF16)
        onesb = S.sb("onesb", [128, 128], BF16)
        ebase = S.sb("ebase", [128, 32], F32)
        S.dma("pool", lst[:], lstrict_d, writes=[lst])
        S.op("pool", lambda: nc.gpsimd.memset(onesb[:], 1.0), writes=[onesb])
        S.op("pool", lambda: nc.gpsimd.memset(Macc[:], 0.0), writes=[Macc])
        for e in range(N_EXPERTS):
            S.op("pool", lambda e=e: nc.gpsimd.memset(ebase[:, e:e + 1], float(e * CAP)), writes=[ebase])
        for t in range(R // 128):
            rows = slice(t * 128, (t + 1) * 128)
            l, w, mi, ii = L[t % 2], W[t % 2], Mi[t % 2], II[t % 2]
            S.dma("sp", l[:], lg_d[rows], writes=[l])
            S.op("dve", lambda l=l, w=w: nc.vector.tensor_reduce(out=w[:, 0:1], in_=l[:, 0:4], axis=AX.X, op=ALU.max), reads=[l], writes=[w])
            S.op("dve", lambda l=l, w=w: nc.vector.tensor_scalar(out=w[:, 4:8], in0=l[:, 0:4], scalar1=w[:, 0:1], scalar2=None, op0=ALU.is_ge),
                 reads=[l, w], writes=[w])
            S.op("dve", lambda w=w: nc.vector.tensor_scalar(out=w[:, 1:2], in0=w[:, 0:1], scalar1=-1.0, scalar2=None, op0=ALU.mult),
                 reads=[w], writes=[w])
            S.op("act", lambda l=l, w=w: nc.scalar.activation(out=w[:, 44:48], in_=l[:, 0:4], func=AF.Exp, bias=w[:, 1:2], scale=1.0),
                 reads=[l, w], writes=[w])
            S.op("dve", lambda w=w: nc.vector.tensor_reduce(out=w[:, 2:3], in_=w[:, 44:48], axis=AX.X, op=ALU.add), reads=[w], writes=[w])
            S.op("dve", lambda w=w: nc.vector.reciprocal(out=w[:, 3:4], in_=w[:, 2:3]), reads=[w], writes=[w])
            S.op("dve", lambda l=l, w=w: nc.vector.tensor_scalar(out=w[:, 8:16], in0=l[:, 4:12], scalar1=w[:, 4:5], scalar2=None, op0=ALU.mult),
                 reads=[l, w], writes=[w])
            for gi in range(1, 4):
                S.op("dve", lambda l=l, w=w, gi=gi: nc.vector.scalar_tensor_tensor(
                    out=w[:, 8:16], in0=l[:, 4 + gi * 8:12 + gi * 8], scalar=w[:, 4 + gi:5 + gi], in1=w[:, 8:16], op0=ALU.mult, op1=ALU.add),
                    reads=[l, w], writes=[w])
            S.op("dve", lambda w=w: nc.vector.max(out=w[:, 16:24], in_=w[:, 8:16]), reads=[w], writes=[w])
            S.op("dve", lambda w=w: nc.vector.tensor_scalar(out=w[:, 24:32], in0=w[:, 8:16], scalar1=w[:, 17:18], scalar2=None, op0=ALU.is_ge),
                 reads=[w], writes=[w])
            S.op("dve", lambda w=w: nc.vector.tensor_scalar(out=w[:, 48:56], in0=w[:, 8:16], scalar1=w[:, 16:17], scalar2=None, op0=ALU.is_ge),
                 reads=[w], writes=[w])
            S.op("dve", lambda w=w: nc.vector.tensor_tensor(out=w[:, 56:64], in0=w[:, 24:32], in1=w[:, 48:56], op=ALU.subtract), reads=[w], writes=[w])
            S.op("dve", lambda w=w: nc.vector.tensor_scalar(out=w[:, 40:41], in0=w[:, 16:17], scalar1=-1.0, scalar2=None, op0=ALU.mult),
                 reads=[w], writes=[w])
            S.op("act", lambda w=w: nc.scalar.activation(out=w[:, 32:40], in_=w[:, 8:16], func=AF.Exp, bias=w[:, 40:41], scale=1.0),
                 reads=[w], writes=[w])
            S.op("act", lambda w=w: nc.scalar.activation(out=w[:, 41:42], in_=w[:, 17:18], func=AF.Exp, bias=w[:, 40:41], scale=1.0),
                 reads=[w], writes=[w])
            S.op("dve", lambda w=w: nc.vector.tensor_scalar(out=w[:, 41:42], in0=w[:, 41:42], scalar1=1.0, scalar2=None, op0=ALU.add),
                 reads=[w], writes=[w])
            S.op("dve", lambda w=w: nc.vector.reciprocal(out=w[:, 42:43], in_=w[:, 41:42]), reads=[w], writes=[w])
            S.op("dve", lambda w=w: nc.vector.tensor_tensor(out=w[:, 42:43], in0=w[:, 42:43], in1=w[:, 3:4], op=ALU.mult), reads=[w], writes=[w])
            S.op("dve", lambda w=w: nc.vector.tensor_scalar(out=w[:, 64:72], in0=w[:, 32:40], scalar1=w[:, 42:43], scalar2=None, op0=ALU.mult),
                 reads=[w], writes=[w])
            for (sc, oc) in ((48, 80), (56, 81)):
                S.op("dve", lambda w=w, sc=sc: nc.vector.tensor_tensor(out=w[:, 72:80], in0=w[:, 64:72], in1=w[:, sc:sc + 8], op=ALU.mult),
                     reads=[w], writes=[w])
                S.op("dve", lambda w=w, oc=oc: nc.vector.tensor_reduce(out=w[:, oc:oc + 1], in_=w[:, 72:80], axis=AX.X, op=ALU.add),
                     reads=[w], writes=[w])
            for gi in range(4):
                S.op("dve", lambda w=w, gi=gi: nc.vector.tensor_scalar(out=w[:, 96 + gi * 8:104 + gi * 8], in0=w[:, 48:56],
                                                                       scalar1=w[:, 4 + gi:5 + gi], scalar2=None, op0=ALU.mult),
                     reads=[w], writes=[w])
                S.op("dve", lambda w=w, gi=gi: nc.vector.tensor_scalar(out=w[:, 128 + gi * 8:136 + gi * 8], in0=w[:, 56:64],
                                                                       scalar1=w[:, 4 + gi:5 + gi], scalar2=None, op0=ALU.mult),
                     reads=[w], writes=[w])
            S.op("dve", lambda w=w, mi=mi: nc.vector.tensor_tensor(out=mi[:], in0=w[:, 96:128], in1=w[:, 128:160], op=ALU.add),
                 reads=[w], writes=[mi])
            a = C.acc[C.nA % 4]
            C.nA += 1
            S.op("pe", lambda a=a, mi=mi: nc.tensor.matmul(out=a[:, 0:32], lhsT=lst[:], rhs=mi[:], start=True, stop=False),
                 reads=[lst, mi], writes=[a])
            S.op("pe", lambda a=a: nc.tensor.matmul(out=a[:, 0:32], lhsT=onesb[:], rhs=Macc[:], start=False, stop=True),
                 reads=[onesb, Macc], writes=[a])
            S.op("dve", lambda mi=mi: nc.vector.tensor_tensor(out=Macc[:], in0=Macc[:], in1=mi[:], op=ALU.add), reads=[Macc, mi], writes=[Macc])
            S.op("dve", lambda a=a, w=w: nc.vector.tensor_tensor(out=w[:, 160:192], in0=a[:, 0:32], in1=ebase[:], op=ALU.add),
                 reads=[a, ebase], writes=[w])
            for (mc, oc) in ((96, 82), (128, 83)):
                S.op("dve", lambda w=w, mc=mc: nc.vector.tensor_tensor(out=w[:, 192:224], in0=w[:, 160:192], in1=w[:, mc:mc + 32], op=ALU.mult),
                     reads=[w], writes=[w])
                S.op("dve", lambda w=w, oc=oc: nc.vector.tensor_reduce(out=w[:, oc:oc + 1], in_=w[:, 192:224], axis=AX.X, op=ALU.add),
                     reads=[w], writes=[w])
            S.op("dve", lambda w=w, ii=ii: nc.vector.tensor_copy(out=ii[:], in_=w[:, 82:84]), reads=[w], writes=[ii])
            S.dma("sp", IDX_d[rows], ii[:], reads=[ii])
            S.dma("sp", GAB_d[rows], w[:, 80:82], reads=[w])


def emit_dispatch(C, x1_d, modrows, modsel, IDX_d, Xs_d, R, NS):
    S, nc = C.S, C.nc
    D = D_MODEL
    I32 = mybir.dt.int32
    xsb = Buf(None)
    with S.scope():
        zt = S.sb("zt", [128, 8, D], BF16)
        S.op("pool", lambda: nc.gpsimd.memset(zt[:], 0.0), writes=[zt])
        for r0 in range(0, NS, 1024):
            S.dma("sp", Xs_d[r0:r0 + 1024].rearrange("(a p) n -> p a n", p=128), zt[:], reads=[zt], writes=[xsb])
        modA = S.sb("modA", [128, 2, D], F32)
        modB = S.sb("modB", [128, 2, D], F32)
        for s_, (ra, rb_) in enumerate(modrows):
            S.dma("sp", modA[:, s_, :], ra.partition_broadcast(128), writes=[modA])
            S.dma("sp", modB[:, s_, :], rb_.partition_broadcast(128), writes=[modB])
        S.op("dve", lambda: nc.vector.tensor_scalar(out=modA[:], in0=modA[:], scalar1=1.0, scalar2=None, op0=ALU.add),
             reads=[modA], writes=[modA])
        xt = [S.sb("x%d" % i, [128, D], F32) for i in range(3)]
        st = [S.sb("st%d" % i, [128, 8, 6], F32) for i in range(2)]
        mv = [S.sb("mv%d" % i, [128, 4], F32) for i in range(2)]
        II = [S.sb("II%d" % i, [128, 2], I32) for i in range(3)]
        xbt = [S.sb("xb%d" % i, [128, D], BF16) for i in range(3)]
        for t in range(R // 128):
            rows = slice(t * 128, (t + 1) * 128)
            x, ii, xb = xt[t % 3], II[t % 3], xbt[t % 3]
            S.dma("sp", x[:], x1_d[rows], writes=[x])
            S.dma("sp", ii[:], IDX_d[rows], writes=[ii])
            m = ln_stats(S, x, D, "d", {"st": st[t % 2], "mv": mv[t % 2]})
            ms = modsel(t)
            S.op("dve", lambda x=x, m=m: nc.vector.tensor_scalar(out=x[:], in0=x[:], scalar1=m[:, 0:1], scalar2=m[:, 2:3],
                                                                 op0=ALU.subtract, op1=ALU.mult), reads=[x, m], writes=[x])
            S.op("dve", lambda x=x, ms=ms: nc.vector.tensor_tensor(out=x[:], in0=x[:], in1=modA[:, ms, :], op=ALU.mult),
                 reads=[x, modA], writes=[x])
            S.op("dve", lambda x=x, xb=xb, ms=ms: nc.vector.tensor_tensor(out=xb[:], in0=x[:], in1=modB[:, ms, :], op=ALU.add),
                 reads=[x, modB], writes=[xb])
            for j in range(2):
                S.idma(Xs_d, xb[:], ii[:, j:j + 1], True, reads=[xb, ii, xsb], writes=[])


def emit_experts(C, Xs_d, Ys_d, w1, w3, w2, CAP):
    S, nc = C.S, C.nc
    D, DE = D_MODEL, D_EXPERT
    NTall = CAP // 128
    NTe = min(8, NTall)
    with S.scope():
        hb = [S.sb("hb%d" % i, [128, D], BF16) for i in range(2)]
        hT = [S.sb("hT%d" % i, [128, 16, 128], BF16) for i in range(NTe)]
        hh = [S.sb("hh%d" % i, [128, DE], BF16) for i in range(NTe)]
        hT2 = [S.sb("hU%d" % i, [128, 8, 128], BF16) for i in range(NTe)]
        w1c = [S.sb("w1c%d" % i, [128, 16, 512], BF16) for i in range(2)]
        w3c = [S.sb("w3c%d" % i, [128, 16, 512], BF16) for i in range(2)]
        w2c = [S.sb("w2c%d" % i, [128, 8, 512], BF16) for i in range(2)]
        stl = [S.sb("stl%d" % i, [128, 512], F32) for i in range(3)]
        ot = [S.sb("ot%d" % i, [128, 512], BF16) for i in range(3)]
        n1 = n2 = n3 = 0
        for e, sub in [(e_, s_) for e_ in range(N_EXPERTS) for s_ in range(0, NTall, 8)]:
            r0 = e * CAP + sub * 128
            NTe = min(8, NTall - sub)
            for t in range(NTe):
                h = hb[n1 % 2]
                n1 += 1
                S.dma("sp", h[:], Xs_d[r0 + t * 128:r0 + (t + 1) * 128], writes=[h])
                for k0 in (0, 8):
                    p = C.pT[C.nT % 2]
                    C.nT += 1
                    for j in range(8):
                        S.op("pe", lambda p=p, h=h, j=j, k0=k0: nc.tensor.transpose(
                            out=p[:, j * 128:(j + 1) * 128], in_=h[:, (k0 + j) * 128:(k0 + j + 1) * 128], identity=C.identb[:]),
                            reads=[h, C.identb], writes=[p])
                    S.op("act", lambda p=p, t=t, k0=k0: nc.scalar.copy(
                        out=hT[t][:, k0:k0 + 8, :], in_=p[:, 0:1024].rearrange("p (a b) -> p a b", b=128)), reads=[p], writes=[hT[t]])
            for jc in range(2):
                wa, wb_ = w1c[n2 % 2], w3c[n2 % 2]
                n2 += 1
                S.dma("pool", wa[:], w1[e][:, jc * 512:(jc + 1) * 512].rearrange("(kc p) n -> p kc n", p=128), writes=[wa])
                S.dma("pool", wb_[:], w3[e][:, jc * 512:(jc + 1) * 512].rearrange("(kc p) n -> p kc n", p=128), writes=[wb_])
                for t in range(NTe):
                    a1 = C.acc[C.nA % 4]
                    a3 = C.acc[(C.nA + 1) % 4]
                    C.nA += 2
                    s_ = stl[n3 % 3]
                    n3 += 1
                    for kc in range(16):
                        S.op("pe", lambda a1=a1, wa=wa, t=t, kc=kc: nc.tensor.matmul(out=a1[:], lhsT=hT[t][:, kc, :], rhs=wa[:, kc, :],
                                                                                    start=(kc == 0), stop=(kc == 15)), reads=[hT[t], wa], writes=[a1])
                    for kc in range(16):
                        S.op("pe", lambda a3=a3, wb_=wb_, t=t, kc=kc: nc.tensor.matmul(out=a3[:], lhsT=hT[t][:, kc, :], rhs=wb_[:, kc, :],
                                                                                      start=(kc == 0), stop=(kc == 15)), reads=[hT[t], wb_], writes=[a3])
                    S.op("act", lambda a1=a1, s_=s_: nc.scalar.activation(out=s_[:], in_=a1[:], func=AF.Silu), reads=[a1], writes=[s_])
                    S.op("dve", lambda a3=a3, s_=s_, t=t, jc=jc: nc.vector.tensor_tensor(out=hh[t][:, jc * 512:(jc + 1) * 512], in0=a3[:],
                                                                                        in1=s_[:], op=ALU.mult), reads=[a3, s_], writes=[hh[t]])
            for t in range(NTe):
                p = C.pT[C.nT % 2]
                C.nT += 1
                for j in range(8):
                    S.op("pe", lambda p=p, t=t, j=j: nc.tensor.transpose(out=p[:, j * 128:(j + 1) * 128], in_=hh[t][:, j * 128:(j + 1) * 128],
                                                                         identity=C.identb[:]), reads=[hh[t], C.identb], writes=[p])
                S.op("act", lambda p=p, t=t: nc.scalar.copy(out=hT2[t][:], in_=p[:, 0:1024].rearrange("p (a b) -> p a b", b=128)),
                     reads=[p], writes=[hT2[t]])
            for n4 in range(4):
                wc = w2c[n2 % 2]
                n2 += 1
                S.dma("pool", wc[:], w2[e][:, n4 * 512:(n4 + 1) * 512].rearrange("(kc p) n -> p kc n", p=128), writes=[wc])
                for t in range(NTe):
                    a = C.acc[C.nA % 4]
                    C.nA += 1
                    o = ot[n3 % 3]
                    n3 += 1
                    for kc in range(8):
                        S.op("pe", lambda a=a, wc=wc, t=t, kc=kc: nc.tensor.matmul(out=a[:], lhsT=hT2[t][:, kc, :], rhs=wc[:, kc, :],
                                                                                  start=(kc == 0), stop=(kc == 7)), reads=[hT2[t], wc], writes=[a])
                    S.op("act", lambda a=a, o=o: nc.scalar.copy(out=o[:], in_=a[:]), reads=[a], writes=[o])
                    S.dma("sp", Ys_d[r0 + t * 128:r0 + (t + 1) * 128, n4 * 512:(n4 + 1) * 512], o[:], reads=[o])


def emit_combine(C, Ys_d, IDX_d, GAB_d, f_d, R):
    S, nc = C.S, C.nc
    D = D_MODEL
    I32 = mybir.dt.int32
    with S.scope():
        ya = [S.sb("ya%d" % i, [128, D], BF16) for i in range(2)]
        yb = [S.sb("yb%d" % i, [128, D], BF16) for i in range(2)]
        fo = [S.sb("fo%d" % i, [128, D], F32) for i in range(2)]
        II = [S.sb("II%d" % i, [128, 2], I32) for i in range(2)]
        GG = [S.sb("GG%d" % i, [128, 2], F32) for i in range(2)]
        for t in range(R // 128):
            rows = slice(t * 128, (t + 1) * 128)
            a, b, ii, gg = ya[t % 2], yb[t % 2], II[t % 2], GG[t % 2]
            S.dma("sp", ii[:], IDX_d[rows], writes=[ii])
            S.dma("sp", gg[:], GAB_d[rows], writes=[gg])
            S.idma(a[:], Ys_d, ii[:, 0:1], False, reads=[ii], writes=[a])
            S.idma(b[:], Ys_d, ii[:, 1:2], False, reads=[ii], writes=[b])
            f = fo[t % 2]
            S.op("dve", lambda a=a, f=f, gg=gg: nc.vector.tensor_scalar(out=f[:], in0=a[:], scalar1=gg[:, 0:1], scalar2=None, op0=ALU.mult),
                 reads=[a, gg], writes=[f])
            S.op("dve", lambda f=f, b=b, gg=gg: nc.vector.scalar_tensor_tensor(out=f[:], in0=b[:], scalar=gg[:, 1:2], in1=f[:],
                                                                               op0=ALU.mult, op1=ALU.add), reads=[f, b, gg], writes=[f])
            S.dma("sp", f_d[rows], f[:], reads=[f])


WNAMES = ["w_ada", "b_ada", "w_in", "b_in", "a_ln_g", "a_ln_b", "a_w_s", "a_b_s", "b_w_dw", "b_b_dw", "b_ln_g", "b_ln_b",
          "d_q_g", "d_w_uq", "d_kv_g", "w_branch", "w_o", "ln1_g", "ln1_b", "e_w1", "e_w3", "e_w2", "ln2_g", "ln2_b"]


def build_program(shapes, RL, NCTX):
    R = RL + NCTX
    D = D_MODEL
    nc, es = new_prog()
    I = {}
    for k, shp in shapes.items():
        I[k] = dram_in(nc, k, shp)
    y_out = dram_out(nc, "y", [RL, D])
    T = lambda n, s: dram_tmp(nc, n, s)
    mod_d = [T("mod%d" % l, [128, 6 * D]) for l in range(DEPTH)]
    z_d = T("z", [R, COL_G])
    zg_d = [T("zg%d" % i, [R, D]) for i in range(4)]
    brA, brB = T("brA", [R, 512]), T("brB", [R, 512])
    PQ = T("PQ", [R, 1024])
    brC = [T("brC%d" % p, [R, 512]) for p in range(4)]
    q_d, kv_d, kr_d = T("q", [R, 768]), T("kv", [R, 1024]), T("kr", [R, 32])
    o_d = T("o", [R, MLA_HEADS, V_HEAD + 1])
    GP = [T("GP%d" % i, [R, D]) for i in range(4)]
    mix_d, x1_d, xb_d = T("mix", [R, D]), T("x1", [R, D]), T("xb", [R, D])
    lg_d, G_d = T("lg", [R, 36]), T("G", [R, 32])
    CAPMAX = 128 * int(math.ceil(3.6 * 2.0 * R / N_EXPERTS / 128))
    Xs_d = dram_tmp(nc, "Xs", [N_EXPERTS * CAPMAX, D], BF16)
    Ys_d = dram_tmp(nc, "Ys", [N_EXPERTS * CAPMAX, D], BF16)
    IDX_d = nc.dram_tensor("IDX", [R, 2], mybir.dt.int32).ap()
    GAB_d = T("GAB", [R, 2])
    f_d = [T("f0", [R, D])]
    NTL = RL // 128
    msel = lambda t: 0 if t < NTL else 1
    with es:
        C = Ctx(nc, es, I["ident"])
        S = C.S
        c512 = [(i * 512, 512, None) for i in range(4)]
        for l in range(DEPTH):
            emit_linear(C, [I["cc"]], lambda ci, l=l: I["w_ada"][l][:, ci * 512:(ci + 1) * 512], mod_d[l], 128, D,
                        [(i * 512, 512, None) for i in range(24)], prologue="silu", bias_d=I["b_ada"][l])
        x_cur = I["x_all"]
        for l in range(DEPTH):
            last = l == DEPTH - 1
            Rm = RL if last else R
            mr = lambda who, i, l=l: mod_d[l][who:who + 1, i * D:(i + 1) * D]
            chunks_in = []
            for c0 in range(0, COL_KV, 512):
                chunks_in.append((c0, 512, AF.Sigmoid if c0 == 1536 else None))
            chunks_in.append((COL_KV, COL_G - COL_KV, None))
            modr1 = [(mr(0, 1), mr(0, 0)), (mr(1, 1), mr(1, 0))]
            emit_linear(C, [x_cur], lambda ci, l=l, ch=chunks_in: I["w_in"][l][:, ch[ci][0]:ch[ci][0] + ch[ci][1]], z_d, R, D,
                        chunks_in, prologue="ln0mod", modrows=modr1, modsel=msel, bias_d=I["b_in"][l])
            for i in range(4):
                g0 = COL_G + i * D
                emit_linear(C, [x_cur], lambda ci, l=l, g0=g0: I["w_in"][l][:, g0 + ci * 512:g0 + (ci + 1) * 512], zg_d[i], Rm, D,
                            [(j * 512, 512, AF.Sigmoid) for j in range(4)], prologue="ln0mod", modrows=modr1, modsel=msel,
                            bias_d=I["b_in"][l][g0:g0 + D])
            emit_chunkgate(C, z_d, I["a_ln_g"][l], I["a_ln_b"][l], I["a_w_s"][l], I["a_b_s"][l], brA, Rm)
            emit_conv(C, z_d, I["b_w_dw"][l], I["b_b_dw"][l], I["b_ln_g"][l], I["b_ln_b"][l], brB,
                      [(0, RL)] + ([] if last else [(RL, NCTX)]))
            emit_linear(C, [z_d[:, COL_C:COL_D]], lambda ci: I["cs128"][:, ci * 512:(ci + 1) * 512], PQ, R, 512,
                        [(0, 512, None), (512, 512, None)])
            K4 = RL // 2
            for p in range(4):
                wsl = PQ[(p % 2) * K4:(p % 2 + 1) * K4, (p // 2) * 512:(p // 2 + 1) * 512]
                emit_linear(C, [I["dftlat"][:, p * K4:(p + 1) * K4]], lambda ci, wsl=wsl: wsl, brC[p], RL, K4, [(0, 512, None)], RB=8)
                if not last:
                    Kc = NCTX // 2
                    wsl2 = PQ[RL + (p % 2) * Kc:RL + (p % 2 + 1) * Kc, (p // 2) * 512:(p // 2 + 1) * 512]
                    emit_linear(C, [I["dftctx"][:, p * Kc:(p + 1) * Kc]], lambda ci, wsl2=wsl2: wsl2, brC[p][RL:R], NCTX, Kc,
                                [(0, 512, None)])
            emit_linear(C, [z_d[:, COL_D:COL_KV]], lambda ci, l=l: I["d_w_uq"][l][:, ci * 512:ci * 512 + (512 if ci == 0 else 256)],
                        q_d, R, 512, [(0, 512, None), (512, 256, None)], prologue="rms", g_d=I["d_q_g"][l])
            emit_linear(C, [z_d[:, COL_KV:COL_KR]], lambda ci, l=l: I["w_ukv"][l][:, ci * 512:(ci + 1) * 512],
                        kv_d, R, 256, [(0, 512, None), (512, 512, None)], prologue="rms", g_d=I["d_kv_g"][l])
            emit_rope(C, q_d, QK_NOPE, MLA_HEADS, QK_NOPE + QK_ROPE, q_d, QK_NOPE, I["rcos"], I["rsin"], RL)
            emit_rope(C, z_d, COL_KR, 1, 32, kr_d, 0, I["rcos"], I["rsin"], RL)
            with S.scope():
                tmpk = S.sb("tmpk", [128, NCTX // 128, 32], F32)
                S.dma("sp", tmpk[:], z_d[RL:R, COL_KR:COL_G].rearrange("(a p) n -> p a n", p=128), writes=[tmpk])
                S.dma("sp", kr_d[RL:R, :].rearrange("(a p) n -> p a n", p=128), tmpk[:], reads=[tmpk])
            emit_attention(C, q_d, kv_d, kr_d, o_d, (0, RL), [(0, R)])
            if not last:
                emit_attention(C, q_d, kv_d, kr_d, o_d, (RL, NCTX), [(RL, NCTX)])
            for i in range(4):
                xs = [[brA], [brB], brC, [o_d]][i]
                emit_linear(C, xs, lambda ci, l=l, i=i: I["w_branch"][l][i][:, ci * 512:(ci + 1) * 512], GP[i], Rm, 512, c512,
                            prologue=("attn" if i == 3 else "none"), mul_d=zg_d[i])
            emit_linear(C, GP, lambda ci, l=l: I["w_o"][l][:, ci * 512:(ci + 1) * 512], mix_d, Rm, D, c512)
            emit_resln(C, x_cur, [mix_d], [mr(0, 2), mr(1, 2)], msel, I["ln1_g"][l], I["ln1_b"][l], x1_d, Rm)
            modr2 = [(mr(0, 4), mr(0, 3)), (mr(1, 4), mr(1, 3))]
            emit_linear(C, [x1_d], lambda ci, l=l: I["r_w"][l], lg_d, Rm, D, [(0, 36, None)], prologue="ln0mod",
                        modrows=modr2, modsel=msel, bias_d=I["r_b"][l])
            CAP = 128 * int(math.ceil(3.6 * 2.0 * Rm / N_EXPERTS / 128))
            emit_route(C, lg_d, IDX_d, GAB_d, I["lstrict"], Rm, CAP)
            emit_dispatch(C, x1_d, modr2, msel, IDX_d, Xs_d, Rm, N_EXPERTS * CAP)
            emit_experts(C, Xs_d, Ys_d, I["e_w1"][l], I["e_w3"][l], I["e_w2"][l], CAP)
            emit_combine(C, Ys_d, IDX_d, GAB_d, f_d[0], Rm)
            dst = y_out if last else xb_d
            emit_resln(C, x1_d, [f_d[0]], [mr(0, 5), mr(1, 5)], msel, I["ln2_g"][l], I["ln2_b"][l], dst, Rm)
            x_cur = xb_d
        S.finish()
        print("program instructions:", S.n, S.cnt, S.dcnt)
    return nc


def host_consts(RL, NCTX):
    rows = RL // GRID_W
    row = np.repeat(np.arange(rows, dtype=np.float32), GRID_W)
    col = np.tile(np.arange(GRID_W, dtype=np.float32), rows)
    half = QK_ROPE // 2
    inv = (ROPE_THETA ** (-np.arange(0, half, 2, dtype=np.float32) / half)).astype(np.float32)
    ar = row[:, None] * inv
    ac = col[:, None] * inv
    ang = np.concatenate([ar, ar, ac, ac], -1)
    rcos = np.cos(ang).astype(np.float32)
    rsin = np.concatenate([-np.sin(ar), np.sin(ar), -np.sin(ac), np.sin(ac)], -1).astype(np.float32)
    cw = BRANCH_W // 4
    k = np.arange(cw)
    a = 2 * np.pi * np.outer(k, k) / cw
    cs = np.zeros((512, 1024), np.float32)
    for g in range(4):
        cs[g * cw:(g + 1) * cw, g * cw:(g + 1) * cw] = np.cos(a)
        cs[g * cw:(g + 1) * cw, 512 + g * cw:512 + (g + 1) * cw] = np.sin(a)

    def dft(n):
        kk = np.arange(n, dtype=np.int64)
        ph = (np.outer(kk, kk) % n).astype(np.float64) * (2 * np.pi / n)
        sc = 1.0 / math.sqrt(n * cw)
        return np.concatenate([np.cos(ph) * sc, -np.sin(ph) * sc], 1).astype(np.float32)

    return {"rcos": rcos, "rsin": rsin, "cs128": cs, "dftlat": dft(RL), "dftctx": dft(NCTX),
            "ident": np.eye(128, dtype=np.float32),
            "lstrict": np.triu(np.ones((128, 128), np.float32), 1)}


def kernel(**inp):
    x = np.asarray(inp["x"], np.float32)
    RL = x.shape[1]
    NCTX = inp["ctx"].shape[1]
    ins = {}
    ins["x_all"] = np.concatenate([x[0], np.asarray(inp["ctx"], np.float32)[0]], 0)
    cc = np.zeros((128, D_MODEL), np.float32)
    cc[0] = np.asarray(inp["c"])[0]
    cc[1] = np.asarray(inp["c_ctx"])
    ins["cc"] = cc
    for k in WNAMES:
        ins[k] = np.ascontiguousarray(np.asarray(inp[k], np.float32))
    ins["w_ukv"] = np.concatenate([np.asarray(inp["d_w_uk"]), np.asarray(inp["d_w_uv"])], -1).astype(np.float32)
    ins["r_w"] = np.concatenate([np.asarray(inp["r_w_grp"]), np.asarray(inp["r_w_exp"])], -1).astype(np.float32)
    ins["r_b"] = np.concatenate([np.asarray(inp["r_b_grp"]), np.asarray(inp["r_b_exp"])], -1).astype(np.float32)
    ins.update(host_consts(RL, NCTX))
    shapes = {k: list(v.shape) for k, v in ins.items()}
    key = ("main", RL, NCTX)
    if key not in _PROG_CACHE:
        _PROG_CACHE[key] = build_program(shapes, RL, NCTX)
    res = run_bass_kernel_spmd(_PROG_CACHE[key], [ins], core_ids=[0])
    return np.asarray(res.results[0]["y"], np.float32)[None]
```

```python
import contextlib
import math
import numpy as np
import ml_dtypes
import concourse.bass as bass
import concourse.mybir as mybir
from concourse.bass_utils import run_bass_kernel_spmd

F32 = mybir.dt.float32
BF16 = mybir.dt.bfloat16
AF = mybir.ActivationFunctionType
ALU = mybir.AluOpType
AX = mybir.AxisListType
NCORES = 8

D_MODEL = 2048
SEQ = 8192
DEPTH = 2
GRID_W = 64
CTX_LEN = 256
BRANCH_W = 512
CHUNK = 128
CONV_W = 31
QK_NOPE = 64
QK_ROPE = 32
V_HEAD = 64
MLA_HEADS = 8
Q_LORA = 512
KV_LORA = 256
ROPE_THETA = 10000.0
MLA_SCALE = (QK_NOPE + QK_ROPE) ** -0.5
N_GROUPS = 4
EXP_PER_GROUP = 8
N_EXPERTS = 32
D_EXPERT = 1024
COL_A = 0
COL_B = 1024
COL_C = 2048
COL_D = 2560
COL_KV = 3072
COL_KR = 3328
COL_G = 3360
D_IN = COL_G + 4 * D_MODEL
ALPHA = (2 * DEPTH) ** 0.25
LN_EPS = 1e-6
TL = SEQ // NCORES


class Buf:
    __slots__ = ("t", "lw", "rd", "name")

    def __init__(self, t, name=""):
        self.t = t
        self.lw = None
        self.rd = {}
        self.name = name

    def __getitem__(self, k):
        return self.t[k]


class Sched:
    RING = 12

    def __init__(self, nc, es):
        self.nc = nc
        self.es = es
        self.engs = {"pe": nc.tensor, "act": nc.scalar, "dve": nc.vector, "pool": nc.gpsimd, "sp": nc.sync}
        self.sem = {e: es.enter_context(nc.semaphore("s_" + e)) for e in ("pe", "act", "dve", "pool")}
        self.cnt = {e: 0 for e in self.sem}
        self.dsem = {q: [es.enter_context(nc.semaphore("d_%s%d" % (q, i))) for i in range(self.RING)]
                     for q in ("sp", "pool")}
        self.dcnt = {"sp": 0, "pool": 0}
        self.waited = {e: {} for e in self.engs}
        self.n = 0

    def sb(self, name, shape, dt):
        return Buf(self.es.enter_context(self.nc.sbuf_tensor("sb%d_" % getattr(self, "uid", 0) + name, list(shape), dt)), name)

    def ps(self, name, shape, dt=F32):
        return Buf(self.es.enter_context(self.nc.psum_tensor("ps_" + name, list(shape), dt)), name)

    def _semval(self, tok):
        if tok[0] == "c":
            return ("c", tok[1]), self.sem[tok[1]], tok[2]
        q, idx = tok[1], tok[2]
        return ("d", q, idx % self.RING), self.dsem[q][idx % self.RING], 16 * (idx // self.RING + 1)

    def _wait(self, eng, toks):
        need = {}
        for tok in toks:
            if tok is None:
                continue
            if tok[0] == "c" and tok[1] == eng and eng == "pe":
                continue
            key, sem, val = self._semval(tok)
            if need.get(key, (None, 0))[1] < val:
                need[key] = (sem, val)
        w = self.waited[eng]
        for key, (sem, val) in need.items():
            if w.get(key, 0) >= val:
                continue
            self.engs[eng].wait_ge(sem, val)
            w[key] = val

    def _deps(self, reads, writes):
        toks = []
        for r in reads:
            toks.append(r.lw)
        for wv in writes:
            toks.append(wv.lw)
            toks.extend(wv.rd.values())
        return toks

    def _commit(self, tok, reads, writes, rkey):
        for r in reads:
            r.rd[rkey] = tok
        for wv in writes:
            wv.lw = tok
            wv.rd = {}

    def op(self, eng, fn, reads=(), writes=()):
        self._wait(eng, self._deps(reads, writes))
        ins = fn()
        self.cnt[eng] += 1
        ins.then_inc(self.sem[eng], 1)
        tok = ("c", eng, self.cnt[eng])
        self._commit(tok, reads, writes, eng)
        self.n += 1
        return tok

    def dma(self, q, out, in_, reads=(), writes=(), **kw):
        idx = self.dcnt[q]
        toks = self._deps(reads, writes)
        if idx >= self.RING:
            toks.append(("d", q, idx - self.RING))
        self._wait(q, toks)
        ins = self.engs[q].dma_start(out=out, in_=in_, **kw)
        ins.then_inc(self.dsem[q][idx % self.RING], 16)
        self.dcnt[q] += 1
        tok = ("d", q, idx)
        self._commit(tok, reads, writes, "dma_" + q + str(idx % self.RING))
        self.n += 1
        return tok

    def idma(self, out, in_, idx_ap, scatter, reads=(), writes=()):
        q = "pool"
        idx = self.dcnt[q]
        toks = self._deps(reads, writes)
        if idx >= self.RING:
            toks.append(("d", q, idx - self.RING))
        self._wait(q, toks)
        off = bass.IndirectOffsetOnAxis(ap=idx_ap, axis=0)
        if scatter:
            ins = self.nc.gpsimd.indirect_dma_start(out=out, out_offset=off, in_=in_, in_offset=None)
        else:
            ins = self.nc.gpsimd.indirect_dma_start(out=out, out_offset=None, in_=in_, in_offset=off)
        ins.then_inc(self.dsem[q][idx % self.RING], 16)
        self.dcnt[q] += 1
        tok = ("d", q, idx)
        self._commit(tok, reads, writes, "dma_" + q + str(idx % self.RING))
        self.n += 1
        return tok

    def barrier(self):
        toks = []
        for q in ("sp", "pool"):
            for i in range(max(0, self.dcnt[q] - self.RING), self.dcnt[q]):
                toks.append(("d", q, i))
        for e in self.cnt:
            if self.cnt[e]:
                toks.append(("c", e, self.cnt[e]))
        for e in self.engs:
            self._wait(e, [t for t in toks if not (t[0] == "c" and t[1] == e)])

    @contextlib.contextmanager
    def scope(self):
        old = self.es
        with contextlib.ExitStack() as es2:
            self.es = es2
            self.uid = getattr(self, "uid", 0) + 1
            yield
            self.barrier()
        self.es = old

    def finish(self):
        toks = []
        for q in ("sp", "pool"):
            for i in range(max(0, self.dcnt[q] - self.RING), self.dcnt[q]):
                toks.append(("d", q, i))
        for e in self.cnt:
            if self.cnt[e]:
                toks.append(("c", e, self.cnt[e]))
        self._wait("sp", toks)


def new_prog():
    nc = bass.Bass("TRN2", target_bir_lowering=False)
    es = contextlib.ExitStack()
    return nc, es


def dram_in(nc, name, shape, dt=F32):
    return nc.dram_tensor(name, list(shape), dt, kind="ExternalInput").ap()


def dram_out(nc, name, shape, dt=F32):
    return nc.dram_tensor(name, list(shape), dt, kind="ExternalOutput").ap()


_PROG_CACHE = {}


def dram_tmp(nc, name, shape, dt=F32):
    return nc.dram_tensor(name, list(shape), dt).ap()


def rstd_of(S, m):
    nc = S.nc
    S.op("act", lambda: nc.scalar.activation(out=m[:, 3:4], in_=m[:, 3:4], func=AF.Sqrt), reads=[m], writes=[m])
    S.op("dve", lambda: nc.vector.reciprocal(out=m[:, 2:3], in_=m[:, 3:4]), reads=[m], writes=[m])


def ln_stats(S, x, K, tag, ring):
    nc = S.nc
    nch = (K + 511) // 512
    st, mv = ring["st"], ring["mv"]
    for c in range(nch):
        w = min(512, K - c * 512)
        S.op("dve", lambda c=c, w=w: nc.vector.bn_stats(out=st[:, c, :], in_=x[:, c * 512:c * 512 + w]),
             reads=[x], writes=[st])
    S.op("dve", lambda: nc.vector.bn_aggr(out=mv[:, 0:2], in_=st[:, 0:nch, :]), reads=[st], writes=[mv])
    S.op("dve", lambda: nc.vector.tensor_scalar(out=mv[:, 3:4], in0=mv[:, 1:2], scalar1=LN_EPS, scalar2=None,
                                                op0=ALU.add), reads=[mv], writes=[mv])
    rstd_of(S, mv)
    return mv


class Ctx:
    def __init__(self, nc, es, ident_d):
        self.nc = nc
        self.S = S = Sched(nc, es)
        self.pT = [S.ps("pT%d" % i, [128, 1024], BF16) for i in range(2)]
        self.acc = [S.ps("acc%d" % i, [128, 512], F32) for i in range(4)]
        self.pB = [S.ps("pB%d" % i, [128, 512], F32) for i in range(2)]
        self.identb = S.sb("identb", [128, 128], BF16)
        S.dma("pool", self.identb[:], ident_d, writes=[self.identb])
        self.identf = S.sb("identf", [128, 128], F32)
        S.dma("sp", self.identf[:], ident_d, writes=[self.identf])
        self.nT = 0
        self.nA = 0


def emit_linear(C, xs, w_ap, y_d, R, K, chunks, prologue="none", modrows=None, modsel=None, bias_d=None,
                mul_d=None, g_d=None, colscale=None, RB=16):
    S, nc = C.S, C.nc
    NT = R // 128
    KC = K // 128
    nsum = len(xs)
    with S.scope():
        hT = [S.sb("hT%d" % t, [128, KC, 128], BF16) for t in range(min(RB, NT))]
        xin = [S.sb("xin%d" % i, [128, K], F32) for i in range(2)]
        xaux = [S.sb("xaux%d" % i, [128, K], F32) for i in range(2)] if nsum > 1 else None
        hb = [S.sb("hb%d" % i, [128, K], BF16) for i in range(2)]
        st = [S.sb("st%d" % i, [128, 8, 6], F32) for i in range(2)]
        mv = [S.sb("mv%d" % i, [128, 4], F32) for i in range(2)]
        if prologue == "ln0mod":
            nsets = len(modrows)
            modA = S.sb("modA", [128, nsets, K], F32)
            modB = S.sb("modB", [128, nsets, K], F32)
            for s_, (ra, rb_) in enumerate(modrows):
                S.dma("sp", modA[:, s_, :], ra.partition_broadcast(128), writes=[modA])
                S.dma("sp", modB[:, s_, :], rb_.partition_broadcast(128), writes=[modB])
            S.op("dve", lambda: nc.vector.tensor_scalar(out=modA[:], in0=modA[:], scalar1=1.0, scalar2=None, op0=ALU.add),
                 reads=[modA], writes=[modA])
        if prologue == "rms":
            gb = S.sb("gb", [128, K], F32)
            S.dma("sp", gb[:], g_d.partition_broadcast(128), writes=[gb])
            junk = S.sb("junk", [128, K], F32)
        if prologue == "attn":
            oin = [S.sb("oin%d" % i, [128, MLA_HEADS, V_HEAD + 1], F32) for i in range(2)]
            rden = [S.sb("rden%d" % i, [128, MLA_HEADS, 1], F32) for i in range(2)]
        wb = [S.sb("wb%d" % i, [128, KC, 512], BF16) for i in range(2)]
        bb = [S.sb("bb%d" % i, [128, 512], F32) for i in range(2)] if bias_d is not None else None
        ot = [S.sb("ot%d" % i, [128, 512], F32) for i in range(3)]
        mt = [S.sb("mt%d" % i, [128, 512], F32) for i in range(3)] if mul_d is not None else None
        if colscale is not None:
            gcol_d, gfn = colscale
            ncol = gcol_d.shape[1]
            gt = [S.sb("gt%d" % t, [128, ncol], F32) for t in range(min(RB, NT))]
        wi = 0
        it = 0
        for t0 in range(0, NT, RB):
            tiles = list(range(t0, min(NT, t0 + RB)))
            for t in tiles:
                x = xin[t % 2]
                h = hb[t % 2]
                rows = slice(t * 128, (t + 1) * 128)
                if colscale is not None:
                    S.dma("sp", gt[t - t0][:], gcol_d[rows], writes=[gt[t - t0]])
                if prologue == "attn":
                    o = oin[t % 2]
                    rd_ = rden[t % 2]
                    S.dma("sp", o[:], xs[0][rows], writes=[o])
                    S.op("dve", lambda o=o, rd_=rd_: nc.vector.reciprocal(out=rd_[:], in_=o[:, :, V_HEAD:V_HEAD + 1]),
                         reads=[o], writes=[rd_])
                    for hh in range(MLA_HEADS):
                        S.op("dve", lambda o=o, rd_=rd_, h=h, hh=hh: nc.vector.tensor_scalar(
                            out=h[:, hh * V_HEAD:(hh + 1) * V_HEAD], in0=o[:, hh, 0:V_HEAD], scalar1=rd_[:, hh, :],
                            scalar2=None, op0=ALU.mult), reads=[o, rd_], writes=[h])
                else:
                    S.dma("sp", x[:], xs[0][rows], writes=[x])
                    for i in range(1, nsum):
                        xa = xaux[i % 2]
                        S.dma("sp", xa[:], xs[i][rows], writes=[xa])
                        S.op("dve", lambda x=x, xa=xa: nc.vector.tensor_tensor(out=x[:], in0=x[:], in1=xa[:], op=ALU.add),
                             reads=[x, xa], writes=[x])
                    if prologue == "none":
                        S.op("act", lambda x=x, h=h: nc.scalar.copy(out=h[:], in_=x[:]), reads=[x], writes=[h])
                    elif prologue == "silu":
                        S.op("act", lambda x=x, h=h: nc.scalar.activation(out=h[:], in_=x[:], func=AF.Silu),
                             reads=[x], writes=[h])
                    elif prologue == "ln0mod":
                        m = ln_stats(S, x, K, "p", {"st": st[t % 2], "mv": mv[t % 2]})
                        ms = modsel(t)
                        S.op("dve", lambda x=x, m=m: nc.vector.tensor_scalar(
                            out=x[:], in0=x[:], scalar1=m[:, 0:1], scalar2=m[:, 2:3], op0=ALU.subtract, op1=ALU.mult),
                            reads=[x, m], writes=[x])
                        S.op("dve", lambda x=x, ms=ms: nc.vector.tensor_tensor(out=x[:], in0=x[:], in1=modA[:, ms, :],
                                                                               op=ALU.mult), reads=[x, modA], writes=[x])
                        S.op("dve", lambda x=x, h=h, ms=ms: nc.vector.tensor_tensor(
                            out=h[:], in0=x[:], in1=modB[:, ms, :], op=ALU.add), reads=[x, modB], writes=[h])
                    elif prologue == "rms":
                        m = mv[t % 2]
                        S.op("act", lambda x=x, m=m: nc.scalar.activation(out=junk[:], in_=x[:], func=AF.Square,
                                                                          accum_out=m[:, 0:1]),
                             reads=[x], writes=[junk, m])
                        S.op("dve", lambda m=m: nc.vector.tensor_scalar(out=m[:, 3:4], in0=m[:, 0:1], scalar1=1.0 / K,
                                                                        scalar2=LN_EPS, op0=ALU.mult, op1=ALU.add),
                             reads=[m], writes=[m])
                        rstd_of(S, m)
                        S.op("dve", lambda x=x, h=h, m=m: nc.vector.scalar_tensor_tensor(
                            out=h[:], in0=x[:], scalar=m[:, 2:3], in1=gb[:], op0=ALU.mult, op1=ALU.mult),
                            reads=[x, m, gb], writes=[h])
                for k0 in range(0, KC, 8):
                    kn = min(8, KC - k0)
                    p = C.pT[C.nT % 2]
                    C.nT += 1
                    for j in range(kn):
                        S.op("pe", lambda p=p, h=h, j=j, k0=k0: nc.tensor.transpose(
                            out=p[:, j * 128:(j + 1) * 128], in_=h[:, (k0 + j) * 128:(k0 + j + 1) * 128],
                            identity=C.identb[:]), reads=[h, C.identb], writes=[p])
                    S.op("act", lambda p=p, t=t, k0=k0, kn=kn: nc.scalar.copy(
                        out=hT[t - t0][:, k0:k0 + kn, :], in_=p[:, 0:kn * 128].rearrange("p (a b) -> p a b", b=128)),
                        reads=[p], writes=[hT[t - t0]])
            for ci, (c0, nw, act) in enumerate(chunks):
                w = wb[wi % 2]
                wi += 1
                wv = w_ap(ci).rearrange("(kc p) n -> p kc n", p=128)
                for k0 in range(0, KC, 16):
                    k1 = min(KC, k0 + 16)
                    S.dma("pool", w[:, k0:k1, 0:nw], wv[:, k0:k1, :], writes=[w])
                if bias_d is not None:
                    b = bb[wi % 2]
                    S.dma("sp", b[:, 0:nw], bias_d[c0:c0 + nw].partition_broadcast(128), writes=[b])
                for t in tiles:
                    a = C.acc[C.nA % 4]
                    C.nA += 1
                    o = ot[it % 3]
                    for kc in range(KC):
                        S.op("pe", lambda a=a, w=w, t=t, kc=kc, nw=nw: nc.tensor.matmul(
                            out=a[:, 0:nw], lhsT=hT[t - t0][:, kc, :], rhs=w[:, kc, 0:nw], start=(kc == 0),
                            stop=(kc == KC - 1)), reads=[hT[t - t0], w], writes=[a])
                    if bias_d is not None:
                        S.op("dve", lambda a=a, o=o, b=b, nw=nw: nc.vector.tensor_tensor(
                            out=o[:, 0:nw], in0=a[:, 0:nw], in1=b[:, 0:nw], op=ALU.add), reads=[a, b], writes=[o])
                        if act is not None:
                            S.op("act", lambda o=o, nw=nw, act=act: nc.scalar.activation(
                                out=o[:, 0:nw], in_=o[:, 0:nw], func=act), reads=[o], writes=[o])
                    else:
                        S.op("act", lambda a=a, o=o, nw=nw, act=act: nc.scalar.activation(
                            out=o[:, 0:nw], in_=a[:, 0:nw], func=(act if act is not None else AF.Copy)),
                            reads=[a], writes=[o])
                    if colscale is not None:
                        e_ = gfn(ci)
                        S.op("dve", lambda o=o, t=t, e_=e_, nw=nw: nc.vector.tensor_scalar(
                            out=o[:, 0:nw], in0=o[:, 0:nw], scalar1=gt[t - t0][:, e_:e_ + 1], scalar2=None, op0=ALU.mult),
                            reads=[o, gt[t - t0]], writes=[o])
                    if mul_d is not None:
                        m_ = mt[it % 3]
                        S.dma("sp", m_[:, 0:nw], mul_d[t * 128:(t + 1) * 128, c0:c0 + nw], writes=[m_])
                        S.op("dve", lambda o=o, m_=m_, nw=nw: nc.vector.tensor_tensor(
                            out=o[:, 0:nw], in0=o[:, 0:nw], in1=m_[:, 0:nw], op=ALU.mult), reads=[o, m_], writes=[o])
                    S.dma("sp", y_d[t * 128:(t + 1) * 128, c0:c0 + nw], o[:, 0:nw], reads=[o])
                    it += 1


def emit_resln(C, x_d, fs, m_rows, msel, g_row, b_row, y_d, R):
    S, nc = C.S, C.nc
    D = D_MODEL
    with S.scope():
        mm = S.sb("mm", [128, len(m_rows), D], F32)
        for i, r in enumerate(m_rows):
            S.dma("sp", mm[:, i, :], r.partition_broadcast(128), writes=[mm])
        gb = S.sb("g", [128, D], F32)
        bb = S.sb("b", [128, D], F32)
        S.dma("sp", gb[:], g_row.partition_broadcast(128), writes=[gb])
        S.dma("sp", bb[:], b_row.partition_broadcast(128), writes=[bb])
        xt = [S.sb("x%d" % i, [128, D], F32) for i in range(2)]
        ft = [S.sb("f%d" % i, [128, D], F32) for i in range(2)]
        fa = [S.sb("fa%d" % i, [128, D], F32) for i in range(2)]
        st = [S.sb("st%d" % i, [128, 8, 6], F32) for i in range(2)]
        mv = [S.sb("mv%d" % i, [128, 4], F32) for i in range(2)]
        for t in range(R // 128):
            rows = slice(t * 128, (t + 1) * 128)
            x, f = xt[t % 2], ft[t % 2]
            S.dma("sp", x[:], x_d[rows], writes=[x])
            S.dma("sp", f[:], fs[0][rows], writes=[f])
            for i in range(1, len(fs)):
                a = fa[i % 2]
                S.dma("sp", a[:], fs[i][rows], writes=[a])
                S.op("dve", lambda f=f, a=a: nc.vector.tensor_tensor(out=f[:], in0=f[:], in1=a[:], op=ALU.add),
                     reads=[f, a], writes=[f])
            ms = msel(t)
            S.op("dve", lambda f=f, ms=ms: nc.vector.tensor_tensor(out=f[:], in0=f[:], in1=mm[:, ms, :], op=ALU.mult),
                 reads=[f, mm], writes=[f])
            S.op("dve", lambda x=x, f=f: nc.vector.scalar_tensor_tensor(out=x[:], in0=x[:], scalar=ALPHA, in1=f[:],
                                                                        op0=ALU.mult, op1=ALU.add),
                 reads=[x, f], writes=[x])
            m = ln_stats(S, x, D, "r", {"st": st[t % 2], "mv": mv[t % 2]})
            S.op("dve", lambda x=x, m=m: nc.vector.tensor_scalar(out=x[:], in0=x[:], scalar1=m[:, 0:1], scalar2=m[:, 2:3],
                                                                 op0=ALU.subtract, op1=ALU.mult), reads=[x, m], writes=[x])
            S.op("dve", lambda x=x: nc.vector.tensor_tensor(out=x[:], in0=x[:], in1=gb[:], op=ALU.mult),
                 reads=[x, gb], writes=[x])
            S.op("dve", lambda x=x: nc.vector.tensor_tensor(out=x[:], in0=x[:], in1=bb[:], op=ALU.add),
                 reads=[x, bb], writes=[x])
            S.dma("sp", y_d[rows], x[:], reads=[x])


def gelu_tanh(S, x, t, W):
    nc = S.nc
    S.op("dve", lambda: nc.vector.tensor_tensor(out=t[:, 0:W], in0=x[:, 0:W], in1=x[:, 0:W], op=ALU.mult), reads=[x], writes=[t])
    S.op("dve", lambda: nc.vector.tensor_scalar(out=t[:, 0:W], in0=t[:, 0:W], scalar1=0.044715, scalar2=1.0, op0=ALU.mult,
                                                op1=ALU.add), reads=[t], writes=[t])
    S.op("dve", lambda: nc.vector.tensor_tensor(out=t[:, 0:W], in0=t[:, 0:W], in1=x[:, 0:W], op=ALU.mult), reads=[t, x], writes=[t])
    S.op("act", lambda: nc.scalar.activation(out=t[:, 0:W], in_=t[:, 0:W], func=AF.Sigmoid, scale=1.5957691216057308),
         reads=[t], writes=[t])
    S.op("dve", lambda: nc.vector.tensor_tensor(out=x[:, 0:W], in0=x[:, 0:W], in1=t[:, 0:W], op=ALU.mult), reads=[t, x], writes=[x])


def ln_rows(S, x, W, st, mv, gb, bb, out=None):
    nc = S.nc
    out = out if out is not None else x
    m = ln_stats(S, x, W, "l", {"st": st, "mv": mv})
    S.op("dve", lambda: nc.vector.tensor_scalar(out=x[:, 0:W], in0=x[:, 0:W], scalar1=m[:, 0:1], scalar2=m[:, 2:3],
                                                op0=ALU.subtract, op1=ALU.mult), reads=[x, m], writes=[x])
    S.op("dve", lambda: nc.vector.tensor_tensor(out=x[:, 0:W], in0=x[:, 0:W], in1=gb[:, 0:W], op=ALU.mult), reads=[x, gb], writes=[x])
    S.op("dve", lambda: nc.vector.tensor_tensor(out=out[:, 0:W], in0=x[:, 0:W], in1=bb[:, 0:W], op=ALU.add),
         reads=[x, bb], writes=[out])


def emit_chunkgate(C, z_d, lng, lnb, ws_d, bs_d, y_d, R):
    S, nc = C.S, C.nc
    with S.scope():
        gb = S.sb("g", [128, 512], F32)
        bb = S.sb("b", [128, 512], F32)
        S.dma("sp", gb[:], lng.partition_broadcast(128), writes=[gb])
        S.dma("sp", bb[:], lnb.partition_broadcast(128), writes=[bb])
        wraw = S.sb("wraw", [128, 4, 128], BF16)
        wT = S.sb("wT", [128, 4, 128], BF16)
        S.dma("pool", wraw[:], ws_d.rearrange("g p q -> p g q"), writes=[wraw])
        p = C.pT[C.nT % 2]
        C.nT += 1
        for g in range(4):
            S.op("pe", lambda g=g: nc.tensor.transpose(out=p[:, g * 128:(g + 1) * 128], in_=wraw[:, g, :], identity=C.identb[:]),
                 reads=[wraw, C.identb], writes=[p])
        S.op("act", lambda: nc.scalar.copy(out=wT[:], in_=p[:, 0:512].rearrange("p (a b) -> p a b", b=128)), reads=[p], writes=[wT])
        bs = S.sb("bs", [128, 4], F32)
        S.dma("sp", bs[:], bs_d.rearrange("g p -> p g"), writes=[bs], allow_slow_non_contiguous=True)
        uv = [S.sb("uv%d" % i, [128, 1024], F32) for i in range(2)]
        tt = [S.sb("tt%d" % i, [128, 1024], F32) for i in range(2)]
        vb = [S.sb("vb%d" % i, [128, 512], BF16) for i in range(2)]
        ot = [S.sb("ot%d" % i, [128, 512], F32) for i in range(2)]
        st = [S.sb("st%d" % i, [128, 8, 6], F32) for i in range(2)]
        mv = [S.sb("mv%d" % i, [128, 4], F32) for i in range(2)]
        for t in range(R // 128):
            rows = slice(t * 128, (t + 1) * 128)
            x, tmp, v, o = uv[t % 2], tt[t % 2], vb[t % 2], ot[t % 2]
            S.dma("sp", x[:], z_d[rows, 0:1024], writes=[x])
            gelu_tanh(S, x, tmp, 1024)
            m = ln_stats(S, Buf(x.t[:, 512:1024]) if False else x, 512, "c", {"st": st[t % 2], "mv": mv[t % 2]}) if False else None
            nchs = 1
            S.op("dve", lambda x=x, t=t: nc.vector.bn_stats(out=st[t % 2][:, 0, :], in_=x[:, 512:1024]), reads=[x], writes=[st[t % 2]])
            mvt = mv[t % 2]
            S.op("dve", lambda t=t, mvt=mvt: nc.vector.bn_aggr(out=mvt[:, 0:2], in_=st[t % 2][:, 0:1, :]), reads=[st[t % 2]], writes=[mvt])
            S.op("dve", lambda mvt=mvt: nc.vector.tensor_scalar(out=mvt[:, 3:4], in0=mvt[:, 1:2], scalar1=LN_EPS, scalar2=None,
                                                                op0=ALU.add), reads=[mvt], writes=[mvt])
            rstd_of(S, mvt)
            S.op("dve", lambda x=x, mvt=mvt: nc.vector.tensor_scalar(out=x[:, 512:1024], in0=x[:, 512:1024], scalar1=mvt[:, 0:1],
                                                                     scalar2=mvt[:, 2:3], op0=ALU.subtract, op1=ALU.mult),
                 reads=[x, mvt], writes=[x])
            S.op("dve", lambda x=x: nc.vector.tensor_tensor(out=x[:, 512:1024], in0=x[:, 512:1024], in1=gb[:], op=ALU.mult),
                 reads=[x, gb], writes=[x])
            S.op("dve", lambda x=x, v=v: nc.vector.tensor_tensor(out=v[:], in0=x[:, 512:1024], in1=bb[:], op=ALU.add),
                 reads=[x, bb], writes=[v])
            a = C.acc[C.nA % 4]
            C.nA += 1
            for g in range(4):
                S.op("pe", lambda a=a, v=v, g=g: nc.tensor.matmul(out=a[:, g * 128:(g + 1) * 128], lhsT=wT[:, g, :],
                                                                  rhs=v[:, g * 128:(g + 1) * 128], start=True, stop=True),
                     reads=[wT, v], writes=[a])
            for g in range(4):
                S.op("dve", lambda a=a, o=o, x=x, g=g: nc.vector.scalar_tensor_tensor(
                    out=o[:, g * 128:(g + 1) * 128], in0=a[:, g * 128:(g + 1) * 128], scalar=bs[:, g:g + 1],
                    in1=x[:, g * 128:(g + 1) * 128], op0=ALU.add, op1=ALU.mult), reads=[a, bs, x], writes=[o])
            S.dma("sp", y_d[rows], o[:], reads=[o])


def emit_conv(C, z_d, wdw_d, bdw_d, lng, lnb, y_d, segs, halo=None):
    S, nc = C.S, C.nc
    with S.scope():
        gb = S.sb("g", [128, 512], F32)
        bb = S.sb("b", [128, 512], F32)
        S.dma("sp", gb[:], lng.partition_broadcast(128), writes=[gb])
        S.dma("sp", bb[:], lnb.partition_broadcast(128), writes=[bb])
        wd = S.sb("wd", [128, 4, CONV_W], F32)
        bd = S.sb("bd", [128, 4], F32)
        for c in range(4):
            S.dma("sp", wd[:, c, :], wdw_d[:, c * 128:(c + 1) * 128].rearrange("k p -> p k"), writes=[wd],
                  allow_slow_non_contiguous=True)
            S.dma("sp", bd[:, c:c + 1], bdw_d[c * 128:(c + 1) * 128].rearrange("(p o) -> p o", o=1), writes=[bd],
                  allow_slow_non_contiguous=True)
        yT = S.sb("yT", [128, 4, 1280], F32)
        cacc = S.sb("cacc", [128, 4, 1024], F32)
        ab = [S.sb("ab%d" % i, [128, 1024], F32) for i in range(2)]
        yy = [S.sb("yy%d" % i, [128, 512], F32) for i in range(2)]
        ot = [S.sb("ot%d" % i, [128, 512], F32) for i in range(2)]
        st = [S.sb("st%d" % i, [128, 8, 6], F32) for i in range(2)]
        mv = [S.sb("mv%d" % i, [128, 4], F32) for i in range(2)]
        it = 0
        for si, (r0, nr) in enumerate(segs):
            for b0 in range(0, nr, 1024):
                bl = min(1024, nr - b0)
                nti = bl // 128
                S.op("pool", lambda: nc.gpsimd.memset(yT[:], 0.0), writes=[yT])
                for j in range(-1, nti + 1):
                    tr = b0 + j * 128
                    src = None
                    if 0 <= tr < nr:
                        src = z_d[r0 + tr:r0 + tr + 128, 1024:2048]
                    elif halo is not None and si == 0:
                        src = halo[0:128, :] if tr < 0 else halo[128:256, :]
                    if src is None:
                        continue
                    x, y = ab[it % 2], yy[it % 2]
                    it += 1
                    S.dma("sp", x[:], src, writes=[x])
                    S.op("dve", lambda x=x, y=y: nc.vector.tensor_tensor(out=y[:], in0=x[:, 0:512], in1=x[:, 512:1024], op=ALU.mult),
                         reads=[x], writes=[y])
                    a = C.acc[C.nA % 4]
                    C.nA += 1
                    for c in range(4):
                        S.op("pe", lambda a=a, y=y, c=c: nc.tensor.transpose(out=a[:, c * 128:(c + 1) * 128],
                                                                             in_=y[:, c * 128:(c + 1) * 128], identity=C.identf[:]),
                             reads=[y, C.identf], writes=[a])
                    S.op("act", lambda a=a, j=j: nc.scalar.copy(out=yT[:, :, (j + 1) * 128:(j + 2) * 128],
                                                                in_=a[:, 0:512].rearrange("p (c t) -> p c t", t=128)),
                         reads=[a], writes=[yT])
                for c in range(4):
                    for k in range(CONV_W):
                        src = yT[:, c, 113 + k:113 + k + bl]
                        if k == 0:
                            S.op("dve", lambda c=c, src=src: nc.vector.tensor_scalar(
                                out=cacc[:, c, 0:bl], in0=src, scalar1=wd[:, c, 0:1], scalar2=bd[:, c:c + 1], op0=ALU.mult,
                                op1=ALU.add), reads=[yT, wd, bd], writes=[cacc])
                        else:
                            S.op("dve", lambda c=c, k=k, src=src: nc.vector.scalar_tensor_tensor(
                                out=cacc[:, c, 0:bl], in0=src, scalar=wd[:, c, k:k + 1], in1=cacc[:, c, 0:bl], op0=ALU.mult,
                                op1=ALU.add), reads=[yT, wd, cacc], writes=[cacc])
                for j in range(nti):
                    a = C.acc[C.nA % 4]
                    C.nA += 1
                    o = ot[j % 2]
                    for c in range(4):
                        S.op("pe", lambda a=a, c=c, j=j: nc.tensor.transpose(out=a[:, c * 128:(c + 1) * 128],
                                                                             in_=cacc[:, c, j * 128:(j + 1) * 128], identity=C.identf[:]),
                             reads=[cacc, C.identf], writes=[a])
                    S.op("act", lambda a=a, o=o: nc.scalar.copy(out=o[:], in_=a[:]), reads=[a], writes=[o])
                    ln_rows(S, o, 512, st[j % 2], mv[j % 2], gb, bb)
                    S.op("act", lambda o=o: nc.scalar.activation(out=o[:], in_=o[:], func=AF.Silu), reads=[o], writes=[o])
                    S.dma("sp", y_d[r0 + b0 + j * 128:r0 + b0 + (j + 1) * 128], o[:], reads=[o])


def emit_rope(C, src_d, c0, H, stride, dst_d, d0, cos_d, sin_d, nrows):
    S, nc = C.S, C.nc
    W = (H - 1) * stride + 32
    with S.scope():
        xt = [S.sb("x%d" % i, [128, W], F32) for i in range(2)]
        cs = [S.sb("cs%d" % i, [128, 32], F32) for i in range(2)]
        sn = [S.sb("sn%d" % i, [128, 32], F32) for i in range(2)]
        t1 = [S.sb("t1%d" % i, [128, 32], F32) for i in range(2)]
        for t in range(nrows // 128):
            rows = slice(t * 128, (t + 1) * 128)
            x, c_, s_, u = xt[t % 2], cs[t % 2], sn[t % 2], t1[t % 2]
            S.dma("sp", x[:], src_d[rows, c0:c0 + W], writes=[x])
            S.dma("sp", c_[:], cos_d[rows], writes=[c_])
            S.dma("sp", s_[:], sin_d[rows], writes=[s_])
            for h in range(H):
                o = h * stride
                for (a0, b0_) in ((0, 8), (8, 0), (16, 24), (24, 16)):
                    S.op("dve", lambda x=x, s_=s_, u=u, o=o, a0=a0, b0_=b0_: nc.vector.tensor_tensor(
                        out=u[:, a0:a0 + 8], in0=x[:, o + b0_:o + b0_ + 8], in1=s_[:, a0:a0 + 8], op=ALU.mult),
                        reads=[x, s_], writes=[u])
                S.op("dve", lambda x=x, c_=c_, o=o: nc.vector.tensor_tensor(out=x[:, o:o + 32], in0=x[:, o:o + 32], in1=c_[:], op=ALU.mult),
                     reads=[x, c_], writes=[x])
                S.op("dve", lambda x=x, u=u, o=o: nc.vector.tensor_tensor(out=x[:, o:o + 32], in0=x[:, o:o + 32], in1=u[:], op=ALU.add),
                     reads=[x, u], writes=[x])
            S.dma("sp", dst_d[rows, d0:d0 + W], x[:], reads=[x])


def emit_attention(C, q_d, kv_d, kr_d, o_d, q_rows, k_rows):
    S, nc = C.S, C.nc
    ktiles = [r0 + i * 128 for (r0, n) in k_rows for i in range(n // 128)]
    NKT = len(ktiles)
    q0, nq = q_rows
    NQT = nq // 128
    DK = QK_NOPE + QK_ROPE
    with S.scope():
        KT = S.sb("KT", [DK, NKT * 128], BF16)
        QT = S.sb("QT", [DK, NQT * 128], BF16)
        Vt = S.sb("Vt", [128, NKT, V_HEAD + 1], BF16)
        kin = [S.sb("kin%d" % i, [128, DK], F32) for i in range(2)]
        kb = [S.sb("kb%d" % i, [128, DK], BF16) for i in range(2)]
        pt = [S.sb("pt%d" % i, [128, 512], BF16) for i in range(3)]
        of = [S.sb("of%d" % i, [V_HEAD + 1, 512], F32) for i in range(2)]
        oo = [S.sb("oo%d" % i, [128, 4, V_HEAD + 1], F32) for i in range(2)]
        ones = S.sb("ones", [128, NKT, 1], F32)
        S.op("pool", lambda: nc.gpsimd.memset(ones[:], 1.0), writes=[ones])
        S.op("act", lambda: nc.scalar.copy(out=Vt[:, :, V_HEAD:V_HEAD + 1], in_=ones[:]), reads=[ones], writes=[Vt])
        n_ = 0
        for h in range(MLA_HEADS):
            for i, r in enumerate(ktiles):
                x, xb = kin[n_ % 2], kb[n_ % 2]
                n_ += 1
                S.dma("sp", x[:, 0:QK_NOPE], kv_d[r:r + 128, h * QK_NOPE:(h + 1) * QK_NOPE], writes=[x])
                S.dma("sp", x[:, QK_NOPE:DK], kr_d[r:r + 128, :], writes=[x])
                S.dma("pool", Vt[:, i, 0:V_HEAD], kv_d[r:r + 128, 512 + h * V_HEAD:512 + (h + 1) * V_HEAD], writes=[Vt])
                S.op("act", lambda x=x, xb=xb: nc.scalar.copy(out=xb[:], in_=x[:]), reads=[x], writes=[xb])
                p = C.pT[C.nT % 2]
                C.nT += 1
                S.op("pe", lambda p=p, xb=xb: nc.tensor.transpose(out=p[0:DK, 0:128], in_=xb[:], identity=C.identb[:]),
                     reads=[xb, C.identb], writes=[p])
                S.op("act", lambda p=p, i=i: nc.scalar.copy(out=KT[:, i * 128:(i + 1) * 128], in_=p[0:DK, 0:128]),
                     reads=[p], writes=[KT])
            for i in range(NQT):
                r = q0 + i * 128
                x, xb = kin[n_ % 2], kb[n_ % 2]
                n_ += 1
                S.dma("sp", x[:], q_d[r:r + 128, h * DK:(h + 1) * DK], writes=[x])
                S.op("act", lambda x=x, xb=xb: nc.scalar.copy(out=xb[:], in_=x[:]), reads=[x], writes=[xb])
                p = C.pT[C.nT % 2]
                C.nT += 1
                S.op("pe", lambda p=p, xb=xb: nc.tensor.transpose(out=p[0:DK, 0:128], in_=xb[:], identity=C.identb[:]),
                     reads=[xb, C.identb], writes=[p])
                S.op("act", lambda p=p, i=i: nc.scalar.copy(out=QT[:, i * 128:(i + 1) * 128], in_=p[0:DK, 0:128]),
                     reads=[p], writes=[QT])
            for qb in range(0, NQT * 128, 512):
                qw = min(512, NQT * 128 - qb)
                ao = C.pB[(qb // 512) % 2]
                for i in range(NKT):
                    a = C.acc[C.nA % 4]
                    C.nA += 1
                    pp = pt[i % 3]
                    S.op("pe", lambda a=a, i=i, qb=qb, qw=qw: nc.tensor.matmul(out=a[:, 0:qw], lhsT=KT[:, i * 128:(i + 1) * 128],
                                                                              rhs=QT[:, qb:qb + qw], start=True, stop=True),
                         reads=[KT, QT], writes=[a])
                    S.op("act", lambda a=a, pp=pp, qw=qw: nc.scalar.activation(out=pp[:, 0:qw], in_=a[:, 0:qw], func=AF.Exp,
                                                                              scale=MLA_SCALE), reads=[a], writes=[pp])
                    S.op("pe", lambda ao=ao, pp=pp, i=i, qw=qw: nc.tensor.matmul(out=ao[0:V_HEAD + 1, 0:qw], lhsT=Vt[:, i, :],
                                                                                rhs=pp[:, 0:qw], start=(i == 0), stop=(i == NKT - 1)),
                         reads=[Vt, pp], writes=[ao])
                o1 = of[(qb // 512) % 2]
                o2 = oo[(qb // 512) % 2]
                S.op("act", lambda ao=ao, o1=o1, qw=qw: nc.scalar.copy(out=o1[:, 0:qw], in_=ao[0:V_HEAD + 1, 0:qw]), reads=[ao], writes=[o1])
                a = C.acc[C.nA % 4]
                C.nA += 1
                nsub = qw // 128
                for j in range(nsub):
                    S.op("pe", lambda a=a, o1=o1, j=j: nc.tensor.transpose(out=a[:, j * 65:(j + 1) * 65], in_=o1[:, j * 128:(j + 1) * 128],
                                                                           identity=C.identf[0:V_HEAD + 1, 0:V_HEAD + 1]),
                         reads=[o1, C.identf], writes=[a])
                S.op("dve", lambda a=a, o2=o2, nsub=nsub: nc.vector.tensor_copy(
                    out=o2[:, 0:nsub, :], in_=a[:, 0:nsub * 65].rearrange("p (j d) -> p j d", d=65)), reads=[a], writes=[o2])
                for j in range(nsub):
                    r = q0 + qb + j * 128
                    S.dma("sp", o_d[r:r + 128, h, :], o2[:, j, :], reads=[o2])


def emit_route(C, lg_d, IDX_d, GAB_d, lstrict_d, R, CAP):
    S, nc = C.S, C.nc
    I32 = mybir.dt.int32
    with S.scope():
        L = [S.sb("L%d" % i, [128, 36], F32) for i in range(2)]
        W = [S.sb("W%d" % i, [128, 256], F32) for i in range(2)]
        Mi = [S.sb("Mi%d" % i, [128, 32], BF16) for i in range(2)]
        II = [S.sb("II%d" % i, [128, 2], I32) for i in range(2)]
        Macc = S.sb("Macc", [128, 32], BF16)
        lst = S.sb("lst", [128, 128], BF16)
        onesb = S.sb("onesb", [128, 128], BF16)
        ebase = S.sb("ebase", [128, 32], F32)
        S.dma("pool", lst[:], lstrict_d, writes=[lst])
        S.op("pool", lambda: nc.gpsimd.memset(onesb[:], 1.0), writes=[onesb])
        S.op("pool", lambda: nc.gpsimd.memset(Macc[:], 0.0), writes=[Macc])
        for e in range(N_EXPERTS):
            S.op("pool", lambda e=e: nc.gpsimd.memset(ebase[:, e:e + 1], float(e * CAP)), writes=[ebase])
        for t in range(R // 128):
            rows = slice(t * 128, (t + 1) * 128)
            l, w, mi, ii = L[t % 2], W[t % 2], Mi[t % 2], II[t % 2]
            S.dma("sp", l[:], lg_d[rows], writes=[l])
            S.op("dve", lambda l=l, w=w: nc.vector.tensor_reduce(out=w[:, 0:1], in_=l[:, 0:4], axis=AX.X, op=ALU.max), reads=[l], writes=[w])
            S.op("dve", lambda l=l, w=w: nc.vector.tensor_scalar(out=w[:, 4:8], in0=l[:, 0:4], scalar1=w[:, 0:1], scalar2=None, op0=ALU.is_ge),
                 reads=[l, w], writes=[w])
            S.op("dve", lambda w=w: nc.vector.tensor_scalar(out=w[:, 1:2], in0=w[:, 0:1], scalar1=-1.0, scalar2=None, op0=ALU.mult),
                 reads=[w], writes=[w])
            S.op("act", lambda l=l, w=w: nc.scalar.activation(out=w[:, 44:48], in_=l[:, 0:4], func=AF.Exp, bias=w[:, 1:2], scale=1.0),
                 reads=[l, w], writes=[w])
            S.op("dve", lambda w=w: nc.vector.tensor_reduce(out=w[:, 2:3], in_=w[:, 44:48], axis=AX.X, op=ALU.add), reads=[w], writes=[w])
            S.op("dve", lambda w=w: nc.vector.reciprocal(out=w[:, 3:4], in_=w[:, 2:3]), reads=[w], writes=[w])
            S.op("dve", lambda l=l, w=w: nc.vector.tensor_scalar(out=w[:, 8:16], in0=l[:, 4:12], scalar1=w[:, 4:5], scalar2=None, op0=ALU.mult),
                 reads=[l, w], writes=[w])
            for gi in range(1, 4):
                S.op("dve", lambda l=l, w=w, gi=gi: nc.vector.scalar_tensor_tensor(
                    out=w[:, 8:16], in0=l[:, 4 + gi * 8:12 + gi * 8], scalar=w[:, 4 + gi:5 + gi], in1=w[:, 8:16], op0=ALU.mult, op1=ALU.add),
                    reads=[l, w], writes=[w])
            S.op("dve", lambda w=w: nc.vector.max(out=w[:, 16:24], in_=w[:, 8:16]), reads=[w], writes=[w])
            S.op("dve", lambda w=w: nc.vector.tensor_scalar(out=w[:, 24:32], in0=w[:, 8:16], scalar1=w[:, 17:18], scalar2=None, op0=ALU.is_ge),
                 reads=[w], writes=[w])
            S.op("dve", lambda w=w: nc.vector.tensor_scalar(out=w[:, 48:56], in0=w[:, 8:16], scalar1=w[:, 16:17], scalar2=None, op0=ALU.is_ge),
                 reads=[w], writes=[w])
            S.op("dve", lambda w=w: nc.vector.tensor_tensor(out=w[:, 56:64], in0=w[:, 24:32], in1=w[:, 48:56], op=ALU.subtract), reads=[w], writes=[w])
            S.op("dve", lambda w=w: nc.vector.tensor_scalar(out=w[:, 40:41], in0=w[:, 16:17], scalar1=-1.0, scalar2=None, op0=ALU.mult),
                 reads=[w], writes=[w])
            S.op("act", lambda w=w: nc.scalar.activation(out=w[:, 32:40], in_=w[:, 8:16], func=AF.Exp, bias=w[:, 40:41], scale=1.0),
                 reads=[w], writes=[w])
            S.op("act", lambda w=w: nc.scalar.activation(out=w[:, 41:42], in_=w[:, 17:18], func=AF.Exp, bias=w[:, 40:41], scale=1.0),
                 reads=[w], writes=[w])
            S.op("dve", lambda w=w: nc.vector.tensor_scalar(out=w[:, 41:42], in0=w[:, 41:42], scalar1=1.0, scalar2=None, op0=ALU.add),
                 reads=[w], writes=[w])
            S.op("dve", lambda w=w: nc.vector.reciprocal(out=w[:, 42:43], in_=w[:, 41:42]), reads=[w], writes=[w])
            S.op("dve", lambda w=w: nc.vector.tensor_tensor(out=w[:, 42:43], in0=w[:, 42:43], in1=w[:, 3:4], op=ALU.mult), reads=[w], writes=[w])
            S.op("dve", lambda w=w: nc.vector.tensor_scalar(out=w[:, 64:72], in0=w[:, 32:40], scalar1=w[:, 42:43], scalar2=None, op0=ALU.mult),
                 reads=[w], writes=[w])
            for (sc, oc) in ((48, 80), (56, 81)):
                S.op("dve", lambda w=w, sc=sc: nc.vector.tensor_tensor(out=w[:, 72:80], in0=w[:, 64:72], in1=w[:, sc:sc + 8], op=ALU.mult),
                     reads=[w], writes=[w])
                S.op("dve", lambda w=w, oc=oc: nc.vector.tensor_reduce(out=w[:, oc:oc + 1], in_=w[:, 72:80], axis=AX.X, op=ALU.add),
                     reads=[w], writes=[w])
            for gi in range(4):
                S.op("dve", lambda w=w, gi=gi: nc.vector.tensor_scalar(out=w[:, 96 + gi * 8:104 + gi * 8], in0=w[:, 48:56],
                                                                       scalar1=w[:, 4 + gi:5 + gi], scalar2=None, op0=ALU.mult),
                     reads=[w], writes=[w])
                S.op("dve", lambda w=w, gi=gi: nc.vector.tensor_scalar(out=w[:, 128 + gi * 8:136 + gi * 8], in0=w[:, 56:64],
                                                                       scalar1=w[:, 4 + gi:5 + gi], scalar2=None, op0=ALU.mult),
                     reads=[w], writes=[w])
            S.op("dve", lambda w=w, mi=mi: nc.vector.tensor_tensor(out=mi[:], in0=w[:, 96:128], in1=w[:, 128:160], op=ALU.add),
                 reads=[w], writes=[mi])
            a = C.acc[C.nA % 4]
            C.nA += 1
            S.op("pe", lambda a=a, mi=mi: nc.tensor.matmul(out=a[:, 0:32], lhsT=lst[:], rhs=mi[:], start=True, stop=False),
                 reads=[lst, mi], writes=[a])
            S.op("pe", lambda a=a: nc.tensor.matmul(out=a[:, 0:32], lhsT=onesb[:], rhs=Macc[:], start=False, stop=True),
                 reads=[onesb, Macc], writes=[a])
            S.op("dve", lambda mi=mi: nc.vector.tensor_tensor(out=Macc[:], in0=Macc[:], in1=mi[:], op=ALU.add), reads=[Macc, mi], writes=[Macc])
            S.op("dve", lambda a=a, w=w: nc.vector.tensor_tensor(out=w[:, 160:192], in0=a[:, 0:32], in1=ebase[:], op=ALU.add),
                 reads=[a, ebase], writes=[w])
            for (mc, oc) in ((96, 82), (128, 83)):
                S.op("dve", lambda w=w, mc=mc: nc.vector.tensor_tensor(out=w[:, 192:224], in0=w[:, 160:192], in1=w[:, mc:mc + 32], op=ALU.mult),
                     reads=[w], writes=[w])
                S.op("dve", lambda w=w, oc=oc: nc.vector.tensor_reduce(out=w[:, oc:oc + 1], in_=w[:, 192:224], axis=AX.X, op=ALU.add),
                     reads=[w], writes=[w])
            S.op("dve", lambda w=w, ii=ii: nc.vector.tensor_copy(out=ii[:], in_=w[:, 82:84]), reads=[w], writes=[ii])
            S.dma("sp", IDX_d[rows], ii[:], reads=[ii])
            S.dma("sp", GAB_d[rows], w[:, 80:82], reads=[w])


def emit_dispatch(C, x1_d, modrows, modsel, IDX_d, Xs_d, R, NS):
    S, nc = C.S, C.nc
    D = D_MODEL
    I32 = mybir.dt.int32
    xsb = Buf(None)
    with S.scope():
        zt = S.sb("zt", [128, 8, D], BF16)
        S.op("pool", lambda: nc.gpsimd.memset(zt[:], 0.0), writes=[zt])
        for r0 in range(0, NS, 1024):
            S.dma("sp", Xs_d[r0:r0 + 1024].rearrange("(a p) n -> p a n", p=128), zt[:], reads=[zt], writes=[xsb])
        modA = S.sb("modA", [128, 2, D], F32)
        modB = S.sb("modB", [128, 2, D], F32)
        for s_, (ra, rb_) in enumerate(modrows):
            S.dma("sp", modA[:, s_, :], ra.partition_broadcast(128), writes=[modA])
            S.dma("sp", modB[:, s_, :], rb_.partition_broadcast(128), writes=[modB])
        S.op("dve", lambda: nc.vector.tensor_scalar(out=modA[:], in0=modA[:], scalar1=1.0, scalar2=None, op0=ALU.add),
             reads=[modA], writes=[modA])
        xt = [S.sb("x%d" % i, [128, D], F32) for i in range(3)]
        st = [S.sb("st%d" % i, [128, 8, 6], F32) for i in range(2)]
        mv = [S.sb("mv%d" % i, [128, 4], F32) for i in range(2)]
        II = [S.sb("II%d" % i, [128, 2], I32) for i in range(3)]
        xbt = [S.sb("xb%d" % i, [128, D], BF16) for i in range(3)]
        for t in range(R // 128):
            rows = slice(t * 128, (t + 1) * 128)
            x, ii, xb = xt[t % 3], II[t % 3], xbt[t % 3]
            S.dma("sp", x[:], x1_d[rows], writes=[x])
            S.dma("sp", ii[:], IDX_d[rows], writes=[ii])
            m = ln_stats(S, x, D, "d", {"st": st[t % 2], "mv": mv[t % 2]})
            ms = modsel(t)
            S.op("dve", lambda x=x, m=m: nc.vector.tensor_scalar(out=x[:], in0=x[:], scalar1=m[:, 0:1], scalar2=m[:, 2:3],
                                                                 op0=ALU.subtract, op1=ALU.mult), reads=[x, m], writes=[x])
            S.op("dve", lambda x=x, ms=ms: nc.vector.tensor_tensor(out=x[:], in0=x[:], in1=modA[:, ms, :], op=ALU.mult),
                 reads=[x, modA], writes=[x])
            S.op("dve", lambda x=x, xb=xb, ms=ms: nc.vector.tensor_tensor(out=xb[:], in0=x[:], in1=modB[:, ms, :], op=ALU.add),
                 reads=[x, modB], writes=[xb])
            for j in range(2):
                S.idma(Xs_d, xb[:], ii[:, j:j + 1], True, reads=[xb, ii, xsb], writes=[])


def emit_experts(C, Xs_d, Ys_d, w1, w3, w2, CAP):
    S, nc = C.S, C.nc
    D, DE = D_MODEL, D_EXPERT
    NTall = CAP // 128
    NTe = min(8, NTall)
    with S.scope():
        hb = [S.sb("hb%d" % i, [128, D], BF16) for i in range(2)]
        hT = [S.sb("hT%d" % i, [128, 16, 128], BF16) for i in range(NTe)]
        hh = [S.sb("hh%d" % i, [128, DE], BF16) for i in range(NTe)]
        hT2 = [S.sb("hU%d" % i, [128, 8, 128], BF16) for i in range(NTe)]
        w1c = [S.sb("w1c%d" % i, [128, 16, 512], BF16) for i in range(2)]
        w3c = [S.sb("w3c%d" % i, [128, 16, 512], BF16) for i in range(2)]
        w2c = [S.sb("w2c%d" % i, [128, 8, 512], BF16) for i in range(2)]
        stl = [S.sb("stl%d" % i, [128, 512], F32) for i in range(3)]
        ot = [S.sb("ot%d" % i, [128, 512], BF16) for i in range(3)]
        n1 = n2 = n3 = 0
        for e, sub in [(e_, s_) for e_ in range(N_EXPERTS) for s_ in range(0, NTall, 8)]:
            r0 = e * CAP + sub * 128
            NTe = min(8, NTall - sub)
            for t in range(NTe):
                h = hb[n1 % 2]
                n1 += 1
                S.dma("sp", h[:], Xs_d[r0 + t * 128:r0 + (t + 1) * 128], writes=[h])
                for k0 in (0, 8):
                    p = C.pT[C.nT % 2]
                    C.nT += 1
                    for j in range(8):
                        S.op("pe", lambda p=p, h=h, j=j, k0=k0: nc.tensor.transpose(
                            out=p[:, j * 128:(j + 1) * 128], in_=h[:, (k0 + j) * 128:(k0 + j + 1) * 128], identity=C.identb[:]),
                            reads=[h, C.identb], writes=[p])
                    S.op("act", lambda p=p, t=t, k0=k0: nc.scalar.copy(
                        out=hT[t][:, k0:k0 + 8, :], in_=p[:, 0:1024].rearrange("p (a b) -> p a b", b=128)), reads=[p], writes=[hT[t]])
            for jc in range(2):
                wa, wb_ = w1c[n2 % 2], w3c[n2 % 2]
                n2 += 1
                S.dma("pool", wa[:], w1[e][:, jc * 512:(jc + 1) * 512].rearrange("(kc p) n -> p kc n", p=128), writes=[wa])
                S.dma("pool", wb_[:], w3[e][:, jc * 512:(jc + 1) * 512].rearrange("(kc p) n -> p kc n", p=128), writes=[wb_])
                for t in range(NTe):
                    a1 = C.acc[C.nA % 4]
                    a3 = C.acc[(C.nA + 1) % 4]
                    C.nA += 2
                    s_ = stl[n3 % 3]
                    n3 += 1
                    for kc in range(16):
                        S.op("pe", lambda a1=a1, wa=wa, t=t, kc=kc: nc.tensor.matmul(out=a1[:], lhsT=hT[t][:, kc, :], rhs=wa[:, kc, :],
                                                                                    start=(kc == 0), stop=(kc == 15)), reads=[hT[t], wa], writes=[a1])
                    for kc in range(16):
                        S.op("pe", lambda a3=a3, wb_=wb_, t=t, kc=kc: nc.tensor.matmul(out=a3[:], lhsT=hT[t][:, kc, :], rhs=wb_[:, kc, :],
                                                                                      start=(kc == 0), stop=(kc == 15)), reads=[hT[t], wb_], writes=[a3])
                    S.op("act", lambda a1=a1, s_=s_: nc.scalar.activation(out=s_[:], in_=a1[:], func=AF.Silu), reads=[a1], writes=[s_])
                    S.op("dve", lambda a3=a3, s_=s_, t=t, jc=jc: nc.vector.tensor_tensor(out=hh[t][:, jc * 512:(jc + 1) * 512], in0=a3[:],
                                                                                        in1=s_[:], op=ALU.mult), reads=[a3, s_], writes=[hh[t]])
            for t in range(NTe):
                p = C.pT[C.nT % 2]
                C.nT += 1
                for j in range(8):
                    S.op("pe", lambda p=p, t=t, j=j: nc.tensor.transpose(out=p[:, j * 128:(j + 1) * 128], in_=hh[t][:, j * 128:(j + 1) * 128],
                                                                         identity=C.identb[:]), reads=[hh[t], C.identb], writes=[p])
                S.op("act", lambda p=p, t=t: nc.scalar.copy(out=hT2[t][:], in_=p[:, 0:1024].rearrange("p (a b) -> p a b", b=128)),
                     reads=[p], writes=[hT2[t]])
            for n4 in range(4):
                wc = w2c[n2 % 2]
                n2 += 1
                S.dma("pool", wc[:], w2[e][:, n4 * 512:(n4 + 1) * 512].rearrange("(kc p) n -> p kc n", p=128), writes=[wc])
                for t in range(NTe):
                    a = C.acc[C.nA % 4]
                    C.nA += 1
                    o = ot[n3 % 3]
                    n3 += 1
                    for kc in range(8):
                        S.op("pe", lambda a=a, wc=wc, t=t, kc=kc: nc.tensor.matmul(out=a[:], lhsT=hT2[t][:, kc, :], rhs=wc[:, kc, :],
                                                                                  start=(kc == 0), stop=(kc == 7)), reads=[hT2[t], wc], writes=[a])
                    S.op("act", lambda a=a, o=o: nc.scalar.copy(out=o[:], in_=a[:]), reads=[a], writes=[o])
                    S.dma("sp", Ys_d[r0 + t * 128:r0 + (t + 1) * 128, n4 * 512:(n4 + 1) * 512], o[:], reads=[o])


def emit_combine(C, Ys_d, IDX_d, GAB_d, f_d, R):
    S, nc = C.S, C.nc
    D = D_MODEL
    I32 = mybir.dt.int32
    with S.scope():
        ya = [S.sb("ya%d" % i, [128, D], BF16) for i in range(2)]
        yb = [S.sb("yb%d" % i, [128, D], BF16) for i in range(2)]
        fo = [S.sb("fo%d" % i, [128, D], F32) for i in range(2)]
        II = [S.sb("II%d" % i, [128, 2], I32) for i in range(2)]
        GG = [S.sb("GG%d" % i, [128, 2], F32) for i in range(2)]
        for t in range(R // 128):
            rows = slice(t * 128, (t + 1) * 128)
            a, b, ii, gg = ya[t % 2], yb[t % 2], II[t % 2], GG[t % 2]
            S.dma("sp", ii[:], IDX_d[rows], writes=[ii])
            S.dma("sp", gg[:], GAB_d[rows], writes=[gg])
            S.idma(a[:], Ys_d, ii[:, 0:1], False, reads=[ii], writes=[a])
            S.idma(b[:], Ys_d, ii[:, 1:2], False, reads=[ii], writes=[b])
            f = fo[t % 2]
            S.op("dve", lambda a=a, f=f, gg=gg: nc.vector.tensor_scalar(out=f[:], in0=a[:], scalar1=gg[:, 0:1], scalar2=None, op0=ALU.mult),
                 reads=[a, gg], writes=[f])
            S.op("dve", lambda f=f, b=b, gg=gg: nc.vector.scalar_tensor_tensor(out=f[:], in0=b[:], scalar=gg[:, 1:2], in1=f[:],
                                                                               op0=ALU.mult, op1=ALU.add), reads=[f, b, gg], writes=[f])
            S.dma("sp", f_d[rows], f[:], reads=[f])


WNAMES = ["w_ada", "b_ada", "w_in", "b_in", "a_ln_g", "a_ln_b", "a_w_s", "a_b_s", "b_w_dw", "b_b_dw", "b_ln_g", "b_ln_b",
          "d_q_g", "d_w_uq", "d_kv_g", "w_branch", "w_o", "ln1_g", "ln1_b", "e_w1", "e_w3", "e_w2", "ln2_g", "ln2_b"]


def build_program(shapes, RL, NCTX):
    R = RL + NCTX
    D = D_MODEL
    nc, es = new_prog()
    I = {}
    for k, shp in shapes.items():
        I[k] = dram_in(nc, k, shp)
    y_out = dram_out(nc, "y", [RL, D])
    T = lambda n, s: dram_tmp(nc, n, s)
    mod_d = [T("mod%d" % l, [128, 6 * D]) for l in range(DEPTH)]
    z_d = T("z", [R, COL_G])
    zg_d = [T("zg%d" % i, [R, D]) for i in range(4)]
    brA, brB = T("brA", [R, 512]), T("brB", [R, 512])
    PQ = T("PQ", [R, 1024])
    brC = [T("brC%d" % p, [R, 512]) for p in range(4)]
    q_d, kv_d, kr_d = T("q", [R, 768]), T("kv", [R, 1024]), T("kr", [R, 32])
    o_d = T("o", [R, MLA_HEADS, V_HEAD + 1])
    GP = [T("GP%d" % i, [R, D]) for i in range(4)]
    mix_d, x1_d, xb_d = T("mix", [R, D]), T("x1", [R, D]), T("xb", [R, D])
    lg_d, G_d = T("lg", [R, 36]), T("G", [R, 32])
    CAPMAX = 128 * int(math.ceil(3.6 * 2.0 * R / N_EXPERTS / 128))
    Xs_d = dram_tmp(nc, "Xs", [N_EXPERTS * CAPMAX, D], BF16)
    Ys_d = dram_tmp(nc, "Ys", [N_EXPERTS * CAPMAX, D], BF16)
    IDX_d = nc.dram_tensor("IDX", [R, 2], mybir.dt.int32).ap()
    GAB_d = T("GAB", [R, 2])
    f_d = [T("f0", [R, D])]
    NTL = RL // 128
    msel = lambda t: 0 if t < NTL else 1
    with es:
        C = Ctx(nc, es, I["ident"])
        S = C.S
        c512 = [(i * 512, 512, None) for i in range(4)]
        for l in range(DEPTH):
            emit_linear(C, [I["cc"]], lambda ci, l=l: I["w_ada"][l][:, ci * 512:(ci + 1) * 512], mod_d[l], 128, D,
                        [(i * 512, 512, None) for i in range(24)], prologue="silu", bias_d=I["b_ada"][l])
        x_cur = I["x_all"]
        for l in range(DEPTH):
            last = l == DEPTH - 1
            Rm = RL if last else R
            mr = lambda who, i, l=l: mod_d[l][who:who + 1, i * D:(i + 1) * D]
            chunks_in = []
            for c0 in range(0, COL_KV, 512):
                chunks_in.append((c0, 512, AF.Sigmoid if c0 == 1536 else None))
            chunks_in.append((COL_KV, COL_G - COL_KV, None))
            modr1 = [(mr(0, 1), mr(0, 0)), (mr(1, 1), mr(1, 0))]
            emit_linear(C, [x_cur], lambda ci, l=l, ch=chunks_in: I["w_in"][l][:, ch[ci][0]:ch[ci][0] + ch[ci][1]], z_d, R, D,
                        chunks_in, prologue="ln0mod", modrows=modr1, modsel=msel, bias_d=I["b_in"][l])
            for i in range(4):
                g0 = COL_G + i * D
                emit_linear(C, [x_cur], lambda ci, l=l, g0=g0: I["w_in"][l][:, g0 + ci * 512:g0 + (ci + 1) * 512], zg_d[i], Rm, D,
                            [(j * 512, 512, AF.Sigmoid) for j in range(4)], prologue="ln0mod", modrows=modr1, modsel=msel,
                            bias_d=I["b_in"][l][g0:g0 + D])
            emit_chunkgate(C, z_d, I["a_ln_g"][l], I["a_ln_b"][l], I["a_w_s"][l], I["a_b_s"][l], brA, Rm)
            emit_conv(C, z_d, I["b_w_dw"][l], I["b_b_dw"][l], I["b_ln_g"][l], I["b_ln_b"][l], brB,
                      [(0, RL)] + ([] if last else [(RL, NCTX)]))
            emit_linear(C, [z_d[:, COL_C:COL_D]], lambda ci: I["cs128"][:, ci * 512:(ci + 1) * 512], PQ, R, 512,
                        [(0, 512, None), (512, 512, None)])
            K4 = RL // 2
            for p in range(4):
                wsl = PQ[(p % 2) * K4:(p % 2 + 1) * K4, (p // 2) * 512:(p // 2 + 1) * 512]
                emit_linear(C, [I["dftlat"][:, p * K4:(p + 1) * K4]], lambda ci, wsl=wsl: wsl, brC[p], RL, K4, [(0, 512, None)], RB=8)
                if not last:
                    Kc = NCTX // 2
                    wsl2 = PQ[RL + (p % 2) * Kc:RL + (p % 2 + 1) * Kc, (p // 2) * 512:(p // 2 + 1) * 512]
                    emit_linear(C, [I["dftctx"][:, p * Kc:(p + 1) * Kc]], lambda ci, wsl2=wsl2: wsl2, brC[p][RL:R], NCTX, Kc,
                                [(0, 512, None)])
            emit_linear(C, [z_d[:, COL_D:COL_KV]], lambda ci, l=l: I["d_w_uq"][l][:, ci * 512:ci * 512 + (512 if ci == 0 else 256)],
                        q_d, R, 512, [(0, 512, None), (512, 256, None)], prologue="rms", g_d=I["d_q_g"][l])
            emit_linear(C, [z_d[:, COL_KV:COL_KR]], lambda ci, l=l: I["w_ukv"][l][:, ci * 512:(ci + 1) * 512],
                        kv_d, R, 256, [(0, 512, None), (512, 512, None)], prologue="rms", g_d=I["d_kv_g"][l])
            emit_rope(C, q_d, QK_NOPE, MLA_HEADS, QK_NOPE + QK_ROPE, q_d, QK_NOPE, I["rcos"], I["rsin"], RL)
            emit_rope(C, z_d, COL_KR, 1, 32, kr_d, 0, I["rcos"], I["rsin"], RL)
            with S.scope():
                tmpk = S.sb("tmpk", [128, NCTX // 128, 32], F32)
                S.dma("sp", tmpk[:], z_d[RL:R, COL_KR:COL_G].rearrange("(a p) n -> p a n", p=128), writes=[tmpk])
                S.dma("sp", kr_d[RL:R, :].rearrange("(a p) n -> p a n", p=128), tmpk[:], reads=[tmpk])
            emit_attention(C, q_d, kv_d, kr_d, o_d, (0, RL), [(0, R)])
            if not last:
                emit_attention(C, q_d, kv_d, kr_d, o_d, (RL, NCTX), [(RL, NCTX)])
            for i in range(4):
                xs = [[brA], [brB], brC, [o_d]][i]
                emit_linear(C, xs, lambda ci, l=l, i=i: I["w_branch"][l][i][:, ci * 512:(ci + 1) * 512], GP[i], Rm, 512, c512,
                            prologue=("attn" if i == 3 else "none"), mul_d=zg_d[i])
            emit_linear(C, GP, lambda ci, l=l: I["w_o"][l][:, ci * 512:(ci + 1) * 512], mix_d, Rm, D, c512)
            emit_resln(C, x_cur, [mix_d], [mr(0, 2), mr(1, 2)], msel, I["ln1_g"][l], I["ln1_b"][l], x1_d, Rm)
            modr2 = [(mr(0, 4), mr(0, 3)), (mr(1, 4), mr(1, 3))]
            emit_linear(C, [x1_d], lambda ci, l=l: I["r_w"][l], lg_d, Rm, D, [(0, 36, None)], prologue="ln0mod",
                        modrows=modr2, modsel=msel, bias_d=I["r_b"][l])
            CAP = 128 * int(math.ceil(3.6 * 2.0 * Rm / N_EXPERTS / 128))
            emit_route(C, lg_d, IDX_d, GAB_d, I["lstrict"], Rm, CAP)
            emit_dispatch(C, x1_d, modr2, msel, IDX_d, Xs_d, Rm, N_EXPERTS * CAP)
            emit_experts(C, Xs_d, Ys_d, I["e_w1"][l], I["e_w3"][l], I["e_w2"][l], CAP)
            emit_combine(C, Ys_d, IDX_d, GAB_d, f_d[0], Rm)
            dst = y_out if last else xb_d
            emit_resln(C, x1_d, [f_d[0]], [mr(0, 5), mr(1, 5)], msel, I["ln2_g"][l], I["ln2_b"][l], dst, Rm)
            x_cur = xb_d
        S.finish()
        print("program instructions:", S.n, S.cnt, S.dcnt)
    return nc


def host_consts(RL, NCTX):
    rows = RL // GRID_W
    row = np.repeat(np.arange(rows, dtype=np.float32), GRID_W)
    col = np.tile(np.arange(GRID_W, dtype=np.float32), rows)
    half = QK_ROPE // 2
    inv = (ROPE_THETA ** (-np.arange(0, half, 2, dtype=np.float32) / half)).astype(np.float32)
    ar = row[:, None] * inv
    ac = col[:, None] * inv
    ang = np.concatenate([ar, ar, ac, ac], -1)
    rcos = np.cos(ang).astype(np.float32)
    rsin = np.concatenate([-np.sin(ar), np.sin(ar), -np.sin(ac), np.sin(ac)], -1).astype(np.float32)
    cw = BRANCH_W // 4
    k = np.arange(cw)
    a = 2 * np.pi * np.outer(k, k) / cw
    cs = np.zeros((512, 1024), np.float32)
    for g in range(4):
        cs[g * cw:(g + 1) * cw, g * cw:(g + 1) * cw] = np.cos(a)
        cs[g * cw:(g + 1) * cw, 512 + g * cw:512 + (g + 1) * cw] = np.sin(a)

    def dft(n):
        kk = np.arange(n, dtype=np.int64)
        ph = (np.outer(kk, kk) % n).astype(np.float64) * (2 * np.pi / n)
        sc = 1.0 / math.sqrt(n * cw)
        return np.concatenate([np.cos(ph) * sc, -np.sin(ph) * sc], 1).astype(np.float32)

    return {"rcos": rcos, "rsin": rsin, "cs128": cs, "dftlat": dft(RL), "dftctx": dft(NCTX),
            "ident": np.eye(128, dtype=np.float32),
            "lstrict": np.triu(np.ones((128, 128), np.float32), 1)}


def kernel_fused(**inp):
    x = np.asarray(inp["x"], np.float32)
    RL = x.shape[1]
    NCTX = inp["ctx"].shape[1]
    ins = {}
    ins["x_all"] = np.concatenate([x[0], np.asarray(inp["ctx"], np.float32)[0]], 0)
    cc = np.zeros((128, D_MODEL), np.float32)
    cc[0] = np.asarray(inp["c"])[0]
    cc[1] = np.asarray(inp["c_ctx"])
    ins["cc"] = cc
    for k in WNAMES:
        ins[k] = np.ascontiguousarray(np.asarray(inp[k], np.float32))
    ins["w_ukv"] = np.concatenate([np.asarray(inp["d_w_uk"]), np.asarray(inp["d_w_uv"])], -1).astype(np.float32)
    ins["r_w"] = np.concatenate([np.asarray(inp["r_w_grp"]), np.asarray(inp["r_w_exp"])], -1).astype(np.float32)
    ins["r_b"] = np.concatenate([np.asarray(inp["r_b_grp"]), np.asarray(inp["r_b_exp"])], -1).astype(np.float32)
    ins.update(host_consts(RL, NCTX))
    shapes = {k: list(v.shape) for k, v in ins.items()}
    key = ("main", RL, NCTX)
    if key not in _PROG_CACHE:
        _PROG_CACHE[key] = build_program(shapes, RL, NCTX)
    res = run_bass_kernel_spmd(_PROG_CACHE[key], [ins], core_ids=[0])
    return np.asarray(res.results[0]["y"], np.float32)[None]


NLAT_ALL = SEQ
RC = TL + CTX_LEN


def _chunks(spec):
    return [(c0, w, a) for (c0, w, a) in spec]


def build_P1(shapes):
    RL, NCTX, R, D = TL, CTX_LEN, RC, D_MODEL
    nc, es = new_prog()
    I = {k: dram_in(nc, k, shp) for k, shp in shapes.items()}
    PQ_o, kv_o, kr_o = dram_out(nc, "PQ_o", [R, 1024]), dram_out(nc, "kv_o", [R, 1024]), dram_out(nc, "kr_o", [R, 32])
    zab_o = dram_out(nc, "zab_o", [256, 1024])
    mod_d = dram_tmp(nc, "mod", [128, 6 * D])
    z_d = dram_tmp(nc, "z", [R, COL_G])
    msel = lambda t: 0 if t < RL // 128 else 1
    with es:
        C = Ctx(nc, es, I["ident"])
        S = C.S
        emit_linear(C, [I["cc"]], lambda ci: I["w_ada"][:, ci * 512:(ci + 1) * 512], mod_d, 128, D,
                    [(i * 512, 512, None) for i in range(24)], prologue="silu", bias_d=I["b_ada"])
        mr = lambda who, i: mod_d[who:who + 1, i * D:(i + 1) * D]
        ch = [(1024, 512, None), (1536, 512, AF.Sigmoid), (2048, 512, None), (COL_KV, COL_G - COL_KV, None)]
        emit_linear(C, [I["x_own"]], lambda ci: I["w_in"][:, ch[ci][0]:ch[ci][0] + ch[ci][1]], z_d, R, D, ch, prologue="ln0mod",
                    modrows=[(mr(0, 1), mr(0, 0)), (mr(1, 1), mr(1, 0))], modsel=msel, bias_d=I["b_in"])
        emit_linear(C, [z_d[:, COL_C:COL_D]], lambda ci: I["cs128"][:, ci * 512:(ci + 1) * 512], PQ_o, R, 512,
                    [(0, 512, None), (512, 512, None)])
        emit_linear(C, [z_d[:, COL_KV:COL_KR]], lambda ci: I["w_ukv"][:, ci * 512:(ci + 1) * 512], kv_o, R, 256,
                    [(0, 512, None), (512, 512, None)], prologue="rms", g_d=I["d_kv_g"])
        emit_rope(C, z_d, COL_KR, 1, 32, kr_o, 0, I["rcos"], I["rsin"], RL)
        with S.scope():
            tmpk = S.sb("tmpk", [128, NCTX // 128, 32], F32)
            S.dma("sp", tmpk[:], z_d[RL:R, COL_KR:COL_G].rearrange("(a p) n -> p a n", p=128), writes=[tmpk])
            S.dma("sp", kr_o[RL:R, :].rearrange("(a p) n -> p a n", p=128), tmpk[:], reads=[tmpk])
            tz = S.sb("tz", [128, 2, 1024], F32)
            S.dma("sp", tz[:, 0, :], z_d[0:128, 1024:2048], writes=[tz])
            S.dma("sp", tz[:, 1, :], z_d[RL - 128:RL, 1024:2048], writes=[tz])
            S.dma("sp", zab_o.rearrange("(a p) n -> p a n", p=128), tz[:], reads=[tz])
        S.finish()
        print("P1 instructions:", S.n)
    return nc


def build_P2(shapes):
    RL, NCTX, R, D = TL, CTX_LEN, RC, D_MODEL
    nc, es = new_prog()
    I = {k: dram_in(nc, k, shp) for k, shp in shapes.items()}
    xn_o = dram_out(nc, "xn", [R, D])
    T = lambda n, s_: dram_tmp(nc, n, s_)
    mod_d = T("mod", [128, 6 * D])
    z_d = T("z", [R, COL_G])
    zg_d = [T("zg%d" % i, [R, D]) for i in range(4)]
    brA, brB = T("brA", [R, 512]), T("brB", [R, 512])
    brC = [T("brC%d" % p, [R, 512]) for p in range(4)]
    q_d = T("q", [R, 768])
    o_d = T("o", [R, MLA_HEADS, V_HEAD + 1])
    GP = [T("GP%d" % i, [R, D]) for i in range(4)]
    mix_d, x1_d = T("mix", [R, D]), T("x1", [R, D])
    lg_d = T("lg", [R, 36])
    CAP = 128 * int(math.ceil(3.6 * 2.0 * R / N_EXPERTS / 128))
    Xs_d = dram_tmp(nc, "Xs", [N_EXPERTS * CAP, D], BF16)
    Ys_d = dram_tmp(nc, "Ys", [N_EXPERTS * CAP, D], BF16)
    IDX_d = nc.dram_tensor("IDX", [R, 2], mybir.dt.int32).ap()
    GAB_d = T("GAB", [R, 2])
    f_d = T("f0", [R, D])
    msel = lambda t: 0 if t < RL // 128 else 1
    with es:
        C = Ctx(nc, es, I["ident"])
        S = C.S
        c512 = [(i * 512, 512, None) for i in range(4)]
        emit_linear(C, [I["cc"]], lambda ci: I["w_ada"][:, ci * 512:(ci + 1) * 512], mod_d, 128, D,
                    [(i * 512, 512, None) for i in range(24)], prologue="silu", bias_d=I["b_ada"])
        mr = lambda who, i: mod_d[who:who + 1, i * D:(i + 1) * D]
        x_cur = I["x_own"]
        ch = [(0, 512, None), (512, 512, None), (1024, 512, None), (1536, 512, AF.Sigmoid), (COL_D, 512, None)]
        modr1 = [(mr(0, 1), mr(0, 0)), (mr(1, 1), mr(1, 0))]
        emit_linear(C, [x_cur], lambda ci: I["w_in"][:, ch[ci][0]:ch[ci][0] + ch[ci][1]], z_d, R, D, ch, prologue="ln0mod",
                    modrows=modr1, modsel=msel, bias_d=I["b_in"])
        for i in range(4):
            g0 = COL_G + i * D
            emit_linear(C, [x_cur], lambda ci, g0=g0: I["w_in"][:, g0 + ci * 512:g0 + (ci + 1) * 512], zg_d[i], R, D,
                        [(j * 512, 512, AF.Sigmoid) for j in range(4)], prologue="ln0mod", modrows=modr1, modsel=msel,
                        bias_d=I["b_in"][g0:g0 + D])
        emit_chunkgate(C, z_d, I["a_ln_g"], I["a_ln_b"], I["a_w_s"], I["a_b_s"], brA, R)
        emit_conv(C, z_d, I["b_w_dw"], I["b_b_dw"], I["b_ln_g"], I["b_ln_b"], brB, [(0, RL), (RL, NCTX)], halo=I["halo"])
        PQ = I["PQ_all"]
        K4 = NLAT_ALL // 2
        for p in range(4):
            wsl = PQ[(p % 2) * K4:(p % 2 + 1) * K4, (p // 2) * 512:(p // 2 + 1) * 512]
            emit_linear(C, [I["dft_own"][:, p * K4:(p + 1) * K4]], lambda ci, wsl=wsl: wsl, brC[p], RL, K4, [(0, 512, None)], RB=8)
            Kc = NCTX // 2
            wsl2 = PQ[NLAT_ALL + (p % 2) * Kc:NLAT_ALL + (p % 2 + 1) * Kc, (p // 2) * 512:(p // 2 + 1) * 512]
            emit_linear(C, [I["dftctx"][:, p * Kc:(p + 1) * Kc]], lambda ci, wsl2=wsl2: wsl2, brC[p][RL:R], NCTX, Kc, [(0, 512, None)])
        emit_linear(C, [z_d[:, COL_D:COL_KV]], lambda ci: I["d_w_uq"][:, ci * 512:ci * 512 + (512 if ci == 0 else 256)],
                    q_d, R, 512, [(0, 512, None), (512, 256, None)], prologue="rms", g_d=I["d_q_g"])
        emit_rope(C, q_d, QK_NOPE, MLA_HEADS, QK_NOPE + QK_ROPE, q_d, QK_NOPE, I["rcos"], I["rsin"], RL)
        RA = NLAT_ALL + NCTX
        emit_attention(C, q_d, I["kv_all"], I["kr_all"], o_d, (0, RL), [(0, RA)])
        emit_attention(C, q_d, I["kv_all"], I["kr_all"], o_d, (RL, NCTX), [(NLAT_ALL, NCTX)])
        for i in range(4):
            xs = [[brA], [brB], brC, [o_d]][i]
            emit_linear(C, xs, lambda ci, i=i: I["w_branch"][i][:, ci * 512:(ci + 1) * 512], GP[i], R, 512, c512,
                        prologue=("attn" if i == 3 else "none"), mul_d=zg_d[i])
        emit_linear(C, GP, lambda ci: I["w_o"][:, ci * 512:(ci + 1) * 512], mix_d, R, D, c512)
        emit_resln(C, x_cur, [mix_d], [mr(0, 2), mr(1, 2)], msel, I["ln1_g"], I["ln1_b"], x1_d, R)
        modr2 = [(mr(0, 4), mr(0, 3)), (mr(1, 4), mr(1, 3))]
        emit_linear(C, [x1_d], lambda ci: I["r_w"], lg_d, R, D, [(0, 36, None)], prologue="ln0mod",
                    modrows=modr2, modsel=msel, bias_d=I["r_b"])
        emit_route(C, lg_d, IDX_d, GAB_d, I["lstrict"], R, CAP)
        emit_dispatch(C, x1_d, modr2, msel, IDX_d, Xs_d, R, N_EXPERTS * CAP)
        emit_experts(C, Xs_d, Ys_d, I["e_w1"], I["e_w3"], I["e_w2"], CAP)
        emit_combine(C, Ys_d, IDX_d, GAB_d, f_d, R)
        emit_resln(C, x1_d, [f_d], [mr(0, 5), mr(1, 5)], msel, I["ln2_g"], I["ln2_b"], xn_o, R)
        S.finish()
        print("P2 instructions:", S.n, S.cnt)
    return nc


def _run(key, builder, in_maps):
    shapes = {k: list(v.shape) for k, v in in_maps[0].items()}
    if key not in _PROG_CACHE:
        _PROG_CACHE[key] = builder(shapes)
    res = run_bass_kernel_spmd(_PROG_CACHE[key], in_maps, core_ids=list(range(NCORES)))
    return res.results


def kernel(**inp):
    f32 = lambda a: np.ascontiguousarray(np.asarray(a, np.float32))
    x = f32(inp["x"])[0]
    ctx = f32(inp["ctx"])[0]
    cc = np.zeros((128, D_MODEL), np.float32)
    cc[0] = f32(inp["c"])[0]
    cc[1] = f32(inp["c_ctx"])
    cst = host_consts(SEQ, CTX_LEN)
    w_ukv = np.concatenate([f32(inp["d_w_uk"]), f32(inp["d_w_uv"])], -1)
    r_w = np.concatenate([f32(inp["r_w_grp"]), f32(inp["r_w_exp"])], -1)
    r_b = np.concatenate([f32(inp["r_b_grp"]), f32(inp["r_b_exp"])], -1)
    xo = [np.concatenate([x[j * TL:(j + 1) * TL], ctx], 0) for j in range(NCORES)]
    zero_t = np.zeros((128, 1024), np.float32)
    for l in range(DEPTH):
        W = lambda k: f32(inp[k][l])
        common1 = {"cc": cc, "w_ada": W("w_ada"), "b_ada": W("b_ada"), "w_in": W("w_in"), "b_in": W("b_in"),
                   "d_kv_g": W("d_kv_g"), "w_ukv": np.ascontiguousarray(w_ukv[l]), "cs128": cst["cs128"], "ident": cst["ident"]}
        ins1 = []
        for j in range(NCORES):
            d = dict(common1)
            d["x_own"] = xo[j]
            d["rcos"] = np.ascontiguousarray(cst["rcos"][j * TL:(j + 1) * TL])
            d["rsin"] = np.ascontiguousarray(cst["rsin"][j * TL:(j + 1) * TL])
            ins1.append(d)
        r1 = _run("P1", build_P1, ins1)
        cat = lambda k: np.concatenate([np.asarray(r1[j][k])[:TL] for j in range(NCORES)] + [np.asarray(r1[0][k])[TL:]], 0)
        PQ_all, kv_all, kr_all = cat("PQ_o"), cat("kv_o"), cat("kr_o")
        common2 = {"cc": cc, "PQ_all": PQ_all, "kv_all": kv_all, "kr_all": kr_all, "dftctx": cst["dftctx"],
                   "lstrict": cst["lstrict"], "ident": cst["ident"],
                   "w_ukv_unused": None}
        del common2["w_ukv_unused"]
        for k in ["w_ada", "b_ada", "w_in", "b_in", "a_ln_g", "a_ln_b", "a_w_s", "a_b_s", "b_w_dw", "b_b_dw", "b_ln_g", "b_ln_b",
                  "d_q_g", "d_w_uq", "w_branch", "w_o", "ln1_g", "ln1_b", "e_w1", "e_w3", "e_w2", "ln2_g", "ln2_b"]:
            common2[k] = W(k)
        common2["r_w"] = np.ascontiguousarray(r_w[l])
        common2["r_b"] = np.ascontiguousarray(r_b[l])
        ins2 = []
        for j in range(NCORES):
            d = dict(common2)
            d["x_own"] = xo[j]
            prev = np.asarray(r1[j - 1]["zab_o"])[128:256] if j > 0 else zero_t
            nxt = np.asarray(r1[j + 1]["zab_o"])[0:128] if j < NCORES - 1 else zero_t
            d["halo"] = np.ascontiguousarray(np.concatenate([prev, nxt], 0))
            d["dft_own"] = np.ascontiguousarray(cst["dftlat"][j * TL:(j + 1) * TL])
            d["rcos"] = np.ascontiguousarray(cst["rcos"][j * TL:(j + 1) * TL])
            d["rsin"] = np.ascontiguousarray(cst["rsin"][j * TL:(j + 1) * TL])
            ins2.append(d)
        r2 = _run("P2", build_P2, ins2)
        xo = [np.ascontiguousarray(np.asarray(r2[j]["xn"], np.float32)) for j in range(NCORES)]
    return np.concatenate([xo[j][:TL] for j in range(NCORES)], 0)[None].astype(np.float32)
```

```python
import contextlib
import math
import numpy as np
import ml_dtypes
import concourse.bass as bass
import concourse.mybir as mybir
from concourse.bass_utils import run_bass_kernel_spmd

F32 = mybir.dt.float32
BF16 = mybir.dt.bfloat16
AF = mybir.ActivationFunctionType
ALU = mybir.AluOpType
AX = mybir.AxisListType
NCORES = 8

D_MODEL = 2048
SEQ = 8192
DEPTH = 2
GRID_W = 64
CTX_LEN = 256
BRANCH_W = 512
CHUNK = 128
CONV_W = 31
QK_NOPE = 64
QK_ROPE = 32
V_HEAD = 64
MLA_HEADS = 8
Q_LORA = 512
KV_LORA = 256
ROPE_THETA = 10000.0
MLA_SCALE = (QK_NOPE + QK_ROPE) ** -0.5
N_GROUPS = 4
EXP_PER_GROUP = 8
N_EXPERTS = 32
D_EXPERT = 1024
COL_A = 0
COL_B = 1024
COL_C = 2048
COL_D = 2560
COL_KV = 3072
COL_KR = 3328
COL_G = 3360
D_IN = COL_G + 4 * D_MODEL
ALPHA = (2 * DEPTH) ** 0.25
LN_EPS = 1e-6
TL = SEQ // NCORES


class Buf:
    __slots__ = ("t", "lw", "rd", "name")

    def __init__(self, t, name=""):
        self.t = t
        self.lw = None
        self.rd = {}
        self.name = name

    def __getitem__(self, k):
        return self.t[k]


class Sched:
    RING = 12

    def __init__(self, nc, es):
        self.nc = nc
        self.es = es
        self.engs = {"pe": nc.tensor, "act": nc.scalar, "dve": nc.vector, "pool": nc.gpsimd, "sp": nc.sync}
        self.sem = {e: es.enter_context(nc.semaphore("s_" + e)) for e in ("pe", "act", "dve", "pool")}
        self.cnt = {e: 0 for e in self.sem}
        self.dsem = {q: [es.enter_context(nc.semaphore("d_%s%d" % (q, i))) for i in range(self.RING)]
                     for q in ("sp", "pool")}
        self.dcnt = {"sp": 0, "pool": 0}
        self.waited = {e: {} for e in self.engs}
        self.n = 0

    def sb(self, name, shape, dt):
        return Buf(self.es.enter_context(self.nc.sbuf_tensor("sb%d_" % getattr(self, "uid", 0) + name, list(shape), dt)), name)

    def ps(self, name, shape, dt=F32):
        return Buf(self.es.enter_context(self.nc.psum_tensor("ps_" + name, list(shape), dt)), name)

    def _semval(self, tok):
        if tok[0] == "c":
            return ("c", tok[1]), self.sem[tok[1]], tok[2]
        q, idx = tok[1], tok[2]
        return ("d", q, idx % self.RING), self.dsem[q][idx % self.RING], 16 * (idx // self.RING + 1)

    def _wait(self, eng, toks):
        need = {}
        for tok in toks:
            if tok is None:
                continue
            if tok[0] == "c" and tok[1] == eng and eng == "pe":
                continue
            key, sem, val = self._semval(tok)
            if need.get(key, (None, 0))[1] < val:
                need[key] = (sem, val)
        w = self.waited[eng]
        for key, (sem, val) in need.items():
            if w.get(key, 0) >= val:
                continue
            self.engs[eng].wait_ge(sem, val)
            w[key] = val

    def _deps(self, reads, writes):
        toks = []
        for r in reads:
            toks.append(r.lw)
        for wv in writes:
            toks.append(wv.lw)
            toks.extend(wv.rd.values())
        return toks

    def _commit(self, tok, reads, writes, rkey):
        for r in reads:
            r.rd[rkey] = tok
        for wv in writes:
            wv.lw = tok
            wv.rd = {}

    def op(self, eng, fn, reads=(), writes=()):
        self._wait(eng, self._deps(reads, writes))
        ins = fn()
        self.cnt[eng] += 1
        ins.then_inc(self.sem[eng], 1)
        tok = ("c", eng, self.cnt[eng])
        self._commit(tok, reads, writes, eng)
        self.n += 1
        return tok

    def dma(self, q, out, in_, reads=(), writes=(), **kw):
        idx = self.dcnt[q]
        toks = self._deps(reads, writes)
        if idx >= self.RING:
            toks.append(("d", q, idx - self.RING))
        self._wait(q, toks)
        ins = self.engs[q].dma_start(out=out, in_=in_, **kw)
        ins.then_inc(self.dsem[q][idx % self.RING], 16)
        self.dcnt[q] += 1
        tok = ("d", q, idx)
        self._commit(tok, reads, writes, "dma_" + q + str(idx % self.RING))
        self.n += 1
        return tok

    def idma(self, out, in_, idx_ap, scatter, reads=(), writes=()):
        q = "pool"
        idx = self.dcnt[q]
        toks = self._deps(reads, writes)
        if idx >= self.RING:
            toks.append(("d", q, idx - self.RING))
        self._wait(q, toks)
        off = bass.IndirectOffsetOnAxis(ap=idx_ap, axis=0)
        if scatter:
            ins = self.nc.gpsimd.indirect_dma_start(out=out, out_offset=off, in_=in_, in_offset=None)
        else:
            ins = self.nc.gpsimd.indirect_dma_start(out=out, out_offset=None, in_=in_, in_offset=off)
        ins.then_inc(self.dsem[q][idx % self.RING], 16)
        self.dcnt[q] += 1
        tok = ("d", q, idx)
        self._commit(tok, reads, writes, "dma_" + q + str(idx % self.RING))
        self.n += 1
        return tok

    def barrier(self):
        toks = []
        for q in ("sp", "pool"):
            for i in range(max(0, self.dcnt[q] - self.RING), self.dcnt[q]):
                toks.append(("d", q, i))
        for e in self.cnt:
            if self.cnt[e]:
                toks.append(("c", e, self.cnt[e]))
        for e in self.engs:
            self._wait(e, [t for t in toks if not (t[0] == "c" and t[1] == e)])

    @contextlib.contextmanager
    def scope(self):
        old = self.es
        with contextlib.ExitStack() as es2:
            self.es = es2
            self.uid = getattr(self, "uid", 0) + 1
            yield
            self.barrier()
        self.es = old

    def finish(self):
        toks = []
        for q in ("sp", "pool"):
            for i in range(max(0, self.dcnt[q] - self.RING), self.dcnt[q]):
                toks.append(("d", q, i))
        for e in self.cnt:
            if self.cnt[e]:
                toks.append(("c", e, self.cnt[e]))
        self._wait("sp", toks)


def new_prog():
    nc = bass.Bass("TRN2", target_bir_lowering=False)
    es = contextlib.ExitStack()
    return nc, es


def dram_in(nc, name, shape, dt=F32):
    return nc.dram_tensor(name, list(shape), dt, kind="ExternalInput").ap()


def dram_out(nc, name, shape, dt=F32):
    return nc.dram_tensor(name, list(shape), dt, kind="ExternalOutput").ap()


_PROG_CACHE = {}


def dram_tmp(nc, name, shape, dt=F32):
    return nc.dram_tensor(name, list(shape), dt).ap()


def rstd_of(S, m):
    nc = S.nc
    S.op("act", lambda: nc.scalar.activation(out=m[:, 3:4], in_=m[:, 3:4], func=AF.Sqrt), reads=[m], writes=[m])
    S.op("dve", lambda: nc.vector.reciprocal(out=m[:, 2:3], in_=m[:, 3:4]), reads=[m], writes=[m])


def ln_stats(S, x, K, tag, ring):
    nc = S.nc
    nch = (K + 511) // 512
    st, mv = ring["st"], ring["mv"]
    for c in range(nch):
        w = min(512, K - c * 512)
        S.op("dve", lambda c=c, w=w: nc.vector.bn_stats(out=st[:, c, :], in_=x[:, c * 512:c * 512 + w]),
             reads=[x], writes=[st])
    S.op("dve", lambda: nc.vector.bn_aggr(out=mv[:, 0:2], in_=st[:, 0:nch, :]), reads=[st], writes=[mv])
    S.op("dve", lambda: nc.vector.tensor_scalar(out=mv[:, 3:4], in0=mv[:, 1:2], scalar1=LN_EPS, scalar2=None,
                                                op0=ALU.add), reads=[mv], writes=[mv])
    rstd_of(S, mv)
    return mv


class Ctx:
    def __init__(self, nc, es, ident_d):
        self.nc = nc
        self.S = S = Sched(nc, es)
        self.pT = [S.ps("pT%d" % i, [128, 1024], BF16) for i in range(2)]
        self.acc = [S.ps("acc%d" % i, [128, 512], F32) for i in range(4)]
        self.pB = [S.ps("pB%d" % i, [128, 512], F32) for i in range(2)]
        self.identb = S.sb("identb", [128, 128], BF16)
        S.dma("pool", self.identb[:], ident_d, writes=[self.identb])
        self.identf = S.sb("identf", [128, 128], F32)
        S.dma("sp", self.identf[:], ident_d, writes=[self.identf])
        self.nT = 0
        self.nA = 0


def emit_linear(C, xs, w_ap, y_d, R, K, chunks, prologue="none", modrows=None, modsel=None, bias_d=None,
                mul_d=None, g_d=None, colscale=None, RB=16):
    S, nc = C.S, C.nc
    NT = R // 128
    KC = K // 128
    nsum = len(xs)
    with S.scope():
        hT = [S.sb("hT%d" % t, [128, KC, 128], BF16) for t in range(min(RB, NT))]
        xin = [S.sb("xin%d" % i, [128, K], F32) for i in range(2)]
        xaux = [S.sb("xaux%d" % i, [128, K], F32) for i in range(2)] if nsum > 1 else None
        hb = [S.sb("hb%d" % i, [128, K], BF16) for i in range(2)]
        st = [S.sb("st%d" % i, [128, 8, 6], F32) for i in range(2)]
        mv = [S.sb("mv%d" % i, [128, 4], F32) for i in range(2)]
        if prologue == "ln0mod":
            nsets = len(modrows)
            modA = S.sb("modA", [128, nsets, K], F32)
            modB = S.sb("modB", [128, nsets, K], F32)
            for s_, (ra, rb_) in enumerate(modrows):
                S.dma("sp", modA[:, s_, :], ra.partition_broadcast(128), writes=[modA])
                S.dma("sp", modB[:, s_, :], rb_.partition_broadcast(128), writes=[modB])
            S.op("dve", lambda: nc.vector.tensor_scalar(out=modA[:], in0=modA[:], scalar1=1.0, scalar2=None, op0=ALU.add),
                 reads=[modA], writes=[modA])
        if prologue == "rms":
            gb = S.sb("gb", [128, K], F32)
            S.dma("sp", gb[:], g_d.partition_broadcast(128), writes=[gb])
            junk = S.sb("junk", [128, K], F32)
        if prologue == "attn":
            oin = [S.sb("oin%d" % i, [128, MLA_HEADS, V_HEAD + 1], F32) for i in range(2)]
            rden = [S.sb("rden%d" % i, [128, MLA_HEADS, 1], F32) for i in range(2)]
        wb = [S.sb("wb%d" % i, [128, KC, 512], BF16) for i in range(2)]
        bb = [S.sb("bb%d" % i, [128, 512], F32) for i in range(2)] if bias_d is not None else None
        ot = [S.sb("ot%d" % i, [128, 512], F32) for i in range(3)]
        mt = [S.sb("mt%d" % i, [128, 512], F32) for i in range(3)] if mul_d is not None else None
        if colscale is not None:
            gcol_d, gfn = colscale
            ncol = gcol_d.shape[1]
            gt = [S.sb("gt%d" % t, [128, ncol], F32) for t in range(min(RB, NT))]
        wi = 0
        it = 0
        for t0 in range(0, NT, RB):
            tiles = list(range(t0, min(NT, t0 + RB)))
            for t in tiles:
                x = xin[t % 2]
                h = hb[t % 2]
                rows = slice(t * 128, (t + 1) * 128)
                if colscale is not None:
                    S.dma("sp", gt[t - t0][:], gcol_d[rows], writes=[gt[t - t0]])
                if prologue == "attn":
                    o = oin[t % 2]
                    rd_ = rden[t % 2]
                    S.dma("sp", o[:], xs[0][rows], writes=[o])
                    S.op("dve", lambda o=o, rd_=rd_: nc.vector.reciprocal(out=rd_[:], in_=o[:, :, V_HEAD:V_HEAD + 1]),
                         reads=[o], writes=[rd_])
                    for hh in range(MLA_HEADS):
                        S.op("dve", lambda o=o, rd_=rd_, h=h, hh=hh: nc.vector.tensor_scalar(
                            out=h[:, hh * V_HEAD:(hh + 1) * V_HEAD], in0=o[:, hh, 0:V_HEAD], scalar1=rd_[:, hh, :],
                            scalar2=None, op0=ALU.mult), reads=[o, rd_], writes=[h])
                else:
                    S.dma("sp", x[:], xs[0][rows], writes=[x])
                    for i in range(1, nsum):
                        xa = xaux[i % 2]
                        S.dma("sp", xa[:], xs[i][rows], writes=[xa])
                        S.op("dve", lambda x=x, xa=xa: nc.vector.tensor_tensor(out=x[:], in0=x[:], in1=xa[:], op=ALU.add),
                             reads=[x, xa], writes=[x])
                    if prologue == "none":
                        S.op("act", lambda x=x, h=h: nc.scalar.copy(out=h[:], in_=x[:]), reads=[x], writes=[h])
                    elif prologue == "silu":
                        S.op("act", lambda x=x, h=h: nc.scalar.activation(out=h[:], in_=x[:], func=AF.Silu),
                             reads=[x], writes=[h])
                    elif prologue == "ln0mod":
                        m = ln_stats(S, x, K, "p", {"st": st[t % 2], "mv": mv[t % 2]})
                        ms = modsel(t)
                        S.op("dve", lambda x=x, m=m: nc.vector.tensor_scalar(
                            out=x[:], in0=x[:], scalar1=m[:, 0:1], scalar2=m[:, 2:3], op0=ALU.subtract, op1=ALU.mult),
                            reads=[x, m], writes=[x])
                        S.op("dve", lambda x=x, ms=ms: nc.vector.tensor_tensor(out=x[:], in0=x[:], in1=modA[:, ms, :],
                                                                               op=ALU.mult), reads=[x, modA], writes=[x])
                        S.op("dve", lambda x=x, h=h, ms=ms: nc.vector.tensor_tensor(
                            out=h[:], in0=x[:], in1=modB[:, ms, :], op=ALU.add), reads=[x, modB], writes=[h])
                    elif prologue == "rms":
                        m = mv[t % 2]
                        S.op("act", lambda x=x, m=m: nc.scalar.activation(out=junk[:], in_=x[:], func=AF.Square,
                                                                          accum_out=m[:, 0:1]),
                             reads=[x], writes=[junk, m])
                        S.op("dve", lambda m=m: nc.vector.tensor_scalar(out=m[:, 3:4], in0=m[:, 0:1], scalar1=1.0 / K,
                                                                        scalar2=LN_EPS, op0=ALU.mult, op1=ALU.add),
                             reads=[m], writes=[m])
                        rstd_of(S, m)
                        S.op("dve", lambda x=x, h=h, m=m: nc.vector.scalar_tensor_tensor(
                            out=h[:], in0=x[:], scalar=m[:, 2:3], in1=gb[:], op0=ALU.mult, op1=ALU.mult),
                            reads=[x, m, gb], writes=[h])
                for k0 in range(0, KC, 8):
                    kn = min(8, KC - k0)
                    p = C.pT[C.nT % 2]
                    C.nT += 1
                    for j in range(kn):
                        S.op("pe", lambda p=p, h=h, j=j, k0=k0: nc.tensor.transpose(
                            out=p[:, j * 128:(j + 1) * 128], in_=h[:, (k0 + j) * 128:(k0 + j + 1) * 128],
                            identity=C.identb[:]), reads=[h, C.identb], writes=[p])
                    S.op("act", lambda p=p, t=t, k0=k0, kn=kn: nc.scalar.copy(
                        out=hT[t - t0][:, k0:k0 + kn, :], in_=p[:, 0:kn * 128].rearrange("p (a b) -> p a b", b=128)),
                        reads=[p], writes=[hT[t - t0]])
            for ci, (c0, nw, act) in enumerate(chunks):
                w = wb[wi % 2]
                wi += 1
                wv = w_ap(ci).rearrange("(kc p) n -> p kc n", p=128)
                for k0 in range(0, KC, 16):
                    k1 = min(KC, k0 + 16)
                    S.dma("pool", w[:, k0:k1, 0:nw], wv[:, k0:k1, :], writes=[w])
                if bias_d is not None:
                    b = bb[wi % 2]
                    S.dma("sp", b[:, 0:nw], bias_d[c0:c0 + nw].partition_broadcast(128), writes=[b])
                for t in tiles:
                    a = C.acc[C.nA % 4]
                    C.nA += 1
                    o = ot[it % 3]
                    for kc in range(KC):
                        S.op("pe", lambda a=a, w=w, t=t, kc=kc, nw=nw: nc.tensor.matmul(
                            out=a[:, 0:nw], lhsT=hT[t - t0][:, kc, :], rhs=w[:, kc, 0:nw], start=(kc == 0),
                            stop=(kc == KC - 1)), reads=[hT[t - t0], w], writes=[a])
                    if bias_d is not None:
                        S.op("dve", lambda a=a, o=o, b=b, nw=nw: nc.vector.tensor_tensor(
                            out=o[:, 0:nw], in0=a[:, 0:nw], in1=b[:, 0:nw], op=ALU.add), reads=[a, b], writes=[o])
                        if act is not None:
                            S.op("act", lambda o=o, nw=nw, act=act: nc.scalar.activation(
                                out=o[:, 0:nw], in_=o[:, 0:nw], func=act), reads=[o], writes=[o])
                    else:
                        S.op("act", lambda a=a, o=o, nw=nw, act=act: nc.scalar.activation(
                            out=o[:, 0:nw], in_=a[:, 0:nw], func=(act if act is not None else AF.Copy)),
                            reads=[a], writes=[o])
                    if colscale is not None:
                        e_ = gfn(ci)
                        S.op("dve", lambda o=o, t=t, e_=e_, nw=nw: nc.vector.tensor_scalar(
                            out=o[:, 0:nw], in0=o[:, 0:nw], scalar1=gt[t - t0][:, e_:e_ + 1], scalar2=None, op0=ALU.mult),
                            reads=[o, gt[t - t0]], writes=[o])
                    if mul_d is not None:
                        m_ = mt[it % 3]
                        S.dma("sp", m_[:, 0:nw], mul_d[t * 128:(t + 1) * 128, c0:c0 + nw], writes=[m_])
                        S.op("dve", lambda o=o, m_=m_, nw=nw: nc.vector.tensor_tensor(
                            out=o[:, 0:nw], in0=o[:, 0:nw], in1=m_[:, 0:nw], op=ALU.mult), reads=[o, m_], writes=[o])
                    S.dma("sp", y_d[t * 128:(t + 1) * 128, c0:c0 + nw], o[:, 0:nw], reads=[o])
                    it += 1


def emit_resln(C, x_d, fs, m_rows, msel, g_row, b_row, y_d, R):
    S, nc = C.S, C.nc
    D = D_MODEL
    with S.scope():
        mm = S.sb("mm", [128, len(m_rows), D], F32)
        for i, r in enumerate(m_rows):
            S.dma("sp", mm[:, i, :], r.partition_broadcast(128), writes=[mm])
        gb = S.sb("g", [128, D], F32)
        bb = S.sb("b", [128, D], F32)
        S.dma("sp", gb[:], g_row.partition_broadcast(128), writes=[gb])
        S.dma("sp", bb[:], b_row.partition_broadcast(128), writes=[bb])
        xt = [S.sb("x%d" % i, [128, D], F32) for i in range(2)]
        ft = [S.sb("f%d" % i, [128, D], F32) for i in range(2)]
        fa = [S.sb("fa%d" % i, [128, D], F32) for i in range(2)]
        st = [S.sb("st%d" % i, [128, 8, 6], F32) for i in range(2)]
        mv = [S.sb("mv%d" % i, [128, 4], F32) for i in range(2)]
        for t in range(R // 128):
            rows = slice(t * 128, (t + 1) * 128)
            x, f = xt[t % 2], ft[t % 2]
            S.dma("sp", x[:], x_d[rows], writes=[x])
            S.dma("sp", f[:], fs[0][rows], writes=[f])
            for i in range(1, len(fs)):
                a = fa[i % 2]
                S.dma("sp", a[:], fs[i][rows], writes=[a])
                S.op("dve", lambda f=f, a=a: nc.vector.tensor_tensor(out=f[:], in0=f[:], in1=a[:], op=ALU.add),
                     reads=[f, a], writes=[f])
            ms = msel(t)
            S.op("dve", lambda f=f, ms=ms: nc.vector.tensor_tensor(out=f[:], in0=f[:], in1=mm[:, ms, :], op=ALU.mult),
                 reads=[f, mm], writes=[f])
            S.op("dve", lambda x=x, f=f: nc.vector.scalar_tensor_tensor(out=x[:], in0=x[:], scalar=ALPHA, in1=f[:],
                                                                        op0=ALU.mult, op1=ALU.add),
                 reads=[x, f], writes=[x])
            m = ln_stats(S, x, D, "r", {"st": st[t % 2], "mv": mv[t % 2]})
            S.op("dve", lambda x=x, m=m: nc.vector.tensor_scalar(out=x[:], in0=x[:], scalar1=m[:, 0:1], scalar2=m[:, 2:3],
                                                                 op0=ALU.subtract, op1=ALU.mult), reads=[x, m], writes=[x])
            S.op("dve", lambda x=x: nc.vector.tensor_tensor(out=x[:], in0=x[:], in1=gb[:], op=ALU.mult),
                 reads=[x, gb], writes=[x])
            S.op("dve", lambda x=x: nc.vector.tensor_tensor(out=x[:], in0=x[:], in1=bb[:], op=ALU.add),
                 reads=[x, bb], writes=[x])
            S.dma("sp", y_d[rows], x[:], reads=[x])


def gelu_tanh(S, x, t, W):
    nc = S.nc
    S.op("dve", lambda: nc.vector.tensor_tensor(out=t[:, 0:W], in0=x[:, 0:W], in1=x[:, 0:W], op=ALU.mult), reads=[x], writes=[t])
    S.op("dve", lambda: nc.vector.tensor_scalar(out=t[:, 0:W], in0=t[:, 0:W], scalar1=0.044715, scalar2=1.0, op0=ALU.mult,
                                                op1=ALU.add), reads=[t], writes=[t])
    S.op("dve", lambda: nc.vector.tensor_tensor(out=t[:, 0:W], in0=t[:, 0:W], in1=x[:, 0:W], op=ALU.mult), reads=[t, x], writes=[t])
    S.op("act", lambda: nc.scalar.activation(out=t[:, 0:W], in_=t[:, 0:W], func=AF.Sigmoid, scale=1.5957691216057308),
         reads=[t], writes=[t])
    S.op("dve", lambda: nc.vector.tensor_tensor(out=x[:, 0:W], in0=x[:, 0:W], in1=t[:, 0:W], op=ALU.mult), reads=[t, x], writes=[x])


def ln_rows(S, x, W, st, mv, gb, bb, out=None):
    nc = S.nc
    out = out if out is not None else x
    m = ln_stats(S, x, W, "l", {"st": st, "mv": mv})
    S.op("dve", lambda: nc.vector.tensor_scalar(out=x[:, 0:W], in0=x[:, 0:W], scalar1=m[:, 0:1], scalar2=m[:, 2:3],
                                                op0=ALU.subtract, op1=ALU.mult), reads=[x, m], writes=[x])
    S.op("dve", lambda: nc.vector.tensor_tensor(out=x[:, 0:W], in0=x[:, 0:W], in1=gb[:, 0:W], op=ALU.mult), reads=[x, gb], writes=[x])
    S.op("dve", lambda: nc.vector.tensor_tensor(out=out[:, 0:W], in0=x[:, 0:W], in1=bb[:, 0:W], op=ALU.add),
         reads=[x, bb], writes=[out])


def emit_chunkgate(C, z_d, lng, lnb, ws_d, bs_d, y_d, R):
    S, nc = C.S, C.nc
    with S.scope():
        gb = S.sb("g", [128, 512], F32)
        bb = S.sb("b", [128, 512], F32)
        S.dma("sp", gb[:], lng.partition_broadcast(128), writes=[gb])
        S.dma("sp", bb[:], lnb.partition_broadcast(128), writes=[bb])
        wraw = S.sb("wraw", [128, 4, 128], BF16)
        wT = S.sb("wT", [128, 4, 128], BF16)
        S.dma("pool", wraw[:], ws_d.rearrange("g p q -> p g q"), writes=[wraw])
        p = C.pT[C.nT % 2]
        C.nT += 1
        for g in range(4):
            S.op("pe", lambda g=g: nc.tensor.transpose(out=p[:, g * 128:(g + 1) * 128], in_=wraw[:, g, :], identity=C.identb[:]),
                 reads=[wraw, C.identb], writes=[p])
        S.op("act", lambda: nc.scalar.copy(out=wT[:], in_=p[:, 0:512].rearrange("p (a b) -> p a b", b=128)), reads=[p], writes=[wT])
        bs = S.sb("bs", [128, 4], F32)
        S.dma("sp", bs[:], bs_d.rearrange("g p -> p g"), writes=[bs], allow_slow_non_contiguous=True)
        uv = [S.sb("uv%d" % i, [128, 1024], F32) for i in range(2)]
        tt = [S.sb("tt%d" % i, [128, 1024], F32) for i in range(2)]
        vb = [S.sb("vb%d" % i, [128, 512], BF16) for i in range(2)]
        ot = [S.sb("ot%d" % i, [128, 512], F32) for i in range(2)]
        st = [S.sb("st%d" % i, [128, 8, 6], F32) for i in range(2)]
        mv = [S.sb("mv%d" % i, [128, 4], F32) for i in range(2)]
        for t in range(R // 128):
            rows = slice(t * 128, (t + 1) * 128)
            x, tmp, v, o = uv[t % 2], tt[t % 2], vb[t % 2], ot[t % 2]
            S.dma("sp", x[:], z_d[rows, 0:1024], writes=[x])
            gelu_tanh(S, x, tmp, 1024)
            m = ln_stats(S, Buf(x.t[:, 512:1024]) if False else x, 512, "c", {"st": st[t % 2], "mv": mv[t % 2]}) if False else None
            nchs = 1
            S.op("dve", lambda x=x, t=t: nc.vector.bn_stats(out=st[t % 2][:, 0, :], in_=x[:, 512:1024]), reads=[x], writes=[st[t % 2]])
            mvt = mv[t % 2]
            S.op("dve", lambda t=t, mvt=mvt: nc.vector.bn_aggr(out=mvt[:, 0:2], in_=st[t % 2][:, 0:1, :]), reads=[st[t % 2]], writes=[mvt])
            S.op("dve", lambda mvt=mvt: nc.vector.tensor_scalar(out=mvt[:, 3:4], in0=mvt[:, 1:2], scalar1=LN_EPS, scalar2=None,
                                                                op0=ALU.add), reads=[mvt], writes=[mvt])
            rstd_of(S, mvt)
            S.op("dve", lambda x=x, mvt=mvt: nc.vector.tensor_scalar(out=x[:, 512:1024], in0=x[:, 512:1024], scalar1=mvt[:, 0:1],
                                                                     scalar2=mvt[:, 2:3], op0=ALU.subtract, op1=ALU.mult),
                 reads=[x, mvt], writes=[x])
            S.op("dve", lambda x=x: nc.vector.tensor_tensor(out=x[:, 512:1024], in0=x[:, 512:1024], in1=gb[:], op=ALU.mult),
                 reads=[x, gb], writes=[x])
            S.op("dve", lambda x=x, v=v: nc.vector.tensor_tensor(out=v[:], in0=x[:, 512:1024], in1=bb[:], op=ALU.add),
                 reads=[x, bb], writes=[v])
            a = C.acc[C.nA % 4]
            C.nA += 1
            for g in range(4):
                S.op("pe", lambda a=a, v=v, g=g: nc.tensor.matmul(out=a[:, g * 128:(g + 1) * 128], lhsT=wT[:, g, :],
                                                                  rhs=v[:, g * 128:(g + 1) * 128], start=True, stop=True),
                     reads=[wT, v], writes=[a])
            for g in range(4):
                S.op("dve", lambda a=a, o=o, x=x, g=g: nc.vector.scalar_tensor_tensor(
                    out=o[:, g * 128:(g + 1) * 128], in0=a[:, g * 128:(g + 1) * 128], scalar=bs[:, g:g + 1],
                    in1=x[:, g * 128:(g + 1) * 128], op0=ALU.add, op1=ALU.mult), reads=[a, bs, x], writes=[o])
            S.dma("sp", y_d[rows], o[:], reads=[o])


def emit_conv(C, z_d, wdw_d, bdw_d, lng, lnb, y_d, segs, halo=None):
    S, nc = C.S, C.nc
    with S.scope():
        gb = S.sb("g", [128, 512], F32)
        bb = S.sb("b", [128, 512], F32)
        S.dma("sp", gb[:], lng.partition_broadcast(128), writes=[gb])
        S.dma("sp", bb[:], lnb.partition_broadcast(128), writes=[bb])
        wd = S.sb("wd", [128, 4, CONV_W], F32)
        bd = S.sb("bd", [128, 4], F32)
        for c in range(4):
            S.dma("sp", wd[:, c, :], wdw_d[:, c * 128:(c + 1) * 128].rearrange("k p -> p k"), writes=[wd],
                  allow_slow_non_contiguous=True)
            S.dma("sp", bd[:, c:c + 1], bdw_d[c * 128:(c + 1) * 128].rearrange("(p o) -> p o", o=1), writes=[bd],
                  allow_slow_non_contiguous=True)
        yT = S.sb("yT", [128, 4, 1280], F32)
        cacc = S.sb("cacc", [128, 4, 1024], F32)
        ab = [S.sb("ab%d" % i, [128, 1024], F32) for i in range(2)]
        yy = [S.sb("yy%d" % i, [128, 512], F32) for i in range(2)]
        ot = [S.sb("ot%d" % i, [128, 512], F32) for i in range(2)]
        st = [S.sb("st%d" % i, [128, 8, 6], F32) for i in range(2)]
        mv = [S.sb("mv%d" % i, [128, 4], F32) for i in range(2)]
        it = 0
        for si, (r0, nr) in enumerate(segs):
            for b0 in range(0, nr, 1024):
                bl = min(1024, nr - b0)
                nti = bl // 128
                S.op("pool", lambda: nc.gpsimd.memset(yT[:], 0.0), writes=[yT])
                for j in range(-1, nti + 1):
                    tr = b0 + j * 128
                    src = None
                    if 0 <= tr < nr:
                        src = z_d[r0 + tr:r0 + tr + 128, 1024:2048]
                    elif halo is not None and si == 0:
                        src = halo[0:128, :] if tr < 0 else halo[128:256, :]
                    if src is None:
                        continue
                    x, y = ab[it % 2], yy[it % 2]
                    it += 1
                    S.dma("sp", x[:], src, writes=[x])
                    S.op("dve", lambda x=x, y=y: nc.vector.tensor_tensor(out=y[:], in0=x[:, 0:512], in1=x[:, 512:1024], op=ALU.mult),
                         reads=[x], writes=[y])
                    a = C.acc[C.nA % 4]
                    C.nA += 1
                    for c in range(4):
                        S.op("pe", lambda a=a, y=y, c=c: nc.tensor.transpose(out=a[:, c * 128:(c + 1) * 128],
                                                                             in_=y[:, c * 128:(c + 1) * 128], identity=C.identf[:]),
                             reads=[y, C.identf], writes=[a])
                    S.op("act", lambda a=a, j=j: nc.scalar.copy(out=yT[:, :, (j + 1) * 128:(j + 2) * 128],
                                                                in_=a[:, 0:512].rearrange("p (c t) -> p c t", t=128)),
                         reads=[a], writes=[yT])
                for c in range(4):
                    for k in range(CONV_W):
                        src = yT[:, c, 113 + k:113 + k + bl]
                        if k == 0:
                            S.op("dve", lambda c=c, src=src: nc.vector.tensor_scalar(
                                out=cacc[:, c, 0:bl], in0=src, scalar1=wd[:, c, 0:1], scalar2=bd[:, c:c + 1], op0=ALU.mult,
                                op1=ALU.add), reads=[yT, wd, bd], writes=[cacc])
                        else:
                            S.op("dve", lambda c=c, k=k, src=src: nc.vector.scalar_tensor_tensor(
                                out=cacc[:, c, 0:bl], in0=src, scalar=wd[:, c, k:k + 1], in1=cacc[:, c, 0:bl], op0=ALU.mult,
                                op1=ALU.add), reads=[yT, wd, cacc], writes=[cacc])
                for j in range(nti):
                    a = C.acc[C.nA % 4]
                    C.nA += 1
                    o = ot[j % 2]
                    for c in range(4):
                        S.op("pe", lambda a=a, c=c, j=j: nc.tensor.transpose(out=a[:, c * 128:(c + 1) * 128],
                                                                             in_=cacc[:, c, j * 128:(j + 1) * 128], identity=C.identf[:]),
                             reads=[cacc, C.identf], writes=[a])
                    S.op("act", lambda a=a, o=o: nc.scalar.copy(out=o[:], in_=a[:]), reads=[a], writes=[o])
                    ln_rows(S, o, 512, st[j % 2], mv[j % 2], gb, bb)
                    S.op("act", lambda o=o: nc.scalar.activation(out=o[:], in_=o[:], func=AF.Silu), reads=[o], writes=[o])
                    S.dma("sp", y_d[r0 + b0 + j * 128:r0 + b0 + (j + 1) * 128], o[:], reads=[o])


def emit_rope(C, src_d, c0, H, stride, dst_d, d0, cos_d, sin_d, nrows):
    S, nc = C.S, C.nc
    W = (H - 1) * stride + 32
    with S.scope():
        xt = [S.sb("x%d" % i, [128, W], F32) for i in range(2)]
        cs = [S.sb("cs%d" % i, [128, 32], F32) for i in range(2)]
        sn = [S.sb("sn%d" % i, [128, 32], F32) for i in range(2)]
        t1 = [S.sb("t1%d" % i, [128, 32], F32) for i in range(2)]
        for t in range(nrows // 128):
            rows = slice(t * 128, (t + 1) * 128)
            x, c_, s_, u = xt[t % 2], cs[t % 2], sn[t % 2], t1[t % 2]
            S.dma("sp", x[:], src_d[rows, c0:c0 + W], writes=[x])
            S.dma("sp", c_[:], cos_d[rows], writes=[c_])
            S.dma("sp", s_[:], sin_d[rows], writes=[s_])
            for h in range(H):
                o = h * stride
                for (a0, b0_) in ((0, 8), (8, 0), (16, 24), (24, 16)):
                    S.op("dve", lambda x=x, s_=s_, u=u, o=o, a0=a0, b0_=b0_: nc.vector.tensor_tensor(
                        out=u[:, a0:a0 + 8], in0=x[:, o + b0_:o + b0_ + 8], in1=s_[:, a0:a0 + 8], op=ALU.mult),
                        reads=[x, s_], writes=[u])
                S.op("dve", lambda x=x, c_=c_, o=o: nc.vector.tensor_tensor(out=x[:, o:o + 32], in0=x[:, o:o + 32], in1=c_[:], op=ALU.mult),
                     reads=[x, c_], writes=[x])
                S.op("dve", lambda x=x, u=u, o=o: nc.vector.tensor_tensor(out=x[:, o:o + 32], in0=x[:, o:o + 32], in1=u[:], op=ALU.add),
                     reads=[x, u], writes=[x])
            S.dma("sp", dst_d[rows, d0:d0 + W], x[:], reads=[x])


def emit_attention(C, q_d, kv_d, kr_d, o_d, q_rows, k_rows):
    S, nc = C.S, C.nc
    ktiles = [r0 + i * 128 for (r0, n) in k_rows for i in range(n // 128)]
    NKT = len(ktiles)
    q0, nq = q_rows
    NQT = nq // 128
    DK = QK_NOPE + QK_ROPE
    with S.scope():
        KT = S.sb("KT", [DK, NKT * 128], BF16)
        QT = S.sb("QT", [DK, NQT * 128], BF16)
        Vt = S.sb("Vt", [128, NKT, V_HEAD + 1], BF16)
        kin = [S.sb("kin%d" % i, [128, DK], F32) for i in range(4)]
        kb = [S.sb("kb%d" % i, [128, DK], BF16) for i in range(4)]
        KTb = [Buf(KT.t) for _ in range(NKT)]
        QTb = [Buf(QT.t) for _ in range(NQT)]
        Vtb = [Buf(Vt.t) for _ in range(NKT)]
        pt = [S.sb("pt%d" % i, [128, 512], BF16) for i in range(3)]
        of = [S.sb("of%d" % i, [V_HEAD + 1, 512], F32) for i in range(2)]
        oo = [S.sb("oo%d" % i, [128, 4, V_HEAD + 1], F32) for i in range(2)]
        ones = S.sb("ones", [128, NKT, 1], F32)
        S.op("pool", lambda: nc.gpsimd.memset(ones[:], 1.0), writes=[ones])
        S.op("act", lambda: nc.scalar.copy(out=Vt[:, :, V_HEAD:V_HEAD + 1], in_=ones[:]), reads=[ones], writes=Vtb)
        n_ = 0
        for h in range(MLA_HEADS):
            for i, r in enumerate(ktiles):
                x, xb = kin[n_ % 4], kb[n_ % 4]
                n_ += 1
                S.dma("sp", x[:, 0:QK_NOPE], kv_d[r:r + 128, h * QK_NOPE:(h + 1) * QK_NOPE], writes=[x])
                S.dma("sp", x[:, QK_NOPE:DK], kr_d[r:r + 128, :], writes=[x])
                S.dma("pool", Vt[:, i, 0:V_HEAD], kv_d[r:r + 128, 512 + h * V_HEAD:512 + (h + 1) * V_HEAD], writes=[Vtb[i]])
                S.op("act", lambda x=x, xb=xb: nc.scalar.copy(out=xb[:], in_=x[:]), reads=[x], writes=[xb])
                p = C.pT[C.nT % 2]
                C.nT += 1
                S.op("pe", lambda p=p, xb=xb: nc.tensor.transpose(out=p[0:DK, 0:128], in_=xb[:], identity=C.identb[:]),
                     reads=[xb, C.identb], writes=[p])
                S.op("act", lambda p=p, i=i: nc.scalar.copy(out=KT[:, i * 128:(i + 1) * 128], in_=p[0:DK, 0:128]),
                     reads=[p], writes=[KTb[i]])
            for i in range(NQT):
                r = q0 + i * 128
                x, xb = kin[n_ % 4], kb[n_ % 4]
                n_ += 1
                S.dma("sp", x[:], q_d[r:r + 128, h * DK:(h + 1) * DK], writes=[x])
                S.op("act", lambda x=x, xb=xb: nc.scalar.copy(out=xb[:], in_=x[:]), reads=[x], writes=[xb])
                p = C.pT[C.nT % 2]
                C.nT += 1
                S.op("pe", lambda p=p, xb=xb: nc.tensor.transpose(out=p[0:DK, 0:128], in_=xb[:], identity=C.identb[:]),
                     reads=[xb, C.identb], writes=[p])
                S.op("act", lambda p=p, i=i: nc.scalar.copy(out=QT[:, i * 128:(i + 1) * 128], in_=p[0:DK, 0:128]),
                     reads=[p], writes=[QTb[i]])
            for qb in range(0, NQT * 128, 512):
                qw = min(512, NQT * 128 - qb)
                ao = C.pB[(qb // 512) % 2]
                for i in range(NKT):
                    a = C.acc[C.nA % 4]
                    C.nA += 1
                    pp = pt[i % 3]
                    S.op("pe", lambda a=a, i=i, qb=qb, qw=qw: nc.tensor.matmul(out=a[:, 0:qw], lhsT=KT[:, i * 128:(i + 1) * 128],
                                                                              rhs=QT[:, qb:qb + qw], start=True, stop=True),
                         reads=[KTb[i]] + QTb[qb // 128:(qb + qw) // 128], writes=[a])
                    S.op("act", lambda a=a, pp=pp, qw=qw: nc.scalar.activation(out=pp[:, 0:qw], in_=a[:, 0:qw], func=AF.Exp,
                                                                              scale=MLA_SCALE), reads=[a], writes=[pp])
                    S.op("pe", lambda ao=ao, pp=pp, i=i, qw=qw: nc.tensor.matmul(out=ao[0:V_HEAD + 1, 0:qw], lhsT=Vt[:, i, :],
                                                                                rhs=pp[:, 0:qw], start=(i == 0), stop=(i == NKT - 1)),
                         reads=[Vtb[i], pp], writes=[ao])
                o1 = of[(qb // 512) % 2]
                o2 = oo[(qb // 512) % 2]
                S.op("act", lambda ao=ao, o1=o1, qw=qw: nc.scalar.copy(out=o1[:, 0:qw], in_=ao[0:V_HEAD + 1, 0:qw]), reads=[ao], writes=[o1])
                a = C.acc[C.nA % 4]
                C.nA += 1
                nsub = qw // 128
                for j in range(nsub):
                    S.op("pe", lambda a=a, o1=o1, j=j: nc.tensor.transpose(out=a[:, j * 65:(j + 1) * 65], in_=o1[:, j * 128:(j + 1) * 128],
                                                                           identity=C.identf[0:V_HEAD + 1, 0:V_HEAD + 1]),
                         reads=[o1, C.identf], writes=[a])
                S.op("dve", lambda a=a, o2=o2, nsub=nsub: nc.vector.tensor_copy(
                    out=o2[:, 0:nsub, :], in_=a[:, 0:nsub * 65].rearrange("p (j d) -> p j d", d=65)), reads=[a], writes=[o2])
                for j in range(nsub):
                    r = q0 + qb + j * 128
                    S.dma("sp", o_d[r:r + 128, h, :], o2[:, j, :], reads=[o2])


def emit_route(C, lg_d, IDX_d, GAB_d, lstrict_d, R, CAP):
    S, nc = C.S, C.nc
    I32 = mybir.dt.int32
    with S.scope():
        L = [S.sb("L%d" % i, [128, 36], F32) for i in range(2)]
        W = [S.sb("W%d" % i, [128, 256], F32) for i in range(2)]
        Mi = [S.sb("Mi%d" % i, [128, 32], BF16) for i in range(2)]
        II = [S.sb("II%d" % i, [128, 2], I32) for i in range(2)]
        Macc = S.sb("Macc", [128, 32], BF16)
        lst = S.sb("lst", [128, 128], BF16)
        onesb = S.sb("onesb", [128, 128], BF16)
        ebase = S.sb("ebase", [128, 32], F32)
        S.dma("pool", lst[:], lstrict_d, writes=[lst])
        S.op("pool", lambda: nc.gpsimd.memset(onesb[:], 1.0), writes=[onesb])
        S.op("pool", lambda: nc.gpsimd.memset(Macc[:], 0.0), writes=[Macc])
        for e in range(N_EXPERTS):
            S.op("pool", lambda e=e: nc.gpsimd.memset(ebase[:, e:e + 1], float(e * CAP)), writes=[ebase])
        for t in range(R // 128):
            rows = slice(t * 128, (t + 1) * 128)
            l, w, mi, ii = L[t % 2], W[t % 2], Mi[t % 2], II[t % 2]
            S.dma("sp", l[:], lg_d[rows], writes=[l])
            S.op("dve", lambda l=l, w=w: nc.vector.tensor_reduce(out=w[:, 0:1], in_=l[:, 0:4], axis=AX.X, op=ALU.max), reads=[l], writes=[w])
            S.op("dve", lambda l=l, w=w: nc.vector.tensor_scalar(out=w[:, 4:8], in0=l[:, 0:4], scalar1=w[:, 0:1], scalar2=None, op0=ALU.is_ge),
                 reads=[l, w], writes=[w])
            S.op("dve", lambda w=w: nc.vector.tensor_scalar(out=w[:, 1:2], in0=w[:, 0:1], scalar1=-1.0, scalar2=None, op0=ALU.mult),
                 reads=[w], writes=[w])
            S.op("act", lambda l=l, w=w: nc.scalar.activation(out=w[:, 44:48], in_=l[:, 0:4], func=AF.Exp, bias=w[:, 1:2], scale=1.0),
                 reads=[l, w], writes=[w])
            S.op("dve", lambda w=w: nc.vector.tensor_reduce(out=w[:, 2:3], in_=w[:, 44:48], axis=AX.X, op=ALU.add), reads=[w], writes=[w])
            S.op("dve", lambda w=w: nc.vector.reciprocal(out=w[:, 3:4], in_=w[:, 2:3]), reads=[w], writes=[w])
            S.op("dve", lambda l=l, w=w: nc.vector.tensor_scalar(out=w[:, 8:16], in0=l[:, 4:12], scalar1=w[:, 4:5], scalar2=None, op0=ALU.mult),
                 reads=[l, w], writes=[w])
            for gi in range(1, 4):
                S.op("dve", lambda l=l, w=w, gi=gi: nc.vector.scalar_tensor_tensor(
                    out=w[:, 8:16], in0=l[:, 4 + gi * 8:12 + gi * 8], scalar=w[:, 4 + gi:5 + gi], in1=w[:, 8:16], op0=ALU.mult, op1=ALU.add),
                    reads=[l, w], writes=[w])
            S.op("dve", lambda w=w: nc.vector.max(out=w[:, 16:24], in_=w[:, 8:16]), reads=[w], writes=[w])
            S.op("dve", lambda w=w: nc.vector.tensor_scalar(out=w[:, 24:32], in0=w[:, 8:16], scalar1=w[:, 17:18], scalar2=None, op0=ALU.is_ge),
                 reads=[w], writes=[w])
            S.op("dve", lambda w=w: nc.vector.tensor_scalar(out=w[:, 48:56], in0=w[:, 8:16], scalar1=w[:, 16:17], scalar2=None, op0=ALU.is_ge),
                 reads=[w], writes=[w])
            S.op("dve", lambda w=w: nc.vector.tensor_tensor(out=w[:, 56:64], in0=w[:, 24:32], in1=w[:, 48:56], op=ALU.subtract), reads=[w], writes=[w])
            S.op("dve", lambda w=w: nc.vector.tensor_scalar(out=w[:, 40:41], in0=w[:, 16:17], scalar1=-1.0, scalar2=None, op0=ALU.mult),
                 reads=[w], writes=[w])
            S.op("act", lambda w=w: nc.scalar.activation(out=w[:, 32:40], in_=w[:, 8:16], func=AF.Exp, bias=w[:, 40:41], scale=1.0),
                 reads=[w], writes=[w])
            S.op("act", lambda w=w: nc.scalar.activation(out=w[:, 41:42], in_=w[:, 17:18], func=AF.Exp, bias=w[:, 40:41], scale=1.0),
                 reads=[w], writes=[w])
            S.op("dve", lambda w=w: nc.vector.tensor_scalar(out=w[:, 41:42], in0=w[:, 41:42], scalar1=1.0, scalar2=None, op0=ALU.add),
                 reads=[w], writes=[w])
            S.op("dve", lambda w=w: nc.vector.reciprocal(out=w[:, 42:43], in_=w[:, 41:42]), reads=[w], writes=[w])
            S.op("dve", lambda w=w: nc.vector.tensor_tensor(out=w[:, 42:43], in0=w[:, 42:43], in1=w[:, 3:4], op=ALU.mult), reads=[w], writes=[w])
            S.op("dve", lambda w=w: nc.vector.tensor_scalar(out=w[:, 64:72], in0=w[:, 32:40], scalar1=w[:, 42:43], scalar2=None, op0=ALU.mult),
                 reads=[w], writes=[w])
            for (sc, oc) in ((48, 80), (56, 81)):
                S.op("dve", lambda w=w, sc=sc: nc.vector.tensor_tensor(out=w[:, 72:80], in0=w[:, 64:72], in1=w[:, sc:sc + 8], op=ALU.mult),
                     reads=[w], writes=[w])
                S.op("dve", lambda w=w, oc=oc: nc.vector.tensor_reduce(out=w[:, oc:oc + 1], in_=w[:, 72:80], axis=AX.X, op=ALU.add),
                     reads=[w], writes=[w])
            for gi in range(4):
                S.op("dve", lambda w=w, gi=gi: nc.vector.tensor_scalar(out=w[:, 96 + gi * 8:104 + gi * 8], in0=w[:, 48:56],
                                                                       scalar1=w[:, 4 + gi:5 + gi], scalar2=None, op0=ALU.mult),
                     reads=[w], writes=[w])
                S.op("dve", lambda w=w, gi=gi: nc.vector.tensor_scalar(out=w[:, 128 + gi * 8:136 + gi * 8], in0=w[:, 56:64],
                                                                       scalar1=w[:, 4 + gi:5 + gi], scalar2=None, op0=ALU.mult),
                     reads=[w], writes=[w])
            S.op("dve", lambda w=w, mi=mi: nc.vector.tensor_tensor(out=mi[:], in0=w[:, 96:128], in1=w[:, 128:160], op=ALU.add),
                 reads=[w], writes=[mi])
            a = C.acc[C.nA % 4]
            C.nA += 1
            S.op("pe", lambda a=a, mi=mi: nc.tensor.matmul(out=a[:, 0:32], lhsT=lst[:], rhs=mi[:], start=True, stop=False),
                 reads=[lst, mi], writes=[a])
            S.op("pe", lambda a=a: nc.tensor.matmul(out=a[:, 0:32], lhsT=onesb[:], rhs=Macc[:], start=False, stop=True),
                 reads=[onesb, Macc], writes=[a])
            S.op("dve", lambda mi=mi: nc.vector.tensor_tensor(out=Macc[:], in0=Macc[:], in1=mi[:], op=ALU.add), reads=[Macc, mi], writes=[Macc])
            S.op("dve", lambda a=a, w=w: nc.vector.tensor_tensor(out=w[:, 160:192], in0=a[:, 0:32], in1=ebase[:], op=ALU.add),
                 reads=[a, ebase], writes=[w])
            for (mc, oc) in ((96, 82), (128, 83)):
                S.op("dve", lambda w=w, mc=mc: nc.vector.tensor_tensor(out=w[:, 192:224], in0=w[:, 160:192], in1=w[:, mc:mc + 32], op=ALU.mult),
                     reads=[w], writes=[w])
                S.op("dve", lambda w=w, oc=oc: nc.vector.tensor_reduce(out=w[:, oc:oc + 1], in_=w[:, 192:224], axis=AX.X, op=ALU.add),
                     reads=[w], writes=[w])
            S.op("dve", lambda w=w, ii=ii: nc.vector.tensor_copy(out=ii[:], in_=w[:, 82:84]), reads=[w], writes=[ii])
            S.dma("sp", IDX_d[rows], ii[:], reads=[ii])
            S.dma("sp", GAB_d[rows], w[:, 80:82], reads=[w])


def emit_dispatch(C, x1_d, modrows, modsel, IDX_d, Xs_d, R, NS):
    S, nc = C.S, C.nc
    D = D_MODEL
    I32 = mybir.dt.int32
    xsb = Buf(None)
    with S.scope():
        zt = S.sb("zt", [128, 8, D], BF16)
        S.op("pool", lambda: nc.gpsimd.memset(zt[:], 0.0), writes=[zt])
        for r0 in range(0, NS, 1024):
            S.dma("sp", Xs_d[r0:r0 + 1024].rearrange("(a p) n -> p a n", p=128), zt[:], reads=[zt], writes=[xsb])
        modA = S.sb("modA", [128, 2, D], F32)
        modB = S.sb("modB", [128, 2, D], F32)
        for s_, (ra, rb_) in enumerate(modrows):
            S.dma("sp", modA[:, s_, :], ra.partition_broadcast(128), writes=[modA])
            S.dma("sp", modB[:, s_, :], rb_.partition_broadcast(128), writes=[modB])
        S.op("dve", lambda: nc.vector.tensor_scalar(out=modA[:], in0=modA[:], scalar1=1.0, scalar2=None, op0=ALU.add),
             reads=[modA], writes=[modA])
        xt = [S.sb("x%d" % i, [128, D], F32) for i in range(3)]
        st = [S.sb("st%d" % i, [128, 8, 6], F32) for i in range(2)]
        mv = [S.sb("mv%d" % i, [128, 4], F32) for i in range(2)]
        II = [S.sb("II%d" % i, [128, 2], I32) for i in range(3)]
        xbt = [S.sb("xb%d" % i, [128, D], BF16) for i in range(3)]
        for t in range(R // 128):
            rows = slice(t * 128, (t + 1) * 128)
            x, ii, xb = xt[t % 3], II[t % 3], xbt[t % 3]
            S.dma("sp", x[:], x1_d[rows], writes=[x])
            S.dma("sp", ii[:], IDX_d[rows], writes=[ii])
            m = ln_stats(S, x, D, "d", {"st": st[t % 2], "mv": mv[t % 2]})
            ms = modsel(t)
            S.op("dve", lambda x=x, m=m: nc.vector.tensor_scalar(out=x[:], in0=x[:], scalar1=m[:, 0:1], scalar2=m[:, 2:3],
                                                                 op0=ALU.subtract, op1=ALU.mult), reads=[x, m], writes=[x])
            S.op("dve", lambda x=x, ms=ms: nc.vector.tensor_tensor(out=x[:], in0=x[:], in1=modA[:, ms, :], op=ALU.mult),
                 reads=[x, modA], writes=[x])
            S.op("dve", lambda x=x, xb=xb, ms=ms: nc.vector.tensor_tensor(out=xb[:], in0=x[:], in1=modB[:, ms, :], op=ALU.add),
                 reads=[x, modB], writes=[xb])
            for j in range(2):
                S.idma(Xs_d, xb[:], ii[:, j:j + 1], True, reads=[xb, ii, xsb], writes=[])


def emit_experts(C, Xs_d, Ys_d, w1, w3, w2, CAP):
    S, nc = C.S, C.nc
    D, DE = D_MODEL, D_EXPERT
    NTall = CAP // 128
    NTe = min(8, NTall)
    with S.scope():
        hb = [S.sb("hb%d" % i, [128, D], BF16) for i in range(2)]
        hT = [S.sb("hT%d" % i, [128, 16, 128], BF16) for i in range(NTe)]
        hh = [S.sb("hh%d" % i, [128, DE], BF16) for i in range(NTe)]
        hT2 = [S.sb("hU%d" % i, [128, 8, 128], BF16) for i in range(NTe)]
        w1c = [S.sb("w1c%d" % i, [128, 16, 512], BF16) for i in range(2)]
        w3c = [S.sb("w3c%d" % i, [128, 16, 512], BF16) for i in range(2)]
        w2c = [S.sb("w2c%d" % i, [128, 8, 512], BF16) for i in range(2)]
        stl = [S.sb("stl%d" % i, [128, 512], F32) for i in range(3)]
        ot = [S.sb("ot%d" % i, [128, 512], BF16) for i in range(3)]
        n1 = n2 = n3 = 0
        for e, sub in [(e_, s_) for e_ in range(N_EXPERTS) for s_ in range(0, NTall, 8)]:
            r0 = e * CAP + sub * 128
            NTe = min(8, NTall - sub)
            for t in range(NTe):
                h = hb[n1 % 2]
                n1 += 1
                S.dma("sp", h[:], Xs_d[r0 + t * 128:r0 + (t + 1) * 128], writes=[h])
                for k0 in (0, 8):
                    p = C.pT[C.nT % 2]
                    C.nT += 1
                    for j in range(8):
                        S.op("pe", lambda p=p, h=h, j=j, k0=k0: nc.tensor.transpose(
                            out=p[:, j * 128:(j + 1) * 128], in_=h[:, (k0 + j) * 128:(k0 + j + 1) * 128], identity=C.identb[:]),
                            reads=[h, C.identb], writes=[p])
                    S.op("act", lambda p=p, t=t, k0=k0: nc.scalar.copy(
                        out=hT[t][:, k0:k0 + 8, :], in_=p[:, 0:1024].rearrange("p (a b) -> p a b", b=128)), reads=[p], writes=[hT[t]])
            for jc in range(2):
                wa, wb_ = w1c[n2 % 2], w3c[n2 % 2]
                n2 += 1
                S.dma("pool", wa[:], w1[e][:, jc * 512:(jc + 1) * 512].rearrange("(kc p) n -> p kc n", p=128), writes=[wa])
                S.dma("pool", wb_[:], w3[e][:, jc * 512:(jc + 1) * 512].rearrange("(kc p) n -> p kc n", p=128), writes=[wb_])
                for t in range(NTe):
                    a1 = C.acc[C.nA % 4]
                    a3 = C.acc[(C.nA + 1) % 4]
                    C.nA += 2
                    s_ = stl[n3 % 3]
                    n3 += 1
                    for kc in range(16):
                        S.op("pe", lambda a1=a1, wa=wa, t=t, kc=kc: nc.tensor.matmul(out=a1[:], lhsT=hT[t][:, kc, :], rhs=wa[:, kc, :],
                                                                                    start=(kc == 0), stop=(kc == 15)), reads=[hT[t], wa], writes=[a1])
                    for kc in range(16):
                        S.op("pe", lambda a3=a3, wb_=wb_, t=t, kc=kc: nc.tensor.matmul(out=a3[:], lhsT=hT[t][:, kc, :], rhs=wb_[:, kc, :],
                                                                                      start=(kc == 0), stop=(kc == 15)), reads=[hT[t], wb_], writes=[a3])
                    S.op("act", lambda a1=a1, s_=s_: nc.scalar.activation(out=s_[:], in_=a1[:], func=AF.Silu), reads=[a1], writes=[s_])
                    S.op("dve", lambda a3=a3, s_=s_, t=t, jc=jc: nc.vector.tensor_tensor(out=hh[t][:, jc * 512:(jc + 1) * 512], in0=a3[:],
                                                                                        in1=s_[:], op=ALU.mult), reads=[a3, s_], writes=[hh[t]])
            for t in range(NTe):
                p = C.pT[C.nT % 2]
                C.nT += 1
                for j in range(8):
                    S.op("pe", lambda p=p, t=t, j=j: nc.tensor.transpose(out=p[:, j * 128:(j + 1) * 128], in_=hh[t][:, j * 128:(j + 1) * 128],
                                                                         identity=C.identb[:]), reads=[hh[t], C.identb], writes=[p])
                S.op("act", lambda p=p, t=t: nc.scalar.copy(out=hT2[t][:], in_=p[:, 0:1024].rearrange("p (a b) -> p a b", b=128)),
                     reads=[p], writes=[hT2[t]])
            for n4 in range(4):
                wc = w2c[n2 % 2]
                n2 += 1
                S.dma("pool", wc[:], w2[e][:, n4 * 512:(n4 + 1) * 512].rearrange("(kc p) n -> p kc n", p=128), writes=[wc])
                for t in range(NTe):
                    a = C.acc[C.nA % 4]
                    C.nA += 1
                    o = ot[n3 % 3]
                    n3 += 1
                    for kc in range(8):
                        S.op("pe", lambda a=a, wc=wc, t=t, kc=kc: nc.tensor.matmul(out=a[:], lhsT=hT2[t][:, kc, :], rhs=wc[:, kc, :],
                                                                                  start=(kc == 0), stop=(kc == 7)), reads=[hT2[t], wc], writes=[a])
                    S.op("act", lambda a=a, o=o: nc.scalar.copy(out=o[:], in_=a[:]), reads=[a], writes=[o])
                    S.dma("sp", Ys_d[r0 + t * 128:r0 + (t + 1) * 128, n4 * 512:(n4 + 1) * 512], o[:], reads=[o])


def emit_combine(C, Ys_d, IDX_d, GAB_d, f_d, R):
    S, nc = C.S, C.nc
    D = D_MODEL
    I32 = mybir.dt.int32
    with S.scope():
        ya = [S.sb("ya%d" % i, [128, D], BF16) for i in range(2)]
        yb = [S.sb("yb%d" % i, [128, D], BF16) for i in range(2)]
        fo = [S.sb("fo%d" % i, [128, D], F32) for i in range(2)]
        II = [S.sb("II%d" % i, [128, 2], I32) for i in range(2)]
        GG = [S.sb("GG%d" % i, [128, 2], F32) for i in range(2)]
        for t in range(R // 128):
            rows = slice(t * 128, (t + 1) * 128)
            a, b, ii, gg = ya[t % 2], yb[t % 2], II[t % 2], GG[t % 2]
            S.dma("sp", ii[:], IDX_d[rows], writes=[ii])
            S.dma("sp", gg[:], GAB_d[rows], writes=[gg])
            S.idma(a[:], Ys_d, ii[:, 0:1], False, reads=[ii], writes=[a])
            S.idma(b[:], Ys_d, ii[:, 1:2], False, reads=[ii], writes=[b])
            f = fo[t % 2]
            S.op("dve", lambda a=a, f=f, gg=gg: nc.vector.tensor_scalar(out=f[:], in0=a[:], scalar1=gg[:, 0:1], scalar2=None, op0=ALU.mult),
                 reads=[a, gg], writes=[f])
            S.op("dve", lambda f=f, b=b, gg=gg: nc.vector.scalar_tensor_tensor(out=f[:], in0=b[:], scalar=gg[:, 1:2], in1=f[:],
                                                                               op0=ALU.mult, op1=ALU.add), reads=[f, b, gg], writes=[f])
            S.dma("sp", f_d[rows], f[:], reads=[f])


WNAMES = ["w_ada", "b_ada", "w_in", "b_in", "a_ln_g", "a_ln_b", "a_w_s", "a_b_s", "b_w_dw", "b_b_dw", "b_ln_g", "b_ln_b",
          "d_q_g", "d_w_uq", "d_kv_g", "w_branch", "w_o", "ln1_g", "ln1_b", "e_w1", "e_w3", "e_w2", "ln2_g", "ln2_b"]


def build_program(shapes, RL, NCTX):
    R = RL + NCTX
    D = D_MODEL
    nc, es = new_prog()
    I = {}
    for k, shp in shapes.items():
        I[k] = dram_in(nc, k, shp)
    y_out = dram_out(nc, "y", [RL, D])
    T = lambda n, s: dram_tmp(nc, n, s)
    mod_d = [T("mod%d" % l, [128, 6 * D]) for l in range(DEPTH)]
    z_d = T("z", [R, COL_G])
    zg_d = [T("zg%d" % i, [R, D]) for i in range(4)]
    brA, brB = T("brA", [R, 512]), T("brB", [R, 512])
    PQ = T("PQ", [R, 1024])
    brC = [T("brC%d" % p, [R, 512]) for p in range(4)]
    q_d, kv_d, kr_d = T("q", [R, 768]), T("kv", [R, 1024]), T("kr", [R, 32])
    o_d = T("o", [R, MLA_HEADS, V_HEAD + 1])
    GP = [T("GP%d" % i, [R, D]) for i in range(4)]
    mix_d, x1_d, xb_d = T("mix", [R, D]), T("x1", [R, D]), T("xb", [R, D])
    lg_d, G_d = T("lg", [R, 36]), T("G", [R, 32])
    CAPMAX = 128 * int(math.ceil(3.6 * 2.0 * R / N_EXPERTS / 128))
    Xs_d = dram_tmp(nc, "Xs", [N_EXPERTS * CAPMAX, D], BF16)
    Ys_d = dram_tmp(nc, "Ys", [N_EXPERTS * CAPMAX, D], BF16)
    IDX_d = nc.dram_tensor("IDX", [R, 2], mybir.dt.int32).ap()
    GAB_d = T("GAB", [R, 2])
    f_d = [T("f0", [R, D])]
    NTL = RL // 128
    msel = lambda t: 0 if t < NTL else 1
    with es:
        C = Ctx(nc, es, I["ident"])
        S = C.S
        c512 = [(i * 512, 512, None) for i in range(4)]
        for l in range(DEPTH):
            emit_linear(C, [I["cc"]], lambda ci, l=l: I["w_ada"][l][:, ci * 512:(ci + 1) * 512], mod_d[l], 128, D,
                        [(i * 512, 512, None) for i in range(24)], prologue="silu", bias_d=I["b_ada"][l])
        x_cur = I["x_all"]
        for l in range(DEPTH):
            last = l == DEPTH - 1
            Rm = RL if last else R
            mr = lambda who, i, l=l: mod_d[l][who:who + 1, i * D:(i + 1) * D]
            chunks_in = []
            for c0 in range(0, COL_KV, 512):
                chunks_in.append((c0, 512, AF.Sigmoid if c0 == 1536 else None))
            chunks_in.append((COL_KV, COL_G - COL_KV, None))
            modr1 = [(mr(0, 1), mr(0, 0)), (mr(1, 1), mr(1, 0))]
            emit_linear(C, [x_cur], lambda ci, l=l, ch=chunks_in: I["w_in"][l][:, ch[ci][0]:ch[ci][0] + ch[ci][1]], z_d, R, D,
                        chunks_in, prologue="ln0mod", modrows=modr1, modsel=msel, bias_d=I["b_in"][l])
            for i in range(4):
                g0 = COL_G + i * D
                emit_linear(C, [x_cur], lambda ci, l=l, g0=g0: I["w_in"][l][:, g0 + ci * 512:g0 + (ci + 1) * 512], zg_d[i], Rm, D,
                            [(j * 512, 512, AF.Sigmoid) for j in range(4)], prologue="ln0mod", modrows=modr1, modsel=msel,
                            bias_d=I["b_in"][l][g0:g0 + D])
            emit_chunkgate(C, z_d, I["a_ln_g"][l], I["a_ln_b"][l], I["a_w_s"][l], I["a_b_s"][l], brA, Rm)
            emit_conv(C, z_d, I["b_w_dw"][l], I["b_b_dw"][l], I["b_ln_g"][l], I["b_ln_b"][l], brB,
                      [(0, RL)] + ([] if last else [(RL, NCTX)]))
            emit_linear(C, [z_d[:, COL_C:COL_D]], lambda ci: I["cs128"][:, ci * 512:(ci + 1) * 512], PQ, R, 512,
                        [(0, 512, None), (512, 512, None)])
            K4 = RL // 2
            for p in range(4):
                wsl = PQ[(p % 2) * K4:(p % 2 + 1) * K4, (p // 2) * 512:(p // 2 + 1) * 512]
                emit_linear(C, [I["dftlat"][:, p * K4:(p + 1) * K4]], lambda ci, wsl=wsl: wsl, brC[p], RL, K4, [(0, 512, None)], RB=8)
                if not last:
                    Kc = NCTX // 2
                    wsl2 = PQ[RL + (p % 2) * Kc:RL + (p % 2 + 1) * Kc, (p // 2) * 512:(p // 2 + 1) * 512]
                    emit_linear(C, [I["dftctx"][:, p * Kc:(p + 1) * Kc]], lambda ci, wsl2=wsl2: wsl2, brC[p][RL:R], NCTX, Kc,
                                [(0, 512, None)])
            emit_linear(C, [z_d[:, COL_D:COL_KV]], lambda ci, l=l: I["d_w_uq"][l][:, ci * 512:ci * 512 + (512 if ci == 0 else 256)],
                        q_d, R, 512, [(0, 512, None), (512, 256, None)], prologue="rms", g_d=I["d_q_g"][l])
            emit_linear(C, [z_d[:, COL_KV:COL_KR]], lambda ci, l=l: I["w_ukv"][l][:, ci * 512:(ci + 1) * 512],
                        kv_d, R, 256, [(0, 512, None), (512, 512, None)], prologue="rms", g_d=I["d_kv_g"][l])
            emit_rope(C, q_d, QK_NOPE, MLA_HEADS, QK_NOPE + QK_ROPE, q_d, QK_NOPE, I["rcos"], I["rsin"], RL)
            emit_rope(C, z_d, COL_KR, 1, 32, kr_d, 0, I["rcos"], I["rsin"], RL)
            with S.scope():
                tmpk = S.sb("tmpk", [128, NCTX // 128, 32], F32)
                S.dma("sp", tmpk[:], z_d[RL:R, COL_KR:COL_G].rearrange("(a p) n -> p a n", p=128), writes=[tmpk])
                S.dma("sp", kr_d[RL:R, :].rearrange("(a p) n -> p a n", p=128), tmpk[:], reads=[tmpk])
            emit_attention(C, q_d, kv_d, kr_d, o_d, (0, RL), [(0, R)])
            if not last:
                emit_attention(C, q_d, kv_d, kr_d, o_d, (RL, NCTX), [(RL, NCTX)])
            for i in range(4):
                xs = [[brA], [brB], brC, [o_d]][i]
                emit_linear(C, xs, lambda ci, l=l, i=i: I["w_branch"][l][i][:, ci * 512:(ci + 1) * 512], GP[i], Rm, 512, c512,
                            prologue=("attn" if i == 3 else "none"), mul_d=zg_d[i])
            emit_linear(C, GP, lambda ci, l=l: I["w_o"][l][:, ci * 512:(ci + 1) * 512], mix_d, Rm, D, c512)
            emit_resln(C, x_cur, [mix_d], [mr(0, 2), mr(1, 2)], msel, I["ln1_g"][l], I["ln1_b"][l], x1_d, Rm)
            modr2 = [(mr(0, 4), mr(0, 3)), (mr(1, 4), mr(1, 3))]
            emit_linear(C, [x1_d], lambda ci, l=l: I["r_w"][l], lg_d, Rm, D, [(0, 36, None)], prologue="ln0mod",
                        modrows=modr2, modsel=msel, bias_d=I["r_b"][l])
            CAP = 128 * int(math.ceil(3.6 * 2.0 * Rm / N_EXPERTS / 128))
            emit_route(C, lg_d, IDX_d, GAB_d, I["lstrict"], Rm, CAP)
            emit_dispatch(C, x1_d, modr2, msel, IDX_d, Xs_d, Rm, N_EXPERTS * CAP)
            emit_experts(C, Xs_d, Ys_d, I["e_w1"][l], I["e_w3"][l], I["e_w2"][l], CAP)
            emit_combine(C, Ys_d, IDX_d, GAB_d, f_d[0], Rm)
            dst = y_out if last else xb_d
            emit_resln(C, x1_d, [f_d[0]], [mr(0, 5), mr(1, 5)], msel, I["ln2_g"][l], I["ln2_b"][l], dst, Rm)
            x_cur = xb_d
        S.finish()
        print("program instructions:", S.n, S.cnt, S.dcnt)
    return nc


def host_consts(RL, NCTX):
    rows = RL // GRID_W
    row = np.repeat(np.arange(rows, dtype=np.float32), GRID_W)
    col = np.tile(np.arange(GRID_W, dtype=np.float32), rows)
    half = QK_ROPE // 2
    inv = (ROPE_THETA ** (-np.arange(0, half, 2, dtype=np.float32) / half)).astype(np.float32)
    ar = row[:, None] * inv
    ac = col[:, None] * inv
    ang = np.concatenate([ar, ar, ac, ac], -1)
    rcos = np.cos(ang).astype(np.float32)
    rsin = np.concatenate([-np.sin(ar), np.sin(ar), -np.sin(ac), np.sin(ac)], -1).astype(np.float32)
    cw = BRANCH_W // 4
    k = np.arange(cw)
    a = 2 * np.pi * np.outer(k, k) / cw
    cs = np.zeros((512, 1024), np.float32)
    for g in range(4):
        cs[g * cw:(g + 1) * cw, g * cw:(g + 1) * cw] = np.cos(a)
        cs[g * cw:(g + 1) * cw, 512 + g * cw:512 + (g + 1) * cw] = np.sin(a)

    def dft(n):
        kk = np.arange(n, dtype=np.int64)
        ph = (np.outer(kk, kk) % n).astype(np.float64) * (2 * np.pi / n)
        sc = 1.0 / math.sqrt(n * cw)
        return np.concatenate([np.cos(ph) * sc, -np.sin(ph) * sc], 1).astype(np.float32)

    return {"rcos": rcos, "rsin": rsin, "cs128": cs, "dftlat": dft(RL), "dftctx": dft(NCTX),
            "ident": np.eye(128, dtype=np.float32),
            "lstrict": np.triu(np.ones((128, 128), np.float32), 1)}


def kernel_fused(**inp):
    x = np.asarray(inp["x"], np.float32)
    RL = x.shape[1]
    NCTX = inp["ctx"].shape[1]
    ins = {}
    ins["x_all"] = np.concatenate([x[0], np.asarray(inp["ctx"], np.float32)[0]], 0)
    cc = np.zeros((128, D_MODEL), np.float32)
    cc[0] = np.asarray(inp["c"])[0]
    cc[1] = np.asarray(inp["c_ctx"])
    ins["cc"] = cc
    for k in WNAMES:
        ins[k] = np.ascontiguousarray(np.asarray(inp[k], np.float32))
    ins["w_ukv"] = np.concatenate([np.asarray(inp["d_w_uk"]), np.asarray(inp["d_w_uv"])], -1).astype(np.float32)
    ins["r_w"] = np.concatenate([np.asarray(inp["r_w_grp"]), np.asarray(inp["r_w_exp"])], -1).astype(np.float32)
    ins["r_b"] = np.concatenate([np.asarray(inp["r_b_grp"]), np.asarray(inp["r_b_exp"])], -1).astype(np.float32)
    ins.update(host_consts(RL, NCTX))
    shapes = {k: list(v.shape) for k, v in ins.items()}
    key = ("main", RL, NCTX)
    if key not in _PROG_CACHE:
        _PROG_CACHE[key] = build_program(shapes, RL, NCTX)
    res = run_bass_kernel_spmd(_PROG_CACHE[key], [ins], core_ids=[0])
    return np.asarray(res.results[0]["y"], np.float32)[None]


NLAT_ALL = SEQ
RC = TL + CTX_LEN


def _chunks(spec):
    return [(c0, w, a) for (c0, w, a) in spec]


def build_P1(shapes):
    RL, NCTX, R, D = TL, CTX_LEN, RC, D_MODEL
    nc, es = new_prog()
    I = {k: dram_in(nc, k, shp) for k, shp in shapes.items()}
    PQ_o, kv_o, kr_o = dram_out(nc, "PQ_o", [R, 1024]), dram_out(nc, "kv_o", [R, 1024]), dram_out(nc, "kr_o", [R, 32])
    zab_o = dram_out(nc, "zab_o", [256, 1024])
    mod_d = dram_tmp(nc, "mod", [128, 6 * D])
    z_d = dram_tmp(nc, "z", [R, COL_G])
    msel = lambda t: 0 if t < RL // 128 else 1
    with es:
        C = Ctx(nc, es, I["ident"])
        S = C.S
        emit_linear(C, [I["cc"]], lambda ci: I["w_ada"][:, ci * 512:(ci + 1) * 512], mod_d, 128, D,
                    [(i * 512, 512, None) for i in range(24)], prologue="silu", bias_d=I["b_ada"])
        mr = lambda who, i: mod_d[who:who + 1, i * D:(i + 1) * D]
        ch = [(1024, 512, None), (1536, 512, AF.Sigmoid), (2048, 512, None), (COL_KV, COL_G - COL_KV, None)]
        emit_linear(C, [I["x_own"]], lambda ci: I["w_in"][:, ch[ci][0]:ch[ci][0] + ch[ci][1]], z_d, R, D, ch, prologue="ln0mod",
                    modrows=[(mr(0, 1), mr(0, 0)), (mr(1, 1), mr(1, 0))], modsel=msel, bias_d=I["b_in"])
        emit_linear(C, [z_d[:, COL_C:COL_D]], lambda ci: I["cs128"][:, ci * 512:(ci + 1) * 512], PQ_o, R, 512,
                    [(0, 512, None), (512, 512, None)])
        emit_linear(C, [z_d[:, COL_KV:COL_KR]], lambda ci: I["w_ukv"][:, ci * 512:(ci + 1) * 512], kv_o, R, 256,
                    [(0, 512, None), (512, 512, None)], prologue="rms", g_d=I["d_kv_g"])
        emit_rope(C, z_d, COL_KR, 1, 32, kr_o, 0, I["rcos"], I["rsin"], RL)
        with S.scope():
            tmpk = S.sb("tmpk", [128, NCTX // 128, 32], F32)
            S.dma("sp", tmpk[:], z_d[RL:R, COL_KR:COL_G].rearrange("(a p) n -> p a n", p=128), writes=[tmpk])
            S.dma("sp", kr_o[RL:R, :].rearrange("(a p) n -> p a n", p=128), tmpk[:], reads=[tmpk])
            tz = S.sb("tz", [128, 2, 1024], F32)
            S.dma("sp", tz[:, 0, :], z_d[0:128, 1024:2048], writes=[tz])
            S.dma("sp", tz[:, 1, :], z_d[RL - 128:RL, 1024:2048], writes=[tz])
            S.dma("sp", zab_o.rearrange("(a p) n -> p a n", p=128), tz[:], reads=[tz])
        S.finish()
        print("P1 instructions:", S.n)
    return nc


def build_P2(shapes):
    RL, NCTX, R, D = TL, CTX_LEN, RC, D_MODEL
    nc, es = new_prog()
    I = {k: dram_in(nc, k, shp) for k, shp in shapes.items()}
    xn_o = dram_out(nc, "xn", [R, D])
    T = lambda n, s_: dram_tmp(nc, n, s_)
    mod_d = T("mod", [128, 6 * D])
    z_d = T("z", [R, D_IN])
    brA, brB = T("brA", [R, 512]), T("brB", [R, 512])
    brC = [T("brC%d" % p, [R, 512]) for p in range(4)]
    q_d = T("q", [R, 768])
    o_d = T("o", [R, MLA_HEADS, V_HEAD + 1])
    GP = [T("GP%d" % i, [R, D]) for i in range(4)]
    mix_d, x1_d = T("mix", [R, D]), T("x1", [R, D])
    lg_d = T("lg", [R, 36])
    CAP = 128 * int(math.ceil(3.6 * 2.0 * R / N_EXPERTS / 128))
    Xs_d = dram_tmp(nc, "Xs", [N_EXPERTS * CAP, D], BF16)
    Ys_d = dram_tmp(nc, "Ys", [N_EXPERTS * CAP, D], BF16)
    IDX_d = nc.dram_tensor("IDX", [R, 2], mybir.dt.int32).ap()
    GAB_d = T("GAB", [R, 2])
    f_d = T("f0", [R, D])
    msel = lambda t: 0 if t < RL // 128 else 1
    with es:
        C = Ctx(nc, es, I["ident"])
        S = C.S
        c512 = [(i * 512, 512, None) for i in range(4)]
        emit_linear(C, [I["cc"]], lambda ci: I["w_ada"][:, ci * 512:(ci + 1) * 512], mod_d, 128, D,
                    [(i * 512, 512, None) for i in range(24)], prologue="silu", bias_d=I["b_ada"])
        mr = lambda who, i: mod_d[who:who + 1, i * D:(i + 1) * D]
        x_cur = I["x_own"]
        ch = [(0, 512, None), (512, 512, None), (1024, 512, None), (1536, 512, AF.Sigmoid), (COL_D, 512, None)]
        ch += [(c0, 512, AF.Sigmoid) for c0 in range(COL_G, D_IN, 512)]
        modr1 = [(mr(0, 1), mr(0, 0)), (mr(1, 1), mr(1, 0))]
        emit_linear(C, [x_cur], lambda ci: I["w_in"][:, ch[ci][0]:ch[ci][0] + ch[ci][1]], z_d, R, D, ch, prologue="ln0mod",
                    modrows=modr1, modsel=msel, bias_d=I["b_in"])
        zg_d = [z_d[:, COL_G + i * D:COL_G + (i + 1) * D] for i in range(4)]
        emit_chunkgate(C, z_d, I["a_ln_g"], I["a_ln_b"], I["a_w_s"], I["a_b_s"], brA, R)
        emit_conv(C, z_d, I["b_w_dw"], I["b_b_dw"], I["b_ln_g"], I["b_ln_b"], brB, [(0, RL), (RL, NCTX)], halo=I["halo"])
        PQ = I["PQ_all"]
        K4 = NLAT_ALL // 2
        for p in range(4):
            wsl = PQ[(p % 2) * K4:(p % 2 + 1) * K4, (p // 2) * 512:(p // 2 + 1) * 512]
            emit_linear(C, [I["dft_own"][:, p * K4:(p + 1) * K4]], lambda ci, wsl=wsl: wsl, brC[p], RL, K4, [(0, 512, None)], RB=8)
            Kc = NCTX // 2
            wsl2 = PQ[NLAT_ALL + (p % 2) * Kc:NLAT_ALL + (p % 2 + 1) * Kc, (p // 2) * 512:(p // 2 + 1) * 512]
            emit_linear(C, [I["dftctx"][:, p * Kc:(p + 1) * Kc]], lambda ci, wsl2=wsl2: wsl2, brC[p][RL:R], NCTX, Kc, [(0, 512, None)])
        emit_linear(C, [z_d[:, COL_D:COL_KV]], lambda ci: I["d_w_uq"][:, ci * 512:ci * 512 + (512 if ci == 0 else 256)],
                    q_d, R, 512, [(0, 512, None), (512, 256, None)], prologue="rms", g_d=I["d_q_g"])
        emit_rope(C, q_d, QK_NOPE, MLA_HEADS, QK_NOPE + QK_ROPE, q_d, QK_NOPE, I["rcos"], I["rsin"], RL)
        RA = NLAT_ALL + NCTX
        emit_attention(C, q_d, I["kv_all"], I["kr_all"], o_d, (0, RL), [(0, RA)])
        emit_attention(C, q_d, I["kv_all"], I["kr_all"], o_d, (RL, NCTX), [(NLAT_ALL, NCTX)])
        for i in range(4):
            xs = [[brA], [brB], brC, [o_d]][i]
            emit_linear(C, xs, lambda ci, i=i: I["w_branch"][i][:, ci * 512:(ci + 1) * 512], GP[i], R, 512, c512,
                        prologue=("attn" if i == 3 else "none"), mul_d=zg_d[i])
        emit_linear(C, GP, lambda ci: I["w_o"][:, ci * 512:(ci + 1) * 512], mix_d, R, D, c512)
        emit_resln(C, x_cur, [mix_d], [mr(0, 2), mr(1, 2)], msel, I["ln1_g"], I["ln1_b"], x1_d, R)
        modr2 = [(mr(0, 4), mr(0, 3)), (mr(1, 4), mr(1, 3))]
        emit_linear(C, [x1_d], lambda ci: I["r_w"], lg_d, R, D, [(0, 36, None)], prologue="ln0mod",
                    modrows=modr2, modsel=msel, bias_d=I["r_b"])
        emit_route(C, lg_d, IDX_d, GAB_d, I["lstrict"], R, CAP)
        emit_dispatch(C, x1_d, modr2, msel, IDX_d, Xs_d, R, N_EXPERTS * CAP)
        emit_experts(C, Xs_d, Ys_d, I["e_w1"], I["e_w3"], I["e_w2"], CAP)
        emit_combine(C, Ys_d, IDX_d, GAB_d, f_d, R)
        emit_resln(C, x1_d, [f_d], [mr(0, 5), mr(1, 5)], msel, I["ln2_g"], I["ln2_b"], xn_o, R)
        S.finish()
        print("P2 instructions:", S.n, S.cnt)
    return nc


def _run(key, builder, in_maps):
    shapes = {k: list(v.shape) for k, v in in_maps[0].items()}
    if key not in _PROG_CACHE:
        _PROG_CACHE[key] = builder(shapes)
    res = run_bass_kernel_spmd(_PROG_CACHE[key], in_maps, core_ids=list(range(NCORES)))
    return res.results


def kernel(**inp):
    f32 = lambda a: np.ascontiguousarray(np.asarray(a, np.float32))
    x = f32(inp["x"])[0]
    ctx = f32(inp["ctx"])[0]
    cc = np.zeros((128, D_MODEL), np.float32)
    cc[0] = f32(inp["c"])[0]
    cc[1] = f32(inp["c_ctx"])
    cst = host_consts(SEQ, CTX_LEN)
    w_ukv = np.concatenate([f32(inp["d_w_uk"]), f32(inp["d_w_uv"])], -1)
    r_w = np.concatenate([f32(inp["r_w_grp"]), f32(inp["r_w_exp"])], -1)
    r_b = np.concatenate([f32(inp["r_b_grp"]), f32(inp["r_b_exp"])], -1)
    xo = [np.concatenate([x[j * TL:(j + 1) * TL], ctx], 0) for j in range(NCORES)]
    zero_t = np.zeros((128, 1024), np.float32)
    for l in range(DEPTH):
        W = lambda k: f32(inp[k][l])
        common1 = {"cc": cc, "w_ada": W("w_ada"), "b_ada": W("b_ada"), "w_in": W("w_in"), "b_in": W("b_in"),
                   "d_kv_g": W("d_kv_g"), "w_ukv": np.ascontiguousarray(w_ukv[l]), "cs128": cst["cs128"], "ident": cst["ident"]}
        ins1 = []
        for j in range(NCORES):
            d = dict(common1)
            d["x_own"] = xo[j]
            d["rcos"] = np.ascontiguousarray(cst["rcos"][j * TL:(j + 1) * TL])
            d["rsin"] = np.ascontiguousarray(cst["rsin"][j * TL:(j + 1) * TL])
            ins1.append(d)
        r1 = _run("P1", build_P1, ins1)
        cat = lambda k: np.concatenate([np.asarray(r1[j][k])[:TL] for j in range(NCORES)] + [np.asarray(r1[0][k])[TL:]], 0)
        PQ_all, kv_all, kr_all = cat("PQ_o"), cat("kv_o"), cat("kr_o")
        common2 = {"cc": cc, "PQ_all": PQ_all, "kv_all": kv_all, "kr_all": kr_all, "dftctx": cst["dftctx"],
                   "lstrict": cst["lstrict"], "ident": cst["ident"],
                   "w_ukv_unused": None}
        del common2["w_ukv_unused"]
        for k in ["w_ada", "b_ada", "w_in", "b_in", "a_ln_g", "a_ln_b", "a_w_s", "a_b_s", "b_w_dw", "b_b_dw", "b_ln_g", "b_ln_b",
                  "d_q_g", "d_w_uq", "w_branch", "w_o", "ln1_g", "ln1_b", "e_w1", "e_w3", "e_w2", "ln2_g", "ln2_b"]:
            common2[k] = W(k)
        common2["r_w"] = np.ascontiguousarray(r_w[l])
        common2["r_b"] = np.ascontiguousarray(r_b[l])
        ins2 = []
        for j in range(NCORES):
            d = dict(common2)
            d["x_own"] = xo[j]
            prev = np.asarray(r1[j - 1]["zab_o"])[128:256] if j > 0 else zero_t
            nxt = np.asarray(r1[j + 1]["zab_o"])[0:128] if j < NCORES - 1 else zero_t
            d["halo"] = np.ascontiguousarray(np.concatenate([prev, nxt], 0))
            d["dft_own"] = np.ascontiguousarray(cst["dftlat"][j * TL:(j + 1) * TL])
            d["rcos"] = np.ascontiguousarray(cst["rcos"][j * TL:(j + 1) * TL])
            d["rsin"] = np.ascontiguousarray(cst["rsin"][j * TL:(j + 1) * TL])
            ins2.append(d)
        r2 = _run("P2", build_P2, ins2)
        xo = [np.ascontiguousarray(np.asarray(r2[j]["xn"], np.float32)) for j in range(NCORES)]
    return np.concatenate([xo[j][:TL] for j in range(NCORES)], 0)[None].astype(np.float32)
```

```python
import contextlib
import math
import numpy as np
import ml_dtypes
import concourse.bass as bass
import concourse.mybir as mybir
from concourse.bass_utils import run_bass_kernel_spmd

F32 = mybir.dt.float32
BF16 = mybir.dt.bfloat16
AF = mybir.ActivationFunctionType
ALU = mybir.AluOpType
AX = mybir.AxisListType
NCORES = 8

D_MODEL = 2048
SEQ = 8192
DEPTH = 2
GRID_W = 64
CTX_LEN = 256
BRANCH_W = 512
CHUNK = 128
CONV_W = 31
QK_NOPE = 64
QK_ROPE = 32
V_HEAD = 64
MLA_HEADS = 8
Q_LORA = 512
KV_LORA = 256
ROPE_THETA = 10000.0
MLA_SCALE = (QK_NOPE + QK_ROPE) ** -0.5
N_GROUPS = 4
EXP_PER_GROUP = 8
N_EXPERTS = 32
D_EXPERT = 1024
COL_A = 0
COL_B = 1024
COL_C = 2048
COL_D = 2560
COL_KV = 3072
COL_KR = 3328
COL_G = 3360
D_IN = COL_G + 4 * D_MODEL
ALPHA = (2 * DEPTH) ** 0.25
LN_EPS = 1e-6
TL = SEQ // NCORES


class Buf:
    __slots__ = ("t", "lw", "rd", "name")

    def __init__(self, t, name=""):
        self.t = t
        self.lw = None
        self.rd = {}
        self.name = name

    def __getitem__(self, k):
        return self.t[k]


class Sched:
    RING = 12

    def __init__(self, nc, es):
        self.nc = nc
        self.es = es
        self.engs = {"pe": nc.tensor, "act": nc.scalar, "dve": nc.vector, "pool": nc.gpsimd, "sp": nc.sync}
        self.sem = {e: es.enter_context(nc.semaphore("s_" + e)) for e in ("pe", "act", "dve", "pool")}
        self.cnt = {e: 0 for e in self.sem}
        self.dsem = {q: [es.enter_context(nc.semaphore("d_%s%d" % (q, i))) for i in range(self.RING)]
                     for q in ("sp", "pool")}
        self.dcnt = {"sp": 0, "pool": 0}
        self.waited = {e: {} for e in self.engs}
        self.n = 0

    def sb(self, name, shape, dt):
        return Buf(self.es.enter_context(self.nc.sbuf_tensor("sb%d_" % getattr(self, "uid", 0) + name, list(shape), dt)), name)

    def ps(self, name, shape, dt=F32):
        return Buf(self.es.enter_context(self.nc.psum_tensor("ps_" + name, list(shape), dt)), name)

    def _semval(self, tok):
        if tok[0] == "c":
            return ("c", tok[1]), self.sem[tok[1]], tok[2]
        q, idx = tok[1], tok[2]
        return ("d", q, idx % self.RING), self.dsem[q][idx % self.RING], 16 * (idx // self.RING + 1)

    def _wait(self, eng, toks):
        need = {}
        for tok in toks:
            if tok is None:
                continue
            if tok[0] == "c" and tok[1] == eng and eng == "pe":
                continue
            key, sem, val = self._semval(tok)
            if need.get(key, (None, 0))[1] < val:
                need[key] = (sem, val)
        w = self.waited[eng]
        for key, (sem, val) in need.items():
            if w.get(key, 0) >= val:
                continue
            self.engs[eng].wait_ge(sem, val)
            w[key] = val

    def _deps(self, reads, writes):
        toks = []
        for r in reads:
            toks.append(r.lw)
        for wv in writes:
            toks.append(wv.lw)
            toks.extend(wv.rd.values())
        return toks

    def _commit(self, tok, reads, writes, rkey):
        for r in reads:
            r.rd[rkey] = tok
        for wv in writes:
            wv.lw = tok
            wv.rd = {}

    def op(self, eng, fn, reads=(), writes=()):
        self._wait(eng, self._deps(reads, writes))
        ins = fn()
        self.cnt[eng] += 1
        ins.then_inc(self.sem[eng], 1)
        tok = ("c", eng, self.cnt[eng])
        self._commit(tok, reads, writes, eng)
        self.n += 1
        return tok

    def dma(self, q, out, in_, reads=(), writes=(), **kw):
        idx = self.dcnt[q]
        toks = self._deps(reads, writes)
        if idx >= self.RING:
            toks.append(("d", q, idx - self.RING))
        self._wait(q, toks)
        ins = self.engs[q].dma_start(out=out, in_=in_, **kw)
        ins.then_inc(self.dsem[q][idx % self.RING], 16)
        self.dcnt[q] += 1
        tok = ("d", q, idx)
        self._commit(tok, reads, writes, "dma_" + q + str(idx % self.RING))
        self.n += 1
        return tok

    def idma(self, out, in_, idx_ap, scatter, reads=(), writes=()):
        q = "pool"
        idx = self.dcnt[q]
        toks = self._deps(reads, writes)
        if idx >= self.RING:
            toks.append(("d", q, idx - self.RING))
        self._wait(q, toks)
        off = bass.IndirectOffsetOnAxis(ap=idx_ap, axis=0)
        if scatter:
            ins = self.nc.gpsimd.indirect_dma_start(out=out, out_offset=off, in_=in_, in_offset=None)
        else:
            ins = self.nc.gpsimd.indirect_dma_start(out=out, out_offset=None, in_=in_, in_offset=off)
        ins.then_inc(self.dsem[q][idx % self.RING], 16)
        self.dcnt[q] += 1
        tok = ("d", q, idx)
        self._commit(tok, reads, writes, "dma_" + q + str(idx % self.RING))
        self.n += 1
        return tok

    def barrier(self):
        toks = []
        for q in ("sp", "pool"):
            for i in range(max(0, self.dcnt[q] - self.RING), self.dcnt[q]):
                toks.append(("d", q, i))
        for e in self.cnt:
            if self.cnt[e]:
                toks.append(("c", e, self.cnt[e]))
        for e in self.engs:
            self._wait(e, [t for t in toks if not (t[0] == "c" and t[1] == e)])

    @contextlib.contextmanager
    def scope(self):
        old = self.es
        with contextlib.ExitStack() as es2:
            self.es = es2
            self.uid = getattr(self, "uid", 0) + 1
            yield
            self.barrier()
        self.es = old

    def finish(self):
        toks = []
        for q in ("sp", "pool"):
            for i in range(max(0, self.dcnt[q] - self.RING), self.dcnt[q]):
                toks.append(("d", q, i))
        for e in self.cnt:
            if self.cnt[e]:
                toks.append(("c", e, self.cnt[e]))
        self._wait("sp", toks)


def new_prog():
    nc = bass.Bass("TRN2", target_bir_lowering=False)
    es = contextlib.ExitStack()
    return nc, es


def dram_in(nc, name, shape, dt=F32):
    return nc.dram_tensor(name, list(shape), dt, kind="ExternalInput").ap()


def dram_out(nc, name, shape, dt=F32):
    return nc.dram_tensor(name, list(shape), dt, kind="ExternalOutput").ap()


_PROG_CACHE = {}


def dram_tmp(nc, name, shape, dt=F32):
    return nc.dram_tensor(name, list(shape), dt).ap()


def rstd_of(S, m):
    nc = S.nc
    S.op("act", lambda: nc.scalar.activation(out=m[:, 3:4], in_=m[:, 3:4], func=AF.Sqrt), reads=[m], writes=[m])
    S.op("dve", lambda: nc.vector.reciprocal(out=m[:, 2:3], in_=m[:, 3:4]), reads=[m], writes=[m])


def ln_stats(S, x, K, tag, ring):
    nc = S.nc
    nch = (K + 511) // 512
    st, mv = ring["st"], ring["mv"]
    for c in range(nch):
        w = min(512, K - c * 512)
        S.op("dve", lambda c=c, w=w: nc.vector.bn_stats(out=st[:, c, :], in_=x[:, c * 512:c * 512 + w]),
             reads=[x], writes=[st])
    S.op("dve", lambda: nc.vector.bn_aggr(out=mv[:, 0:2], in_=st[:, 0:nch, :]), reads=[st], writes=[mv])
    S.op("dve", lambda: nc.vector.tensor_scalar(out=mv[:, 3:4], in0=mv[:, 1:2], scalar1=LN_EPS, scalar2=None,
                                                op0=ALU.add), reads=[mv], writes=[mv])
    rstd_of(S, mv)
    return mv


class Ctx:
    def __init__(self, nc, es, ident_d):
        self.nc = nc
        self.S = S = Sched(nc, es)
        self.pT = [S.ps("pT%d" % i, [128, 1024], BF16) for i in range(2)]
        self.acc = [S.ps("acc%d" % i, [128, 512], F32) for i in range(4)]
        self.pB = [S.ps("pB%d" % i, [128, 512], F32) for i in range(2)]
        self.identb = S.sb("identb", [128, 128], BF16)
        S.dma("pool", self.identb[:], ident_d, writes=[self.identb])
        self.identf = S.sb("identf", [128, 128], F32)
        S.dma("sp", self.identf[:], ident_d, writes=[self.identf])
        self.nT = 0
        self.nA = 0


def emit_linear(C, xs, w_ap, y_d, R, K, chunks, prologue="none", modrows=None, modsel=None, bias_d=None,
                mul_d=None, g_d=None, colscale=None, RB=16):
    S, nc = C.S, C.nc
    NT = R // 128
    KC = K // 128
    nsum = len(xs)
    with S.scope():
        hT = [S.sb("hT%d" % t, [128, KC, 128], BF16) for t in range(min(RB, NT))]
        xin = [S.sb("xin%d" % i, [128, K], F32) for i in range(2)]
        xaux = [S.sb("xaux%d" % i, [128, K], F32) for i in range(2)] if nsum > 1 else None
        hb = [S.sb("hb%d" % i, [128, K], BF16) for i in range(2)]
        st = [S.sb("st%d" % i, [128, 8, 6], F32) for i in range(2)]
        mv = [S.sb("mv%d" % i, [128, 4], F32) for i in range(2)]
        if prologue == "ln0mod":
            nsets = len(modrows)
            modA = S.sb("modA", [128, nsets, K], F32)
            modB = S.sb("modB", [128, nsets, K], F32)
            for s_, (ra, rb_) in enumerate(modrows):
                S.dma("sp", modA[:, s_, :], ra.partition_broadcast(128), writes=[modA])
                S.dma("sp", modB[:, s_, :], rb_.partition_broadcast(128), writes=[modB])
            S.op("dve", lambda: nc.vector.tensor_scalar(out=modA[:], in0=modA[:], scalar1=1.0, scalar2=None, op0=ALU.add),
                 reads=[modA], writes=[modA])
        if prologue == "rms":
            gb = S.sb("gb", [128, K], F32)
            S.dma("sp", gb[:], g_d.partition_broadcast(128), writes=[gb])
            junk = S.sb("junk", [128, K], F32)
        if prologue == "attn":
            oin = [S.sb("oin%d" % i, [128, MLA_HEADS, V_HEAD + 1], F32) for i in range(2)]
            rden = [S.sb("rden%d" % i, [128, MLA_HEADS, 1], F32) for i in range(2)]
        wb = [S.sb("wb%d" % i, [128, KC, 512], BF16) for i in range(2)]
        bb = [S.sb("bb%d" % i, [128, 512], F32) for i in range(2)] if bias_d is not None else None
        ot = [S.sb("ot%d" % i, [128, 512], F32) for i in range(3)]
        mt = [S.sb("mt%d" % i, [128, 512], F32) for i in range(3)] if mul_d is not None else None
        if colscale is not None:
            gcol_d, gfn = colscale
            ncol = gcol_d.shape[1]
            gt = [S.sb("gt%d" % t, [128, ncol], F32) for t in range(min(RB, NT))]
        wi = 0
        it = 0
        for t0 in range(0, NT, RB):
            tiles = list(range(t0, min(NT, t0 + RB)))
            for t in tiles:
                x = xin[t % 2]
                h = hb[t % 2]
                rows = slice(t * 128, (t + 1) * 128)
                if colscale is not None:
                    S.dma("sp", gt[t - t0][:], gcol_d[rows], writes=[gt[t - t0]])
                if prologue == "attn":
                    o = oin[t % 2]
                    rd_ = rden[t % 2]
                    S.dma("sp", o[:], xs[0][rows], writes=[o])
                    S.op("dve", lambda o=o, rd_=rd_: nc.vector.reciprocal(out=rd_[:], in_=o[:, :, V_HEAD:V_HEAD + 1]),
                         reads=[o], writes=[rd_])
                    for hh in range(MLA_HEADS):
                        S.op("dve", lambda o=o, rd_=rd_, h=h, hh=hh: nc.vector.tensor_scalar(
                            out=h[:, hh * V_HEAD:(hh + 1) * V_HEAD], in0=o[:, hh, 0:V_HEAD], scalar1=rd_[:, hh, :],
                            scalar2=None, op0=ALU.mult), reads=[o, rd_], writes=[h])
                else:
                    S.dma("sp", x[:], xs[0][rows], writes=[x])
                    for i in range(1, nsum):
                        xa = xaux[i % 2]
                        S.dma("sp", xa[:], xs[i][rows], writes=[xa])
                        S.op("dve", lambda x=x, xa=xa: nc.vector.tensor_tensor(out=x[:], in0=x[:], in1=xa[:], op=ALU.add),
                             reads=[x, xa], writes=[x])
                    if prologue == "none":
                        S.op("act", lambda x=x, h=h: nc.scalar.copy(out=h[:], in_=x[:]), reads=[x], writes=[h])
                    elif prologue == "silu":
                        S.op("act", lambda x=x, h=h: nc.scalar.activation(out=h[:], in_=x[:], func=AF.Silu),
                             reads=[x], writes=[h])
                    elif prologue == "ln0mod":
                        m = ln_stats(S, x, K, "p", {"st": st[t % 2], "mv": mv[t % 2]})
                        ms = modsel(t)
                        S.op("dve", lambda x=x, m=m: nc.vector.tensor_scalar(
                            out=x[:], in0=x[:], scalar1=m[:, 0:1], scalar2=m[:, 2:3], op0=ALU.subtract, op1=ALU.mult),
                            reads=[x, m], writes=[x])
                        S.op("dve", lambda x=x, ms=ms: nc.vector.tensor_tensor(out=x[:], in0=x[:], in1=modA[:, ms, :],
                                                                               op=ALU.mult), reads=[x, modA], writes=[x])
                        S.op("dve", lambda x=x, h=h, ms=ms: nc.vector.tensor_tensor(
                            out=h[:], in0=x[:], in1=modB[:, ms, :], op=ALU.add), reads=[x, modB], writes=[h])
                    elif prologue == "rms":
                        m = mv[t % 2]
                        S.op("act", lambda x=x, m=m: nc.scalar.activation(out=junk[:], in_=x[:], func=AF.Square,
                                                                          accum_out=m[:, 0:1]),
                             reads=[x], writes=[junk, m])
                        S.op("dve", lambda m=m: nc.vector.tensor_scalar(out=m[:, 3:4], in0=m[:, 0:1], scalar1=1.0 / K,
                                                                        scalar2=LN_EPS, op0=ALU.mult, op1=ALU.add),
                             reads=[m], writes=[m])
                        rstd_of(S, m)
                        S.op("dve", lambda x=x, h=h, m=m: nc.vector.scalar_tensor_tensor(
                            out=h[:], in0=x[:], scalar=m[:, 2:3], in1=gb[:], op0=ALU.mult, op1=ALU.mult),
                            reads=[x, m, gb], writes=[h])
                for k0 in range(0, KC, 8):
                    kn = min(8, KC - k0)
                    p = C.pT[C.nT % 2]
                    C.nT += 1
                    for j in range(kn):
                        S.op("pe", lambda p=p, h=h, j=j, k0=k0: nc.tensor.transpose(
                            out=p[:, j * 128:(j + 1) * 128], in_=h[:, (k0 + j) * 128:(k0 + j + 1) * 128],
                            identity=C.identb[:]), reads=[h, C.identb], writes=[p])
                    S.op("act", lambda p=p, t=t, k0=k0, kn=kn: nc.scalar.copy(
                        out=hT[t - t0][:, k0:k0 + kn, :], in_=p[:, 0:kn * 128].rearrange("p (a b) -> p a b", b=128)),
                        reads=[p], writes=[hT[t - t0]])
            for ci, (c0, nw, act) in enumerate(chunks):
                w = wb[wi % 2]
                wi += 1
                wv = w_ap(ci).rearrange("(kc p) n -> p kc n", p=128)
                for k0 in range(0, KC, 16):
                    k1 = min(KC, k0 + 16)
                    S.dma("pool", w[:, k0:k1, 0:nw], wv[:, k0:k1, :], writes=[w])
                if bias_d is not None:
                    b = bb[wi % 2]
                    S.dma("sp", b[:, 0:nw], bias_d[c0:c0 + nw].partition_broadcast(128), writes=[b])
                for t in tiles:
                    a = C.acc[C.nA % 4]
                    C.nA += 1
                    o = ot[it % 3]
                    for kc in range(KC):
                        S.op("pe", lambda a=a, w=w, t=t, kc=kc, nw=nw: nc.tensor.matmul(
                            out=a[:, 0:nw], lhsT=hT[t - t0][:, kc, :], rhs=w[:, kc, 0:nw], start=(kc == 0),
                            stop=(kc == KC - 1)), reads=[hT[t - t0], w], writes=[a])
                    if bias_d is not None:
                        S.op("dve", lambda a=a, o=o, b=b, nw=nw: nc.vector.tensor_tensor(
                            out=o[:, 0:nw], in0=a[:, 0:nw], in1=b[:, 0:nw], op=ALU.add), reads=[a, b], writes=[o])
                        if act is not None:
                            S.op("act", lambda o=o, nw=nw, act=act: nc.scalar.activation(
                                out=o[:, 0:nw], in_=o[:, 0:nw], func=act), reads=[o], writes=[o])
                    else:
                        S.op("act", lambda a=a, o=o, nw=nw, act=act: nc.scalar.activation(
                            out=o[:, 0:nw], in_=a[:, 0:nw], func=(act if act is not None else AF.Copy)),
                            reads=[a], writes=[o])
                    if colscale is not None:
                        e_ = gfn(ci)
                        S.op("dve", lambda o=o, t=t, e_=e_, nw=nw: nc.vector.tensor_scalar(
                            out=o[:, 0:nw], in0=o[:, 0:nw], scalar1=gt[t - t0][:, e_:e_ + 1], scalar2=None, op0=ALU.mult),
                            reads=[o, gt[t - t0]], writes=[o])
                    if mul_d is not None:
                        m_ = mt[it % 3]
                        S.dma("sp", m_[:, 0:nw], mul_d[t * 128:(t + 1) * 128, c0:c0 + nw], writes=[m_])
                        S.op("dve", lambda o=o, m_=m_, nw=nw: nc.vector.tensor_tensor(
                            out=o[:, 0:nw], in0=o[:, 0:nw], in1=m_[:, 0:nw], op=ALU.mult), reads=[o, m_], writes=[o])
                    S.dma("sp", y_d[t * 128:(t + 1) * 128, c0:c0 + nw], o[:, 0:nw], reads=[o])
                    it += 1


def emit_resln(C, x_d, fs, m_rows, msel, g_row, b_row, y_d, R):
    S, nc = C.S, C.nc
    D = D_MODEL
    with S.scope():
        mm = S.sb("mm", [128, len(m_rows), D], F32)
        for i, r in enumerate(m_rows):
            S.dma("sp", mm[:, i, :], r.partition_broadcast(128), writes=[mm])
        gb = S.sb("g", [128, D], F32)
        bb = S.sb("b", [128, D], F32)
        S.dma("sp", gb[:], g_row.partition_broadcast(128), writes=[gb])
        S.dma("sp", bb[:], b_row.partition_broadcast(128), writes=[bb])
        xt = [S.sb("x%d" % i, [128, D], F32) for i in range(2)]
        ft = [S.sb("f%d" % i, [128, D], F32) for i in range(2)]
        fa = [S.sb("fa%d" % i, [128, D], F32) for i in range(2)]
        st = [S.sb("st%d" % i, [128, 8, 6], F32) for i in range(2)]
        mv = [S.sb("mv%d" % i, [128, 4], F32) for i in range(2)]
        for t in range(R // 128):
            rows = slice(t * 128, (t + 1) * 128)
            x, f = xt[t % 2], ft[t % 2]
            S.dma("sp", x[:], x_d[rows], writes=[x])
            S.dma("sp", f[:], fs[0][rows], writes=[f])
            for i in range(1, len(fs)):
                a = fa[i % 2]
                S.dma("sp", a[:], fs[i][rows], writes=[a])
                S.op("dve", lambda f=f, a=a: nc.vector.tensor_tensor(out=f[:], in0=f[:], in1=a[:], op=ALU.add),
                     reads=[f, a], writes=[f])
            ms = msel(t)
            S.op("dve", lambda f=f, ms=ms: nc.vector.tensor_tensor(out=f[:], in0=f[:], in1=mm[:, ms, :], op=ALU.mult),
                 reads=[f, mm], writes=[f])
            S.op("dve", lambda x=x, f=f: nc.vector.scalar_tensor_tensor(out=x[:], in0=x[:], scalar=ALPHA, in1=f[:],
                                                                        op0=ALU.mult, op1=ALU.add),
                 reads=[x, f], writes=[x])
            m = ln_stats(S, x, D, "r", {"st": st[t % 2], "mv": mv[t % 2]})
            S.op("dve", lambda x=x, m=m: nc.vector.tensor_scalar(out=x[:], in0=x[:], scalar1=m[:, 0:1], scalar2=m[:, 2:3],
                                                                 op0=ALU.subtract, op1=ALU.mult), reads=[x, m], writes=[x])
            S.op("dve", lambda x=x: nc.vector.tensor_tensor(out=x[:], in0=x[:], in1=gb[:], op=ALU.mult),
                 reads=[x, gb], writes=[x])
            S.op("dve", lambda x=x: nc.vector.tensor_tensor(out=x[:], in0=x[:], in1=bb[:], op=ALU.add),
                 reads=[x, bb], writes=[x])
            S.dma("sp", y_d[rows], x[:], reads=[x])


def gelu_tanh(S, x, t, W):
    nc = S.nc
    S.op("dve", lambda: nc.vector.tensor_tensor(out=t[:, 0:W], in0=x[:, 0:W], in1=x[:, 0:W], op=ALU.mult), reads=[x], writes=[t])
    S.op("dve", lambda: nc.vector.tensor_scalar(out=t[:, 0:W], in0=t[:, 0:W], scalar1=0.044715, scalar2=1.0, op0=ALU.mult,
                                                op1=ALU.add), reads=[t], writes=[t])
    S.op("dve", lambda: nc.vector.tensor_tensor(out=t[:, 0:W], in0=t[:, 0:W], in1=x[:, 0:W], op=ALU.mult), reads=[t, x], writes=[t])
    S.op("act", lambda: nc.scalar.activation(out=t[:, 0:W], in_=t[:, 0:W], func=AF.Sigmoid, scale=1.5957691216057308),
         reads=[t], writes=[t])
    S.op("dve", lambda: nc.vector.tensor_tensor(out=x[:, 0:W], in0=x[:, 0:W], in1=t[:, 0:W], op=ALU.mult), reads=[t, x], writes=[x])


def ln_rows(S, x, W, st, mv, gb, bb, out=None):
    nc = S.nc
    out = out if out is not None else x
    m = ln_stats(S, x, W, "l", {"st": st, "mv": mv})
    S.op("dve", lambda: nc.vector.tensor_scalar(out=x[:, 0:W], in0=x[:, 0:W], scalar1=m[:, 0:1], scalar2=m[:, 2:3],
                                                op0=ALU.subtract, op1=ALU.mult), reads=[x, m], writes=[x])
    S.op("dve", lambda: nc.vector.tensor_tensor(out=x[:, 0:W], in0=x[:, 0:W], in1=gb[:, 0:W], op=ALU.mult), reads=[x, gb], writes=[x])
    S.op("dve", lambda: nc.vector.tensor_tensor(out=out[:, 0:W], in0=x[:, 0:W], in1=bb[:, 0:W], op=ALU.add),
         reads=[x, bb], writes=[out])


def emit_chunkgate(C, z_d, lng, lnb, ws_d, bs_d, y_d, R):
    S, nc = C.S, C.nc
    with S.scope():
        gb = S.sb("g", [128, 512], F32)
        bb = S.sb("b", [128, 512], F32)
        S.dma("sp", gb[:], lng.partition_broadcast(128), writes=[gb])
        S.dma("sp", bb[:], lnb.partition_broadcast(128), writes=[bb])
        wraw = S.sb("wraw", [128, 4, 128], BF16)
        wT = S.sb("wT", [128, 4, 128], BF16)
        S.dma("pool", wraw[:], ws_d.rearrange("g p q -> p g q"), writes=[wraw])
        p = C.pT[C.nT % 2]
        C.nT += 1
        for g in range(4):
            S.op("pe", lambda g=g: nc.tensor.transpose(out=p[:, g * 128:(g + 1) * 128], in_=wraw[:, g, :], identity=C.identb[:]),
                 reads=[wraw, C.identb], writes=[p])
        S.op("act", lambda: nc.scalar.copy(out=wT[:], in_=p[:, 0:512].rearrange("p (a b) -> p a b", b=128)), reads=[p], writes=[wT])
        bs = S.sb("bs", [128, 4], F32)
        S.dma("sp", bs[:], bs_d.rearrange("g p -> p g"), writes=[bs], allow_slow_non_contiguous=True)
        uv = [S.sb("uv%d" % i, [128, 1024], F32) for i in range(2)]
        tt = [S.sb("tt%d" % i, [128, 1024], F32) for i in range(2)]
        vb = [S.sb("vb%d" % i, [128, 512], BF16) for i in range(2)]
        ot = [S.sb("ot%d" % i, [128, 512], F32) for i in range(2)]
        st = [S.sb("st%d" % i, [128, 8, 6], F32) for i in range(2)]
        mv = [S.sb("mv%d" % i, [128, 4], F32) for i in range(2)]
        for t in range(R // 128):
            rows = slice(t * 128, (t + 1) * 128)
            x, tmp, v, o = uv[t % 2], tt[t % 2], vb[t % 2], ot[t % 2]
            S.dma("sp", x[:], z_d[rows, 0:1024], writes=[x])
            gelu_tanh(S, x, tmp, 1024)
            m = ln_stats(S, Buf(x.t[:, 512:1024]) if False else x, 512, "c", {"st": st[t % 2], "mv": mv[t % 2]}) if False else None
            nchs = 1
            S.op("dve", lambda x=x, t=t: nc.vector.bn_stats(out=st[t % 2][:, 0, :], in_=x[:, 512:1024]), reads=[x], writes=[st[t % 2]])
            mvt = mv[t % 2]
            S.op("dve", lambda t=t, mvt=mvt: nc.vector.bn_aggr(out=mvt[:, 0:2], in_=st[t % 2][:, 0:1, :]), reads=[st[t % 2]], writes=[mvt])
            S.op("dve", lambda mvt=mvt: nc.vector.tensor_scalar(out=mvt[:, 3:4], in0=mvt[:, 1:2], scalar1=LN_EPS, scalar2=None,
                                                                op0=ALU.add), reads=[mvt], writes=[mvt])
            rstd_of(S, mvt)
            S.op("dve", lambda x=x, mvt=mvt: nc.vector.tensor_scalar(out=x[:, 512:1024], in0=x[:, 512:1024], scalar1=mvt[:, 0:1],
                                                                     scalar2=mvt[:, 2:3], op0=ALU.subtract, op1=ALU.mult),
                 reads=[x, mvt], writes=[x])
            S.op("dve", lambda x=x: nc.vector.tensor_tensor(out=x[:, 512:1024], in0=x[:, 512:1024], in1=gb[:], op=ALU.mult),
                 reads=[x, gb], writes=[x])
            S.op("dve", lambda x=x, v=v: nc.vector.tensor_tensor(out=v[:], in0=x[:, 512:1024], in1=bb[:], op=ALU.add),
                 reads=[x, bb], writes=[v])
            a = C.acc[C.nA % 4]
            C.nA += 1
            for g in range(4):
                S.op("pe", lambda a=a, v=v, g=g: nc.tensor.matmul(out=a[:, g * 128:(g + 1) * 128], lhsT=wT[:, g, :],
                                                                  rhs=v[:, g * 128:(g + 1) * 128], start=True, stop=True),
                     reads=[wT, v], writes=[a])
            for g in range(4):
                S.op("dve", lambda a=a, o=o, x=x, g=g: nc.vector.scalar_tensor_tensor(
                    out=o[:, g * 128:(g + 1) * 128], in0=a[:, g * 128:(g + 1) * 128], scalar=bs[:, g:g + 1],
                    in1=x[:, g * 128:(g + 1) * 128], op0=ALU.add, op1=ALU.mult), reads=[a, bs, x], writes=[o])
            S.dma("sp", y_d[rows], o[:], reads=[o])


def emit_conv(C, z_d, wdw_d, bdw_d, lng, lnb, y_d, segs, halo=None):
    S, nc = C.S, C.nc
    with S.scope():
        gb = S.sb("g", [128, 512], F32)
        bb = S.sb("b", [128, 512], F32)
        S.dma("sp", gb[:], lng.partition_broadcast(128), writes=[gb])
        S.dma("sp", bb[:], lnb.partition_broadcast(128), writes=[bb])
        wd = S.sb("wd", [128, 4, CONV_W], F32)
        bd = S.sb("bd", [128, 4], F32)
        for c in range(4):
            S.dma("sp", wd[:, c, :], wdw_d[:, c * 128:(c + 1) * 128].rearrange("k p -> p k"), writes=[wd],
                  allow_slow_non_contiguous=True)
            S.dma("sp", bd[:, c:c + 1], bdw_d[c * 128:(c + 1) * 128].rearrange("(p o) -> p o", o=1), writes=[bd],
                  allow_slow_non_contiguous=True)
        yT = S.sb("yT", [128, 4, 1280], F32)
        cacc = S.sb("cacc", [128, 4, 1024], F32)
        ab = [S.sb("ab%d" % i, [128, 1024], F32) for i in range(2)]
        yy = [S.sb("yy%d" % i, [128, 512], F32) for i in range(2)]
        ot = [S.sb("ot%d" % i, [128, 512], F32) for i in range(2)]
        st = [S.sb("st%d" % i, [128, 8, 6], F32) for i in range(2)]
        mv = [S.sb("mv%d" % i, [128, 4], F32) for i in range(2)]
        it = 0
        for si, (r0, nr) in enumerate(segs):
            for b0 in range(0, nr, 1024):
                bl = min(1024, nr - b0)
                nti = bl // 128
                S.op("pool", lambda: nc.gpsimd.memset(yT[:], 0.0), writes=[yT])
                for j in range(-1, nti + 1):
                    tr = b0 + j * 128
                    src = None
                    if 0 <= tr < nr:
                        src = z_d[r0 + tr:r0 + tr + 128, 1024:2048]
                    elif halo is not None and si == 0:
                        src = halo[0:128, :] if tr < 0 else halo[128:256, :]
                    if src is None:
                        continue
                    x, y = ab[it % 2], yy[it % 2]
                    it += 1
                    S.dma("sp", x[:], src, writes=[x])
                    S.op("dve", lambda x=x, y=y: nc.vector.tensor_tensor(out=y[:], in0=x[:, 0:512], in1=x[:, 512:1024], op=ALU.mult),
                         reads=[x], writes=[y])
                    a = C.acc[C.nA % 4]
                    C.nA += 1
                    for c in range(4):
                        S.op("pe", lambda a=a, y=y, c=c: nc.tensor.transpose(out=a[:, c * 128:(c + 1) * 128],
                                                                             in_=y[:, c * 128:(c + 1) * 128], identity=C.identf[:]),
                             reads=[y, C.identf], writes=[a])
                    S.op("act", lambda a=a, j=j: nc.scalar.copy(out=yT[:, :, (j + 1) * 128:(j + 2) * 128],
                                                                in_=a[:, 0:512].rearrange("p (c t) -> p c t", t=128)),
                         reads=[a], writes=[yT])
                for c in range(4):
                    for k in range(CONV_W):
                        src = yT[:, c, 113 + k:113 + k + bl]
                        if k == 0:
                            S.op("dve", lambda c=c, src=src: nc.vector.tensor_scalar(
                                out=cacc[:, c, 0:bl], in0=src, scalar1=wd[:, c, 0:1], scalar2=bd[:, c:c + 1], op0=ALU.mult,
                                op1=ALU.add), reads=[yT, wd, bd], writes=[cacc])
                        else:
                            S.op("dve", lambda c=c, k=k, src=src: nc.vector.scalar_tensor_tensor(
                                out=cacc[:, c, 0:bl], in0=src, scalar=wd[:, c, k:k + 1], in1=cacc[:, c, 0:bl], op0=ALU.mult,
                                op1=ALU.add), reads=[yT, wd, cacc], writes=[cacc])
                for j in range(nti):
                    a = C.acc[C.nA % 4]
                    C.nA += 1
                    o = ot[j % 2]
                    for c in range(4):
                        S.op("pe", lambda a=a, c=c, j=j: nc.tensor.transpose(out=a[:, c * 128:(c + 1) * 128],
                                                                             in_=cacc[:, c, j * 128:(j + 1) * 128], identity=C.identf[:]),
                             reads=[cacc, C.identf], writes=[a])
                    S.op("act", lambda a=a, o=o: nc.scalar.copy(out=o[:], in_=a[:]), reads=[a], writes=[o])
                    ln_rows(S, o, 512, st[j % 2], mv[j % 2], gb, bb)
                    S.op("act", lambda o=o: nc.scalar.activation(out=o[:], in_=o[:], func=AF.Silu), reads=[o], writes=[o])
                    S.dma("sp", y_d[r0 + b0 + j * 128:r0 + b0 + (j + 1) * 128], o[:], reads=[o])


def emit_rope(C, src_d, c0, H, stride, dst_d, d0, cos_d, sin_d, nrows):
    S, nc = C.S, C.nc
    W = (H - 1) * stride + 32
    with S.scope():
        xt = [S.sb("x%d" % i, [128, W], F32) for i in range(2)]
        cs = [S.sb("cs%d" % i, [128, 32], F32) for i in range(2)]
        sn = [S.sb("sn%d" % i, [128, 32], F32) for i in range(2)]
        t1 = [S.sb("t1%d" % i, [128, 32], F32) for i in range(2)]
        for t in range(nrows // 128):
            rows = slice(t * 128, (t + 1) * 128)
            x, c_, s_, u = xt[t % 2], cs[t % 2], sn[t % 2], t1[t % 2]
            S.dma("sp", x[:], src_d[rows, c0:c0 + W], writes=[x])
            S.dma("sp", c_[:], cos_d[rows], writes=[c_])
            S.dma("sp", s_[:], sin_d[rows], writes=[s_])
            for h in range(H):
                o = h * stride
                for (a0, b0_) in ((0, 8), (8, 0), (16, 24), (24, 16)):
                    S.op("dve", lambda x=x, s_=s_, u=u, o=o, a0=a0, b0_=b0_: nc.vector.tensor_tensor(
                        out=u[:, a0:a0 + 8], in0=x[:, o + b0_:o + b0_ + 8], in1=s_[:, a0:a0 + 8], op=ALU.mult),
                        reads=[x, s_], writes=[u])
                S.op("dve", lambda x=x, c_=c_, o=o: nc.vector.tensor_tensor(out=x[:, o:o + 32], in0=x[:, o:o + 32], in1=c_[:], op=ALU.mult),
                     reads=[x, c_], writes=[x])
                S.op("dve", lambda x=x, u=u, o=o: nc.vector.tensor_tensor(out=x[:, o:o + 32], in0=x[:, o:o + 32], in1=u[:], op=ALU.add),
                     reads=[x, u], writes=[x])
            S.dma("sp", dst_d[rows, d0:d0 + W], x[:], reads=[x])


def emit_attention(C, q_d, kv_d, kr_d, o_d, q_rows, k_rows):
    S, nc = C.S, C.nc
    ktiles = [r0 + i * 128 for (r0, n) in k_rows for i in range(n // 128)]
    NKT = len(ktiles)
    q0, nq = q_rows
    NQT = nq // 128
    DK = QK_NOPE + QK_ROPE
    with S.scope():
        KT = S.sb("KT", [DK, NKT * 128], BF16)
        QT = S.sb("QT", [DK, NQT * 128], BF16)
        Vt = S.sb("Vt", [128, NKT, V_HEAD + 1], BF16)
        kin = [S.sb("kin%d" % i, [128, DK], F32) for i in range(4)]
        kb = [S.sb("kb%d" % i, [128, DK], BF16) for i in range(4)]
        KTb = [Buf(KT.t) for _ in range(NKT)]
        QTb = [Buf(QT.t) for _ in range(NQT)]
        Vtb = [Buf(Vt.t) for _ in range(NKT)]
        pt = [S.sb("pt%d" % i, [128, 512], BF16) for i in range(3)]
        of = [S.sb("of%d" % i, [V_HEAD + 1, 512], F32) for i in range(2)]
        oo = [S.sb("oo%d" % i, [128, 4, V_HEAD + 1], F32) for i in range(2)]
        ones = S.sb("ones", [128, NKT, 1], F32)
        S.op("pool", lambda: nc.gpsimd.memset(ones[:], 1.0), writes=[ones])
        S.op("act", lambda: nc.scalar.copy(out=Vt[:, :, V_HEAD:V_HEAD + 1], in_=ones[:]), reads=[ones], writes=Vtb)
        n_ = 0
        for h in range(MLA_HEADS):
            for i, r in enumerate(ktiles):
                x, xb = kin[n_ % 4], kb[n_ % 4]
                n_ += 1
                S.dma("sp", x[:, 0:QK_NOPE], kv_d[r:r + 128, h * QK_NOPE:(h + 1) * QK_NOPE], writes=[x])
                S.dma("sp", x[:, QK_NOPE:DK], kr_d[r:r + 128, :], writes=[x])
                S.dma("pool", Vt[:, i, 0:V_HEAD], kv_d[r:r + 128, 512 + h * V_HEAD:512 + (h + 1) * V_HEAD], writes=[Vtb[i]])
                S.op("act", lambda x=x, xb=xb: nc.scalar.copy(out=xb[:], in_=x[:]), reads=[x], writes=[xb])
                p = C.pT[C.nT % 2]
                C.nT += 1
                S.op("pe", lambda p=p, xb=xb: nc.tensor.transpose(out=p[0:DK, 0:128], in_=xb[:], identity=C.identb[:]),
                     reads=[xb, C.identb], writes=[p])
                S.op("act", lambda p=p, i=i: nc.scalar.copy(out=KT[:, i * 128:(i + 1) * 128], in_=p[0:DK, 0:128]),
                     reads=[p], writes=[KTb[i]])
            for i in range(NQT):
                r = q0 + i * 128
                x, xb = kin[n_ % 4], kb[n_ % 4]
                n_ += 1
                S.dma("sp", x[:], q_d[r:r + 128, h * DK:(h + 1) * DK], writes=[x])
                S.op("act", lambda x=x, xb=xb: nc.scalar.copy(out=xb[:], in_=x[:]), reads=[x], writes=[xb])
                p = C.pT[C.nT % 2]
                C.nT += 1
                S.op("pe", lambda p=p, xb=xb: nc.tensor.transpose(out=p[0:DK, 0:128], in_=xb[:], identity=C.identb[:]),
                     reads=[xb, C.identb], writes=[p])
                S.op("act", lambda p=p, i=i: nc.scalar.copy(out=QT[:, i * 128:(i + 1) * 128], in_=p[0:DK, 0:128]),
                     reads=[p], writes=[QTb[i]])
            for qb in range(0, NQT * 128, 512):
                qw = min(512, NQT * 128 - qb)
                ao = C.pB[(qb // 512) % 2]
                for i in range(NKT):
                    a = C.acc[C.nA % 4]
                    C.nA += 1
                    pp = pt[i % 3]
                    S.op("pe", lambda a=a, i=i, qb=qb, qw=qw: nc.tensor.matmul(out=a[:, 0:qw], lhsT=KT[:, i * 128:(i + 1) * 128],
                                                                              rhs=QT[:, qb:qb + qw], start=True, stop=True),
                         reads=[KTb[i]] + QTb[qb // 128:(qb + qw) // 128], writes=[a])
                    S.op("act", lambda a=a, pp=pp, qw=qw: nc.scalar.activation(out=pp[:, 0:qw], in_=a[:, 0:qw], func=AF.Exp,
                                                                              scale=MLA_SCALE), reads=[a], writes=[pp])
                    S.op("pe", lambda ao=ao, pp=pp, i=i, qw=qw: nc.tensor.matmul(out=ao[0:V_HEAD + 1, 0:qw], lhsT=Vt[:, i, :],
                                                                                rhs=pp[:, 0:qw], start=(i == 0), stop=(i == NKT - 1)),
                         reads=[Vtb[i], pp], writes=[ao])
                o1 = of[(qb // 512) % 2]
                o2 = oo[(qb // 512) % 2]
                S.op("act", lambda ao=ao, o1=o1, qw=qw: nc.scalar.copy(out=o1[:, 0:qw], in_=ao[0:V_HEAD + 1, 0:qw]), reads=[ao], writes=[o1])
                a = C.acc[C.nA % 4]
                C.nA += 1
                nsub = qw // 128
                for j in range(nsub):
                    S.op("pe", lambda a=a, o1=o1, j=j: nc.tensor.transpose(out=a[:, j * 65:(j + 1) * 65], in_=o1[:, j * 128:(j + 1) * 128],
                                                                           identity=C.identf[0:V_HEAD + 1, 0:V_HEAD + 1]),
                         reads=[o1, C.identf], writes=[a])
                S.op("dve", lambda a=a, o2=o2, nsub=nsub: nc.vector.tensor_copy(
                    out=o2[:, 0:nsub, :], in_=a[:, 0:nsub * 65].rearrange("p (j d) -> p j d", d=65)), reads=[a], writes=[o2])
                for j in range(nsub):
                    r = q0 + qb + j * 128
                    S.dma("sp", o_d[r:r + 128, h, :], o2[:, j, :], reads=[o2])


def emit_route(C, lg_d, IDX_d, GAB_d, lstrict_d, R, CAP):
    S, nc = C.S, C.nc
    I32 = mybir.dt.int32
    with S.scope():
        L = [S.sb("L%d" % i, [128, 36], F32) for i in range(2)]
        W = [S.sb("W%d" % i, [128, 256], F32) for i in range(2)]
        Mi = [S.sb("Mi%d" % i, [128, 32], BF16) for i in range(2)]
        II = [S.sb("II%d" % i, [128, 2], I32) for i in range(2)]
        Macc = S.sb("Macc", [128, 32], BF16)
        lst = S.sb("lst", [128, 128], BF16)
        onesb = S.sb("onesb", [128, 128], BF16)
        ebase = S.sb("ebase", [128, 32], F32)
        S.dma("pool", lst[:], lstrict_d, writes=[lst])
        S.op("pool", lambda: nc.gpsimd.memset(onesb[:], 1.0), writes=[onesb])
        S.op("pool", lambda: nc.gpsimd.memset(Macc[:], 0.0), writes=[Macc])
        for e in range(N_EXPERTS):
            S.op("pool", lambda e=e: nc.gpsimd.memset(ebase[:, e:e + 1], float(e * CAP)), writes=[ebase])
        for t in range(R // 128):
            rows = slice(t * 128, (t + 1) * 128)
            l, w, mi, ii = L[t % 2], W[t % 2], Mi[t % 2], II[t % 2]
            S.dma("sp", l[:], lg_d[rows], writes=[l])
            S.op("dve", lambda l=l, w=w: nc.vector.tensor_reduce(out=w[:, 0:1], in_=l[:, 0:4], axis=AX.X, op=ALU.max), reads=[l], writes=[w])
            S.op("dve", lambda l=l, w=w: nc.vector.tensor_scalar(out=w[:, 4:8], in0=l[:, 0:4], scalar1=w[:, 0:1], scalar2=None, op0=ALU.is_ge),
                 reads=[l, w], writes=[w])
            S.op("dve", lambda w=w: nc.vector.tensor_scalar(out=w[:, 1:2], in0=w[:, 0:1], scalar1=-1.0, scalar2=None, op0=ALU.mult),
                 reads=[w], writes=[w])
            S.op("act", lambda l=l, w=w: nc.scalar.activation(out=w[:, 44:48], in_=l[:, 0:4], func=AF.Exp, bias=w[:, 1:2], scale=1.0),
                 reads=[l, w], writes=[w])
            S.op("dve", lambda w=w: nc.vector.tensor_reduce(out=w[:, 2:3], in_=w[:, 44:48], axis=AX.X, op=ALU.add), reads=[w], writes=[w])
            S.op("dve", lambda w=w: nc.vector.reciprocal(out=w[:, 3:4], in_=w[:, 2:3]), reads=[w], writes=[w])
            S.op("dve", lambda l=l, w=w: nc.vector.tensor_scalar(out=w[:, 8:16], in0=l[:, 4:12], scalar1=w[:, 4:5], scalar2=None, op0=ALU.mult),
                 reads=[l, w], writes=[w])
            for gi in range(1, 4):
                S.op("dve", lambda l=l, w=w, gi=gi: nc.vector.scalar_tensor_tensor(
                    out=w[:, 8:16], in0=l[:, 4 + gi * 8:12 + gi * 8], scalar=w[:, 4 + gi:5 + gi], in1=w[:, 8:16], op0=ALU.mult, op1=ALU.add),
                    reads=[l, w], writes=[w])
            S.op("dve", lambda w=w: nc.vector.max(out=w[:, 16:24], in_=w[:, 8:16]), reads=[w], writes=[w])
            S.op("dve", lambda w=w: nc.vector.tensor_scalar(out=w[:, 24:32], in0=w[:, 8:16], scalar1=w[:, 17:18], scalar2=None, op0=ALU.is_ge),
                 reads=[w], writes=[w])
            S.op("dve", lambda w=w: nc.vector.tensor_scalar(out=w[:, 48:56], in0=w[:, 8:16], scalar1=w[:, 16:17], scalar2=None, op0=ALU.is_ge),
                 reads=[w], writes=[w])
            S.op("dve", lambda w=w: nc.vector.tensor_tensor(out=w[:, 56:64], in0=w[:, 24:32], in1=w[:, 48:56], op=ALU.subtract), reads=[w], writes=[w])
            S.op("dve", lambda w=w: nc.vector.tensor_scalar(out=w[:, 40:41], in0=w[:, 16:17], scalar1=-1.0, scalar2=None, op0=ALU.mult),
                 reads=[w], writes=[w])
            S.op("act", lambda w=w: nc.scalar.activation(out=w[:, 32:40], in_=w[:, 8:16], func=AF.Exp, bias=w[:, 40:41], scale=1.0),
                 reads=[w], writes=[w])
            S.op("act", lambda w=w: nc.scalar.activation(out=w[:, 41:42], in_=w[:, 17:18], func=AF.Exp, bias=w[:, 40:41], scale=1.0),
                 reads=[w], writes=[w])
            S.op("dve", lambda w=w: nc.vector.tensor_scalar(out=w[:, 41:42], in0=w[:, 41:42], scalar1=1.0, scalar2=None, op0=ALU.add),
                 reads=[w], writes=[w])
            S.op("dve", lambda w=w: nc.vector.reciprocal(out=w[:, 42:43], in_=w[:, 41:42]), reads=[w], writes=[w])
            S.op("dve", lambda w=w: nc.vector.tensor_tensor(out=w[:, 42:43], in0=w[:, 42:43], in1=w[:, 3:4], op=ALU.mult), reads=[w], writes=[w])
            S.op("dve", lambda w=w: nc.vector.tensor_scalar(out=w[:, 64:72], in0=w[:, 32:40], scalar1=w[:, 42:43], scalar2=None, op0=ALU.mult),
                 reads=[w], writes=[w])
            for (sc, oc) in ((48, 80), (56, 81)):
                S.op("dve", lambda w=w, sc=sc: nc.vector.tensor_tensor(out=w[:, 72:80], in0=w[:, 64:72], in1=w[:, sc:sc + 8], op=ALU.mult),
                     reads=[w], writes=[w])
                S.op("dve", lambda w=w, oc=oc: nc.vector.tensor_reduce(out=w[:, oc:oc + 1], in_=w[:, 72:80], axis=AX.X, op=ALU.add),
                     reads=[w], writes=[w])
            for gi in range(4):
                S.op("dve", lambda w=w, gi=gi: nc.vector.tensor_scalar(out=w[:, 96 + gi * 8:104 + gi * 8], in0=w[:, 48:56],
                                                                       scalar1=w[:, 4 + gi:5 + gi], scalar2=None, op0=ALU.mult),
                     reads=[w], writes=[w])
                S.op("dve", lambda w=w, gi=gi: nc.vector.tensor_scalar(out=w[:, 128 + gi * 8:136 + gi * 8], in0=w[:, 56:64],
                                                                       scalar1=w[:, 4 + gi:5 + gi], scalar2=None, op0=ALU.mult),
                     reads=[w], writes=[w])
            S.op("dve", lambda w=w, mi=mi: nc.vector.tensor_tensor(out=mi[:], in0=w[:, 96:128], in1=w[:, 128:160], op=ALU.add),
                 reads=[w], writes=[mi])
            a = C.acc[C.nA % 4]
            C.nA += 1
            S.op("pe", lambda a=a, mi=mi: nc.tensor.matmul(out=a[:, 0:32], lhsT=lst[:], rhs=mi[:], start=True, stop=False),
                 reads=[lst, mi], writes=[a])
            S.op("pe", lambda a=a: nc.tensor.matmul(out=a[:, 0:32], lhsT=onesb[:], rhs=Macc[:], start=False, stop=True),
                 reads=[onesb, Macc], writes=[a])
            S.op("dve", lambda mi=mi: nc.vector.tensor_tensor(out=Macc[:], in0=Macc[:], in1=mi[:], op=ALU.add), reads=[Macc, mi], writes=[Macc])
            S.op("dve", lambda a=a, w=w: nc.vector.tensor_tensor(out=w[:, 160:192], in0=a[:, 0:32], in1=ebase[:], op=ALU.add),
                 reads=[a, ebase], writes=[w])
            for (mc, oc) in ((96, 82), (128, 83)):
                S.op("dve", lambda w=w, mc=mc: nc.vector.tensor_tensor(out=w[:, 192:224], in0=w[:, 160:192], in1=w[:, mc:mc + 32], op=ALU.mult),
                     reads=[w], writes=[w])
                S.op("dve", lambda w=w, oc=oc: nc.vector.tensor_reduce(out=w[:, oc:oc + 1], in_=w[:, 192:224], axis=AX.X, op=ALU.add),
                     reads=[w], writes=[w])
            S.op("dve", lambda w=w, ii=ii: nc.vector.tensor_copy(out=ii[:], in_=w[:, 82:84]), reads=[w], writes=[ii])
            S.dma("sp", IDX_d[rows], ii[:], reads=[ii])
            S.dma("sp", GAB_d[rows], w[:, 80:82], reads=[w])


def emit_dispatch(C, x1_d, modrows, modsel, IDX_d, Xs_d, R, NS):
    S, nc = C.S, C.nc
    D = D_MODEL
    I32 = mybir.dt.int32
    xsb = Buf(None)
    with S.scope():
        zt = S.sb("zt", [128, 8, D], BF16)
        S.op("pool", lambda: nc.gpsimd.memset(zt[:], 0.0), writes=[zt])
        for r0 in range(0, NS, 1024):
            S.dma("sp", Xs_d[r0:r0 + 1024].rearrange("(a p) n -> p a n", p=128), zt[:], reads=[zt], writes=[xsb])
        modA = S.sb("modA", [128, 2, D], F32)
        modB = S.sb("modB", [128, 2, D], F32)
        for s_, (ra, rb_) in enumerate(modrows):
            S.dma("sp", modA[:, s_, :], ra.partition_broadcast(128), writes=[modA])
            S.dma("sp", modB[:, s_, :], rb_.partition_broadcast(128), writes=[modB])
        S.op("dve", lambda: nc.vector.tensor_scalar(out=modA[:], in0=modA[:], scalar1=1.0, scalar2=None, op0=ALU.add),
             reads=[modA], writes=[modA])
        xt = [S.sb("x%d" % i, [128, D], F32) for i in range(3)]
        st = [S.sb("st%d" % i, [128, 8, 6], F32) for i in range(2)]
        mv = [S.sb("mv%d" % i, [128, 4], F32) for i in range(2)]
        II = [S.sb("II%d" % i, [128, 2], I32) for i in range(3)]
        xbt = [S.sb("xb%d" % i, [128, D], BF16) for i in range(3)]
        for t in range(R // 128):
            rows = slice(t * 128, (t + 1) * 128)
            x, ii, xb = xt[t % 3], II[t % 3], xbt[t % 3]
            S.dma("sp", x[:], x1_d[rows], writes=[x])
            S.dma("sp", ii[:], IDX_d[rows], writes=[ii])
            m = ln_stats(S, x, D, "d", {"st": st[t % 2], "mv": mv[t % 2]})
            ms = modsel(t)
            S.op("dve", lambda x=x, m=m: nc.vector.tensor_scalar(out=x[:], in0=x[:], scalar1=m[:, 0:1], scalar2=m[:, 2:3],
                                                                 op0=ALU.subtract, op1=ALU.mult), reads=[x, m], writes=[x])
            S.op("dve", lambda x=x, ms=ms: nc.vector.tensor_tensor(out=x[:], in0=x[:], in1=modA[:, ms, :], op=ALU.mult),
                 reads=[x, modA], writes=[x])
            S.op("dve", lambda x=x, xb=xb, ms=ms: nc.vector.tensor_tensor(out=xb[:], in0=x[:], in1=modB[:, ms, :], op=ALU.add),
                 reads=[x, modB], writes=[xb])
            for j in range(2):
                S.idma(Xs_d, xb[:], ii[:, j:j + 1], True, reads=[xb, ii, xsb], writes=[])


def emit_experts(C, Xs_d, Ys_d, w1, w3, w2, CAP):
    S, nc = C.S, C.nc
    D, DE = D_MODEL, D_EXPERT
    NTall = CAP // 128
    NTe = min(8, NTall)
    with S.scope():
        hb = [S.sb("hb%d" % i, [128, D], BF16) for i in range(2)]
        hT = [S.sb("hT%d" % i, [128, 16, 128], BF16) for i in range(NTe)]
        hh = [S.sb("hh%d" % i, [128, DE], BF16) for i in range(NTe)]
        hT2 = [S.sb("hU%d" % i, [128, 8, 128], BF16) for i in range(NTe)]
        w1c = [S.sb("w1c%d" % i, [128, 16, 512], BF16) for i in range(2)]
        w3c = [S.sb("w3c%d" % i, [128, 16, 512], BF16) for i in range(2)]
        w2c = [S.sb("w2c%d" % i, [128, 8, 512], BF16) for i in range(2)]
        stl = [S.sb("stl%d" % i, [128, 512], F32) for i in range(3)]
        ot = [S.sb("ot%d" % i, [128, 512], BF16) for i in range(3)]
        n1 = n2 = n3 = 0
        for e, sub in [(e_, s_) for e_ in range(N_EXPERTS) for s_ in range(0, NTall, 8)]:
            r0 = e * CAP + sub * 128
            NTe = min(8, NTall - sub)
            for t in range(NTe):
                h = hb[n1 % 2]
                n1 += 1
                S.dma("sp", h[:], Xs_d[r0 + t * 128:r0 + (t + 1) * 128], writes=[h])
                for k0 in (0, 8):
                    p = C.pT[C.nT % 2]
                    C.nT += 1
                    for j in range(8):
                        S.op("pe", lambda p=p, h=h, j=j, k0=k0: nc.tensor.transpose(
                            out=p[:, j * 128:(j + 1) * 128], in_=h[:, (k0 + j) * 128:(k0 + j + 1) * 128], identity=C.identb[:]),
                            reads=[h, C.identb], writes=[p])
                    S.op("act", lambda p=p, t=t, k0=k0: nc.scalar.copy(
                        out=hT[t][:, k0:k0 + 8, :], in_=p[:, 0:1024].rearrange("p (a b) -> p a b", b=128)), reads=[p], writes=[hT[t]])
            for jc in range(2):
                wa, wb_ = w1c[n2 % 2], w3c[n2 % 2]
                n2 += 1
                S.dma("pool", wa[:], w1[e][:, jc * 512:(jc + 1) * 512].rearrange("(kc p) n -> p kc n", p=128), writes=[wa])
                S.dma("pool", wb_[:], w3[e][:, jc * 512:(jc + 1) * 512].rearrange("(kc p) n -> p kc n", p=128), writes=[wb_])
                for t in range(NTe):
                    a1 = C.acc[C.nA % 4]
                    a3 = C.acc[(C.nA + 1) % 4]
                    C.nA += 2
                    s_ = stl[n3 % 3]
                    n3 += 1
                    for kc in range(16):
                        S.op("pe", lambda a1=a1, wa=wa, t=t, kc=kc: nc.tensor.matmul(out=a1[:], lhsT=hT[t][:, kc, :], rhs=wa[:, kc, :],
                                                                                    start=(kc == 0), stop=(kc == 15)), reads=[hT[t], wa], writes=[a1])
                    for kc in range(16):
                        S.op("pe", lambda a3=a3, wb_=wb_, t=t, kc=kc: nc.tensor.matmul(out=a3[:], lhsT=hT[t][:, kc, :], rhs=wb_[:, kc, :],
                                                                                      start=(kc == 0), stop=(kc == 15)), reads=[hT[t], wb_], writes=[a3])
                    S.op("act", lambda a1=a1, s_=s_: nc.scalar.activation(out=s_[:], in_=a1[:], func=AF.Silu), reads=[a1], writes=[s_])
                    S.op("dve", lambda a3=a3, s_=s_, t=t, jc=jc: nc.vector.tensor_tensor(out=hh[t][:, jc * 512:(jc + 1) * 512], in0=a3[:],
                                                                                        in1=s_[:], op=ALU.mult), reads=[a3, s_], writes=[hh[t]])
            for t in range(NTe):
                p = C.pT[C.nT % 2]
                C.nT += 1
                for j in range(8):
                    S.op("pe", lambda p=p, t=t, j=j: nc.tensor.transpose(out=p[:, j * 128:(j + 1) * 128], in_=hh[t][:, j * 128:(j + 1) * 128],
                                                                         identity=C.identb[:]), reads=[hh[t], C.identb], writes=[p])
                S.op("act", lambda p=p, t=t: nc.scalar.copy(out=hT2[t][:], in_=p[:, 0:1024].rearrange("p (a b) -> p a b", b=128)),
                     reads=[p], writes=[hT2[t]])
            for n4 in range(4):
                wc = w2c[n2 % 2]
                n2 += 1
                S.dma("pool", wc[:], w2[e][:, n4 * 512:(n4 + 1) * 512].rearrange("(kc p) n -> p kc n", p=128), writes=[wc])
                for t in range(NTe):
                    a = C.acc[C.nA % 4]
                    C.nA += 1
                    o = ot[n3 % 3]
                    n3 += 1
                    for kc in range(8):
                        S.op("pe", lambda a=a, wc=wc, t=t, kc=kc: nc.tensor.matmul(out=a[:], lhsT=hT2[t][:, kc, :], rhs=wc[:, kc, :],
                                                                                  start=(kc == 0), stop=(kc == 7)), reads=[hT2[t], wc], writes=[a])
                    S.op("act", lambda a=a, o=o: nc.scalar.copy(out=o[:], in_=a[:]), reads=[a], writes=[o])
                    S.dma("sp", Ys_d[r0 + t * 128:r0 + (t + 1) * 128, n4 * 512:(n4 + 1) * 512], o[:], reads=[o])


def emit_combine(C, Ys_d, IDX_d, GAB_d, f_d, R):
    S, nc = C.S, C.nc
    D = D_MODEL
    I32 = mybir.dt.int32
    with S.scope():
        ya = [S.sb("ya%d" % i, [128, D], BF16) for i in range(2)]
        yb = [S.sb("yb%d" % i, [128, D], BF16) for i in range(2)]
        fo = [S.sb("fo%d" % i, [128, D], F32) for i in range(2)]
        II = [S.sb("II%d" % i, [128, 2], I32) for i in range(2)]
        GG = [S.sb("GG%d" % i, [128, 2], F32) for i in range(2)]
        for t in range(R // 128):
            rows = slice(t * 128, (t + 1) * 128)
            a, b, ii, gg = ya[t % 2], yb[t % 2], II[t % 2], GG[t % 2]
            S.dma("sp", ii[:], IDX_d[rows], writes=[ii])
            S.dma("sp", gg[:], GAB_d[rows], writes=[gg])
            S.idma(a[:], Ys_d, ii[:, 0:1], False, reads=[ii], writes=[a])
            S.idma(b[:], Ys_d, ii[:, 1:2], False, reads=[ii], writes=[b])
            f = fo[t % 2]
            S.op("dve", lambda a=a, f=f, gg=gg: nc.vector.tensor_scalar(out=f[:], in0=a[:], scalar1=gg[:, 0:1], scalar2=None, op0=ALU.mult),
                 reads=[a, gg], writes=[f])
            S.op("dve", lambda f=f, b=b, gg=gg: nc.vector.scalar_tensor_tensor(out=f[:], in0=b[:], scalar=gg[:, 1:2], in1=f[:],
                                                                               op0=ALU.mult, op1=ALU.add), reads=[f, b, gg], writes=[f])
            S.dma("sp", f_d[rows], f[:], reads=[f])


WNAMES = ["w_ada", "b_ada", "w_in", "b_in", "a_ln_g", "a_ln_b", "a_w_s", "a_b_s", "b_w_dw", "b_b_dw", "b_ln_g", "b_ln_b",
          "d_q_g", "d_w_uq", "d_kv_g", "w_branch", "w_o", "ln1_g", "ln1_b", "e_w1", "e_w3", "e_w2", "ln2_g", "ln2_b"]


def build_program(shapes, RL, NCTX):
    R = RL + NCTX
    D = D_MODEL
    nc, es = new_prog()
    I = {}
    for k, shp in shapes.items():
        I[k] = dram_in(nc, k, shp)
    y_out = dram_out(nc, "y", [RL, D])
    T = lambda n, s: dram_tmp(nc, n, s)
    mod_d = [T("mod%d" % l, [128, 6 * D]) for l in range(DEPTH)]
    z_d = T("z", [R, COL_G])
    zg_d = [T("zg%d" % i, [R, D]) for i in range(4)]
    brA, brB = T("brA", [R, 512]), T("brB", [R, 512])
    PQ = T("PQ", [R, 1024])
    brC = [T("brC%d" % p, [R, 512]) for p in range(4)]
    q_d, kv_d, kr_d = T("q", [R, 768]), T("kv", [R, 1024]), T("kr", [R, 32])
    o_d = T("o", [R, MLA_HEADS, V_HEAD + 1])
    GP = [T("GP%d" % i, [R, D]) for i in range(4)]
    mix_d, x1_d, xb_d = T("mix", [R, D]), T("x1", [R, D]), T("xb", [R, D])
    lg_d, G_d = T("lg", [R, 36]), T("G", [R, 32])
    CAPMAX = 128 * int(math.ceil(3.6 * 2.0 * R / N_EXPERTS / 128))
    Xs_d = dram_tmp(nc, "Xs", [N_EXPERTS * CAPMAX, D], BF16)
    Ys_d = dram_tmp(nc, "Ys", [N_EXPERTS * CAPMAX, D], BF16)
    IDX_d = nc.dram_tensor("IDX", [R, 2], mybir.dt.int32).ap()
    GAB_d = T("GAB", [R, 2])
    f_d = [T("f0", [R, D])]
    NTL = RL // 128
    msel = lambda t: 0 if t < NTL else 1
    with es:
        C = Ctx(nc, es, I["ident"])
        S = C.S
        c512 = [(i * 512, 512, None) for i in range(4)]
        for l in range(DEPTH):
            emit_linear(C, [I["cc"]], lambda ci, l=l: I["w_ada"][l][:, ci * 512:(ci + 1) * 512], mod_d[l], 128, D,
                        [(i * 512, 512, None) for i in range(24)], prologue="silu", bias_d=I["b_ada"][l])
        x_cur = I["x_all"]
        for l in range(DEPTH):
            last = l == DEPTH - 1
            Rm = RL if last else R
            mr = lambda who, i, l=l: mod_d[l][who:who + 1, i * D:(i + 1) * D]
            chunks_in = []
            for c0 in range(0, COL_KV, 512):
                chunks_in.append((c0, 512, AF.Sigmoid if c0 == 1536 else None))
            chunks_in.append((COL_KV, COL_G - COL_KV, None))
            modr1 = [(mr(0, 1), mr(0, 0)), (mr(1, 1), mr(1, 0))]
            emit_linear(C, [x_cur], lambda ci, l=l, ch=chunks_in: I["w_in"][l][:, ch[ci][0]:ch[ci][0] + ch[ci][1]], z_d, R, D,
                        chunks_in, prologue="ln0mod", modrows=modr1, modsel=msel, bias_d=I["b_in"][l])
            for i in range(4):
                g0 = COL_G + i * D
                emit_linear(C, [x_cur], lambda ci, l=l, g0=g0: I["w_in"][l][:, g0 + ci * 512:g0 + (ci + 1) * 512], zg_d[i], Rm, D,
                            [(j * 512, 512, AF.Sigmoid) for j in range(4)], prologue="ln0mod", modrows=modr1, modsel=msel,
                            bias_d=I["b_in"][l][g0:g0 + D])
            emit_chunkgate(C, z_d, I["a_ln_g"][l], I["a_ln_b"][l], I["a_w_s"][l], I["a_b_s"][l], brA, Rm)
            emit_conv(C, z_d, I["b_w_dw"][l], I["b_b_dw"][l], I["b_ln_g"][l], I["b_ln_b"][l], brB,
                      [(0, RL)] + ([] if last else [(RL, NCTX)]))
            emit_linear(C, [z_d[:, COL_C:COL_D]], lambda ci: I["cs128"][:, ci * 512:(ci + 1) * 512], PQ, R, 512,
                        [(0, 512, None), (512, 512, None)])
            K4 = RL // 2
            for p in range(4):
                wsl = PQ[(p % 2) * K4:(p % 2 + 1) * K4, (p // 2) * 512:(p // 2 + 1) * 512]
                emit_linear(C, [I["dftlat"][:, p * K4:(p + 1) * K4]], lambda ci, wsl=wsl: wsl, brC[p], RL, K4, [(0, 512, None)], RB=8)
                if not last:
                    Kc = NCTX // 2
                    wsl2 = PQ[RL + (p % 2) * Kc:RL + (p % 2 + 1) * Kc, (p // 2) * 512:(p // 2 + 1) * 512]
                    emit_linear(C, [I["dftctx"][:, p * Kc:(p + 1) * Kc]], lambda ci, wsl2=wsl2: wsl2, brC[p][RL:R], NCTX, Kc,
                                [(0, 512, None)])
            emit_linear(C, [z_d[:, COL_D:COL_KV]], lambda ci, l=l: I["d_w_uq"][l][:, ci * 512:ci * 512 + (512 if ci == 0 else 256)],
                        q_d, R, 512, [(0, 512, None), (512, 256, None)], prologue="rms", g_d=I["d_q_g"][l])
            emit_linear(C, [z_d[:, COL_KV:COL_KR]], lambda ci, l=l: I["w_ukv"][l][:, ci * 512:(ci + 1) * 512],
                        kv_d, R, 256, [(0, 512, None), (512, 512, None)], prologue="rms", g_d=I["d_kv_g"][l])
            emit_rope(C, q_d, QK_NOPE, MLA_HEADS, QK_NOPE + QK_ROPE, q_d, QK_NOPE, I["rcos"], I["rsin"], RL)
            emit_rope(C, z_d, COL_KR, 1, 32, kr_d, 0, I["rcos"], I["rsin"], RL)
            with S.scope():
                tmpk = S.sb("tmpk", [128, NCTX // 128, 32], F32)
                S.dma("sp", tmpk[:], z_d[RL:R, COL_KR:COL_G].rearrange("(a p) n -> p a n", p=128), writes=[tmpk])
                S.dma("sp", kr_d[RL:R, :].rearrange("(a p) n -> p a n", p=128), tmpk[:], reads=[tmpk])
            emit_attention(C, q_d, kv_d, kr_d, o_d, (0, RL), [(0, R)])
            if not last:
                emit_attention(C, q_d, kv_d, kr_d, o_d, (RL, NCTX), [(RL, NCTX)])
            for i in range(4):
                xs = [[brA], [brB], brC, [o_d]][i]
                emit_linear(C, xs, lambda ci, l=l, i=i: I["w_branch"][l][i][:, ci * 512:(ci + 1) * 512], GP[i], Rm, 512, c512,
                            prologue=("attn" if i == 3 else "none"), mul_d=zg_d[i])
            emit_linear(C, GP, lambda ci, l=l: I["w_o"][l][:, ci * 512:(ci + 1) * 512], mix_d, Rm, D, c512)
            emit_resln(C, x_cur, [mix_d], [mr(0, 2), mr(1, 2)], msel, I["ln1_g"][l], I["ln1_b"][l], x1_d, Rm)
            modr2 = [(mr(0, 4), mr(0, 3)), (mr(1, 4), mr(1, 3))]
            emit_linear(C, [x1_d], lambda ci, l=l: I["r_w"][l], lg_d, Rm, D, [(0, 36, None)], prologue="ln0mod",
                        modrows=modr2, modsel=msel, bias_d=I["r_b"][l])
            CAP = 128 * int(math.ceil(3.6 * 2.0 * Rm / N_EXPERTS / 128))
            emit_route(C, lg_d, IDX_d, GAB_d, I["lstrict"], Rm, CAP)
            emit_dispatch(C, x1_d, modr2, msel, IDX_d, Xs_d, Rm, N_EXPERTS * CAP)
            emit_experts(C, Xs_d, Ys_d, I["e_w1"][l], I["e_w3"][l], I["e_w2"][l], CAP)
            emit_combine(C, Ys_d, IDX_d, GAB_d, f_d[0], Rm)
            dst = y_out if last else xb_d
            emit_resln(C, x1_d, [f_d[0]], [mr(0, 5), mr(1, 5)], msel, I["ln2_g"][l], I["ln2_b"][l], dst, Rm)
            x_cur = xb_d
        S.finish()
        print("program instructions:", S.n, S.cnt, S.dcnt)
    return nc


def host_consts(RL, NCTX):
    rows = RL // GRID_W
    row = np.repeat(np.arange(rows, dtype=np.float32), GRID_W)
    col = np.tile(np.arange(GRID_W, dtype=np.float32), rows)
    half = QK_ROPE // 2
    inv = (ROPE_THETA ** (-np.arange(0, half, 2, dtype=np.float32) / half)).astype(np.float32)
    ar = row[:, None] * inv
    ac = col[:, None] * inv
    ang = np.concatenate([ar, ar, ac, ac], -1)
    rcos = np.cos(ang).astype(np.float32)
    rsin = np.concatenate([-np.sin(ar), np.sin(ar), -np.sin(ac), np.sin(ac)], -1).astype(np.float32)
    cw = BRANCH_W // 4
    k = np.arange(cw)
    a = 2 * np.pi * np.outer(k, k) / cw
    cs = np.zeros((512, 1024), np.float32)
    for g in range(4):
        cs[g * cw:(g + 1) * cw, g * cw:(g + 1) * cw] = np.cos(a)
        cs[g * cw:(g + 1) * cw, 512 + g * cw:512 + (g + 1) * cw] = np.sin(a)

    def dft(n):
        kk = np.arange(n, dtype=np.int64)
        ph = (np.outer(kk, kk) % n).astype(np.float64) * (2 * np.pi / n)
        sc = 1.0 / math.sqrt(n * cw)
        return np.concatenate([np.cos(ph) * sc, -np.sin(ph) * sc], 1).astype(np.float32)

    return {"rcos": rcos, "rsin": rsin, "cs128": cs, "dftlat": dft(RL), "dftctx": dft(NCTX),
            "ident": np.eye(128, dtype=np.float32),
            "lstrict": np.triu(np.ones((128, 128), np.float32), 1)}


def kernel_fused(**inp):
    x = np.asarray(inp["x"], np.float32)
    RL = x.shape[1]
    NCTX = inp["ctx"].shape[1]
    ins = {}
    ins["x_all"] = np.concatenate([x[0], np.asarray(inp["ctx"], np.float32)[0]], 0)
    cc = np.zeros((128, D_MODEL), np.float32)
    cc[0] = np.asarray(inp["c"])[0]
    cc[1] = np.asarray(inp["c_ctx"])
    ins["cc"] = cc
    for k in WNAMES:
        ins[k] = np.ascontiguousarray(np.asarray(inp[k], np.float32))
    ins["w_ukv"] = np.concatenate([np.asarray(inp["d_w_uk"]), np.asarray(inp["d_w_uv"])], -1).astype(np.float32)
    ins["r_w"] = np.concatenate([np.asarray(inp["r_w_grp"]), np.asarray(inp["r_w_exp"])], -1).astype(np.float32)
    ins["r_b"] = np.concatenate([np.asarray(inp["r_b_grp"]), np.asarray(inp["r_b_exp"])], -1).astype(np.float32)
    ins.update(host_consts(RL, NCTX))
    shapes = {k: list(v.shape) for k, v in ins.items()}
    key = ("main", RL, NCTX)
    if key not in _PROG_CACHE:
        _PROG_CACHE[key] = build_program(shapes, RL, NCTX)
    res = run_bass_kernel_spmd(_PROG_CACHE[key], [ins], core_ids=[0])
    return np.asarray(res.results[0]["y"], np.float32)[None]


NLAT_ALL = SEQ
RC = TL + CTX_LEN


def _chunks(spec):
    return [(c0, w, a) for (c0, w, a) in spec]


def build_P1(shapes):
    RL, NCTX, R, D = TL, CTX_LEN, RC, D_MODEL
    nc, es = new_prog()
    I = {k: dram_in(nc, k, shp) for k, shp in shapes.items()}
    PQ_o, kv_o, kr_o = dram_out(nc, "PQ_o", [R, 1024]), dram_out(nc, "kv_o", [R, 1024]), dram_out(nc, "kr_o", [R, 32])
    zab_o = dram_out(nc, "zab_o", [256, 1024])
    mod_d = dram_out(nc, "mod_o", [128, 6 * D])
    z_d = dram_tmp(nc, "z", [R, COL_G])
    msel = lambda t: 0 if t < RL // 128 else 1
    with es:
        C = Ctx(nc, es, I["ident"])
        S = C.S
        emit_linear(C, [I["cc"]], lambda ci: I["w_ada"][:, ci * 512:(ci + 1) * 512], mod_d, 128, D,
                    [(i * 512, 512, None) for i in range(24)], prologue="silu", bias_d=I["b_ada"])
        mr = lambda who, i: mod_d[who:who + 1, i * D:(i + 1) * D]
        ch = [(1024, 512, None), (1536, 512, AF.Sigmoid), (2048, 512, None), (COL_KV, COL_G - COL_KV, None)]
        emit_linear(C, [I["x_own"]], lambda ci: I["w_in"][:, ch[ci][0]:ch[ci][0] + ch[ci][1]], z_d, R, D, ch, prologue="ln0mod",
                    modrows=[(mr(0, 1), mr(0, 0)), (mr(1, 1), mr(1, 0))], modsel=msel, bias_d=I["b_in"])
        emit_linear(C, [z_d[:, COL_C:COL_D]], lambda ci: I["cs128"][:, ci * 512:(ci + 1) * 512], PQ_o, R, 512,
                    [(0, 512, None), (512, 512, None)])
        emit_linear(C, [z_d[:, COL_KV:COL_KR]], lambda ci: I["w_ukv"][:, ci * 512:(ci + 1) * 512], kv_o, R, 256,
                    [(0, 512, None), (512, 512, None)], prologue="rms", g_d=I["d_kv_g"])
        emit_rope(C, z_d, COL_KR, 1, 32, kr_o, 0, I["rcos"], I["rsin"], RL)
        with S.scope():
            tmpk = S.sb("tmpk", [128, NCTX // 128, 32], F32)
            S.dma("sp", tmpk[:], z_d[RL:R, COL_KR:COL_G].rearrange("(a p) n -> p a n", p=128), writes=[tmpk])
            S.dma("sp", kr_o[RL:R, :].rearrange("(a p) n -> p a n", p=128), tmpk[:], reads=[tmpk])
            tz = S.sb("tz", [128, 2, 1024], F32)
            S.dma("sp", tz[:, 0, :], z_d[0:128, 1024:2048], writes=[tz])
            S.dma("sp", tz[:, 1, :], z_d[RL - 128:RL, 1024:2048], writes=[tz])
            S.dma("sp", zab_o.rearrange("(a p) n -> p a n", p=128), tz[:], reads=[tz])
        S.finish()
        print("P1 instructions:", S.n)
    return nc


def build_P2(shapes):
    RL, NCTX, R, D = TL, CTX_LEN, RC, D_MODEL
    nc, es = new_prog()
    I = {k: dram_in(nc, k, shp) for k, shp in shapes.items()}
    xn_o = dram_out(nc, "xn", [R, D])
    T = lambda n, s_: dram_tmp(nc, n, s_)
    mod_d = I["mod_in"]
    z_d = T("z", [R, D_IN])
    brA, brB = T("brA", [R, 512]), T("brB", [R, 512])
    brC = [T("brC%d" % p, [R, 512]) for p in range(4)]
    q_d = T("q", [R, 768])
    o_d = T("o", [R, MLA_HEADS, V_HEAD + 1])
    GP = [T("GP%d" % i, [R, D]) for i in range(4)]
    mix_d, x1_d = T("mix", [R, D]), T("x1", [R, D])
    lg_d = T("lg", [R, 36])
    CAP = 128 * int(math.ceil(3.6 * 2.0 * R / N_EXPERTS / 128))
    Xs_d = dram_tmp(nc, "Xs", [N_EXPERTS * CAP, D], BF16)
    Ys_d = dram_tmp(nc, "Ys", [N_EXPERTS * CAP, D], BF16)
    IDX_d = nc.dram_tensor("IDX", [R, 2], mybir.dt.int32).ap()
    GAB_d = T("GAB", [R, 2])
    f_d = T("f0", [R, D])
    msel = lambda t: 0 if t < RL // 128 else 1
    with es:
        C = Ctx(nc, es, I["ident"])
        S = C.S
        c512 = [(i * 512, 512, None) for i in range(4)]
        mr = lambda who, i: mod_d[who:who + 1, i * D:(i + 1) * D]
        x_cur = I["x_own"]
        ch = [(0, 512, None), (512, 512, None), (1024, 512, None), (1536, 512, AF.Sigmoid), (COL_D, 512, None)]
        ch += [(c0, 512, AF.Sigmoid) for c0 in range(COL_G, D_IN, 512)]
        modr1 = [(mr(0, 1), mr(0, 0)), (mr(1, 1), mr(1, 0))]
        emit_linear(C, [x_cur], lambda ci: I["w_in"][:, ch[ci][0]:ch[ci][0] + ch[ci][1]], z_d, R, D, ch, prologue="ln0mod",
                    modrows=modr1, modsel=msel, bias_d=I["b_in"])
        zg_d = [z_d[:, COL_G + i * D:COL_G + (i + 1) * D] for i in range(4)]
        emit_chunkgate(C, z_d, I["a_ln_g"], I["a_ln_b"], I["a_w_s"], I["a_b_s"], brA, R)
        emit_conv(C, z_d, I["b_w_dw"], I["b_b_dw"], I["b_ln_g"], I["b_ln_b"], brB, [(0, RL), (RL, NCTX)], halo=I["halo"])
        PQ = I["PQ_all"]
        K4 = NLAT_ALL // 2
        for p in range(4):
            wsl = PQ[(p % 2) * K4:(p % 2 + 1) * K4, (p // 2) * 512:(p // 2 + 1) * 512]
            emit_linear(C, [I["dft_own"][:, p * K4:(p + 1) * K4]], lambda ci, wsl=wsl: wsl, brC[p], RL, K4, [(0, 512, None)], RB=8)
            Kc = NCTX // 2
            wsl2 = PQ[NLAT_ALL + (p % 2) * Kc:NLAT_ALL + (p % 2 + 1) * Kc, (p // 2) * 512:(p // 2 + 1) * 512]
            emit_linear(C, [I["dftctx"][:, p * Kc:(p + 1) * Kc]], lambda ci, wsl2=wsl2: wsl2, brC[p][RL:R], NCTX, Kc, [(0, 512, None)])
        emit_linear(C, [z_d[:, COL_D:COL_KV]], lambda ci: I["d_w_uq"][:, ci * 512:ci * 512 + (512 if ci == 0 else 256)],
                    q_d, R, 512, [(0, 512, None), (512, 256, None)], prologue="rms", g_d=I["d_q_g"])
        emit_rope(C, q_d, QK_NOPE, MLA_HEADS, QK_NOPE + QK_ROPE, q_d, QK_NOPE, I["rcos"], I["rsin"], RL)
        RA = NLAT_ALL + NCTX
        emit_attention(C, q_d, I["kv_all"], I["kr_all"], o_d, (0, RL), [(0, RA)])
        emit_attention(C, q_d, I["kv_all"], I["kr_all"], o_d, (RL, NCTX), [(NLAT_ALL, NCTX)])
        for i in range(4):
            xs = [[brA], [brB], brC, [o_d]][i]
            emit_linear(C, xs, lambda ci, i=i: I["w_branch"][i][:, ci * 512:(ci + 1) * 512], GP[i], R, 512, c512,
                        prologue=("attn" if i == 3 else "none"), mul_d=zg_d[i])
        emit_linear(C, GP, lambda ci: I["w_o"][:, ci * 512:(ci + 1) * 512], mix_d, R, D, c512)
        emit_resln(C, x_cur, [mix_d], [mr(0, 2), mr(1, 2)], msel, I["ln1_g"], I["ln1_b"], x1_d, R)
        modr2 = [(mr(0, 4), mr(0, 3)), (mr(1, 4), mr(1, 3))]
        emit_linear(C, [x1_d], lambda ci: I["r_w"], lg_d, R, D, [(0, 36, None)], prologue="ln0mod",
                    modrows=modr2, modsel=msel, bias_d=I["r_b"])
        emit_route(C, lg_d, IDX_d, GAB_d, I["lstrict"], R, CAP)
        emit_dispatch(C, x1_d, modr2, msel, IDX_d, Xs_d, R, N_EXPERTS * CAP)
        emit_experts(C, Xs_d, Ys_d, I["e_w1"], I["e_w3"], I["e_w2"], CAP)
        emit_combine(C, Ys_d, IDX_d, GAB_d, f_d, R)
        emit_resln(C, x1_d, [f_d], [mr(0, 5), mr(1, 5)], msel, I["ln2_g"], I["ln2_b"], xn_o, R)
        S.finish()
        print("P2 instructions:", S.n, S.cnt)
    return nc


def _run(key, builder, in_maps):
    shapes = {k: list(v.shape) for k, v in in_maps[0].items()}
    if key not in _PROG_CACHE:
        _PROG_CACHE[key] = builder(shapes)
    res = run_bass_kernel_spmd(_PROG_CACHE[key], in_maps, core_ids=list(range(NCORES)))
    return res.results


def kernel(**inp):
    f32 = lambda a: np.ascontiguousarray(np.asarray(a, np.float32))
    x = f32(inp["x"])[0]
    ctx = f32(inp["ctx"])[0]
    cc = np.zeros((128, D_MODEL), np.float32)
    cc[0] = f32(inp["c"])[0]
    cc[1] = f32(inp["c_ctx"])
    cst = host_consts(SEQ, CTX_LEN)
    w_ukv = np.concatenate([f32(inp["d_w_uk"]), f32(inp["d_w_uv"])], -1)
    r_w = np.concatenate([f32(inp["r_w_grp"]), f32(inp["r_w_exp"])], -1)
    r_b = np.concatenate([f32(inp["r_b_grp"]), f32(inp["r_b_exp"])], -1)
    xo = [np.concatenate([x[j * TL:(j + 1) * TL], ctx], 0) for j in range(NCORES)]
    zero_t = np.zeros((128, 1024), np.float32)
    for l in range(DEPTH):
        W = lambda k: f32(inp[k][l])
        common1 = {"cc": cc, "w_ada": W("w_ada"), "b_ada": W("b_ada"), "w_in": W("w_in"), "b_in": W("b_in"),
                   "d_kv_g": W("d_kv_g"), "w_ukv": np.ascontiguousarray(w_ukv[l]), "cs128": cst["cs128"], "ident": cst["ident"]}
        ins1 = []
        for j in range(NCORES):
            d = dict(common1)
            d["x_own"] = xo[j]
            d["rcos"] = np.ascontiguousarray(cst["rcos"][j * TL:(j + 1) * TL])
            d["rsin"] = np.ascontiguousarray(cst["rsin"][j * TL:(j + 1) * TL])
            ins1.append(d)
        r1 = _run("P1", build_P1, ins1)
        cat = lambda k: np.concatenate([np.asarray(r1[j][k])[:TL] for j in range(NCORES)] + [np.asarray(r1[0][k])[TL:]], 0)
        PQ_all, kv_all, kr_all = cat("PQ_o"), cat("kv_o"), cat("kr_o")
        common2 = {"mod_in": np.ascontiguousarray(np.asarray(r1[0]["mod_o"], np.float32)), "PQ_all": PQ_all, "kv_all": kv_all, "kr_all": kr_all, "dftctx": cst["dftctx"],
                   "lstrict": cst["lstrict"], "ident": cst["ident"],
                   "w_ukv_unused": None}
        del common2["w_ukv_unused"]
        for k in ["w_in", "b_in", "a_ln_g", "a_ln_b", "a_w_s", "a_b_s", "b_w_dw", "b_b_dw", "b_ln_g", "b_ln_b",
                  "d_q_g", "d_w_uq", "w_branch", "w_o", "ln1_g", "ln1_b", "e_w1", "e_w3", "e_w2", "ln2_g", "ln2_b"]:
            common2[k] = W(k)
        common2["r_w"] = np.ascontiguousarray(r_w[l])
        common2["r_b"] = np.ascontiguousarray(r_b[l])
        ins2 = []
        for j in range(NCORES):
            d = dict(common2)
            d["x_own"] = xo[j]
            prev = np.asarray(r1[j - 1]["zab_o"])[128:256] if j > 0 else zero_t
            nxt = np.asarray(r1[j + 1]["zab_o"])[0:128] if j < NCORES - 1 else zero_t
            d["halo"] = np.ascontiguousarray(np.concatenate([prev, nxt], 0))
            d["dft_own"] = np.ascontiguousarray(cst["dftlat"][j * TL:(j + 1) * TL])
            d["rcos"] = np.ascontiguousarray(cst["rcos"][j * TL:(j + 1) * TL])
            d["rsin"] = np.ascontiguousarray(cst["rsin"][j * TL:(j + 1) * TL])
            ins2.append(d)
        r2 = _run("P2", build_P2, ins2)
        xo = [np.ascontiguousarray(np.asarray(r2[j]["xn"], np.float32)) for j in range(NCORES)]
    return np.concatenate([xo[j][:TL] for j in range(NCORES)], 0)[None].astype(np.float32)
```
